# Optimizing a Trainium2 kernel written in Bass

```python
import jax, jax.numpy as jnp
from jax import lax
import numpy as np

D_MODEL = 1024
BATCH = 8
SEQ = 4096
DEPTH = 2

HEAD_DIM = 64
N_HEADS_A = 12
N_KV_A = 4
IDX_HEADS = 8
IDX_DIM = 64
IDX_ROPE_DIM = 32
TOPK_MAX = 256
DIL_PATTERNS = ((128, 1), (512, 4), (2048, 16))
HEADS_PER_DIL = 4
N_MEM_HEADS = 4
MEM_TOKENS = 256
D_FF = ((8 * D_MODEL // 3 + 255) // 256) * 256
BLK = 128
ROPE_THETA = 10000.0
EPS = 1e-6
NEG = -1e30

A_SIZES = (N_HEADS_A * HEAD_DIM, N_KV_A * HEAD_DIM, N_KV_A * HEAD_DIM,
           IDX_HEADS * IDX_DIM, IDX_DIM, IDX_HEADS, N_MEM_HEADS * HEAD_DIM)
A_IN = sum(A_SIZES)
A_OUT = (N_HEADS_A + N_MEM_HEADS) * HEAD_DIM
B_SIZES = (HEADS_PER_DIL * HEAD_DIM,) * (3 * len(DIL_PATTERNS)) + (N_MEM_HEADS * HEAD_DIM,)
B_IN = sum(B_SIZES)
B_OUT = (HEADS_PER_DIL + N_MEM_HEADS) * HEAD_DIM

kernel_name = 'hybrid_dsa_dilated_decoder'


def _split(t, sizes):
    offs = np.cumsum(sizes)[:-1].tolist()
    return jnp.split(t, offs, axis=-1)


def rms_norm(x, g):
    xf = x.astype(jnp.float32)
    y = xf * lax.rsqrt(jnp.mean(xf * xf, axis=-1, keepdims=True) + EPS)
    return (y * g.astype(jnp.float32)).astype(x.dtype)


def rope(x, pos, rot_dim):
    half = rot_dim // 2
    freqs = ROPE_THETA ** (-jnp.arange(half, dtype=jnp.float32) / half)
    ang = pos.astype(jnp.float32)[:, :, None, None] * freqs
    cos, sin = jnp.cos(ang), jnp.sin(ang)
    xr = x[..., :rot_dim].astype(jnp.float32)
    x1, x2 = xr[..., :half], xr[..., half:]
    rot = jnp.concatenate([x1 * cos - x2 * sin, x2 * cos + x1 * sin], axis=-1).astype(x.dtype)
    return jnp.concatenate([rot, x[..., rot_dim:]], axis=-1)


def dsa_mixer(proj, pos):
    bsz, seq, _ = proj.shape
    q, k, v, qi, ki, wi, qm = _split(proj, A_SIZES)
    q = rope(q.reshape(bsz, seq, N_HEADS_A, HEAD_DIM), pos, HEAD_DIM)
    k = rope(k.reshape(bsz, seq, N_KV_A, HEAD_DIM), pos, HEAD_DIM)
    v = v.reshape(bsz, seq, N_KV_A, HEAD_DIM)
    qi = rope(qi.reshape(bsz, seq, IDX_HEADS, IDX_DIM), pos, IDX_ROPE_DIM)
    ki = rope(ki.reshape(bsz, seq, 1, IDX_DIM), pos, IDX_ROPE_DIM)[:, :, 0]
    wi = wi.astype(jnp.float32) * (IDX_HEADS ** -0.5 * IDX_DIM ** -0.5)
    n_sel = min(TOPK_MAX, seq // 4)
    grp = N_HEADS_A // N_KV_A
    scale = HEAD_DIM ** -0.5
    key_pos = jnp.arange(seq)
    b_idx = jnp.arange(bsz)[:, None, None]

    def block(i):
        start = i * BLK
        qb = lax.dynamic_slice_in_dim(q, start, BLK, axis=1)
        qib = lax.dynamic_slice_in_dim(qi, start, BLK, axis=1)
        wib = lax.dynamic_slice_in_dim(wi, start, BLK, axis=1)
        t_pos = start + jnp.arange(BLK)
        logits = jnp.einsum('bqhd,bsd->bqhs', qib, ki, preferred_element_type=jnp.float32)
        score = jnp.einsum('bqhs,bqh->bqs', jax.nn.relu(logits), wib)
        score = jnp.where(key_pos[None, None, :] <= t_pos[None, :, None], score, NEG)
        _, idx = lax.top_k(score, n_sel)
        k_sel = k[b_idx, idx]
        v_sel = v[b_idx, idx]
        qg = qb.reshape(bsz, BLK, N_KV_A, grp, HEAD_DIM)
        s = jnp.einsum('bqkgd,bqskd->bqkgs', qg, k_sel, preferred_element_type=jnp.float32) * scale
        valid = (idx <= t_pos[None, :, None])[:, :, None, None, :]
        p = jax.nn.softmax(jnp.where(valid, s, NEG), axis=-1)
        o = jnp.einsum('bqkgs,bqskd->bqkgd', p, v_sel.astype(jnp.float32))
        return o.reshape(bsz, BLK, N_HEADS_A * HEAD_DIM).astype(proj.dtype)

    out = lax.map(block, jnp.arange(seq // BLK))
    out = out.transpose(1, 0, 2, 3).reshape(bsz, seq, N_HEADS_A * HEAD_DIM)
    return out, qm


def dilated_group(q, k, v, window, dil):
    bsz, seq, nh, hd = q.shape
    steps = window // dil
    n = seq // dil
    nb = -(-n // BLK)
    n_pad = nb * BLK

    def to_sub(t, front):
        t = t.reshape(bsz, n, dil, nh, hd).transpose(0, 2, 3, 1, 4)
        return jnp.pad(t, ((0, 0), (0, 0), (0, 0), (front, n_pad - n), (0, 0)))

    qs = to_sub(q, 0).reshape(bsz, dil, nh, nb, BLK, hd)
    kb = to_sub(k, BLK).reshape(bsz, dil, nh, nb + 1, BLK, hd)
    vb = to_sub(v, BLK).reshape(bsz, dil, nh, nb + 1, BLK, hd)
    kw = jnp.concatenate([kb[:, :, :, :-1], kb[:, :, :, 1:]], axis=4)
    vw = jnp.concatenate([vb[:, :, :, :-1], vb[:, :, :, 1:]], axis=4)
    s = jnp.einsum('brhnqd,brhnkd->brhnqk', qs, kw, preferred_element_type=jnp.float32) * (hd ** -0.5)
    qi = jnp.arange(BLK)[None, :, None]
    kj = jnp.arange(2 * BLK)[None, None, :]
    blk = jnp.arange(nb)[:, None, None]
    dist = qi + BLK - kj
    valid = (dist >= 0) & (dist <= steps) & (blk * BLK + kj - BLK >= 0)
    s = jnp.where(valid, s, NEG)
    m = jnp.max(s, axis=-1, keepdims=True)
    p = jnp.exp(s - m)
    den = jnp.sum(p, axis=-1, keepdims=True)
    o = jnp.einsum('brhnqk,brhnkd->brhnqd', p, vw.astype(jnp.float32)) / den
    lse = m[..., 0] + jnp.log(den[..., 0])
    o = o.reshape(bsz, dil, nh, n_pad, hd)[:, :, :, :n].transpose(0, 3, 1, 2, 4).reshape(bsz, seq, nh, hd)
    lse = lse.reshape(bsz, dil, nh, n_pad)[..., :n].transpose(0, 3, 1, 2).reshape(bsz, seq, nh)
    return o, lse


def dilated_mixer(proj, pos):
    bsz, seq, _ = proj.shape
    parts = _split(proj, B_SIZES)
    outs, lses = [], []
    for g, (window, dil) in enumerate(DIL_PATTERNS):
        q, k, v = [parts[3 * g + j].reshape(bsz, seq, HEADS_PER_DIL, HEAD_DIM) for j in range(3)]
        o, lse = dilated_group(rope(q, pos, HEAD_DIM), rope(k, pos, HEAD_DIM), v, window, dil)
        outs.append(o)
        lses.append(lse)
    wts = jax.nn.softmax(jnp.stack(lses, axis=-1), axis=-1)
    o = jnp.sum(jnp.stack(outs, axis=-2) * wts[..., None], axis=-2)
    return o.reshape(bsz, seq, HEADS_PER_DIL * HEAD_DIM).astype(proj.dtype), parts[-1]


def memory_attention(qm, mem, g_mem, w_mem_kv):
    bsz, seq, _ = qm.shape
    kv = (rms_norm(mem, g_mem) @ w_mem_kv).reshape(bsz, mem.shape[1], 2, N_MEM_HEADS, HEAD_DIM)
    q = qm.reshape(bsz, seq, N_MEM_HEADS, HEAD_DIM)
    s = jnp.einsum('bshd,bmhd->bhsm', q, kv[:, :, 0], preferred_element_type=jnp.float32) * (HEAD_DIM ** -0.5)
    p = jax.nn.softmax(s, axis=-1)
    o = jnp.einsum('bhsm,bmhd->bshd', p, kv[:, :, 1].astype(jnp.float32))
    return o.reshape(bsz, seq, N_MEM_HEADS * HEAD_DIM).astype(qm.dtype)


def swiglu_ffn(h, w_gate_up, w_down):
    g, u = jnp.split(h @ w_gate_up, 2, axis=-1)
    return (jax.nn.silu(g) * u) @ w_down


def hybrid_layer(x, mem, pos, layer_idx, norm_mix, norm_mem, w_in, w_mem_kv, w_out,
                 norm_ffn, w_gate_up, w_down):
    proj = rms_norm(x, norm_mix) @ w_in
    if layer_idx % 2 == 0:
        mix, qm = dsa_mixer(proj, pos)
    else:
        mix, qm = dilated_mixer(proj, pos)
    mo = memory_attention(qm, mem, norm_mem, w_mem_kv)
    x = x + jnp.concatenate([mix, mo], axis=-1) @ w_out
    x = x + swiglu_ffn(rms_norm(x, norm_ffn), w_gate_up, w_down)
    return x


def setup_inputs(seed: int = 0) -> dict:
    key = jax.random.key(seed)
    ks = jax.random.split(key, 24)
    f32 = jnp.float32

    def dense(k, fi, fo):
        return jax.random.normal(k, (fi, fo), f32) * fi ** -0.5

    def gain(k):
        return 1.0 + 0.02 * jax.random.normal(k, (D_MODEL,), f32)

    x = jax.random.normal(ks[0], (BATCH, SEQ, D_MODEL), f32)
    mem = jax.random.normal(ks[1], (BATCH, MEM_TOKENS, D_MODEL), f32)
    offs = jax.random.randint(ks[2], (BATCH, 1), 0, 1024, dtype=jnp.int32)
    positions = (jnp.arange(SEQ, dtype=jnp.int32)[None, :] + offs).astype(jnp.int32)
    return {
        'x': x, 'mem': mem, 'positions': positions,
        'l0_norm_mix': gain(ks[3]), 'l0_norm_mem': gain(ks[4]),
        'l0_w_in': dense(ks[5], D_MODEL, A_IN),
        'l0_w_mem_kv': dense(ks[6], D_MODEL, 2 * N_MEM_HEADS * HEAD_DIM),
        'l0_w_out': dense(ks[7], A_OUT, D_MODEL),
        'l0_norm_ffn': gain(ks[8]),
        'l0_w_gate_up': dense(ks[9], D_MODEL, 2 * D_FF),
        'l0_w_down': dense(ks[10], D_FF, D_MODEL),
        'l1_norm_mix': gain(ks[11]), 'l1_norm_mem': gain(ks[12]),
        'l1_w_in': dense(ks[13], D_MODEL, B_IN),
        'l1_w_mem_kv': dense(ks[14], D_MODEL, 2 * N_MEM_HEADS * HEAD_DIM),
        'l1_w_out': dense(ks[15], B_OUT, D_MODEL),
        'l1_norm_ffn': gain(ks[16]),
        'l1_w_gate_up': dense(ks[17], D_MODEL, 2 * D_FF),
        'l1_w_down': dense(ks[18], D_FF, D_MODEL),
        'final_norm': gain(ks[19]),
    }


def reference(x, mem, positions,
              l0_norm_mix, l0_norm_mem, l0_w_in, l0_w_mem_kv, l0_w_out, l0_norm_ffn, l0_w_gate_up, l0_w_down,
              l1_norm_mix, l1_norm_mem, l1_w_in, l1_w_mem_kv, l1_w_out, l1_norm_ffn, l1_w_gate_up, l1_w_down,
              final_norm):
    layers = (
        (l0_norm_mix, l0_norm_mem, l0_w_in, l0_w_mem_kv, l0_w_out, l0_norm_ffn, l0_w_gate_up, l0_w_down),
        (l1_norm_mix, l1_norm_mem, l1_w_in, l1_w_mem_kv, l1_w_out, l1_norm_ffn, l1_w_gate_up, l1_w_down),
    )
    for i in range(DEPTH):
        x = hybrid_layer(x, mem, positions, i, *layers[i])
    return rms_norm(x, final_norm)
```

```python
import contextlib
import numpy as np
import ml_dtypes
import concourse.bass as bass
import concourse.mybir as mybir
from concourse.bass_utils import run_bass_kernel_spmd

F32 = mybir.dt.float32
BF16 = mybir.dt.bfloat16
I32 = mybir.dt.int32
AF = mybir.ActivationFunctionType
ALU = mybir.AluOpType
AX = mybir.AxisListType

SAME_ENGINE_SYNC = True


class Tok:
    def __init__(self, name):
        self.name = name
        self.w = None
        self.r = {}


class Buf(Tok):
    def __init__(self, name, handle):
        super().__init__(name)
        self.h = handle

    def ap(self):
        return self.h[:]

    def __getitem__(self, idx):
        return self.h[idx]


class _Eng:
    def __init__(self, name, handle, sem):
        self.name = name
        self.h = handle
        self.sem = sem
        self.count = 0
        self.seen = {}


class Prog:
    def __init__(self, nc, n_dma_sems=8):
        self.nc = nc
        self.stack = contextlib.ExitStack()
        self.engs = {}
        for name in ("tensor", "vector", "scalar", "gpsimd", "sync"):
            sem = self.stack.enter_context(nc.semaphore("s_" + name))
            self.engs[name] = _Eng(name, getattr(nc, name), sem)
        self.dma_sems = {}
        for q in ("sync", "gpsimd", "scalar"):
            self.dma_sems[q] = [[self.stack.enter_context(nc.semaphore("d_%s%d" % (q, i))), 0] for i in range(n_dma_sems)]
        self.dma_rr = {"sync": 0, "gpsimd": 0, "scalar": 0}
        self.out_events = []
        self.n_inst = 0
        self.scopes = [self.stack]
        import os
        self.max_ops = int(os.environ["KMAXOPS"]) if "KMAXOPS" in os.environ else None

    @contextlib.contextmanager
    def scope(self):
        st = contextlib.ExitStack()
        self.scopes.append(st)
        try:
            yield
        finally:
            self.barrier()
            self.scopes.pop()
            st.close()

    def barrier(self):
        if getattr(self, "finished", False):
            return
        for eng in self.engs.values():
            for other in self.engs.values():
                if other is not eng and other.count > 0:
                    self._wait(eng, (other.name, other.sem, other.count))
            for q, pool in self.dma_sems.items():
                for i, slot in enumerate(pool):
                    if slot[1] > 0:
                        self._wait(eng, ("d_%s%d" % (q, i), slot[0], 16 * slot[1]))

    def tok(self, name):
        return Tok(name)

    def sb(self, name, shape, dtype):
        self.uid = getattr(self, "uid", 0) + 1
        name = "%s_u%d" % (name, self.uid)
        return Buf(name, self.scopes[-1].enter_context(self.nc.sbuf_tensor(name, list(shape), dtype)))

    def ps(self, name, shape, dtype):
        b = Buf(name, self.scopes[-1].enter_context(self.nc.psum_tensor(name, list(shape), dtype)))
        b.excl = True
        return b

    def dram(self, name, shape, dtype):
        return self.nc.dram_tensor(name, list(shape), dtype, kind="Internal").ap()

    def _wait(self, eng, ev):
        key, sem, val = ev
        if eng.seen.get(key, 0) >= val:
            return
        if key == eng.name and not (SAME_ENGINE_SYNC and eng.name != "tensor" and eng.name != "sync"):
            return
        eng.h.wait_ge(sem, val)
        eng.seen[key] = val

    def _deps(self, eng, reads, writes):
        for t in reads:
            if t.w is not None:
                self._wait(eng, t.w)
        for t in writes:
            if t.w is not None:
                self._wait(eng, t.w)
            for ev in t.r.values():
                self._wait(eng, ev)

    def _record(self, ev, reads, writes):
        for t in writes:
            t.w = ev
            t.r = {}
        for t in reads:
            if t in writes:
                continue
            t.r[ev[0]] = ev

    def op(self, engname, fn, reads=(), writes=()):
        if self.max_ops is not None and self.n_inst >= self.max_ops:
            return None
        eng = self.engs[engname]
        ex = [t for t in reads if getattr(t, "excl", False) and t not in writes]
        if ex:
            reads = [t for t in reads if t not in ex]
            writes = list(writes) + ex
        self._deps(eng, reads, writes)
        inst = fn(eng.h)
        eng.count += 1
        inst.then_inc(eng.sem, 1)
        ev = (eng.name, eng.sem, eng.count)
        self._record(ev, reads, writes)
        self.n_inst += 1
        return ev

    def dma(self, q, out_ap, in_ap, reads=(), writes=(), out=False, **kw):
        if self.max_ops is not None and self.n_inst >= self.max_ops:
            return None
        eng = self.engs[q]
        self._deps(eng, reads, writes)
        pool = self.dma_sems[q]
        i = self.dma_rr[q]
        self.dma_rr[q] = (i + 1) % len(pool)
        slot = pool[i]
        key = "d_%s%d" % (q, i)
        if slot[1] > 0:
            self._wait(eng, (key, slot[0], 16 * slot[1]))
        eng.h.dma_start(out=out_ap, in_=in_ap, **kw).then_inc(slot[0], 16)
        slot[1] += 1
        ev = (key, slot[0], 16 * slot[1])
        self._record(ev, reads, writes)
        if out:
            self.out_events.append(ev)
        self.n_inst += 1
        return ev

    def finish(self):
        eng = self.engs["sync"]
        for q, pool in self.dma_sems.items():
            for i, slot in enumerate(pool):
                if slot[1] > 0:
                    self._wait(eng, ("d_%s%d" % (q, i), slot[0], 16 * slot[1]))
        for name, e in self.engs.items():
            if name != "sync" and e.count > 0:
                self._wait(eng, (name, e.sem, e.count))
        self.finished = True
        for st in reversed(self.scopes):
            st.close()

    def make_identity(self, idt):
        tmp = self.sb(idt.name + "_i", [128, 128], I32)
        self.op("gpsimd", lambda e: e.iota(tmp.ap(), pattern=[[1, 128]], base=0, channel_multiplier=-1), writes=[tmp])
        self.op("vector", lambda e: e.tensor_scalar(out=idt.ap(), in0=tmp.ap(), scalar1=0.0, scalar2=None, op0=ALU.is_equal),
                reads=[tmp], writes=[idt])


S = 4096
D = 1024
NT = 32
DFF = 2816
NFC = 22
EPS = 1e-6
NEG = -1.0e30
MASKV = -30000.0
VW = 66
N_BISECT = 16
IDX_SCALE = (8 ** -0.5) * (64 ** -0.5)
QPERM = [0, 3, 1, 4, 2, 5, 6, 9, 7, 10, 8, 11]

SPEC0 = dict(
    ncols=2184,
    chunks=[(0, 512, [("rope64", 0, 8)]), (512, 512, [("rope64", 0, 8)]), (1024, 512, [("rope32", 0, 8)]),
            (1536, 136, [("rope32", 0, 2), ("wi", 128, 8)]), (1672, 512, [("copy", 0, 512)])],
    tcols=[128 * k for k in range(13)] + [1928, 2056],
    vcols=(1672, 1928),
)
SPEC1 = dict(
    ncols=2560,
    chunks=[(0, 512, [("rope64", 0, 8)]), (512, 512, [("rope64", 0, 8)]), (1024, 512, [("rope64", 0, 8)]),
            (1536, 512, [("copy", 0, 512)]), (2048, 512, [("copy", 0, 512)])],
    tcols=[128 * k for k in range(12)] + [2304, 2432],
    vcols=(1536, 2304),
)


class Ctx:
    pass


def load_weight(P, name, dram, nk, ncols, q="gpsimd"):
    W = P.sb(name, [128, nk, ncols], BF16)
    toks = [P.tok("%s_%d" % (name, k)) for k in range(nk)]
    for k in range(nk):
        c0 = 0
        while c0 < ncols:
            c1 = min(ncols, c0 + 2048)
            P.dma(q, W[:, k, c0:c1], dram[k * 128:(k + 1) * 128, c0:c1], writes=[toks[k]])
            c0 = c1
    return W, toks


def setup_consts(P, C, pos_in):
    C.ident = P.sb("ident", [128, 128], BF16)
    P.make_identity(C.ident)
    C.negthr = P.sb("negthr", [128, 1], F32)
    P.op("vector", lambda e: e.memset(C.negthr.ap(), -1.0e29), writes=[C.negthr])
    C.pow2 = P.sb("pow2", [128, N_BISECT + 2], F32)
    for k in range(N_BISECT + 2):
        P.op("gpsimd", lambda e: e.memset(C.pow2[:, k:k + 1], 2.0 ** (1 - k)), writes=[C.pow2])
    C.MdT = P.sb("MdT", [128, 128], BF16)
    C.MpT = P.sb("MpT", [128, 128], BF16)
    zt = P.sb("zt", [128, 128], F32)
    zm = P.sb("zm", [128, 128], F32)
    P.op("vector", lambda e: e.memset(zt.ap(), 0.0), writes=[zt])
    P.op("gpsimd", lambda e: e.affine_select(out=zm.ap(), in_=zt.ap(), pattern=[[1, 128]], compare_op=ALU.is_ge, fill=MASKV, base=0, channel_multiplier=-1),
         reads=[zt], writes=[zm])
    P.op("vector", lambda e: e.tensor_copy(C.MdT.ap(), zm.ap()), reads=[zm], writes=[C.MdT])
    P.op("gpsimd", lambda e: e.affine_select(out=zm.ap(), in_=zt.ap(), pattern=[[-1, 128]], compare_op=ALU.is_ge, fill=MASKV, base=0, channel_multiplier=1),
         reads=[zt, C.MdT], writes=[zm])
    P.op("vector", lambda e: e.tensor_copy(C.MpT.ap(), zm.ap()), reads=[zm], writes=[C.MpT])


def build_rope_tables(P, C, pos_in):
    C.cos64 = P.sb("cos64", [128, NT, 32], F32)
    C.sin64 = P.sb("sin64", [128, NT, 32], F32)
    C.nsin64 = P.sb("nsin64", [128, NT, 32], F32)
    C.cos16 = P.sb("cos16", [128, NT, 16], F32)
    C.sin16 = P.sb("sin16", [128, NT, 16], F32)
    C.nsin16 = P.sb("nsin16", [128, NT, 16], F32)
    with P.scope():
        posi = P.sb("posi", [128, NT], I32)
        posf = P.sb("posf", [128, NT], F32)
        P.dma("sync", posi.ap(), pos_in, writes=[posi])
        P.op("vector", lambda e: e.tensor_copy(posf.ap(), posi.ap()), reads=[posi], writes=[posf])
        for half, cosT, sinT, nsinT in ((32, C.cos64, C.sin64, C.nsin64), (16, C.cos16, C.sin16, C.nsin16)):
            n = NT * half
            fr = P.sb("fr%d" % half, [128, half], F32)
            a = P.sb("a%d" % half, [128, NT, half], F32)
            ki = P.sb("ki%d" % half, [128, NT, half], I32)
            kf = P.sb("kf%d" % half, [128, NT, half], F32)
            fr1 = P.sb("fr1%d" % half, [128, NT, half], F32)
            m1 = P.sb("m1%d" % half, [128, NT, half], F32)
            f0 = 0 if half == 32 else 32
            P.dma("sync", fr.ap(), C.freqs_in[:, f0:f0 + half], writes=[fr])
            P.op("vector", lambda e: e.tensor_tensor(out=a.ap(), in0=posf.ap().unsqueeze(2).broadcast_to([128, NT, half]),
                                                     in1=fr.ap().unsqueeze(1).broadcast_to([128, NT, half]), op=ALU.mult),
                 reads=[posf, fr], writes=[a])
            P.op("vector", lambda e: e.tensor_scalar(out=a.ap(), in0=a.ap(), scalar1=float(1.0 / (2.0 * np.pi)), scalar2=None, op0=ALU.mult),
                 reads=[a], writes=[a])
            for shift, outT, neg in ((0.0, sinT, False), (0.25, cosT, False), (0.5, nsinT, False)):
                src = a
                if shift != 0.0:
                    P.op("vector", lambda e: e.tensor_scalar(out=fr1.ap(), in0=a.ap(), scalar1=shift, scalar2=None, op0=ALU.add),
                         reads=[a], writes=[fr1])
                    src = fr1
                P.op("vector", lambda e: e.tensor_copy(ki.ap(), src.ap()), reads=[src], writes=[ki])
                P.op("vector", lambda e: e.tensor_copy(kf.ap(), ki.ap()), reads=[ki], writes=[kf])
                P.op("vector", lambda e: e.tensor_tensor(out=kf.ap(), in0=src.ap(), in1=kf.ap(), op=ALU.subtract), reads=[src, kf], writes=[kf])
                P.op("vector", lambda e: e.tensor_scalar(out=m1.ap(), in0=kf.ap(), scalar1=0.5, scalar2=None, op0=ALU.is_gt), reads=[kf], writes=[m1])
                P.op("vector", lambda e: e.tensor_tensor(out=kf.ap(), in0=kf.ap(), in1=m1.ap(), op=ALU.subtract), reads=[kf, m1], writes=[kf])
                P.op("vector", lambda e: e.tensor_scalar(out=m1.ap(), in0=kf.ap(), scalar1=-0.5, scalar2=None, op0=ALU.is_lt), reads=[kf], writes=[m1])
                P.op("vector", lambda e: e.tensor_tensor(out=kf.ap(), in0=kf.ap(), in1=m1.ap(), op=ALU.add), reads=[kf, m1], writes=[kf])
                P.op("scalar", lambda e: e.activation(out=outT.ap(), in_=kf.ap(), func=AF.Sin, scale=float(2.0 * np.pi) * (1.0 - 1e-6)),
                     reads=[kf], writes=[outT])


def alloc_norm_work(P, C, tag):
    W = Ctx()
    W.sqj = P.sb(tag + "sqj", [128, D], BF16)
    W.small = [[P.sb("%ssm%d_%d" % (tag, r, j), [128, 1], F32) for j in range(4)] for r in range(2)]
    W.hb = [P.sb("%shb%d" % (tag, r), [128, D], BF16) for r in range(2)]
    W.k = 0
    return W


def rmsnorm_to_hT(P, C, W, xt, gb, hT_ap, hT_tok, bank, evac_eng="scalar"):
    r = W.k % 2
    W.k += 1
    ssq, t1, t2, rstd = W.small[r]
    hb = W.hb[r]
    P.op("scalar", lambda e: e.activation(out=W.sqj.ap(), in_=xt.ap(), func=AF.Square, accum_out=ssq.ap()), reads=[xt], writes=[W.sqj, ssq])
    P.op("vector", lambda e: e.tensor_scalar(out=t1.ap(), in0=ssq.ap(), scalar1=1.0 / D, scalar2=EPS, op0=ALU.mult, op1=ALU.add), reads=[ssq], writes=[t1])
    P.op("scalar", lambda e: e.activation(out=t2.ap(), in_=t1.ap(), func=AF.Sqrt), reads=[t1], writes=[t2])
    P.op("vector", lambda e: e.reciprocal(out=rstd.ap(), in_=t2.ap()), reads=[t2], writes=[rstd])
    P.op("vector", lambda e: e.scalar_tensor_tensor(out=hb.ap(), in0=xt.ap(), scalar=rstd.ap(), in1=gb.ap(), op0=ALU.mult, op1=ALU.mult),
         reads=[xt, rstd, gb], writes=[hb])
    bbf = bank.ap().bitcast(BF16)
    for c in range(8):
        P.op("tensor", lambda e: e.transpose(bbf[:, c * 128:(c + 1) * 128], hb[:, c * 128:(c + 1) * 128], C.ident.ap()),
             reads=[hb, C.ident], writes=[bank])
    srcv = bbf if len(hT_ap.shape) == 2 else bbf.rearrange("p (c t) -> p c t", t=128)
    if evac_eng == "scalar":
        P.op("scalar", lambda e: e.activation(out=hT_ap, in_=srcv, func=AF.Copy), reads=[bank], writes=[hT_tok])
    else:
        P.op("vector", lambda e: e.tensor_copy(hT_ap, srcv), reads=[bank], writes=[hT_tok])
    return rstd


def load_gain(P, C, name, gains, row):
    gb = P.sb(name, [128, D], F32)
    P.dma("sync", gb.ap(), gains[row].partition_broadcast(128), writes=[gb])
    return gb


def phase_A(P, C, tag, xsrc, w_dram, gains, grow, spec, fT, vS, wiAll, pos_in):
    ncols = spec["ncols"]
    with P.scope():
        build_rope_tables(P, C, pos_in)
        Win, Wt = load_weight(P, tag + "Win", w_dram, 8, ncols)
        gb = load_gain(P, C, tag + "gbA", gains, grow)
        NW = alloc_norm_work(P, C, tag + "A")
        xts = [P.sb("%sxt%d" % (tag, r), [128, D], F32) for r in range(2)]
        hTs = [P.sb("%shT%d" % (tag, r), [128, D], BF16) for r in range(2)]
        pts = [P.sb("%spt%d" % (tag, r), [128, ncols], BF16) for r in range(2)]
        t1s = [P.sb("%st1_%d" % (tag, r), [128, 512], F32) for r in range(2)]
        t2s = [P.sb("%st2_%d" % (tag, r), [128, 512], F32) for r in range(2)]
        ntc = len(spec["tcols"])
        fTs = [P.sb("%sfTs%d" % (tag, r), [128, ntc, 128], BF16) for r in range(2)]
        fT_r = fT.rearrange("c p t -> p c t")
        cc = 0
        for i in range(NT):
            xt = xts[i % 2]
            hT = hTs[i % 2]
            pt = pts[i % 2]
            P.dma("sync", xt.ap(), xsrc[i * 128:(i + 1) * 128, :], writes=[xt])
            rmsnorm_to_hT(P, C, NW, xt, gb, hT.ap(), hT, C.banks[0], evac_eng="scalar")
            for (col0, width, handlers) in spec["chunks"]:
                bank = C.banks[1 + (cc % 4)]
                t1 = t1s[cc % 2]
                t2 = t2s[cc % 2]
                cc += 1
                for k in range(8):
                    P.op("tensor", lambda e: e.matmul(bank[:, 0:width], lhsT=hT[:, k * 128:(k + 1) * 128], rhs=Win[:, k, col0:col0 + width],
                                                      start=(k == 0), stop=(k == 7)), reads=[hT, Wt[k]], writes=[bank])
                for (kind, l0, n) in handlers:
                    if kind == "rope64":
                        nh = n
                        w = nh * 64
                        xv2 = bank[:, l0:l0 + w].rearrange("p (h d) -> p h d", d=32)
                        xv = bank[:, l0:l0 + w].rearrange("p (h d) -> p h d", d=64)
                        t1v2 = t1[:, 0:w].rearrange("p (h d) -> p h d", d=32)
                        t2v = t2[:, 0:w].rearrange("p (h d) -> p h d", d=64)
                        cosb = C.cos64[:, i:i + 1, :].broadcast_to([128, 2 * nh, 32])
                        sinb = C.sin64[:, i:i + 1, :].broadcast_to([128, nh, 32])
                        nsinb = C.nsin64[:, i:i + 1, :].broadcast_to([128, nh, 32])
                        P.op("vector", lambda e: e.tensor_tensor(out=t1v2, in0=xv2, in1=cosb, op=ALU.mult), reads=[bank, C.cos64], writes=[t1])
                        P.op("vector", lambda e: e.tensor_tensor(out=t2v[:, :, 0:32], in0=xv[:, :, 32:64], in1=nsinb, op=ALU.mult),
                             reads=[bank, C.nsin64], writes=[t2])
                        P.op("vector", lambda e: e.tensor_tensor(out=t2v[:, :, 32:64], in0=xv[:, :, 0:32], in1=sinb, op=ALU.mult),
                             reads=[bank, C.sin64], writes=[t2])
                        P.op("gpsimd", lambda e: e.tensor_tensor(out=pt[:, col0 + l0:col0 + l0 + w], in0=t1[:, 0:w], in1=t2[:, 0:w], op=ALU.add),
                             reads=[t1, t2], writes=[pt])
                    elif kind == "rope32":
                        nh = n
                        w = nh * 64
                        xv = bank[:, l0:l0 + w].rearrange("p (h d) -> p h d", d=64)
                        xr4 = bank[:, l0:l0 + w].rearrange("p (h t d) -> p h t d", t=4, d=16)
                        t1r4 = t1[:, 0:w].rearrange("p (h t d) -> p h t d", t=4, d=16)
                        t2v = t2[:, 0:w].rearrange("p (h d) -> p h d", d=64)
                        t1v = t1[:, 0:w].rearrange("p (h d) -> p h d", d=64)
                        ptv = pt[:, col0 + l0:col0 + l0 + w].rearrange("p (h d) -> p h d", d=64)
                        cosb = C.cos16[:, i:i + 1, :].unsqueeze(1).broadcast_to([128, nh, 2, 16])
                        sinb = C.sin16[:, i:i + 1, :].broadcast_to([128, nh, 16])
                        nsinb = C.nsin16[:, i:i + 1, :].broadcast_to([128, nh, 16])
                        P.op("vector", lambda e: e.tensor_tensor(out=t1r4[:, :, 0:2, :], in0=xr4[:, :, 0:2, :], in1=cosb, op=ALU.mult),
                             reads=[bank, C.cos16], writes=[t1])
                        P.op("vector", lambda e: e.tensor_tensor(out=t2v[:, :, 0:16], in0=xv[:, :, 16:32], in1=nsinb, op=ALU.mult),
                             reads=[bank, C.nsin16], writes=[t2])
                        P.op("vector", lambda e: e.tensor_tensor(out=t2v[:, :, 16:32], in0=xv[:, :, 0:16], in1=sinb, op=ALU.mult),
                             reads=[bank, C.sin16], writes=[t2])
                        P.op("gpsimd", lambda e: e.tensor_tensor(out=ptv[:, :, 0:32], in0=t1v[:, :, 0:32], in1=t2v[:, :, 0:32], op=ALU.add),
                             reads=[t1, t2], writes=[pt])
                        P.op("scalar", lambda e: e.activation(out=ptv[:, :, 32:64], in_=xv[:, :, 32:64], func=AF.Copy), reads=[bank], writes=[pt])
                    elif kind == "copy":
                        P.op("scalar", lambda e: e.activation(out=pt[:, col0 + l0:col0 + l0 + n], in_=bank[:, l0:l0 + n], func=AF.Copy),
                             reads=[bank], writes=[pt])
                    elif kind == "wi":
                        P.op("scalar", lambda e: e.activation(out=wiAll[:, i, :], in_=bank[:, l0:l0 + n], func=AF.Copy, scale=float(IDX_SCALE)),
                             reads=[bank], writes=[wiAll])
            fts = fTs[i % 2]
            for g0 in range(0, ntc, 8):
                g1 = min(ntc, g0 + 8)
                bank = C.banks[5 + (g0 // 8)]
                bbf = bank.ap().bitcast(BF16)
                for k in range(g0, g1):
                    c0 = spec["tcols"][k]
                    P.op("tensor", lambda e: e.transpose(bbf[:, (k - g0) * 128:(k - g0 + 1) * 128], pt[:, c0:c0 + 128], C.ident.ap()),
                         reads=[pt, C.ident], writes=[bank])
                eng = "vector" if g0 == 0 else "scalar"
                if eng == "vector":
                    P.op("vector", lambda e: e.tensor_copy(fts[:, g0:g1, :], bbf[:, 0:(g1 - g0) * 128].rearrange("p (c t) -> p c t", t=128)),
                         reads=[bank], writes=[fts])
                else:
                    P.op("scalar", lambda e: e.activation(out=fts[:, g0:g1, :], in_=bbf[:, 0:(g1 - g0) * 128].rearrange("p (c t) -> p c t", t=128),
                                                          func=AF.Copy), reads=[bank], writes=[fts])
            P.dma("sync", fT_r[:, :, i * 128:(i + 1) * 128], fts.ap(), reads=[fts])
            v0, v1 = spec["vcols"]
            P.dma("sync", vS[i * 128:(i + 1) * 128, :], pt[:, v0:v1], reads=[pt])


def phase_M(P, C, tag, mem_in, w_dram, gains, grow, kmT, Vm):
    with P.scope():
        Wm, Wt = load_weight(P, tag + "Wm", w_dram, 8, 512)
        gb = load_gain(P, C, tag + "gbM", gains, grow)
        NW = alloc_norm_work(P, C, tag + "M")
        xts = [P.sb("%smx%d" % (tag, r), [128, D], F32) for r in range(2)]
        hTs = [P.sb("%smhT%d" % (tag, r), [128, D], BF16) for r in range(2)]
        kb16 = [P.sb("%skb16_%d" % (tag, r), [128, 256], BF16) for r in range(2)]
        P.op("gpsimd", lambda e: e.memset(Vm.ap(), 1.0), writes=[Vm])
        for mb in range(2):
            xt, hT = xts[mb], hTs[mb]
            P.dma("sync", xt.ap(), mem_in[mb * 128:(mb + 1) * 128, :], writes=[xt])
            rmsnorm_to_hT(P, C, NW, xt, gb, hT.ap(), hT, C.banks[0])
            bank = C.banks[1 + mb]
            for k in range(8):
                P.op("tensor", lambda e: e.matmul(bank.ap(), lhsT=hT[:, k * 128:(k + 1) * 128], rhs=Wm[:, k, :], start=(k == 0), stop=(k == 7)),
                     reads=[hT, Wt[k]], writes=[bank])
            P.op("scalar", lambda e: e.activation(out=kb16[mb].ap(), in_=bank[:, 0:256], func=AF.Copy), reads=[bank], writes=[kb16[mb]])
            P.op("vector", lambda e: e.tensor_copy(Vm[:, mb, :, 0:64], bank[:, 256:512].rearrange("p (h d) -> p h d", d=64)),
                 reads=[bank], writes=[Vm])
            tb = C.banks[3 + mb]
            tbf = tb.ap().bitcast(BF16)
            for c in range(2):
                P.op("tensor", lambda e: e.transpose(tbf[:, c * 128:(c + 1) * 128], kb16[mb][:, c * 128:(c + 1) * 128], C.ident.ap()),
                     reads=[kb16[mb], C.ident], writes=[tb])
            P.op("vector", lambda e: e.tensor_copy(kmT[:, :, mb * 128:(mb + 1) * 128], tbf[:, 0:256].rearrange("p (c t) -> p c t", t=128)),
                 reads=[tb], writes=[kmT])


def attn_finish(P, C, W, xsrc, xdst, i, nheads, Wout, Wot, nkc):
    r = i % 2
    attn = W.attn[r]
    rec = W.rec[r]
    xt = W.xts[r]
    P.dma("sync", xt.ap(), xsrc[i * 128:(i + 1) * 128, :], writes=[xt])
    h0 = 0
    for (bank, oap, nh) in W.osrc(r):
        ov = oap.rearrange("p (h d) -> p h d", d=65)
        P.op("vector", lambda e: e.reciprocal(out=rec[:, h0:h0 + nh], in_=ov[:, :, 64]), reads=[bank], writes=[rec])
        P.op("vector", lambda e: e.tensor_tensor(out=attn[:, h0 * 64:(h0 + nh) * 64].rearrange("p (h d) -> p h d", d=64), in0=ov[:, :, 0:64],
                                                 in1=rec[:, h0:h0 + nh].unsqueeze(2).broadcast_to([128, nh, 64]), op=ALU.mult),
             reads=[bank, rec], writes=[attn])
        h0 += nh
    tb = W.tbank
    tbf = tb.ap().bitcast(BF16)
    aT = W.attnT[r]
    for c in range(nkc):
        P.op("tensor", lambda e: e.transpose(tbf[:, c * 128:(c + 1) * 128], attn[:, c * 128:(c + 1) * 128], C.ident.ap()),
             reads=[attn, C.ident], writes=[tb])
    P.op("scalar", lambda e: e.activation(out=aT[:, 0:nkc * 128], in_=tbf[:, 0:nkc * 128], func=AF.Copy), reads=[tb], writes=[aT])
    xn = W.xn[r]
    for half in range(2):
        yb = W.ybanks[half]
        for c in range(nkc):
            P.op("tensor", lambda e: e.matmul(yb.ap(), lhsT=aT[:, c * 128:(c + 1) * 128], rhs=Wout[:, c, half * 512:(half + 1) * 512],
                                              start=(c == 0), stop=(c == nkc - 1)), reads=[aT, Wot[c]], writes=[yb])
        P.op("vector", lambda e: e.tensor_tensor(out=xn[:, half * 512:(half + 1) * 512], in0=yb.ap(), in1=xt[:, half * 512:(half + 1) * 512], op=ALU.add),
             reads=[yb, xt], writes=[xn])
    P.dma("sync", xdst[i * 128:(i + 1) * 128, :], xn.ap(), reads=[xn])


def mem_heads(P, C, W, qall, qch0, kmT, Vm, obank, sbank, r):
    PT = W.PTm[r]
    for half in range(2):
        sb_ = sbank[half]
        for hh in range(2):
            hm = half * 2 + hh
            for mb in range(2):
                j = hh * 2 + mb
                P.op("tensor", lambda e: e.matmul(sb_[:, j * 128:(j + 1) * 128], lhsT=kmT[:, hm // 2, mb * 128:(mb + 1) * 128],
                                                  rhs=qall[:, qch0 + hm, :], start=True, stop=True),
                     reads=[kmT, qall], writes=[sb_])
        P.op("scalar", lambda e: e.activation(out=PT[:, half * 512:(half + 1) * 512], in_=sb_.ap(), func=AF.Exp, scale=0.125),
             reads=[sb_], writes=[PT])
    for hm in range(4):
        for mb in range(2):
            j = hm * 2 + mb
            P.op("tensor", lambda e: e.matmul(obank[:, hm * 65:(hm + 1) * 65], lhsT=PT[:, j * 128:(j + 1) * 128], rhs=Vm[:, mb, hm, 0:65],
                                              start=(mb == 0), stop=(mb == 1)), reads=[PT, Vm], writes=[obank])


def alloc_finish_work(P, C, tag, obanks, tbank, ybanks):
    W = Ctx()
    W.attn = [P.sb("%sattn%d" % (tag, r), [128, D], BF16) for r in range(2)]
    W.attnT = [P.sb("%sattnT%d" % (tag, r), [128, D], BF16) for r in range(2)]
    W.rec = [P.sb("%srec%d" % (tag, r), [128, 16], F32) for r in range(2)]
    W.xts = [P.sb("%sfx%d" % (tag, r), [128, D], F32) for r in range(2)]
    W.xn = [P.sb("%sxn%d" % (tag, r), [128, D], F32) for r in range(2)]
    W.PTm = [P.sb("%sPTm%d" % (tag, r), [128, 1024], BF16) for r in range(2)]
    W.osrc = lambda r: [(bk, bk[:, 0:nh * 65], nh) for (bk, nh) in obanks]
    W.tbank = tbank
    W.ybanks = ybanks
    return W


def phase_B0(P, C, xsrc, xdst, fT, vS, wiAll, kmT, Vm, wout_dram, nblocks=NT):
    with P.scope():
        Wout, Wot = load_weight(P, "Wout0", wout_dram, 8, D)
        kT = P.sb("kT", [128, 2, S], BF16)
        kiT = P.sb("kiT", [128, S], BF16)
        Va = P.sb("Va", [128, NT, 4, VW], BF16)
        P.op("gpsimd", lambda e: e.memset(Va.ap(), 1.0), writes=[Va])
        for c in range(2):
            P.dma("sync", kT[:, c, :], fT[6 + c], writes=[kT])
        P.dma("sync", kiT.ap(), fT[12], writes=[kiT])
        vr = vS.rearrange("(i p) (g d) -> p i g d", p=128, d=64)
        for i0 in range(NT):
            P.dma("sync", Va[:, i0, :, 0:64], vr[:, i0, :, :], writes=[Va])
        score = P.sb("score", [128, S], F32)
        junk = P.sb("junk", [128, S], BF16)
        Bs = [P.sb("Bm%d" % r, [128, S], BF16) for r in range(2)]
        Rs = [P.sb("R%d" % r, [128, 512], F32) for r in range(3)]
        PTs = [P.sb("PT%d" % r, [128, 512], BF16) for r in range(3)]
        qalls = [P.sb("qall%d" % r, [128, 24, 128], BF16) for r in range(2)]
        for r in range(2):
            P.op("gpsimd", lambda e: e.memset(qalls[r].ap(), 0.0), writes=[qalls[r]])
        sm = [[P.sb("bs%d_%d" % (r, j), [128, 1], F32) for j in range(6)] for r in range(2)]
        wtabs = [P.sb("wtab%d" % r, [128, N_BISECT + 2], F32) for r in range(2)]
        FW = alloc_finish_work(P, C, "b0", [(C.banks[3], 6), (C.banks[4], 6), (C.banks[5], 4)], C.banks[0], [C.banks[1], C.banks[2]])
        fT_r = fT.rearrange("c p t -> p c t")
        cnts = dict(lc=0, sc=0)

        def prep(b):
            lc = cnts["lc"]
            r = b % 2
            N = 128 * (b + 1)
            qall = qalls[r]
            B = Bs[r]
            q0 = b * 128
            for (slot0, nch, ch0) in ((0, 6, 0), (12, 4, 8), (20, 2, 13)):
                for base in (0, 64):
                    P.dma("sync", qall[base:base + 64, slot0 + base // 64:slot0 + 2 * nch:2, :], fT_r[base:base + 64, ch0:ch0 + nch, q0:q0 + 128],
                          writes=[qall])
            for c0 in range(0, N, 512):
                wc = min(512, N - c0)
                for h in range(8):
                    bank = C.banks[6 + (lc % 2)]
                    R = Rs[lc % 3]
                    lc += 1
                    P.op("tensor", lambda e: e.matmul(bank[:, 0:wc], lhsT=qall[:, 12 + h, :], rhs=kiT[:, c0:c0 + wc],
                                                      start=True, stop=True), reads=[qall, kiT], writes=[bank])
                    P.op("scalar", lambda e: e.activation(out=R[:, 0:wc], in_=bank[:, 0:wc], func=AF.Relu), reads=[bank], writes=[R])
                    if h == 0:
                        P.op("vector", lambda e: e.tensor_scalar(out=score[:, c0:c0 + wc], in0=R[:, 0:wc], scalar1=wiAll[:, b, 0:1], scalar2=None, op0=ALU.mult),
                             reads=[R, wiAll], writes=[score])
                    else:
                        P.op("vector", lambda e: e.scalar_tensor_tensor(out=score[:, c0:c0 + wc], in0=R[:, 0:wc], scalar=wiAll[:, b, h:h + 1],
                                                                       in1=score[:, c0:c0 + wc], op0=ALU.mult, op1=ALU.add),
                             reads=[R, wiAll, score], writes=[score])
            mn, mx, mid, cnt, aa, thr = sm[r]
            if b >= 2:
                wtab = wtabs[r]
                P.op("vector", lambda e: e.tensor_reduce(out=mn.ap(), in_=score[:, 0:N - 128], axis=AX.X, op=ALU.min), reads=[score], writes=[mn])
            P.op("gpsimd", lambda e: e.affine_select(out=score[:, q0:q0 + 128], in_=score[:, q0:q0 + 128], pattern=[[-1, 128]], compare_op=ALU.is_ge,
                                                    fill=NEG, base=0, channel_multiplier=1), reads=[score], writes=[score])
            if b >= 2:
                P.op("vector", lambda e: e.tensor_reduce(out=mx.ap(), in_=score[:, 0:N], axis=AX.X, op=ALU.max), reads=[score], writes=[mx])
                P.op("vector", lambda e: e.tensor_tensor(out=aa.ap(), in0=mx.ap(), in1=mn.ap(), op=ALU.subtract), reads=[mx, mn], writes=[aa])
                P.op("vector", lambda e: e.tensor_scalar(out=wtab.ap(), in0=C.pow2.ap(), scalar1=aa.ap(), scalar2=0.5, op0=ALU.mult, op1=ALU.mult),
                     reads=[C.pow2, aa], writes=[wtab])
                P.op("vector", lambda e: e.tensor_tensor(out=mid.ap(), in0=mn.ap(), in1=wtab[:, 1:2], op=ALU.add), reads=[mn, wtab], writes=[mid])
                for k in range(N_BISECT):
                    P.op("vector", lambda e: e.tensor_scalar(out=junk[:, 0:N], in0=score[:, 0:N], scalar1=mid.ap(), scalar2=None, op0=ALU.is_ge, op1=ALU.add,
                                                             accum_out=cnt.ap()), reads=[score, mid], writes=[junk, cnt])
                    P.op("vector", lambda e: e.tensor_scalar(out=aa.ap(), in0=cnt.ap(), scalar1=255.5, scalar2=-0.5, op0=ALU.is_ge, op1=ALU.add),
                         reads=[cnt], writes=[aa])
                    P.op("vector", lambda e: e.scalar_tensor_tensor(out=mid.ap(), in0=aa.ap(), scalar=wtab[:, k + 1:k + 2], in1=mid.ap(), op0=ALU.mult, op1=ALU.add),
                         reads=[aa, wtab, mid], writes=[mid])
                P.op("vector", lambda e: e.tensor_tensor(out=thr.ap(), in0=mid.ap(), in1=wtab[:, N_BISECT + 1:N_BISECT + 2], op=ALU.subtract),
                     reads=[mid, wtab], writes=[thr])
                thr_t = thr
            else:
                thr_t = C.negthr
            P.op("vector", lambda e: e.tensor_scalar(out=B[:, 0:N], in0=score[:, 0:N], scalar1=thr_t.ap(), scalar2=MASKV, op0=ALU.is_lt, op1=ALU.mult),
                 reads=[score, thr_t], writes=[B])
            if C.dbg is not None and b == C.dbg_block:
                P.dma("sync", C.dbg["score"], score.ap(), reads=[score])
                P.dma("sync", C.dbg["thr"], thr_t.ap(), reads=[thr_t])
                P.dma("sync", C.dbg["Bm"], B.ap(), reads=[B])
            cnts["lc"] = lc

        def attend(b):
            sc = cnts["sc"]
            r = b % 2
            qall = qalls[r]
            B = Bs[r]
            jobs = []
            for h in range(12):
                pos = QPERM.index(h)
                g = h // 3
                obank = C.banks[3 + h // 6]
                ocol = (h % 6) * 65
                for kb0 in range(0, b + 1, 4):
                    kb1 = min(b + 1, kb0 + 4)
                    jobs.append((pos, g, obank, ocol, kb0, kb1))
            prev = None
            for job in jobs + [None]:
                if job is not None:
                    pos, g, obank, ocol, kb0, kb1 = job
                    sbank = C.banks[sc % 3]
                    PT = PTs[sc % 3]
                    sc += 1
                    for kb in range(kb0, kb1):
                        j = kb - kb0
                        P.op("tensor", lambda e: e.matmul(sbank[:, j * 128:(j + 1) * 128], lhsT=kT[:, g // 2, kb * 128:(kb + 1) * 128],
                                                          rhs=qall[:, pos, :], start=True, stop=False), reads=[kT, qall], writes=[sbank])
                        P.op("tensor", lambda e: e.matmul(sbank[:, j * 128:(j + 1) * 128], lhsT=B[:, kb * 128:(kb + 1) * 128], rhs=C.ident.ap(),
                                                          start=False, stop=True), reads=[B, C.ident], writes=[sbank])
                    nw = (kb1 - kb0) * 128
                    P.op("scalar", lambda e: e.activation(out=PT[:, 0:nw], in_=sbank[:, 0:nw], func=AF.Exp, scale=0.125), reads=[sbank], writes=[PT])
                if prev is not None:
                    (pos_, g_, obank_, ocol_, kb0_, kb1_), PT_ = prev
                    for kb in range(kb0_, kb1_):
                        j = kb - kb0_
                        P.op("tensor", lambda e: e.matmul(obank_[:, ocol_:ocol_ + 65], lhsT=PT_[:, j * 128:(j + 1) * 128], rhs=Va[:, kb, g_, 0:65],
                                                          start=(kb == 0), stop=(kb == b)), reads=[PT_, Va], writes=[obank_])
                prev = (job, PT) if job is not None else None
            cnts["sc"] = sc
            mem_heads(P, C, FW, qall, 20, kmT, Vm, C.banks[5], [C.banks[6], C.banks[7]], r)
            attn_finish(P, C, FW, xsrc, xdst, b, 16, Wout, Wot, 8)

        prep(0)
        for b in range(nblocks):
            if b + 1 < nblocks:
                prep(b + 1)
            attend(b)


def phase_F(P, C, tag, xnorm, xbase, xdst, wgu_dram, wd_dram, gains, grow, f0, f1, final_row=None, ntiles=NT):
    nf = f1 - f0
    with P.scope():
        Wgu = P.sb(tag + "Wgu", [128, 8, 2 * nf * 128], BF16)
        Wgt = [P.tok("%sWgt%d" % (tag, k)) for k in range(8)]
        for k in range(8):
            for half in range(2):
                c0 = half * DFF + f0 * 128
                P.dma("gpsimd", Wgu[:, k, half * nf * 128:(half + 1) * nf * 128], wgu_dram[k * 128:(k + 1) * 128, c0:c0 + nf * 128], writes=[Wgt[k]])
        Wd = P.sb(tag + "Wd", [128, nf, D], BF16)
        Wdt = [P.tok("%sWdt%d" % (tag, k)) for k in range(nf)]
        for k in range(nf):
            P.dma("gpsimd", Wd[:, k, :], wd_dram[(f0 + k) * 128:(f0 + k + 1) * 128, :], writes=[Wdt[k]])
        gb = load_gain(P, C, tag + "gbF", gains, grow)
        gfin = load_gain(P, C, tag + "gfin", gains, final_row) if final_row is not None else None
        NW = alloc_norm_work(P, C, tag + "F")
        xts = [P.sb("%sFx%d" % (tag, r), [128, D], F32) for r in range(2)]
        hT2 = [P.sb("%sFhT%d" % (tag, r), [128, 8, 256], BF16) for r in range(2)]
        hTt = [[P.tok("%sFhTt%d_%d" % (tag, r, t)) for t in range(2)] for r in range(2)]
        actT = [P.sb("%sactT%d" % (tag, r), [128, nf, 256], BF16) for r in range(2)]
        sg = [P.sb("%ssg%d" % (tag, r), [128, 256], F32) for r in range(2)]
        xn = [P.sb("%sFxn%d" % (tag, r), [128, D], F32) for r in range(4)]
        fsm = [[P.sb("%sfs%d_%d" % (tag, r, j), [128, 1], F32) for j in range(4)] for r in range(2)]
        fj = P.sb(tag + "fj", [128, D], BF16)
        gc = 0
        for G in range(ntiles // 2):
            hT = hT2[G % 2]
            aT = actT[G % 2]
            for t in range(2):
                i = 2 * G + t
                xt = xts[i % 2]
                P.dma("sync", xt.ap(), xnorm[i * 128:(i + 1) * 128, :], writes=[xt])
                rmsnorm_to_hT(P, C, NW, xt, gb, hT[:, :, t * 128:(t + 1) * 128], hTt[G % 2][t], C.banks[0],
                              evac_eng="scalar" if t == 0 else "vector")
                xo = xn[i % 4]
                P.dma("sync", xo.ap(), xbase[i * 128:(i + 1) * 128, :], writes=[xo])
            for fc in range(nf):
                bank = C.banks[1 + (gc % 3)]
                s_ = sg[gc % 2]
                gc += 1
                for half in range(2):
                    coff = half * nf * 128 + fc * 128
                    for k in range(8):
                        P.op("tensor", lambda e: e.matmul(bank[:, half * 256:(half + 1) * 256], lhsT=Wgu[:, k, coff:coff + 128], rhs=hT[:, k, :],
                                                          start=(k == 0), stop=(k == 7)), reads=[Wgt[k]] + hTt[G % 2], writes=[bank])
                P.op("scalar", lambda e: e.activation(out=s_.ap(), in_=bank[:, 0:256], func=AF.Silu), reads=[bank], writes=[s_])
                P.op("vector", lambda e: e.tensor_tensor(out=aT[:, fc, :], in0=bank[:, 256:512], in1=s_.ap(), op=ALU.mult), reads=[bank, s_], writes=[aT])
            for t in range(2):
                i = 2 * G + t
                xo = xn[i % 4]
                for half in range(2):
                    yb = C.banks[4 + (2 * t + half) % 4]
                    for fc in range(nf):
                        P.op("tensor", lambda e: e.matmul(yb.ap(), lhsT=aT[:, fc, t * 128:(t + 1) * 128], rhs=Wd[:, fc, half * 512:(half + 1) * 512],
                                                          start=(fc == 0), stop=(fc == nf - 1)), reads=[aT, Wdt[fc]], writes=[yb])
                    P.op("vector", lambda e: e.tensor_tensor(out=xo[:, half * 512:(half + 1) * 512], in0=yb.ap(), in1=xo[:, half * 512:(half + 1) * 512], op=ALU.add),
                         reads=[yb, xo], writes=[xo])
                if gfin is not None:
                    ssq, t1, t2, rstd = fsm[i % 2]
                    P.op("scalar", lambda e: e.activation(out=fj.ap(), in_=xo.ap(), func=AF.Square, accum_out=ssq.ap()), reads=[xo], writes=[fj, ssq])
                    P.op("vector", lambda e: e.tensor_scalar(out=t1.ap(), in0=ssq.ap(), scalar1=1.0 / D, scalar2=EPS, op0=ALU.mult, op1=ALU.add), reads=[ssq], writes=[t1])
                    P.op("scalar", lambda e: e.activation(out=t2.ap(), in_=t1.ap(), func=AF.Sqrt), reads=[t1], writes=[t2])
                    P.op("vector", lambda e: e.reciprocal(out=rstd.ap(), in_=t2.ap()), reads=[t2], writes=[rstd])
                    P.op("vector", lambda e: e.scalar_tensor_tensor(out=xo.ap(), in0=xo.ap(), scalar=rstd.ap(), in1=gfin.ap(), op0=ALU.mult, op1=ALU.mult),
                         reads=[xo, rstd, gfin], writes=[xo])
                P.dma("sync", xdst[i * 128:(i + 1) * 128, :], xo.ap(), reads=[xo], out=(gfin is not None))


def phase_B1(P, C, fT, vS, Og):
    for g, dil in enumerate((1, 4, 16)):
        nb = NT // dil
        with P.scope():
            qz = P.sb("qz1", [128, 4, S], BF16)
            kTg = P.sb("kTg", [128, 2, S], BF16)
            Vg = P.sb("Vg", [128, NT, 4, VW], BF16)
            P.op("gpsimd", lambda e: e.memset(qz.ap(), 0.0), writes=[qz])
            P.op("vector", lambda e: e.memset(Vg.ap(), 1.0), writes=[Vg])
            for c in range(2):
                for hh in range(2):
                    base = hh * 64
                    P.dma("sync", qz[base:base + 64, 2 * c + hh, :], fT[4 * g + c][base:base + 64, :], writes=[qz])
                P.dma("sync", kTg[:, c, :], fT[4 * g + 2 + c], writes=[kTg])
            vg = vS[:, g * 256:(g + 1) * 256].rearrange("(mb p dd) (h e) -> dd p mb h e", p=128, dd=dil, e=64)
            for r in range(dil):
                for m0 in range(nb):
                    P.dma("sync", Vg[:, r * nb + m0, :, 0:64], vg[r][:, m0, :, :], writes=[Vg])
            PTs = [P.sb("PTg%d" % k, [128, 512], BF16) for k in range(3)]
            Os = [P.sb("Os%d" % k, [128, 260], F32) for k in range(2)]
            Ogr = Og[g].rearrange("(m dd) c -> dd m c", dd=dil)
            sc = 0
            jobs = []
            qb = 0
            for r in range(dil):
                for mb in range(nb):
                    for hp in range(2):
                        jobs.append((r, mb, hp, qb))
                    qb += 1
            prev = None
            for job in jobs + [None]:
                if job is not None:
                    r, mb, hp, qb = job
                    qsl = slice(mb * 128 * dil + r, (mb * 128 + 127) * dil + r + 1, dil)
                    kbs = ([mb - 1] if mb > 0 else []) + [mb]
                    sbank = C.banks[sc % 3]
                    PT = PTs[sc % 3]
                    sc += 1
                    tiles = []
                    for hh in range(2):
                        j = hp * 2 + hh
                        for kb in kbs:
                            t = len(tiles)
                            tiles.append((j, kb))
                            ksl = slice(kb * 128 * dil + r, (kb * 128 + 127) * dil + r + 1, dil)
                            P.op("tensor", lambda e: e.matmul(sbank[:, t * 128:(t + 1) * 128], lhsT=kTg[:, j // 2, ksl], rhs=qz[:, j, qsl],
                                                              start=True, stop=False), reads=[kTg, qz], writes=[sbank])
                            M = C.MdT if kb == mb else C.MpT
                            P.op("tensor", lambda e: e.matmul(sbank[:, t * 128:(t + 1) * 128], lhsT=C.ident.ap(), rhs=M.ap(), start=False, stop=True),
                                 reads=[C.ident, M], writes=[sbank])
                    nw = len(tiles) * 128
                    P.op("scalar", lambda e: e.activation(out=PT[:, 0:nw], in_=sbank[:, 0:nw], func=AF.Exp, scale=0.125), reads=[sbank], writes=[PT])
                if prev is not None:
                    (r_, mb_, hp_, qb_), PT_, tiles_ = prev
                    obank = C.banks[6 + qb_ % 2]
                    kfirst = mb_ - 1 if mb_ > 0 else mb_
                    for t, (j, kb) in enumerate(tiles_):
                        P.op("tensor", lambda e: e.matmul(obank[:, j * 65:(j + 1) * 65], lhsT=PT_[:, t * 128:(t + 1) * 128], rhs=Vg[:, r_ * nb + kb, j, 0:65],
                                                          start=(kb == kfirst), stop=(kb == mb_)), reads=[PT_, Vg], writes=[obank])
                    if hp_ == 1:
                        O = Os[qb_ % 2]
                        P.op("vector", lambda e: e.tensor_copy(O.ap(), obank[:, 0:260]), reads=[obank], writes=[O])
                        P.dma("sync", Ogr[r_][mb_ * 128:(mb_ + 1) * 128, :], O.ap(), reads=[O])
                prev = (job, PT, tiles) if job is not None else None


def phase_B2(P, C, xsrc, xdst, fT, Og, kmT, Vm, wout_dram):
    with P.scope():
        Wout, Wot = load_weight(P, "Wout1", wout_dram, 4, D)
        qzs = [P.sb("qzm%d" % r, [128, 4, 128], BF16) for r in range(2)]
        for r in range(2):
            P.op("gpsimd", lambda e: e.memset(qzs[r].ap(), 0.0), writes=[qzs[r]])
        Ot = [[P.sb("Ot%d_%d" % (r, g), [128, 260], F32) for g in range(3)] for r in range(2)]
        FW = alloc_finish_work(P, C, "b2", [], C.banks[0], [C.banks[1], C.banks[2]])
        FW.osrc = lambda r: [(Ot[r][0], Ot[r][0].ap(), 4), (C.banks[5], C.banks[5][:, 0:260], 4)]
        fT_r = fT.rearrange("c p t -> p c t")
        for i in range(NT):
            r = i % 2
            qz = qzs[r]
            q0 = i * 128
            for base in (0, 64):
                P.dma("sync", qz[base:base + 64, base // 64:4:2, :], fT_r[base:base + 64, 12:14, q0:q0 + 128], writes=[qz])
            for g in range(3):
                P.dma("sync", Ot[r][g].ap(), Og[g][q0:q0 + 128, :], writes=[Ot[r][g]])
            mem_heads(P, C, FW, qz, 0, kmT, Vm, C.banks[5], [C.banks[6], C.banks[7]], r)
            P.op("gpsimd", lambda e: e.tensor_tensor(out=Ot[r][0].ap(), in0=Ot[r][0].ap(), in1=Ot[r][1].ap(), op=ALU.add),
                 reads=[Ot[r][0], Ot[r][1]], writes=[Ot[r][0]])
            P.op("gpsimd", lambda e: e.tensor_tensor(out=Ot[r][0].ap(), in0=Ot[r][0].ap(), in1=Ot[r][2].ap(), op=ALU.add),
                 reads=[Ot[r][0], Ot[r][2]], writes=[Ot[r][0]])
            attn_finish(P, C, FW, xsrc, xdst, i, 8, Wout, Wot, 4)


def build_program(stop_after=None, dbg_block=None, nblocks0=NT, skip_l0=False):
    nc = bass.Bass("TRN2", target_bir_lowering=False)
    P = Prog(nc)
    C = Ctx()
    I = {}

    def inp(name, shape, dt=F32):
        I[name] = nc.dram_tensor(name, list(shape), dt, kind="ExternalInput").ap()
        return I[name]

    x_in = inp("x", [S, D])
    mem_in = inp("mem", [256, D])
    pos_in = inp("pos", [128, NT], I32)
    gains = inp("gains", [7, D])
    C.freqs_in = inp("freqs", [128, 48])
    w_in0 = inp("w_in0", [D, SPEC0["ncols"]])
    w_in1 = inp("w_in1", [D, SPEC1["ncols"]])
    w_mkv = [inp("w_mkv%d" % l, [D, 512]) for l in range(2)]
    w_out0 = inp("w_out0", [1024, D])
    w_out1 = inp("w_out1", [512, D])
    w_gu = [inp("w_gu%d" % l, [D, 2 * DFF]) for l in range(2)]
    w_dn = [inp("w_dn%d" % l, [DFF, D]) for l in range(2)]
    out = nc.dram_tensor("out", [S, D], F32, kind="ExternalOutput").ap()
    C.dbg = None
    C.dbg_block = dbg_block
    if dbg_block is not None:
        C.dbg = dict(score=nc.dram_tensor("dbg_score", [128, S], F32, kind="ExternalOutput").ap(),
                     thr=nc.dram_tensor("dbg_thr", [128, 1], F32, kind="ExternalOutput").ap(),
                     Bm=nc.dram_tensor("dbg_Bm", [128, S], BF16, kind="ExternalOutput").ap())
    fT0 = P.dram("fT0", [15, 128, S], BF16)
    vS0 = P.dram("vS0", [S, 256], BF16)
    fT1 = P.dram("fT1", [14, 128, S], BF16)
    vS1 = P.dram("vS1", [S, 768], BF16)
    Og = [P.dram("Og%d" % g, [S, 260], F32) for g in range(3)]
    xa = P.dram("xa", [S, D], F32)
    xb = P.dram("xb", [S, D], F32)
    xc = P.dram("xc", [S, D], F32)

    C.banks = [P.ps("bank%d" % k, [128, 512], F32) for k in range(8)]
    setup_consts(P, C, pos_in)
    HF = NFC // 2

    def final(src):
        print("n_inst before final", P.n_inst)
        P.max_ops = None
        with P.scope():
            t = [P.sb("fin%d" % r, [128, D], F32) for r in range(2)]
            for i in range(NT):
                P.dma("sync", t[i % 2].ap(), src[i * 128:(i + 1) * 128, :], writes=[t[i % 2]])
                P.dma("sync", out[i * 128:(i + 1) * 128, :], t[i % 2].ap(), reads=[t[i % 2]], out=True)
        P.finish()
        return nc

    with (P.scope() if not skip_l0 else contextlib.nullcontext()):
      if not skip_l0:
        wiAll = P.sb("wiAll", [128, NT, 8], F32)
        kmT = P.sb("kmT0", [128, 2, 256], BF16)
        Vm = P.sb("Vm0", [128, 2, 4, VW], BF16)
        phase_M(P, C, "m0", mem_in, w_mkv[0], gains, 1, kmT, Vm)
        if stop_after == "M":
            d1 = nc.dram_tensor("dbg_kmT", [128, 2, 256], BF16, kind="ExternalOutput").ap()
            d2 = nc.dram_tensor("dbg_Vm", [128, 2, 4, VW], BF16, kind="ExternalOutput").ap()
            P.dma("sync", d1, kmT.ap(), reads=[kmT])
            P.dma("sync", d2, Vm.ap(), reads=[Vm])
            return final(x_in)
        phase_A(P, C, "a0", x_in, w_in0, gains, 0, SPEC0, fT0, vS0, wiAll, pos_in)
        if stop_after == "A":
            d1 = nc.dram_tensor("dbg_fT0", [15, 128, S], BF16, kind="ExternalOutput").ap()
            d2 = nc.dram_tensor("dbg_vS0", [S, 256], BF16, kind="ExternalOutput").ap()
            d3 = nc.dram_tensor("dbg_wi", [128, NT, 8], F32, kind="ExternalOutput").ap()
            with P.scope():
                tb = P.sb("dbgt", [128, S], BF16)
                for c in range(15):
                    P.dma("sync", tb.ap(), fT0[c], writes=[tb])
                    P.dma("sync", d1[c], tb.ap(), reads=[tb])
                for c in range(2):
                    P.dma("sync", tb[:, 0:2048].rearrange("p (a b) -> p a b", b=256), vS0[c * 2048:(c + 1) * 2048, :].rearrange("(a p) b -> p a b", p=128), writes=[tb])
                    P.dma("sync", d2[c * 2048:(c + 1) * 2048, :].rearrange("(a p) b -> p a b", p=128), tb[:, 0:2048].rearrange("p (a b) -> p a b", b=256), reads=[tb])
                P.dma("sync", d3, wiAll.ap(), reads=[wiAll])
            return final(x_in)
        phase_B0(P, C, x_in, xa, fT0, vS0, wiAll, kmT, Vm, w_out0, nblocks=nblocks0)
    if stop_after == "B0":
        return final(xa)
    if not skip_l0:
        phase_F(P, C, "f0a", xa, xa, xb, w_gu[0], w_dn[0], gains, 2, 0, HF)
        phase_F(P, C, "f0b", xa, xb, xc, w_gu[0], w_dn[0], gains, 2, HF, NFC)
    else:
        xc = x_in
    if stop_after == "F0":
        return final(xc)
    with P.scope():
        kmT = P.sb("kmT1", [128, 2, 256], BF16)
        Vm = P.sb("Vm1", [128, 2, 4, VW], BF16)
        phase_M(P, C, "m1", mem_in, w_mkv[1], gains, 4, kmT, Vm)
        phase_A(P, C, "a1", xc, w_in1, gains, 3, SPEC1, fT1, vS1, None, pos_in)
        phase_B1(P, C, fT1, vS1, Og)
        phase_B2(P, C, xc, xa, fT1, Og, kmT, Vm, w_out1)
    if stop_after == "B2":
        return final(xa)
    phase_F(P, C, "f1a", xa, xa, xb, w_gu[1], w_dn[1], gains, 5, 0, HF)
    phase_F(P, C, "f1b", xa, xb, out, w_gu[1], w_dn[1], gains, 5, HF, NFC, final_row=6)
    P.finish()
    return nc


def prep_inputs(inputs):
    f = lambda a: np.ascontiguousarray(np.asarray(a, dtype=np.float32))
    w0 = f(inputs["l0_w_in"])
    q, k, v, qi, ki, wi, qm = np.split(w0, np.cumsum([768, 256, 256, 512, 64, 8])[:], axis=1)
    qp = np.concatenate([q[:, h * 64:(h + 1) * 64] for h in QPERM], axis=1)
    w_in0 = np.ascontiguousarray(np.concatenate([qp, k, qi, ki, ki, wi, v, qm], axis=1))
    w1 = f(inputs["l1_w_in"])
    parts = [w1[:, j * 256:(j + 1) * 256] for j in range(10)]
    w_in1 = np.ascontiguousarray(np.concatenate([parts[0], parts[1], parts[3], parts[4], parts[6], parts[7], parts[2], parts[5], parts[8], parts[9]], axis=1))
    gains = np.ascontiguousarray(np.stack([f(inputs[n]) for n in ("l0_norm_mix", "l0_norm_mem", "l0_norm_ffn", "l1_norm_mix", "l1_norm_mem",
                                                                 "l1_norm_ffn", "final_norm")], axis=0))
    fr64 = (np.float32(10000.0) ** (-np.arange(32, dtype=np.float32) / np.float32(32))).astype(np.float32)
    fr16 = (np.float32(10000.0) ** (-np.arange(16, dtype=np.float32) / np.float32(16))).astype(np.float32)
    freqs = np.ascontiguousarray(np.broadcast_to(np.concatenate([fr64, fr16])[None, :], (128, 48)).astype(np.float32))
    shared = dict(freqs=freqs, gains=gains, w_in0=w_in0, w_in1=w_in1, w_mkv0=f(inputs["l0_w_mem_kv"]), w_mkv1=f(inputs["l1_w_mem_kv"]),
                  w_out0=f(inputs["l0_w_out"]), w_out1=f(inputs["l1_w_out"]), w_gu0=f(inputs["l0_w_gate_up"]), w_gu1=f(inputs["l1_w_gate_up"]),
                  w_dn0=f(inputs["l0_w_down"]), w_dn1=f(inputs["l1_w_down"]))
    x = f(inputs["x"])
    mem = f(inputs["mem"])
    pos = np.ascontiguousarray(np.asarray(inputs["positions"], dtype=np.int32))
    maps = []
    for c in range(x.shape[0]):
        m = dict(shared)
        m["x"] = x[c]
        m["mem"] = mem[c]
        m["pos"] = np.ascontiguousarray(pos[c].reshape(NT, 128).T)
        maps.append(m)
    return maps


_NC_CACHE = {}


def kernel(**inputs):
    maps = prep_inputs(inputs)
    if "nc" not in _NC_CACHE:
        _NC_CACHE["nc"] = build_program()
    nc = _NC_CACHE["nc"]
    res = run_bass_kernel_spmd(nc, maps, core_ids=list(range(len(maps))))
    return np.stack([np.asarray(r["out"], dtype=np.float32) for r in res.results], axis=0)
```

```python
import contextlib
import numpy as np
import ml_dtypes
import concourse.bass as bass
import concourse.mybir as mybir
from concourse.bass_utils import run_bass_kernel_spmd

F32 = mybir.dt.float32
BF16 = mybir.dt.bfloat16
I32 = mybir.dt.int32
AF = mybir.ActivationFunctionType
ALU = mybir.AluOpType
AX = mybir.AxisListType

SAME_ENGINE_SYNC = True


class Tok:
    def __init__(self, name):
        self.name = name
        self.w = None
        self.r = {}


class Buf(Tok):
    def __init__(self, name, handle):
        super().__init__(name)
        self.h = handle

    def ap(self):
        return self.h[:]

    def __getitem__(self, idx):
        return self.h[idx]


class _Eng:
    def __init__(self, name, handle, sem):
        self.name = name
        self.h = handle
        self.sem = sem
        self.count = 0
        self.seen = {}


class Prog:
    def __init__(self, nc, n_dma_sems=8):
        self.nc = nc
        self.stack = contextlib.ExitStack()
        self.engs = {}
        for name in ("tensor", "vector", "scalar", "gpsimd", "sync"):
            sem = self.stack.enter_context(nc.semaphore("s_" + name))
            self.engs[name] = _Eng(name, getattr(nc, name), sem)
        self.dma_sems = {}
        for q in ("sync", "gpsimd", "scalar"):
            self.dma_sems[q] = [[self.stack.enter_context(nc.semaphore("d_%s%d" % (q, i))), 0] for i in range(n_dma_sems)]
        self.dma_rr = {"sync": 0, "gpsimd": 0, "scalar": 0}
        self.out_events = []
        self.n_inst = 0
        self.scopes = [self.stack]
        import os
        self.max_ops = int(os.environ["KMAXOPS"]) if "KMAXOPS" in os.environ else None

    @contextlib.contextmanager
    def scope(self):
        st = contextlib.ExitStack()
        self.scopes.append(st)
        try:
            yield
        finally:
            self.barrier()
            self.scopes.pop()
            st.close()

    def barrier(self):
        if getattr(self, "finished", False):
            return
        for eng in self.engs.values():
            for other in self.engs.values():
                if other is not eng and other.count > 0:
                    self._wait(eng, (other.name, other.sem, other.count))
            for q, pool in self.dma_sems.items():
                for i, slot in enumerate(pool):
                    if slot[1] > 0:
                        self._wait(eng, ("d_%s%d" % (q, i), slot[0], 16 * slot[1]))

    def tok(self, name):
        return Tok(name)

    def sb(self, name, shape, dtype):
        self.uid = getattr(self, "uid", 0) + 1
        name = "%s_u%d" % (name, self.uid)
        return Buf(name, self.scopes[-1].enter_context(self.nc.sbuf_tensor(name, list(shape), dtype)))

    def ps(self, name, shape, dtype):
        b = Buf(name, self.scopes[-1].enter_context(self.nc.psum_tensor(name, list(shape), dtype)))
        b.excl = True
        return b

    def dram(self, name, shape, dtype):
        return self.nc.dram_tensor(name, list(shape), dtype, kind="Internal").ap()

    def _wait(self, eng, ev):
        key, sem, val = ev
        if eng.seen.get(key, 0) >= val:
            return
        if key == eng.name and not (SAME_ENGINE_SYNC and eng.name != "tensor" and eng.name != "sync"):
            return
        eng.h.wait_ge(sem, val)
        eng.seen[key] = val

    def _deps(self, eng, reads, writes):
        for t in reads:
            if t.w is not None:
                self._wait(eng, t.w)
        for t in writes:
            if t.w is not None:
                self._wait(eng, t.w)
            for ev in t.r.values():
                self._wait(eng, ev)

    def _record(self, ev, reads, writes):
        for t in writes:
            t.w = ev
            t.r = {}
        for t in reads:
            if t in writes:
                continue
            t.r[ev[0]] = ev

    def op(self, engname, fn, reads=(), writes=()):
        if self.max_ops is not None and self.n_inst >= self.max_ops:
            return None
        eng = self.engs[engname]
        ex = [t for t in reads if getattr(t, "excl", False) and t not in writes]
        if ex:
            reads = [t for t in reads if t not in ex]
            writes = list(writes) + ex
        self._deps(eng, reads, writes)
        inst = fn(eng.h)
        eng.count += 1
        inst.then_inc(eng.sem, 1)
        ev = (eng.name, eng.sem, eng.count)
        self._record(ev, reads, writes)
        self.n_inst += 1
        return ev

    def dma(self, q, out_ap, in_ap, reads=(), writes=(), out=False, **kw):
        if self.max_ops is not None and self.n_inst >= self.max_ops:
            return None
        eng = self.engs[q]
        self._deps(eng, reads, writes)
        pool = self.dma_sems[q]
        i = self.dma_rr[q]
        self.dma_rr[q] = (i + 1) % len(pool)
        slot = pool[i]
        key = "d_%s%d" % (q, i)
        if slot[1] > 0:
            self._wait(eng, (key, slot[0], 16 * slot[1]))
        eng.h.dma_start(out=out_ap, in_=in_ap, **kw).then_inc(slot[0], 16)
        slot[1] += 1
        ev = (key, slot[0], 16 * slot[1])
        self._record(ev, reads, writes)
        if out:
            self.out_events.append(ev)
        self.n_inst += 1
        return ev

    def finish(self):
        eng = self.engs["sync"]
        for q, pool in self.dma_sems.items():
            for i, slot in enumerate(pool):
                if slot[1] > 0:
                    self._wait(eng, ("d_%s%d" % (q, i), slot[0], 16 * slot[1]))
        for name, e in self.engs.items():
            if name != "sync" and e.count > 0:
                self._wait(eng, (name, e.sem, e.count))
        self.finished = True
        for st in reversed(self.scopes):
            st.close()

    def make_identity(self, idt):
        tmp = self.sb(idt.name + "_i", [128, 128], I32)
        self.op("gpsimd", lambda e: e.iota(tmp.ap(), pattern=[[1, 128]], base=0, channel_multiplier=-1), writes=[tmp])
        self.op("vector", lambda e: e.tensor_scalar(out=idt.ap(), in0=tmp.ap(), scalar1=0.0, scalar2=None, op0=ALU.is_equal),
                reads=[tmp], writes=[idt])


S = 4096
D = 1024
NT = 32
DFF = 2816
NFC = 22
EPS = 1e-6
NEG = -1.0e30
MASKV = -30000.0
VW = 66
N_BISECT = 16
IDX_SCALE = (8 ** -0.5) * (64 ** -0.5)
QPERM = [0, 3, 1, 4, 2, 5, 6, 9, 7, 10, 8, 11]

SPEC0 = dict(
    ncols=2184,
    chunks=[(0, 512, [("rope64", 0, 8)]), (512, 512, [("rope64", 0, 8)]), (1024, 512, [("rope32", 0, 8)]),
            (1536, 136, [("rope32", 0, 2), ("wi", 128, 8)]), (1672, 512, [("copy", 0, 512)])],
    tcols=[128 * k for k in range(13)] + [1928, 2056],
    vcols=(1672, 1928),
)
SPEC1 = dict(
    ncols=2560,
    chunks=[(0, 512, [("rope64", 0, 8)]), (512, 512, [("rope64", 0, 8)]), (1024, 512, [("rope64", 0, 8)]),
            (1536, 512, [("copy", 0, 512)]), (2048, 512, [("copy", 0, 512)])],
    tcols=[128 * k for k in range(12)] + [2304, 2432],
    vcols=(1536, 2304),
)


class Ctx:
    pass


def load_weight(P, name, dram, nk, ncols, q="gpsimd"):
    W = P.sb(name, [128, nk, ncols], BF16)
    toks = [P.tok("%s_%d" % (name, k)) for k in range(nk)]
    for k in range(nk):
        c0 = 0
        while c0 < ncols:
            c1 = min(ncols, c0 + 2048)
            P.dma(q, W[:, k, c0:c1], dram[k * 128:(k + 1) * 128, c0:c1], writes=[toks[k]])
            c0 = c1
    return W, toks


def setup_consts(P, C, pos_in):
    C.ident = P.sb("ident", [128, 128], BF16)
    P.make_identity(C.ident)
    C.negthr = P.sb("negthr", [128, 1], F32)
    P.op("vector", lambda e: e.memset(C.negthr.ap(), -1.0e29), writes=[C.negthr])
    C.pow2 = P.sb("pow2", [128, N_BISECT + 2], F32)
    for k in range(N_BISECT + 2):
        P.op("gpsimd", lambda e: e.memset(C.pow2[:, k:k + 1], 2.0 ** (1 - k)), writes=[C.pow2])
    C.MdT = P.sb("MdT", [128, 128], BF16)
    C.MpT = P.sb("MpT", [128, 128], BF16)
    zt = P.sb("zt", [128, 128], F32)
    zm = P.sb("zm", [128, 128], F32)
    P.op("vector", lambda e: e.memset(zt.ap(), 0.0), writes=[zt])
    P.op("gpsimd", lambda e: e.affine_select(out=zm.ap(), in_=zt.ap(), pattern=[[1, 128]], compare_op=ALU.is_ge, fill=MASKV, base=0, channel_multiplier=-1),
         reads=[zt], writes=[zm])
    P.op("vector", lambda e: e.tensor_copy(C.MdT.ap(), zm.ap()), reads=[zm], writes=[C.MdT])
    P.op("gpsimd", lambda e: e.affine_select(out=zm.ap(), in_=zt.ap(), pattern=[[-1, 128]], compare_op=ALU.is_ge, fill=MASKV, base=0, channel_multiplier=1),
         reads=[zt, C.MdT], writes=[zm])
    P.op("vector", lambda e: e.tensor_copy(C.MpT.ap(), zm.ap()), reads=[zm], writes=[C.MpT])


def build_rope_tables(P, C, pos_in):
    C.cos64 = P.sb("cos64", [128, NT, 32], F32)
    C.sin64 = P.sb("sin64", [128, NT, 32], F32)
    C.nsin64 = P.sb("nsin64", [128, NT, 32], F32)
    C.cos16 = P.sb("cos16", [128, NT, 16], F32)
    C.sin16 = P.sb("sin16", [128, NT, 16], F32)
    C.nsin16 = P.sb("nsin16", [128, NT, 16], F32)
    with P.scope():
        posi = P.sb("posi", [128, NT], I32)
        posf = P.sb("posf", [128, NT], F32)
        P.dma("sync", posi.ap(), pos_in, writes=[posi])
        P.op("vector", lambda e: e.tensor_copy(posf.ap(), posi.ap()), reads=[posi], writes=[posf])
        for half, cosT, sinT, nsinT in ((32, C.cos64, C.sin64, C.nsin64), (16, C.cos16, C.sin16, C.nsin16)):
            n = NT * half
            fr = P.sb("fr%d" % half, [128, half], F32)
            a = P.sb("a%d" % half, [128, NT, half], F32)
            ki = P.sb("ki%d" % half, [128, NT, half], I32)
            kf = P.sb("kf%d" % half, [128, NT, half], F32)
            fr1 = P.sb("fr1%d" % half, [128, NT, half], F32)
            m1 = P.sb("m1%d" % half, [128, NT, half], F32)
            f0 = 0 if half == 32 else 32
            P.dma("sync", fr.ap(), C.freqs_in[:, f0:f0 + half], writes=[fr])
            P.op("vector", lambda e: e.tensor_tensor(out=a.ap(), in0=posf.ap().unsqueeze(2).broadcast_to([128, NT, half]),
                                                     in1=fr.ap().unsqueeze(1).broadcast_to([128, NT, half]), op=ALU.mult),
                 reads=[posf, fr], writes=[a])
            P.op("vector", lambda e: e.tensor_scalar(out=a.ap(), in0=a.ap(), scalar1=float(1.0 / (2.0 * np.pi)), scalar2=None, op0=ALU.mult),
                 reads=[a], writes=[a])
            for shift, outT, neg in ((0.0, sinT, False), (0.25, cosT, False), (0.5, nsinT, False)):
                src = a
                if shift != 0.0:
                    P.op("vector", lambda e: e.tensor_scalar(out=fr1.ap(), in0=a.ap(), scalar1=shift, scalar2=None, op0=ALU.add),
                         reads=[a], writes=[fr1])
                    src = fr1
                P.op("vector", lambda e: e.tensor_copy(ki.ap(), src.ap()), reads=[src], writes=[ki])
                P.op("vector", lambda e: e.tensor_copy(kf.ap(), ki.ap()), reads=[ki], writes=[kf])
                P.op("vector", lambda e: e.tensor_tensor(out=kf.ap(), in0=src.ap(), in1=kf.ap(), op=ALU.subtract), reads=[src, kf], writes=[kf])
                P.op("vector", lambda e: e.tensor_scalar(out=m1.ap(), in0=kf.ap(), scalar1=0.5, scalar2=None, op0=ALU.is_gt), reads=[kf], writes=[m1])
                P.op("vector", lambda e: e.tensor_tensor(out=kf.ap(), in0=kf.ap(), in1=m1.ap(), op=ALU.subtract), reads=[kf, m1], writes=[kf])
                P.op("vector", lambda e: e.tensor_scalar(out=m1.ap(), in0=kf.ap(), scalar1=-0.5, scalar2=None, op0=ALU.is_lt), reads=[kf], writes=[m1])
                P.op("vector", lambda e: e.tensor_tensor(out=kf.ap(), in0=kf.ap(), in1=m1.ap(), op=ALU.add), reads=[kf, m1], writes=[kf])
                P.op("scalar", lambda e: e.activation(out=outT.ap(), in_=kf.ap(), func=AF.Sin, scale=float(2.0 * np.pi) * (1.0 - 1e-6)),
                     reads=[kf], writes=[outT])


def alloc_norm_work(P, C, tag):
    W = Ctx()
    W.sqj = P.sb(tag + "sqj", [128, D], BF16)
    W.small = [[P.sb("%ssm%d_%d" % (tag, r, j), [128, 1], F32) for j in range(4)] for r in range(2)]
    W.hb = [P.sb("%shb%d" % (tag, r), [128, D], BF16) for r in range(2)]
    W.k = 0
    return W


def rmsnorm_to_hT(P, C, W, xt, gb, hT_ap, hT_tok, bank, evac_eng="scalar"):
    r = W.k % 2
    W.k += 1
    ssq, t1, t2, rstd = W.small[r]
    hb = W.hb[r]
    P.op("scalar", lambda e: e.activation(out=W.sqj.ap(), in_=xt.ap(), func=AF.Square, accum_out=ssq.ap()), reads=[xt], writes=[W.sqj, ssq])
    P.op("vector", lambda e: e.tensor_scalar(out=t1.ap(), in0=ssq.ap(), scalar1=1.0 / D, scalar2=EPS, op0=ALU.mult, op1=ALU.add), reads=[ssq], writes=[t1])
    P.op("scalar", lambda e: e.activation(out=t2.ap(), in_=t1.ap(), func=AF.Sqrt), reads=[t1], writes=[t2])
    P.op("vector", lambda e: e.reciprocal(out=rstd.ap(), in_=t2.ap()), reads=[t2], writes=[rstd])
    P.op("vector", lambda e: e.scalar_tensor_tensor(out=hb.ap(), in0=xt.ap(), scalar=rstd.ap(), in1=gb.ap(), op0=ALU.mult, op1=ALU.mult),
         reads=[xt, rstd, gb], writes=[hb])
    if hT_ap is None:
        return hb
    return norm_p2(P, C, hb, hT_ap, hT_tok, bank, evac_eng)


def norm_p2(P, C, hb, hT_ap, hT_tok, bank, evac_eng="scalar"):
    bbf = bank.ap().bitcast(BF16)
    for c in range(8):
        P.op("tensor", lambda e: e.transpose(bbf[:, c * 128:(c + 1) * 128], hb[:, c * 128:(c + 1) * 128], C.ident.ap()),
             reads=[hb, C.ident], writes=[bank])
    srcv = bbf if len(hT_ap.shape) == 2 else bbf.rearrange("p (c t) -> p c t", t=128)
    if evac_eng == "scalar":
        P.op("scalar", lambda e: e.activation(out=hT_ap, in_=srcv, func=AF.Copy), reads=[bank], writes=[hT_tok])
    else:
        P.op("vector", lambda e: e.tensor_copy(hT_ap, srcv), reads=[bank], writes=[hT_tok])


def load_gain(P, C, name, gains, row):
    gb = P.sb(name, [128, D], F32)
    P.dma("sync", gb.ap(), gains[row].partition_broadcast(128), writes=[gb])
    return gb


def phase_A(P, C, tag, xsrc, w_dram, gains, grow, spec, fT, vS, wiAll, pos_in):
    ncols = spec["ncols"]
    with P.scope():
        build_rope_tables(P, C, pos_in)
        Win, Wt = load_weight(P, tag + "Win", w_dram, 8, ncols)
        gb = load_gain(P, C, tag + "gbA", gains, grow)
        NW = alloc_norm_work(P, C, tag + "A")
        xts = [P.sb("%sxt%d" % (tag, r), [128, D], F32) for r in range(2)]
        hTs = [P.sb("%shT%d" % (tag, r), [128, D], BF16) for r in range(2)]
        pts = [P.sb("%spt%d" % (tag, r), [128, ncols], BF16) for r in range(2)]
        t1s = [P.sb("%st1_%d" % (tag, r), [128, 512], F32) for r in range(2)]
        t2s = [P.sb("%st2_%d" % (tag, r), [128, 512], F32) for r in range(2)]
        ntc = len(spec["tcols"])
        fTs = [P.sb("%sfTs%d" % (tag, r), [128, ntc, 128], BF16) for r in range(2)]
        fT_r = fT.rearrange("c p t -> p c t")
        cc = 0
        hbs = {}

        def n1(i):
            xt = xts[i % 2]
            P.dma("sync", xt.ap(), xsrc[i * 128:(i + 1) * 128, :], writes=[xt])
            hbs[i] = rmsnorm_to_hT(P, C, NW, xt, gb, None, None, None)

        def n2(i):
            norm_p2(P, C, hbs.pop(i), hTs[i % 2].ap(), hTs[i % 2], C.banks[0], evac_eng="scalar")

        n1(0)
        n2(0)
        for i in range(NT):
            hT = hTs[i % 2]
            pt = pts[i % 2]
            if i + 1 < NT:
                n1(i + 1)
            for (col0, width, handlers) in spec["chunks"]:
                bank = C.banks[1 + (cc % 4)]
                t1 = t1s[cc % 2]
                t2 = t2s[cc % 2]
                cc += 1
                for k in range(8):
                    P.op("tensor", lambda e: e.matmul(bank[:, 0:width], lhsT=hT[:, k * 128:(k + 1) * 128], rhs=Win[:, k, col0:col0 + width],
                                                      start=(k == 0), stop=(k == 7)), reads=[hT, Wt[k]], writes=[bank])
                for (kind, l0, n) in handlers:
                    if kind == "rope64":
                        nh = n
                        w = nh * 64
                        xv2 = bank[:, l0:l0 + w].rearrange("p (h d) -> p h d", d=32)
                        xv = bank[:, l0:l0 + w].rearrange("p (h d) -> p h d", d=64)
                        t1v2 = t1[:, 0:w].rearrange("p (h d) -> p h d", d=32)
                        t2v = t2[:, 0:w].rearrange("p (h d) -> p h d", d=64)
                        cosb = C.cos64[:, i:i + 1, :].broadcast_to([128, 2 * nh, 32])
                        sinb = C.sin64[:, i:i + 1, :].broadcast_to([128, nh, 32])
                        nsinb = C.nsin64[:, i:i + 1, :].broadcast_to([128, nh, 32])
                        P.op("vector", lambda e: e.tensor_tensor(out=t1v2, in0=xv2, in1=cosb, op=ALU.mult), reads=[bank, C.cos64], writes=[t1])
                        P.op("vector", lambda e: e.tensor_tensor(out=t2v[:, :, 0:32], in0=xv[:, :, 32:64], in1=nsinb, op=ALU.mult),
                             reads=[bank, C.nsin64], writes=[t2])
                        P.op("vector", lambda e: e.tensor_tensor(out=t2v[:, :, 32:64], in0=xv[:, :, 0:32], in1=sinb, op=ALU.mult),
                             reads=[bank, C.sin64], writes=[t2])
                        P.op("gpsimd", lambda e: e.tensor_tensor(out=pt[:, col0 + l0:col0 + l0 + w], in0=t1[:, 0:w], in1=t2[:, 0:w], op=ALU.add),
                             reads=[t1, t2], writes=[pt])
                    elif kind == "rope32":
                        nh = n
                        w = nh * 64
                        xv = bank[:, l0:l0 + w].rearrange("p (h d) -> p h d", d=64)
                        xr4 = bank[:, l0:l0 + w].rearrange("p (h t d) -> p h t d", t=4, d=16)
                        t1r4 = t1[:, 0:w].rearrange("p (h t d) -> p h t d", t=4, d=16)
                        t2v = t2[:, 0:w].rearrange("p (h d) -> p h d", d=64)
                        t1v = t1[:, 0:w].rearrange("p (h d) -> p h d", d=64)
                        ptv = pt[:, col0 + l0:col0 + l0 + w].rearrange("p (h d) -> p h d", d=64)
                        cosb = C.cos16[:, i:i + 1, :].unsqueeze(1).broadcast_to([128, nh, 2, 16])
                        sinb = C.sin16[:, i:i + 1, :].broadcast_to([128, nh, 16])
                        nsinb = C.nsin16[:, i:i + 1, :].broadcast_to([128, nh, 16])
                        P.op("vector", lambda e: e.tensor_tensor(out=t1r4[:, :, 0:2, :], in0=xr4[:, :, 0:2, :], in1=cosb, op=ALU.mult),
                             reads=[bank, C.cos16], writes=[t1])
                        P.op("vector", lambda e: e.tensor_tensor(out=t2v[:, :, 0:16], in0=xv[:, :, 16:32], in1=nsinb, op=ALU.mult),
                             reads=[bank, C.nsin16], writes=[t2])
                        P.op("vector", lambda e: e.tensor_tensor(out=t2v[:, :, 16:32], in0=xv[:, :, 0:16], in1=sinb, op=ALU.mult),
                             reads=[bank, C.sin16], writes=[t2])
                        P.op("gpsimd", lambda e: e.tensor_tensor(out=ptv[:, :, 0:32], in0=t1v[:, :, 0:32], in1=t2v[:, :, 0:32], op=ALU.add),
                             reads=[t1, t2], writes=[pt])
                        P.op("scalar", lambda e: e.activation(out=ptv[:, :, 32:64], in_=xv[:, :, 32:64], func=AF.Copy), reads=[bank], writes=[pt])
                    elif kind == "copy":
                        P.op("scalar", lambda e: e.activation(out=pt[:, col0 + l0:col0 + l0 + n], in_=bank[:, l0:l0 + n], func=AF.Copy),
                             reads=[bank], writes=[pt])
                    elif kind == "wi":
                        P.op("scalar", lambda e: e.activation(out=wiAll[:, i, :], in_=bank[:, l0:l0 + n], func=AF.Copy, scale=float(IDX_SCALE)),
                             reads=[bank], writes=[wiAll])
            if i + 1 < NT:
                n2(i + 1)
            fts = fTs[i % 2]
            for g0 in range(0, ntc, 8):
                g1 = min(ntc, g0 + 8)
                bank = C.banks[5 + (g0 // 8)]
                bbf = bank.ap().bitcast(BF16)
                for k in range(g0, g1):
                    c0 = spec["tcols"][k]
                    P.op("tensor", lambda e: e.transpose(bbf[:, (k - g0) * 128:(k - g0 + 1) * 128], pt[:, c0:c0 + 128], C.ident.ap()),
                         reads=[pt, C.ident], writes=[bank])
                eng = "vector" if g0 == 0 else "scalar"
                if eng == "vector":
                    P.op("vector", lambda e: e.tensor_copy(fts[:, g0:g1, :], bbf[:, 0:(g1 - g0) * 128].rearrange("p (c t) -> p c t", t=128)),
                         reads=[bank], writes=[fts])
                else:
                    P.op("scalar", lambda e: e.activation(out=fts[:, g0:g1, :], in_=bbf[:, 0:(g1 - g0) * 128].rearrange("p (c t) -> p c t", t=128),
                                                          func=AF.Copy), reads=[bank], writes=[fts])
            P.dma("sync", fT_r[:, :, i * 128:(i + 1) * 128], fts.ap(), reads=[fts])
            v0, v1 = spec["vcols"]
            P.dma("sync", vS[i * 128:(i + 1) * 128, :], pt[:, v0:v1], reads=[pt])


def phase_M(P, C, tag, mem_in, w_dram, gains, grow, kmT, Vm):
    with P.scope():
        Wm, Wt = load_weight(P, tag + "Wm", w_dram, 8, 512)
        gb = load_gain(P, C, tag + "gbM", gains, grow)
        NW = alloc_norm_work(P, C, tag + "M")
        xts = [P.sb("%smx%d" % (tag, r), [128, D], F32) for r in range(2)]
        hTs = [P.sb("%smhT%d" % (tag, r), [128, D], BF16) for r in range(2)]
        kb16 = [P.sb("%skb16_%d" % (tag, r), [128, 256], BF16) for r in range(2)]
        P.op("gpsimd", lambda e: e.memset(Vm.ap(), 1.0), writes=[Vm])
        for mb in range(2):
            xt, hT = xts[mb], hTs[mb]
            P.dma("sync", xt.ap(), mem_in[mb * 128:(mb + 1) * 128, :], writes=[xt])
            rmsnorm_to_hT(P, C, NW, xt, gb, hT.ap(), hT, C.banks[0])
            bank = C.banks[1 + mb]
            for k in range(8):
                P.op("tensor", lambda e: e.matmul(bank.ap(), lhsT=hT[:, k * 128:(k + 1) * 128], rhs=Wm[:, k, :], start=(k == 0), stop=(k == 7)),
                     reads=[hT, Wt[k]], writes=[bank])
            P.op("scalar", lambda e: e.activation(out=kb16[mb].ap(), in_=bank[:, 0:256], func=AF.Copy), reads=[bank], writes=[kb16[mb]])
            P.op("vector", lambda e: e.tensor_copy(Vm[:, mb, :, 0:64], bank[:, 256:512].rearrange("p (h d) -> p h d", d=64)),
                 reads=[bank], writes=[Vm])
            tb = C.banks[3 + mb]
            tbf = tb.ap().bitcast(BF16)
            for c in range(2):
                P.op("tensor", lambda e: e.transpose(tbf[:, c * 128:(c + 1) * 128], kb16[mb][:, c * 128:(c + 1) * 128], C.ident.ap()),
                     reads=[kb16[mb], C.ident], writes=[tb])
            P.op("vector", lambda e: e.tensor_copy(kmT[:, :, mb * 128:(mb + 1) * 128], tbf[:, 0:256].rearrange("p (c t) -> p c t", t=128)),
                 reads=[tb], writes=[kmT])


def attn_finish(P, C, W, xsrc, xdst, i, nheads, Wout, Wot, nkc):
    r = i % 2
    attn = W.attn[r]
    rec = W.rec[r]
    xt = W.xts[r]
    P.dma("sync", xt.ap(), xsrc[i * 128:(i + 1) * 128, :], writes=[xt])
    h0 = 0
    for (bank, oap, nh) in W.osrc(r):
        ov = oap.rearrange("p (h d) -> p h d", d=65)
        P.op("vector", lambda e: e.reciprocal(out=rec[:, h0:h0 + nh], in_=ov[:, :, 64]), reads=[bank], writes=[rec])
        P.op("vector", lambda e: e.tensor_tensor(out=attn[:, h0 * 64:(h0 + nh) * 64].rearrange("p (h d) -> p h d", d=64), in0=ov[:, :, 0:64],
                                                 in1=rec[:, h0:h0 + nh].unsqueeze(2).broadcast_to([128, nh, 64]), op=ALU.mult),
             reads=[bank, rec], writes=[attn])
        h0 += nh
    tb = W.tbank
    tbf = tb.ap().bitcast(BF16)
    aT = W.attnT[r]
    for c in range(nkc):
        P.op("tensor", lambda e: e.transpose(tbf[:, c * 128:(c + 1) * 128], attn[:, c * 128:(c + 1) * 128], C.ident.ap()),
             reads=[attn, C.ident], writes=[tb])
    P.op("scalar", lambda e: e.activation(out=aT[:, 0:nkc * 128], in_=tbf[:, 0:nkc * 128], func=AF.Copy), reads=[tb], writes=[aT])
    xn = W.xn[r]
    for half in range(2):
        yb = W.ybanks[half]
        for c in range(nkc):
            P.op("tensor", lambda e: e.matmul(yb.ap(), lhsT=aT[:, c * 128:(c + 1) * 128], rhs=Wout[:, c, half * 512:(half + 1) * 512],
                                              start=(c == 0), stop=(c == nkc - 1)), reads=[aT, Wot[c]], writes=[yb])
        P.op("vector", lambda e: e.tensor_tensor(out=xn[:, half * 512:(half + 1) * 512], in0=yb.ap(), in1=xt[:, half * 512:(half + 1) * 512], op=ALU.add),
             reads=[yb, xt], writes=[xn])
    P.dma("sync", xdst[i * 128:(i + 1) * 128, :], xn.ap(), reads=[xn])


def mem_heads(P, C, W, qall, qch0, kmT, Vm, obank, sbank, r):
    PT = W.PTm[r]
    for half in range(2):
        sb_ = sbank[half]
        for hh in range(2):
            hm = half * 2 + hh
            for mb in range(2):
                j = hh * 2 + mb
                P.op("tensor", lambda e: e.matmul(sb_[:, j * 128:(j + 1) * 128], lhsT=kmT[:, hm // 2, mb * 128:(mb + 1) * 128],
                                                  rhs=qall[:, qch0 + hm, :], start=True, stop=True),
                     reads=[kmT, qall], writes=[sb_])
        P.op("scalar", lambda e: e.activation(out=PT[:, half * 512:(half + 1) * 512], in_=sb_.ap(), func=AF.Exp, scale=0.125),
             reads=[sb_], writes=[PT])
    for hm in range(4):
        for mb in range(2):
            j = hm * 2 + mb
            P.op("tensor", lambda e: e.matmul(obank[:, hm * 65:(hm + 1) * 65], lhsT=PT[:, j * 128:(j + 1) * 128], rhs=Vm[:, mb, hm, 0:65],
                                              start=(mb == 0), stop=(mb == 1)), reads=[PT, Vm], writes=[obank])


def alloc_finish_work(P, C, tag, obanks, tbank, ybanks):
    W = Ctx()
    W.attn = [P.sb("%sattn%d" % (tag, r), [128, D], BF16) for r in range(2)]
    W.attnT = [P.sb("%sattnT%d" % (tag, r), [128, D], BF16) for r in range(2)]
    W.rec = [P.sb("%srec%d" % (tag, r), [128, 16], F32) for r in range(2)]
    W.xts = [P.sb("%sfx%d" % (tag, r), [128, D], F32) for r in range(2)]
    W.xn = [P.sb("%sxn%d" % (tag, r), [128, D], F32) for r in range(2)]
    W.PTm = [P.sb("%sPTm%d" % (tag, r), [128, 1024], BF16) for r in range(2)]
    W.osrc = lambda r: [(bk, bk[:, 0:nh * 65], nh) for (bk, nh) in obanks]
    W.tbank = tbank
    W.ybanks = ybanks
    return W


def phase_B0(P, C, xsrc, xdst, fT, vS, wiAll, kmT, Vm, wout_dram, nblocks=NT):
    with P.scope():
        Wout, Wot = load_weight(P, "Wout0", wout_dram, 8, D)
        kT = P.sb("kT", [128, 2, S], BF16)
        kiT = P.sb("kiT", [128, S], BF16)
        Va = P.sb("Va", [128, NT, 4, VW], BF16)
        P.op("gpsimd", lambda e: e.memset(Va.ap(), 1.0), writes=[Va])
        for c in range(2):
            P.dma("sync", kT[:, c, :], fT[6 + c], writes=[kT])
        P.dma("sync", kiT.ap(), fT[12], writes=[kiT])
        vr = vS.rearrange("(i p) (g d) -> p i g d", p=128, d=64)
        for i0 in range(NT):
            P.dma("sync", Va[:, i0, :, 0:64], vr[:, i0, :, :], writes=[Va])
        score = P.sb("score", [128, S], F32)
        junk = P.sb("junk", [128, S], BF16)
        Bs = [P.sb("Bm%d" % r, [128, S], BF16) for r in range(2)]
        Rs = [P.sb("R%d" % r, [128, 512], F32) for r in range(3)]
        PTs = [P.sb("PT%d" % r, [128, 512], BF16) for r in range(3)]
        qalls = [P.sb("qall%d" % r, [128, 24, 128], BF16) for r in range(2)]
        for r in range(2):
            P.op("gpsimd", lambda e: e.memset(qalls[r].ap(), 0.0), writes=[qalls[r]])
        sm = [[P.sb("bs%d_%d" % (r, j), [128, 1], F32) for j in range(6)] for r in range(2)]
        wtabs = [P.sb("wtab%d" % r, [128, N_BISECT + 2], F32) for r in range(2)]
        FW = alloc_finish_work(P, C, "b0", [(C.banks[3], 6), (C.banks[4], 6), (C.banks[5], 4)], C.banks[0], [C.banks[1], C.banks[2]])
        fT_r = fT.rearrange("c p t -> p c t")
        cnts = dict(lc=0, sc=0)

        def prep(b):
            lc = cnts["lc"]
            r = b % 2
            N = 128 * (b + 1)
            qall = qalls[r]
            B = Bs[r]
            q0 = b * 128
            for (slot0, nch, ch0) in ((0, 6, 0), (12, 4, 8), (20, 2, 13)):
                for base in (0, 64):
                    P.dma("sync", qall[base:base + 64, slot0 + base // 64:slot0 + 2 * nch:2, :], fT_r[base:base + 64, ch0:ch0 + nch, q0:q0 + 128],
                          writes=[qall])
            for c0 in range(0, N, 512):
                wc = min(512, N - c0)
                for h in range(8):
                    bank = C.banks[6 + (lc % 2)]
                    R = Rs[lc % 3]
                    lc += 1
                    P.op("tensor", lambda e: e.matmul(bank[:, 0:wc], lhsT=qall[:, 12 + h, :], rhs=kiT[:, c0:c0 + wc],
                                                      start=True, stop=True), reads=[qall, kiT], writes=[bank])
                    P.op("scalar", lambda e: e.activation(out=R[:, 0:wc], in_=bank[:, 0:wc], func=AF.Relu), reads=[bank], writes=[R])
                    if h == 0:
                        P.op("vector", lambda e: e.tensor_scalar(out=score[:, c0:c0 + wc], in0=R[:, 0:wc], scalar1=wiAll[:, b, 0:1], scalar2=None, op0=ALU.mult),
                             reads=[R, wiAll], writes=[score])
                    else:
                        P.op("vector", lambda e: e.scalar_tensor_tensor(out=score[:, c0:c0 + wc], in0=R[:, 0:wc], scalar=wiAll[:, b, h:h + 1],
                                                                       in1=score[:, c0:c0 + wc], op0=ALU.mult, op1=ALU.add),
                             reads=[R, wiAll, score], writes=[score])
            mn, mx, mid, cnt, aa, thr = sm[r]
            if b >= 2:
                wtab = wtabs[r]
                P.op("vector", lambda e: e.tensor_reduce(out=mn.ap(), in_=score[:, 0:N - 128], axis=AX.X, op=ALU.min), reads=[score], writes=[mn])
            P.op("gpsimd", lambda e: e.affine_select(out=score[:, q0:q0 + 128], in_=score[:, q0:q0 + 128], pattern=[[-1, 128]], compare_op=ALU.is_ge,
                                                    fill=NEG, base=0, channel_multiplier=1), reads=[score], writes=[score])
            if b >= 2:
                P.op("vector", lambda e: e.tensor_reduce(out=mx.ap(), in_=score[:, 0:N], axis=AX.X, op=ALU.max), reads=[score], writes=[mx])
                P.op("vector", lambda e: e.tensor_tensor(out=aa.ap(), in0=mx.ap(), in1=mn.ap(), op=ALU.subtract), reads=[mx, mn], writes=[aa])
                P.op("vector", lambda e: e.tensor_scalar(out=wtab.ap(), in0=C.pow2.ap(), scalar1=aa.ap(), scalar2=0.5, op0=ALU.mult, op1=ALU.mult),
                     reads=[C.pow2, aa], writes=[wtab])
                P.op("vector", lambda e: e.tensor_tensor(out=mid.ap(), in0=mn.ap(), in1=wtab[:, 1:2], op=ALU.add), reads=[mn, wtab], writes=[mid])
                for k in range(N_BISECT):
                    P.op("vector", lambda e: e.tensor_scalar(out=junk[:, 0:N], in0=score[:, 0:N], scalar1=mid.ap(), scalar2=None, op0=ALU.is_ge, op1=ALU.add,
                                                             accum_out=cnt.ap()), reads=[score, mid], writes=[junk, cnt])
                    P.op("vector", lambda e: e.tensor_scalar(out=aa.ap(), in0=cnt.ap(), scalar1=255.5, scalar2=-0.5, op0=ALU.is_ge, op1=ALU.add),
                         reads=[cnt], writes=[aa])
                    P.op("vector", lambda e: e.scalar_tensor_tensor(out=mid.ap(), in0=aa.ap(), scalar=wtab[:, k + 1:k + 2], in1=mid.ap(), op0=ALU.mult, op1=ALU.add),
                         reads=[aa, wtab, mid], writes=[mid])
                P.op("vector", lambda e: e.tensor_tensor(out=thr.ap(), in0=mid.ap(), in1=wtab[:, N_BISECT + 1:N_BISECT + 2], op=ALU.subtract),
                     reads=[mid, wtab], writes=[thr])
                thr_t = thr
            else:
                thr_t = C.negthr
            P.op("vector", lambda e: e.tensor_scalar(out=B[:, 0:N], in0=score[:, 0:N], scalar1=thr_t.ap(), scalar2=MASKV, op0=ALU.is_lt, op1=ALU.mult),
                 reads=[score, thr_t], writes=[B])
            if C.dbg is not None and b == C.dbg_block:
                P.dma("sync", C.dbg["score"], score.ap(), reads=[score])
                P.dma("sync", C.dbg["thr"], thr_t.ap(), reads=[thr_t])
                P.dma("sync", C.dbg["Bm"], B.ap(), reads=[B])
            cnts["lc"] = lc

        def attend(b):
            sc = cnts["sc"]
            r = b % 2
            qall = qalls[r]
            B = Bs[r]
            jobs = []
            for h in range(12):
                pos = QPERM.index(h)
                g = h // 3
                obank = C.banks[3 + h // 6]
                ocol = (h % 6) * 65
                for kb0 in range(0, b + 1, 4):
                    kb1 = min(b + 1, kb0 + 4)
                    jobs.append((pos, g, obank, ocol, kb0, kb1))
            prev = None
            for job in jobs + [None]:
                if job is not None:
                    pos, g, obank, ocol, kb0, kb1 = job
                    sbank = C.banks[sc % 3]
                    PT = PTs[sc % 3]
                    sc += 1
                    for kb in range(kb0, kb1):
                        j = kb - kb0
                        P.op("tensor", lambda e: e.matmul(sbank[:, j * 128:(j + 1) * 128], lhsT=kT[:, g // 2, kb * 128:(kb + 1) * 128],
                                                          rhs=qall[:, pos, :], start=True, stop=False), reads=[kT, qall], writes=[sbank])
                        P.op("tensor", lambda e: e.matmul(sbank[:, j * 128:(j + 1) * 128], lhsT=B[:, kb * 128:(kb + 1) * 128], rhs=C.ident.ap(),
                                                          start=False, stop=True), reads=[B, C.ident], writes=[sbank])
                    nw = (kb1 - kb0) * 128
                    P.op("scalar", lambda e: e.activation(out=PT[:, 0:nw], in_=sbank[:, 0:nw], func=AF.Exp, scale=0.125), reads=[sbank], writes=[PT])
                if prev is not None:
                    (pos_, g_, obank_, ocol_, kb0_, kb1_), PT_ = prev
                    for kb in range(kb0_, kb1_):
                        j = kb - kb0_
                        P.op("tensor", lambda e: e.matmul(obank_[:, ocol_:ocol_ + 65], lhsT=PT_[:, j * 128:(j + 1) * 128], rhs=Va[:, kb, g_, 0:65],
                                                          start=(kb == 0), stop=(kb == b)), reads=[PT_, Va], writes=[obank_])
                prev = (job, PT) if job is not None else None
            cnts["sc"] = sc
            mem_heads(P, C, FW, qall, 20, kmT, Vm, C.banks[5], [C.banks[6], C.banks[7]], r)
            attn_finish(P, C, FW, xsrc, xdst, b, 16, Wout, Wot, 8)

        prep(0)
        for b in range(nblocks):
            if b + 1 < nblocks:
                prep(b + 1)
            attend(b)


def phase_F(P, C, tag, xnorm, xbase, xdst, wgu_dram, wd_dram, gains, grow, f0, f1, final_row=None, ntiles=NT):
    nf = f1 - f0
    with P.scope():
        Wgu = P.sb(tag + "Wgu", [128, 8, 2 * nf * 128], BF16)
        Wgt = [P.tok("%sWgt%d" % (tag, k)) for k in range(8)]
        for k in range(8):
            for half in range(2):
                c0 = half * DFF + f0 * 128
                P.dma("gpsimd", Wgu[:, k, half * nf * 128:(half + 1) * nf * 128], wgu_dram[k * 128:(k + 1) * 128, c0:c0 + nf * 128], writes=[Wgt[k]])
        Wd = P.sb(tag + "Wd", [128, nf, D], BF16)
        Wdt = [P.tok("%sWdt%d" % (tag, k)) for k in range(nf)]
        for k in range(nf):
            P.dma("gpsimd", Wd[:, k, :], wd_dram[(f0 + k) * 128:(f0 + k + 1) * 128, :], writes=[Wdt[k]])
        gb = load_gain(P, C, tag + "gbF", gains, grow)
        gfin = load_gain(P, C, tag + "gfin", gains, final_row) if final_row is not None else None
        NW = alloc_norm_work(P, C, tag + "F")
        xts = [P.sb("%sFx%d" % (tag, r), [128, D], F32) for r in range(2)]
        hT2 = [P.sb("%sFhT%d" % (tag, r), [128, 8, 256], BF16) for r in range(2)]
        hTt = [[P.tok("%sFhTt%d_%d" % (tag, r, t)) for t in range(2)] for r in range(2)]
        actT = [P.sb("%sactT%d" % (tag, r), [128, nf, 256], BF16) for r in range(2)]
        sg = [P.sb("%ssg%d" % (tag, r), [128, 256], F32) for r in range(2)]
        xn = [P.sb("%sFxn%d" % (tag, r), [128, D], F32) for r in range(4)]
        fsm = [[P.sb("%sfs%d_%d" % (tag, r, j), [128, 1], F32) for j in range(4)] for r in range(2)]
        fj = P.sb(tag + "fj", [128, D], BF16)
        gc = 0
        hbs = {}

        def norm1(G):
            for t in range(2):
                i = 2 * G + t
                xt = xts[i % 2]
                P.dma("sync", xt.ap(), xnorm[i * 128:(i + 1) * 128, :], writes=[xt])
                hbs[(G, t)] = rmsnorm_to_hT(P, C, NW, xt, gb, None, None, None)
                xo = xn[i % 4]
                P.dma("sync", xo.ap(), xbase[i * 128:(i + 1) * 128, :], writes=[xo])

        def norm2(G):
            for t in range(2):
                norm_p2(P, C, hbs.pop((G, t)), hT2[G % 2][:, :, t * 128:(t + 1) * 128], hTt[G % 2][t], C.banks[0],
                        evac_eng="scalar" if t == 0 else "vector")

        NG = ntiles // 2
        norm1(0)
        norm2(0)
        for G in range(NG):
            hT = hT2[G % 2]
            aT = actT[G % 2]
            for fc in range(nf):
                if fc == nf // 2 and G + 1 < NG:
                    norm1(G + 1)
                bank = C.banks[1 + (gc % 3)]
                s_ = sg[gc % 2]
                gc += 1
                for half in range(2):
                    coff = half * nf * 128 + fc * 128
                    for k in range(8):
                        P.op("tensor", lambda e: e.matmul(bank[:, half * 256:(half + 1) * 256], lhsT=Wgu[:, k, coff:coff + 128], rhs=hT[:, k, :],
                                                          start=(k == 0), stop=(k == 7)), reads=[Wgt[k]] + hTt[G % 2], writes=[bank])
                P.op("scalar", lambda e: e.activation(out=s_.ap(), in_=bank[:, 0:256], func=AF.Silu), reads=[bank], writes=[s_])
                P.op("vector", lambda e: e.tensor_tensor(out=aT[:, fc, :], in0=bank[:, 256:512], in1=s_.ap(), op=ALU.mult), reads=[bank, s_], writes=[aT])
            if G + 1 < NG:
                norm2(G + 1)
            for t in range(2):
                i = 2 * G + t
                xo = xn[i % 4]
                for half in range(2):
                    yb = C.banks[4 + (2 * t + half) % 4]
                    for fc in range(nf):
                        P.op("tensor", lambda e: e.matmul(yb.ap(), lhsT=aT[:, fc, t * 128:(t + 1) * 128], rhs=Wd[:, fc, half * 512:(half + 1) * 512],
                                                          start=(fc == 0), stop=(fc == nf - 1)), reads=[aT, Wdt[fc]], writes=[yb])
                    P.op("vector", lambda e: e.tensor_tensor(out=xo[:, half * 512:(half + 1) * 512], in0=yb.ap(), in1=xo[:, half * 512:(half + 1) * 512], op=ALU.add),
                         reads=[yb, xo], writes=[xo])
                if gfin is not None:
                    ssq, t1, t2, rstd = fsm[i % 2]
                    P.op("scalar", lambda e: e.activation(out=fj.ap(), in_=xo.ap(), func=AF.Square, accum_out=ssq.ap()), reads=[xo], writes=[fj, ssq])
                    P.op("vector", lambda e: e.tensor_scalar(out=t1.ap(), in0=ssq.ap(), scalar1=1.0 / D, scalar2=EPS, op0=ALU.mult, op1=ALU.add), reads=[ssq], writes=[t1])
                    P.op("scalar", lambda e: e.activation(out=t2.ap(), in_=t1.ap(), func=AF.Sqrt), reads=[t1], writes=[t2])
                    P.op("vector", lambda e: e.reciprocal(out=rstd.ap(), in_=t2.ap()), reads=[t2], writes=[rstd])
                    P.op("vector", lambda e: e.scalar_tensor_tensor(out=xo.ap(), in0=xo.ap(), scalar=rstd.ap(), in1=gfin.ap(), op0=ALU.mult, op1=ALU.mult),
                         reads=[xo, rstd, gfin], writes=[xo])
                P.dma("sync", xdst[i * 128:(i + 1) * 128, :], xo.ap(), reads=[xo], out=(gfin is not None))


def phase_B1(P, C, fT, vS, Og):
    for g, dil in enumerate((1, 4, 16)):
        nb = NT // dil
        with P.scope():
            qz = P.sb("qz1", [128, 4, S], BF16)
            kTg = P.sb("kTg", [128, 2, S], BF16)
            Vg = P.sb("Vg", [128, NT, 4, VW], BF16)
            P.op("gpsimd", lambda e: e.memset(qz.ap(), 0.0), writes=[qz])
            P.op("vector", lambda e: e.memset(Vg.ap(), 1.0), writes=[Vg])
            for c in range(2):
                for hh in range(2):
                    base = hh * 64
                    P.dma("sync", qz[base:base + 64, 2 * c + hh, :], fT[4 * g + c][base:base + 64, :], writes=[qz])
                P.dma("sync", kTg[:, c, :], fT[4 * g + 2 + c], writes=[kTg])
            vg = vS[:, g * 256:(g + 1) * 256].rearrange("(mb p dd) (h e) -> dd p mb h e", p=128, dd=dil, e=64)
            for r in range(dil):
                for m0 in range(nb):
                    P.dma("sync", Vg[:, r * nb + m0, :, 0:64], vg[r][:, m0, :, :], writes=[Vg])
            PTs = [P.sb("PTg%d" % k, [128, 512], BF16) for k in range(3)]
            Os = [P.sb("Os%d" % k, [128, 260], F32) for k in range(2)]
            Ogr = Og[g].rearrange("(m dd) c -> dd m c", dd=dil)
            sc = 0
            jobs = []
            qb = 0
            for r in range(dil):
                for mb in range(nb):
                    for hp in range(2):
                        jobs.append((r, mb, hp, qb))
                    qb += 1
            prev = None
            for job in jobs + [None]:
                if job is not None:
                    r, mb, hp, qb = job
                    qsl = slice(mb * 128 * dil + r, (mb * 128 + 127) * dil + r + 1, dil)
                    kbs = ([mb - 1] if mb > 0 else []) + [mb]
                    sbank = C.banks[sc % 3]
                    PT = PTs[sc % 3]
                    sc += 1
                    tiles = []
                    for hh in range(2):
                        j = hp * 2 + hh
                        for kb in kbs:
                            t = len(tiles)
                            tiles.append((j, kb))
                            ksl = slice(kb * 128 * dil + r, (kb * 128 + 127) * dil + r + 1, dil)
                            P.op("tensor", lambda e: e.matmul(sbank[:, t * 128:(t + 1) * 128], lhsT=kTg[:, j // 2, ksl], rhs=qz[:, j, qsl],
                                                              start=True, stop=False), reads=[kTg, qz], writes=[sbank])
                            M = C.MdT if kb == mb else C.MpT
                            P.op("tensor", lambda e: e.matmul(sbank[:, t * 128:(t + 1) * 128], lhsT=C.ident.ap(), rhs=M.ap(), start=False, stop=True),
                                 reads=[C.ident, M], writes=[sbank])
                    nw = len(tiles) * 128
                    P.op("scalar", lambda e: e.activation(out=PT[:, 0:nw], in_=sbank[:, 0:nw], func=AF.Exp, scale=0.125), reads=[sbank], writes=[PT])
                if prev is not None:
                    (r_, mb_, hp_, qb_), PT_, tiles_ = prev
                    obank = C.banks[6 + qb_ % 2]
                    kfirst = mb_ - 1 if mb_ > 0 else mb_
                    for t, (j, kb) in enumerate(tiles_):
                        P.op("tensor", lambda e: e.matmul(obank[:, j * 65:(j + 1) * 65], lhsT=PT_[:, t * 128:(t + 1) * 128], rhs=Vg[:, r_ * nb + kb, j, 0:65],
                                                          start=(kb == kfirst), stop=(kb == mb_)), reads=[PT_, Vg], writes=[obank])
                    if hp_ == 1:
                        O = Os[qb_ % 2]
                        P.op("vector", lambda e: e.tensor_copy(O.ap(), obank[:, 0:260]), reads=[obank], writes=[O])
                        P.dma("sync", Ogr[r_][mb_ * 128:(mb_ + 1) * 128, :], O.ap(), reads=[O])
                prev = (job, PT, tiles) if job is not None else None


def phase_B2(P, C, xsrc, xdst, fT, Og, kmT, Vm, wout_dram):
    with P.scope():
        Wout, Wot = load_weight(P, "Wout1", wout_dram, 4, D)
        qzs = [P.sb("qzm%d" % r, [128, 4, 128], BF16) for r in range(2)]
        for r in range(2):
            P.op("gpsimd", lambda e: e.memset(qzs[r].ap(), 0.0), writes=[qzs[r]])
        Ot = [[P.sb("Ot%d_%d" % (r, g), [128, 260], F32) for g in range(3)] for r in range(2)]
        FW = alloc_finish_work(P, C, "b2", [], C.banks[0], [C.banks[1], C.banks[2]])
        FW.osrc = lambda r: [(Ot[r][0], Ot[r][0].ap(), 4), (C.banks[5], C.banks[5][:, 0:260], 4)]
        fT_r = fT.rearrange("c p t -> p c t")
        for i in range(NT):
            r = i % 2
            qz = qzs[r]
            q0 = i * 128
            for base in (0, 64):
                P.dma("sync", qz[base:base + 64, base // 64:4:2, :], fT_r[base:base + 64, 12:14, q0:q0 + 128], writes=[qz])
            for g in range(3):
                P.dma("sync", Ot[r][g].ap(), Og[g][q0:q0 + 128, :], writes=[Ot[r][g]])
            mem_heads(P, C, FW, qz, 0, kmT, Vm, C.banks[5], [C.banks[6], C.banks[7]], r)
            P.op("gpsimd", lambda e: e.tensor_tensor(out=Ot[r][0].ap(), in0=Ot[r][0].ap(), in1=Ot[r][1].ap(), op=ALU.add),
                 reads=[Ot[r][0], Ot[r][1]], writes=[Ot[r][0]])
            P.op("gpsimd", lambda e: e.tensor_tensor(out=Ot[r][0].ap(), in0=Ot[r][0].ap(), in1=Ot[r][2].ap(), op=ALU.add),
                 reads=[Ot[r][0], Ot[r][2]], writes=[Ot[r][0]])
            attn_finish(P, C, FW, xsrc, xdst, i, 8, Wout, Wot, 4)


def build_program(stop_after=None, dbg_block=None, nblocks0=NT, skip_l0=False):
    nc = bass.Bass("TRN2", target_bir_lowering=False)
    P = Prog(nc)
    C = Ctx()
    I = {}

    def inp(name, shape, dt=F32):
        I[name] = nc.dram_tensor(name, list(shape), dt, kind="ExternalInput").ap()
        return I[name]

    x_in = inp("x", [S, D])
    mem_in = inp("mem", [256, D])
    pos_in = inp("pos", [128, NT], I32)
    gains = inp("gains", [7, D])
    C.freqs_in = inp("freqs", [128, 48])
    w_in0 = inp("w_in0", [D, SPEC0["ncols"]])
    w_in1 = inp("w_in1", [D, SPEC1["ncols"]])
    w_mkv = [inp("w_mkv%d" % l, [D, 512]) for l in range(2)]
    w_out0 = inp("w_out0", [1024, D])
    w_out1 = inp("w_out1", [512, D])
    w_gu = [inp("w_gu%d" % l, [D, 2 * DFF]) for l in range(2)]
    w_dn = [inp("w_dn%d" % l, [DFF, D]) for l in range(2)]
    out = nc.dram_tensor("out", [S, D], F32, kind="ExternalOutput").ap()
    C.dbg = None
    C.dbg_block = dbg_block
    if dbg_block is not None:
        C.dbg = dict(score=nc.dram_tensor("dbg_score", [128, S], F32, kind="ExternalOutput").ap(),
                     thr=nc.dram_tensor("dbg_thr", [128, 1], F32, kind="ExternalOutput").ap(),
                     Bm=nc.dram_tensor("dbg_Bm", [128, S], BF16, kind="ExternalOutput").ap())
    fT0 = P.dram("fT0", [15, 128, S], BF16)
    vS0 = P.dram("vS0", [S, 256], BF16)
    fT1 = P.dram("fT1", [14, 128, S], BF16)
    vS1 = P.dram("vS1", [S, 768], BF16)
    Og = [P.dram("Og%d" % g, [S, 260], F32) for g in range(3)]
    xa = P.dram("xa", [S, D], F32)
    xb = P.dram("xb", [S, D], F32)
    xc = P.dram("xc", [S, D], F32)

    C.banks = [P.ps("bank%d" % k, [128, 512], F32) for k in range(8)]
    setup_consts(P, C, pos_in)
    HF = NFC // 2

    def final(src):
        print("n_inst before final", P.n_inst)
        P.max_ops = None
        with P.scope():
            t = [P.sb("fin%d" % r, [128, D], F32) for r in range(2)]
            for i in range(NT):
                P.dma("sync", t[i % 2].ap(), src[i * 128:(i + 1) * 128, :], writes=[t[i % 2]])
                P.dma("sync", out[i * 128:(i + 1) * 128, :], t[i % 2].ap(), reads=[t[i % 2]], out=True)
        P.finish()
        return nc

    with (P.scope() if not skip_l0 else contextlib.nullcontext()):
      if not skip_l0:
        wiAll = P.sb("wiAll", [128, NT, 8], F32)
        kmT = P.sb("kmT0", [128, 2, 256], BF16)
        Vm = P.sb("Vm0", [128, 2, 4, VW], BF16)
        phase_M(P, C, "m0", mem_in, w_mkv[0], gains, 1, kmT, Vm)
        if stop_after == "M":
            d1 = nc.dram_tensor("dbg_kmT", [128, 2, 256], BF16, kind="ExternalOutput").ap()
            d2 = nc.dram_tensor("dbg_Vm", [128, 2, 4, VW], BF16, kind="ExternalOutput").ap()
            P.dma("sync", d1, kmT.ap(), reads=[kmT])
            P.dma("sync", d2, Vm.ap(), reads=[Vm])
            return final(x_in)
        phase_A(P, C, "a0", x_in, w_in0, gains, 0, SPEC0, fT0, vS0, wiAll, pos_in)
        if stop_after == "A":
            d1 = nc.dram_tensor("dbg_fT0", [15, 128, S], BF16, kind="ExternalOutput").ap()
            d2 = nc.dram_tensor("dbg_vS0", [S, 256], BF16, kind="ExternalOutput").ap()
            d3 = nc.dram_tensor("dbg_wi", [128, NT, 8], F32, kind="ExternalOutput").ap()
            with P.scope():
                tb = P.sb("dbgt", [128, S], BF16)
                for c in range(15):
                    P.dma("sync", tb.ap(), fT0[c], writes=[tb])
                    P.dma("sync", d1[c], tb.ap(), reads=[tb])
                for c in range(2):
                    P.dma("sync", tb[:, 0:2048].rearrange("p (a b) -> p a b", b=256), vS0[c * 2048:(c + 1) * 2048, :].rearrange("(a p) b -> p a b", p=128), writes=[tb])
                    P.dma("sync", d2[c * 2048:(c + 1) * 2048, :].rearrange("(a p) b -> p a b", p=128), tb[:, 0:2048].rearrange("p (a b) -> p a b", b=256), reads=[tb])
                P.dma("sync", d3, wiAll.ap(), reads=[wiAll])
            return final(x_in)
        phase_B0(P, C, x_in, xa, fT0, vS0, wiAll, kmT, Vm, w_out0, nblocks=nblocks0)
    if stop_after == "B0":
        return final(xa)
    if not skip_l0:
        phase_F(P, C, "f0a", xa, xa, xb, w_gu[0], w_dn[0], gains, 2, 0, HF)
        phase_F(P, C, "f0b", xa, xb, xc, w_gu[0], w_dn[0], gains, 2, HF, NFC)
    else:
        xc = x_in
    if stop_after == "F0":
        return final(xc)
    with P.scope():
        kmT = P.sb("kmT1", [128, 2, 256], BF16)
        Vm = P.sb("Vm1", [128, 2, 4, VW], BF16)
        phase_M(P, C, "m1", mem_in, w_mkv[1], gains, 4, kmT, Vm)
        phase_A(P, C, "a1", xc, w_in1, gains, 3, SPEC1, fT1, vS1, None, pos_in)
        phase_B1(P, C, fT1, vS1, Og)
        phase_B2(P, C, xc, xa, fT1, Og, kmT, Vm, w_out1)
    if stop_after == "B2":
        return final(xa)
    phase_F(P, C, "f1a", xa, xa, xb, w_gu[1], w_dn[1], gains, 5, 0, HF)
    phase_F(P, C, "f1b", xa, xb, out, w_gu[1], w_dn[1], gains, 5, HF, NFC, final_row=6)
    P.finish()
    return nc


def prep_inputs(inputs):
    f = lambda a: np.ascontiguousarray(np.asarray(a, dtype=np.float32))
    w0 = f(inputs["l0_w_in"])
    q, k, v, qi, ki, wi, qm = np.split(w0, np.cumsum([768, 256, 256, 512, 64, 8])[:], axis=1)
    qp = np.concatenate([q[:, h * 64:(h + 1) * 64] for h in QPERM], axis=1)
    w_in0 = np.ascontiguousarray(np.concatenate([qp, k, qi, ki, ki, wi, v, qm], axis=1))
    w1 = f(inputs["l1_w_in"])
    parts = [w1[:, j * 256:(j + 1) * 256] for j in range(10)]
    w_in1 = np.ascontiguousarray(np.concatenate([parts[0], parts[1], parts[3], parts[4], parts[6], parts[7], parts[2], parts[5], parts[8], parts[9]], axis=1))
    gains = np.ascontiguousarray(np.stack([f(inputs[n]) for n in ("l0_norm_mix", "l0_norm_mem", "l0_norm_ffn", "l1_norm_mix", "l1_norm_mem",
                                                                 "l1_norm_ffn", "final_norm")], axis=0))
    fr64 = (np.float32(10000.0) ** (-np.arange(32, dtype=np.float32) / np.float32(32))).astype(np.float32)
    fr16 = (np.float32(10000.0) ** (-np.arange(16, dtype=np.float32) / np.float32(16))).astype(np.float32)
    freqs = np.ascontiguousarray(np.broadcast_to(np.concatenate([fr64, fr16])[None, :], (128, 48)).astype(np.float32))
    shared = dict(freqs=freqs, gains=gains, w_in0=w_in0, w_in1=w_in1, w_mkv0=f(inputs["l0_w_mem_kv"]), w_mkv1=f(inputs["l1_w_mem_kv"]),
                  w_out0=f(inputs["l0_w_out"]), w_out1=f(inputs["l1_w_out"]), w_gu0=f(inputs["l0_w_gate_up"]), w_gu1=f(inputs["l1_w_gate_up"]),
                  w_dn0=f(inputs["l0_w_down"]), w_dn1=f(inputs["l1_w_down"]))
    x = f(inputs["x"])
    mem = f(inputs["mem"])
    pos = np.ascontiguousarray(np.asarray(inputs["positions"], dtype=np.int32))
    maps = []
    for c in range(x.shape[0]):
        m = dict(shared)
        m["x"] = x[c]
        m["mem"] = mem[c]
        m["pos"] = np.ascontiguousarray(pos[c].reshape(NT, 128).T)
        maps.append(m)
    return maps


_NC_CACHE = {}


def kernel(**inputs):
    maps = prep_inputs(inputs)
    if "nc" not in _NC_CACHE:
        _NC_CACHE["nc"] = build_program()
    nc = _NC_CACHE["nc"]
    res = run_bass_kernel_spmd(nc, maps, core_ids=list(range(len(maps))))
    return np.stack([np.asarray(r["out"], dtype=np.float32) for r in res.results], axis=0)
```

```python
import contextlib
import numpy as np
import ml_dtypes
import concourse.bass as bass
import concourse.mybir as mybir
from concourse.bass_utils import run_bass_kernel_spmd

F32 = mybir.dt.float32
BF16 = mybir.dt.bfloat16
I32 = mybir.dt.int32
AF = mybir.ActivationFunctionType
ALU = mybir.AluOpType
AX = mybir.AxisListType

SAME_ENGINE_SYNC = True


class Tok:
    def __init__(self, name):
        self.name = name
        self.w = None
        self.r = {}


class Buf(Tok):
    def __init__(self, name, handle):
        super().__init__(name)
        self.h = handle

    def ap(self):
        return self.h[:]

    def __getitem__(self, idx):
        return self.h[idx]


class _Eng:
    def __init__(self, name, handle, sem):
        self.name = name
        self.h = handle
        self.sem = sem
        self.count = 0
        self.seen = {}


class Prog:
    def __init__(self, nc, n_dma_sems=8):
        self.nc = nc
        self.stack = contextlib.ExitStack()
        self.engs = {}
        for name in ("tensor", "vector", "scalar", "gpsimd", "sync"):
            sem = self.stack.enter_context(nc.semaphore("s_" + name))
            self.engs[name] = _Eng(name, getattr(nc, name), sem)
        self.dma_sems = {}
        for q in ("sync", "gpsimd", "scalar"):
            self.dma_sems[q] = [[self.stack.enter_context(nc.semaphore("d_%s%d" % (q, i))), 0] for i in range(n_dma_sems)]
        self.dma_rr = {"sync": 0, "gpsimd": 0, "scalar": 0}
        self.out_events = []
        self.n_inst = 0
        self.scopes = [self.stack]
        import os
        self.max_ops = int(os.environ["KMAXOPS"]) if "KMAXOPS" in os.environ else None

    @contextlib.contextmanager
    def scope(self):
        st = contextlib.ExitStack()
        self.scopes.append(st)
        try:
            yield
        finally:
            self.barrier()
            self.scopes.pop()
            st.close()

    def barrier(self):
        if getattr(self, "finished", False):
            return
        for eng in self.engs.values():
            for other in self.engs.values():
                if other is not eng and other.count > 0:
                    self._wait(eng, (other.name, other.sem, other.count))
            for q, pool in self.dma_sems.items():
                for i, slot in enumerate(pool):
                    if slot[1] > 0:
                        self._wait(eng, ("d_%s%d" % (q, i), slot[0], 16 * slot[1]))

    def tok(self, name):
        return Tok(name)

    def sb(self, name, shape, dtype):
        self.uid = getattr(self, "uid", 0) + 1
        name = "%s_u%d" % (name, self.uid)
        return Buf(name, self.scopes[-1].enter_context(self.nc.sbuf_tensor(name, list(shape), dtype)))

    def ps(self, name, shape, dtype):
        b = Buf(name, self.scopes[-1].enter_context(self.nc.psum_tensor(name, list(shape), dtype)))
        b.excl = True
        return b

    def dram(self, name, shape, dtype):
        return self.nc.dram_tensor(name, list(shape), dtype, kind="Internal").ap()

    def _wait(self, eng, ev):
        key, sem, val = ev
        if eng.seen.get(key, 0) >= val:
            return
        if key == eng.name and not (SAME_ENGINE_SYNC and eng.name != "tensor" and eng.name != "sync"):
            return
        eng.h.wait_ge(sem, val)
        eng.seen[key] = val

    def _deps(self, eng, reads, writes):
        for t in reads:
            if t.w is not None:
                self._wait(eng, t.w)
        for t in writes:
            if t.w is not None:
                self._wait(eng, t.w)
            for ev in t.r.values():
                self._wait(eng, ev)

    def _record(self, ev, reads, writes):
        for t in writes:
            t.w = ev
            t.r = {}
        for t in reads:
            if t in writes:
                continue
            t.r[ev[0]] = ev

    def op(self, engname, fn, reads=(), writes=()):
        if self.max_ops is not None and self.n_inst >= self.max_ops:
            return None
        eng = self.engs[engname]
        ex = [t for t in reads if getattr(t, "excl", False) and t not in writes]
        if ex:
            reads = [t for t in reads if t not in ex]
            writes = list(writes) + ex
        self._deps(eng, reads, writes)
        inst = fn(eng.h)
        eng.count += 1
        inst.then_inc(eng.sem, 1)
        ev = (eng.name, eng.sem, eng.count)
        self._record(ev, reads, writes)
        self.n_inst += 1
        return ev

    def dma(self, q, out_ap, in_ap, reads=(), writes=(), out=False, **kw):
        if self.max_ops is not None and self.n_inst >= self.max_ops:
            return None
        eng = self.engs[q]
        self._deps(eng, reads, writes)
        pool = self.dma_sems[q]
        i = self.dma_rr[q]
        self.dma_rr[q] = (i + 1) % len(pool)
        slot = pool[i]
        key = "d_%s%d" % (q, i)
        if slot[1] > 0:
            self._wait(eng, (key, slot[0], 16 * slot[1]))
        eng.h.dma_start(out=out_ap, in_=in_ap, **kw).then_inc(slot[0], 16)
        slot[1] += 1
        ev = (key, slot[0], 16 * slot[1])
        self._record(ev, reads, writes)
        if out:
            self.out_events.append(ev)
        self.n_inst += 1
        return ev

    def finish(self):
        eng = self.engs["sync"]
        for q, pool in self.dma_sems.items():
            for i, slot in enumerate(pool):
                if slot[1] > 0:
                    self._wait(eng, ("d_%s%d" % (q, i), slot[0], 16 * slot[1]))
        for name, e in self.engs.items():
            if name != "sync" and e.count > 0:
                self._wait(eng, (name, e.sem, e.count))
        self.finished = True
        for st in reversed(self.scopes):
            st.close()

    def make_identity(self, idt):
        tmp = self.sb(idt.name + "_i", [128, 128], I32)
        self.op("gpsimd", lambda e: e.iota(tmp.ap(), pattern=[[1, 128]], base=0, channel_multiplier=-1), writes=[tmp])
        self.op("vector", lambda e: e.tensor_scalar(out=idt.ap(), in0=tmp.ap(), scalar1=0.0, scalar2=None, op0=ALU.is_equal),
                reads=[tmp], writes=[idt])


S = 4096
D = 1024
NT = 32
DFF = 2816
NFC = 22
EPS = 1e-6
NEG = -1.0e30
MASKV = -30000.0
VW = 66
N_BISECT = 16
IDX_SCALE = (8 ** -0.5) * (64 ** -0.5)
QPERM = [0, 3, 1, 4, 2, 5, 6, 9, 7, 10, 8, 11]

SPEC0 = dict(
    ncols=2184,
    chunks=[(0, 512, [("rope64", 0, 8)]), (512, 512, [("rope64", 0, 8)]), (1024, 512, [("rope32", 0, 8)]),
            (1536, 136, [("rope32", 0, 2), ("wi", 128, 8)]), (1672, 512, [("copy", 0, 512)])],
    tcols=[128 * k for k in range(13)] + [1928, 2056],
    vcols=(1672, 1928),
)
SPEC1 = dict(
    ncols=2560,
    chunks=[(0, 512, [("rope64", 0, 8)]), (512, 512, [("rope64", 0, 8)]), (1024, 512, [("rope64", 0, 8)]),
            (1536, 512, [("copy", 0, 512)]), (2048, 512, [("copy", 0, 512)])],
    tcols=[128 * k for k in range(12)] + [2304, 2432],
    vcols=(1536, 2304),
)


class Ctx:
    pass


def load_weight(P, name, dram, nk, ncols, q="gpsimd"):
    W = P.sb(name, [128, nk, ncols], BF16)
    toks = [P.tok("%s_%d" % (name, k)) for k in range(nk)]
    for k in range(nk):
        c0 = 0
        while c0 < ncols:
            c1 = min(ncols, c0 + 2048)
            P.dma(q, W[:, k, c0:c1], dram[k * 128:(k + 1) * 128, c0:c1], writes=[toks[k]])
            c0 = c1
    return W, toks


def setup_consts(P, C, pos_in):
    C.ident = P.sb("ident", [128, 128], BF16)
    P.make_identity(C.ident)
    C.negthr = P.sb("negthr", [128, 1], F32)
    P.op("vector", lambda e: e.memset(C.negthr.ap(), -1.0e29), writes=[C.negthr])
    C.pow2 = P.sb("pow2", [128, N_BISECT + 2], F32)
    for k in range(N_BISECT + 2):
        P.op("gpsimd", lambda e: e.memset(C.pow2[:, k:k + 1], 2.0 ** (1 - k)), writes=[C.pow2])
    C.MdT = P.sb("MdT", [128, 128], BF16)
    C.MpT = P.sb("MpT", [128, 128], BF16)
    zt = P.sb("zt", [128, 128], F32)
    zm = P.sb("zm", [128, 128], F32)
    P.op("vector", lambda e: e.memset(zt.ap(), 0.0), writes=[zt])
    P.op("gpsimd", lambda e: e.affine_select(out=zm.ap(), in_=zt.ap(), pattern=[[1, 128]], compare_op=ALU.is_ge, fill=MASKV, base=0, channel_multiplier=-1),
         reads=[zt], writes=[zm])
    P.op("vector", lambda e: e.tensor_copy(C.MdT.ap(), zm.ap()), reads=[zm], writes=[C.MdT])
    P.op("gpsimd", lambda e: e.affine_select(out=zm.ap(), in_=zt.ap(), pattern=[[-1, 128]], compare_op=ALU.is_ge, fill=MASKV, base=0, channel_multiplier=1),
         reads=[zt, C.MdT], writes=[zm])
    P.op("vector", lambda e: e.tensor_copy(C.MpT.ap(), zm.ap()), reads=[zm], writes=[C.MpT])


def build_rope_tables(P, C, pos_in):
    C.cos64 = P.sb("cos64", [128, NT, 32], F32)
    C.sin64 = P.sb("sin64", [128, NT, 32], F32)
    C.nsin64 = P.sb("nsin64", [128, NT, 32], F32)
    C.cos16 = P.sb("cos16", [128, NT, 16], F32)
    C.sin16 = P.sb("sin16", [128, NT, 16], F32)
    C.nsin16 = P.sb("nsin16", [128, NT, 16], F32)
    with P.scope():
        posi = P.sb("posi", [128, NT], I32)
        posf = P.sb("posf", [128, NT], F32)
        P.dma("sync", posi.ap(), pos_in, writes=[posi])
        P.op("vector", lambda e: e.tensor_copy(posf.ap(), posi.ap()), reads=[posi], writes=[posf])
        for half, cosT, sinT, nsinT in ((32, C.cos64, C.sin64, C.nsin64), (16, C.cos16, C.sin16, C.nsin16)):
            n = NT * half
            fr = P.sb("fr%d" % half, [128, half], F32)
            a = P.sb("a%d" % half, [128, NT, half], F32)
            ki = P.sb("ki%d" % half, [128, NT, half], I32)
            kf = P.sb("kf%d" % half, [128, NT, half], F32)
            fr1 = P.sb("fr1%d" % half, [128, NT, half], F32)
            m1 = P.sb("m1%d" % half, [128, NT, half], F32)
            f0 = 0 if half == 32 else 32
            P.dma("sync", fr.ap(), C.freqs_in[:, f0:f0 + half], writes=[fr])
            P.op("vector", lambda e: e.tensor_tensor(out=a.ap(), in0=posf.ap().unsqueeze(2).broadcast_to([128, NT, half]),
                                                     in1=fr.ap().unsqueeze(1).broadcast_to([128, NT, half]), op=ALU.mult),
                 reads=[posf, fr], writes=[a])
            P.op("vector", lambda e: e.tensor_scalar(out=a.ap(), in0=a.ap(), scalar1=float(1.0 / (2.0 * np.pi)), scalar2=None, op0=ALU.mult),
                 reads=[a], writes=[a])
            for shift, outT, neg in ((0.0, sinT, False), (0.25, cosT, False), (0.5, nsinT, False)):
                src = a
                if shift != 0.0:
                    P.op("vector", lambda e: e.tensor_scalar(out=fr1.ap(), in0=a.ap(), scalar1=shift, scalar2=None, op0=ALU.add),
                         reads=[a], writes=[fr1])
                    src = fr1
                P.op("vector", lambda e: e.tensor_copy(ki.ap(), src.ap()), reads=[src], writes=[ki])
                P.op("vector", lambda e: e.tensor_copy(kf.ap(), ki.ap()), reads=[ki], writes=[kf])
                P.op("vector", lambda e: e.tensor_tensor(out=kf.ap(), in0=src.ap(), in1=kf.ap(), op=ALU.subtract), reads=[src, kf], writes=[kf])
                P.op("vector", lambda e: e.tensor_scalar(out=m1.ap(), in0=kf.ap(), scalar1=0.5, scalar2=None, op0=ALU.is_gt), reads=[kf], writes=[m1])
                P.op("vector", lambda e: e.tensor_tensor(out=kf.ap(), in0=kf.ap(), in1=m1.ap(), op=ALU.subtract), reads=[kf, m1], writes=[kf])
                P.op("vector", lambda e: e.tensor_scalar(out=m1.ap(), in0=kf.ap(), scalar1=-0.5, scalar2=None, op0=ALU.is_lt), reads=[kf], writes=[m1])
                P.op("vector", lambda e: e.tensor_tensor(out=kf.ap(), in0=kf.ap(), in1=m1.ap(), op=ALU.add), reads=[kf, m1], writes=[kf])
                P.op("scalar", lambda e: e.activation(out=outT.ap(), in_=kf.ap(), func=AF.Sin, scale=float(2.0 * np.pi) * (1.0 - 1e-6)),
                     reads=[kf], writes=[outT])


def alloc_norm_work(P, C, tag):
    W = Ctx()
    W.sqj = P.sb(tag + "sqj", [128, D], BF16)
    W.small = [[P.sb("%ssm%d_%d" % (tag, r, j), [128, 1], F32) for j in range(4)] for r in range(2)]
    W.hb = [P.sb("%shb%d" % (tag, r), [128, D], BF16) for r in range(2)]
    W.k = 0
    return W


def rmsnorm_to_hT(P, C, W, xt, gb, hT_ap, hT_tok, bank, evac_eng="scalar"):
    r = W.k % 2
    W.k += 1
    ssq, t1, t2, rstd = W.small[r]
    hb = W.hb[r]
    P.op("scalar", lambda e: e.activation(out=W.sqj.ap(), in_=xt.ap(), func=AF.Square, accum_out=ssq.ap()), reads=[xt], writes=[W.sqj, ssq])
    P.op("vector", lambda e: e.tensor_scalar(out=t1.ap(), in0=ssq.ap(), scalar1=1.0 / D, scalar2=EPS, op0=ALU.mult, op1=ALU.add), reads=[ssq], writes=[t1])
    P.op("scalar", lambda e: e.activation(out=t2.ap(), in_=t1.ap(), func=AF.Sqrt), reads=[t1], writes=[t2])
    P.op("vector", lambda e: e.reciprocal(out=rstd.ap(), in_=t2.ap()), reads=[t2], writes=[rstd])
    P.op("vector", lambda e: e.scalar_tensor_tensor(out=hb.ap(), in0=xt.ap(), scalar=rstd.ap(), in1=gb.ap(), op0=ALU.mult, op1=ALU.mult),
         reads=[xt, rstd, gb], writes=[hb])
    if hT_ap is None:
        return hb
    return norm_p2(P, C, hb, hT_ap, hT_tok, bank, evac_eng)


def norm_p2(P, C, hb, hT_ap, hT_tok, bank, evac_eng="scalar"):
    bbf = bank.ap().bitcast(BF16)
    for c in range(8):
        P.op("tensor", lambda e: e.transpose(bbf[:, c * 128:(c + 1) * 128], hb[:, c * 128:(c + 1) * 128], C.ident.ap()),
             reads=[hb, C.ident], writes=[bank])
    srcv = bbf if len(hT_ap.shape) == 2 else bbf.rearrange("p (c t) -> p c t", t=128)
    if evac_eng == "scalar":
        P.op("scalar", lambda e: e.activation(out=hT_ap, in_=srcv, func=AF.Copy), reads=[bank], writes=[hT_tok])
    else:
        P.op("vector", lambda e: e.tensor_copy(hT_ap, srcv), reads=[bank], writes=[hT_tok])


def load_gain(P, C, name, gains, row):
    gb = P.sb(name, [128, D], F32)
    P.dma("sync", gb.ap(), gains[row].partition_broadcast(128), writes=[gb])
    return gb


def phase_A(P, C, tag, xsrc, w_dram, gains, grow, spec, fT, vS, wiAll, pos_in):
    ncols = spec["ncols"]
    with P.scope():
        build_rope_tables(P, C, pos_in)
        Win, Wt = load_weight(P, tag + "Win", w_dram, 8, ncols)
        gb = load_gain(P, C, tag + "gbA", gains, grow)
        NW = alloc_norm_work(P, C, tag + "A")
        xts = [P.sb("%sxt%d" % (tag, r), [128, D], F32) for r in range(2)]
        hTs = [P.sb("%shT%d" % (tag, r), [128, D], BF16) for r in range(2)]
        pts = [P.sb("%spt%d" % (tag, r), [128, ncols], BF16) for r in range(2)]
        t1s = [P.sb("%st1_%d" % (tag, r), [128, 512], F32) for r in range(2)]
        t2s = [P.sb("%st2_%d" % (tag, r), [128, 512], F32) for r in range(2)]
        ntc = len(spec["tcols"])
        fTs = [P.sb("%sfTs%d" % (tag, r), [128, ntc, 128], BF16) for r in range(2)]
        fT_r = fT.rearrange("c p t -> p c t")
        cc = 0
        hbs = {}

        def n1(i):
            xt = xts[i % 2]
            P.dma("sync", xt.ap(), xsrc[i * 128:(i + 1) * 128, :], writes=[xt])
            hbs[i] = rmsnorm_to_hT(P, C, NW, xt, gb, None, None, None)

        def n2(i):
            norm_p2(P, C, hbs.pop(i), hTs[i % 2].ap(), hTs[i % 2], C.banks[0], evac_eng="scalar")

        n1(0)
        n2(0)
        for i in range(NT):
            hT = hTs[i % 2]
            pt = pts[i % 2]
            if i + 1 < NT:
                n1(i + 1)
            for (col0, width, handlers) in spec["chunks"]:
                bank = C.banks[1 + (cc % 4)]
                t1 = t1s[cc % 2]
                t2 = t2s[cc % 2]
                cc += 1
                for k in range(8):
                    P.op("tensor", lambda e: e.matmul(bank[:, 0:width], lhsT=hT[:, k * 128:(k + 1) * 128], rhs=Win[:, k, col0:col0 + width],
                                                      start=(k == 0), stop=(k == 7)), reads=[hT, Wt[k]], writes=[bank])
                for (kind, l0, n) in handlers:
                    if kind == "rope64":
                        nh = n
                        w = nh * 64
                        xv2 = bank[:, l0:l0 + w].rearrange("p (h d) -> p h d", d=32)
                        xv = bank[:, l0:l0 + w].rearrange("p (h d) -> p h d", d=64)
                        t1v2 = t1[:, 0:w].rearrange("p (h d) -> p h d", d=32)
                        t2v = t2[:, 0:w].rearrange("p (h d) -> p h d", d=64)
                        cosb = C.cos64[:, i:i + 1, :].broadcast_to([128, 2 * nh, 32])
                        sinb = C.sin64[:, i:i + 1, :].broadcast_to([128, nh, 32])
                        nsinb = C.nsin64[:, i:i + 1, :].broadcast_to([128, nh, 32])
                        P.op("vector", lambda e: e.tensor_tensor(out=t1v2, in0=xv2, in1=cosb, op=ALU.mult), reads=[bank, C.cos64], writes=[t1])
                        P.op("vector", lambda e: e.tensor_tensor(out=t2v[:, :, 0:32], in0=xv[:, :, 32:64], in1=nsinb, op=ALU.mult),
                             reads=[bank, C.nsin64], writes=[t2])
                        P.op("vector", lambda e: e.tensor_tensor(out=t2v[:, :, 32:64], in0=xv[:, :, 0:32], in1=sinb, op=ALU.mult),
                             reads=[bank, C.sin64], writes=[t2])
                        P.op("gpsimd", lambda e: e.tensor_tensor(out=pt[:, col0 + l0:col0 + l0 + w], in0=t1[:, 0:w], in1=t2[:, 0:w], op=ALU.add),
                             reads=[t1, t2], writes=[pt])
                    elif kind == "rope32":
                        nh = n
                        w = nh * 64
                        xv = bank[:, l0:l0 + w].rearrange("p (h d) -> p h d", d=64)
                        xr4 = bank[:, l0:l0 + w].rearrange("p (h t d) -> p h t d", t=4, d=16)
                        t1r4 = t1[:, 0:w].rearrange("p (h t d) -> p h t d", t=4, d=16)
                        t2v = t2[:, 0:w].rearrange("p (h d) -> p h d", d=64)
                        t1v = t1[:, 0:w].rearrange("p (h d) -> p h d", d=64)
                        ptv = pt[:, col0 + l0:col0 + l0 + w].rearrange("p (h d) -> p h d", d=64)
                        cosb = C.cos16[:, i:i + 1, :].unsqueeze(1).broadcast_to([128, nh, 2, 16])
                        sinb = C.sin16[:, i:i + 1, :].broadcast_to([128, nh, 16])
                        nsinb = C.nsin16[:, i:i + 1, :].broadcast_to([128, nh, 16])
                        P.op("vector", lambda e: e.tensor_tensor(out=t1r4[:, :, 0:2, :], in0=xr4[:, :, 0:2, :], in1=cosb, op=ALU.mult),
                             reads=[bank, C.cos16], writes=[t1])
                        P.op("vector", lambda e: e.tensor_tensor(out=t2v[:, :, 0:16], in0=xv[:, :, 16:32], in1=nsinb, op=ALU.mult),
                             reads=[bank, C.nsin16], writes=[t2])
                        P.op("vector", lambda e: e.tensor_tensor(out=t2v[:, :, 16:32], in0=xv[:, :, 0:16], in1=sinb, op=ALU.mult),
                             reads=[bank, C.sin16], writes=[t2])
                        P.op("gpsimd", lambda e: e.tensor_tensor(out=ptv[:, :, 0:32], in0=t1v[:, :, 0:32], in1=t2v[:, :, 0:32], op=ALU.add),
                             reads=[t1, t2], writes=[pt])
                        P.op("scalar", lambda e: e.activation(out=ptv[:, :, 32:64], in_=xv[:, :, 32:64], func=AF.Copy), reads=[bank], writes=[pt])
                    elif kind == "copy":
                        P.op("scalar", lambda e: e.activation(out=pt[:, col0 + l0:col0 + l0 + n], in_=bank[:, l0:l0 + n], func=AF.Copy),
                             reads=[bank], writes=[pt])
                    elif kind == "wi":
                        P.op("scalar", lambda e: e.activation(out=wiAll[:, i, :], in_=bank[:, l0:l0 + n], func=AF.Copy, scale=float(IDX_SCALE)),
                             reads=[bank], writes=[wiAll])
            if i + 1 < NT:
                n2(i + 1)
            fts = fTs[i % 2]
            for g0 in range(0, ntc, 8):
                g1 = min(ntc, g0 + 8)
                bank = C.banks[5 + (g0 // 8)]
                bbf = bank.ap().bitcast(BF16)
                for k in range(g0, g1):
                    c0 = spec["tcols"][k]
                    P.op("tensor", lambda e: e.transpose(bbf[:, (k - g0) * 128:(k - g0 + 1) * 128], pt[:, c0:c0 + 128], C.ident.ap()),
                         reads=[pt, C.ident], writes=[bank])
                eng = "vector" if g0 == 0 else "scalar"
                if eng == "vector":
                    P.op("vector", lambda e: e.tensor_copy(fts[:, g0:g1, :], bbf[:, 0:(g1 - g0) * 128].rearrange("p (c t) -> p c t", t=128)),
                         reads=[bank], writes=[fts])
                else:
                    P.op("scalar", lambda e: e.activation(out=fts[:, g0:g1, :], in_=bbf[:, 0:(g1 - g0) * 128].rearrange("p (c t) -> p c t", t=128),
                                                          func=AF.Copy), reads=[bank], writes=[fts])
            P.dma("sync", fT_r[:, :, i * 128:(i + 1) * 128], fts.ap(), reads=[fts])
            v0, v1 = spec["vcols"]
            P.dma("sync", vS[i * 128:(i + 1) * 128, :], pt[:, v0:v1], reads=[pt])


def phase_M(P, C, tag, mem_in, w_dram, gains, grow, kmT, Vm):
    with P.scope():
        Wm, Wt = load_weight(P, tag + "Wm", w_dram, 8, 512)
        gb = load_gain(P, C, tag + "gbM", gains, grow)
        NW = alloc_norm_work(P, C, tag + "M")
        xts = [P.sb("%smx%d" % (tag, r), [128, D], F32) for r in range(2)]
        hTs = [P.sb("%smhT%d" % (tag, r), [128, D], BF16) for r in range(2)]
        kb16 = [P.sb("%skb16_%d" % (tag, r), [128, 256], BF16) for r in range(2)]
        P.op("gpsimd", lambda e: e.memset(Vm.ap(), 1.0), writes=[Vm])
        for mb in range(2):
            xt, hT = xts[mb], hTs[mb]
            P.dma("sync", xt.ap(), mem_in[mb * 128:(mb + 1) * 128, :], writes=[xt])
            rmsnorm_to_hT(P, C, NW, xt, gb, hT.ap(), hT, C.banks[0])
            bank = C.banks[1 + mb]
            for k in range(8):
                P.op("tensor", lambda e: e.matmul(bank.ap(), lhsT=hT[:, k * 128:(k + 1) * 128], rhs=Wm[:, k, :], start=(k == 0), stop=(k == 7)),
                     reads=[hT, Wt[k]], writes=[bank])
            P.op("scalar", lambda e: e.activation(out=kb16[mb].ap(), in_=bank[:, 0:256], func=AF.Copy), reads=[bank], writes=[kb16[mb]])
            P.op("vector", lambda e: e.tensor_copy(Vm[:, mb, :, 0:64], bank[:, 256:512].rearrange("p (h d) -> p h d", d=64)),
                 reads=[bank], writes=[Vm])
            tb = C.banks[3 + mb]
            tbf = tb.ap().bitcast(BF16)
            for c in range(2):
                P.op("tensor", lambda e: e.transpose(tbf[:, c * 128:(c + 1) * 128], kb16[mb][:, c * 128:(c + 1) * 128], C.ident.ap()),
                     reads=[kb16[mb], C.ident], writes=[tb])
            P.op("vector", lambda e: e.tensor_copy(kmT[:, :, mb * 128:(mb + 1) * 128], tbf[:, 0:256].rearrange("p (c t) -> p c t", t=128)),
                 reads=[tb], writes=[kmT])


def attn_finish(P, C, W, xsrc, xdst, i, nheads, Wout, Wot, nkc):
    r = i % 2
    attn = W.attn[r]
    rec = W.rec[r]
    xt = W.xts[r]
    P.dma("sync", xt.ap(), xsrc[i * 128:(i + 1) * 128, :], writes=[xt])
    h0 = 0
    for (bank, oap, nh) in W.osrc(r):
        ov = oap.rearrange("p (h d) -> p h d", d=65)
        P.op("vector", lambda e: e.reciprocal(out=rec[:, h0:h0 + nh], in_=ov[:, :, 64]), reads=[bank], writes=[rec])
        P.op("vector", lambda e: e.tensor_tensor(out=attn[:, h0 * 64:(h0 + nh) * 64].rearrange("p (h d) -> p h d", d=64), in0=ov[:, :, 0:64],
                                                 in1=rec[:, h0:h0 + nh].unsqueeze(2).broadcast_to([128, nh, 64]), op=ALU.mult),
             reads=[bank, rec], writes=[attn])
        h0 += nh
    tb = W.tbank
    tbf = tb.ap().bitcast(BF16)
    aT = W.attnT[r]
    for c in range(nkc):
        P.op("tensor", lambda e: e.transpose(tbf[:, c * 128:(c + 1) * 128], attn[:, c * 128:(c + 1) * 128], C.ident.ap()),
             reads=[attn, C.ident], writes=[tb])
    P.op("scalar", lambda e: e.activation(out=aT[:, 0:nkc * 128], in_=tbf[:, 0:nkc * 128], func=AF.Copy), reads=[tb], writes=[aT])
    xn = W.xn[r]
    for half in range(2):
        yb = W.ybanks[half]
        for c in range(nkc):
            P.op("tensor", lambda e: e.matmul(yb.ap(), lhsT=aT[:, c * 128:(c + 1) * 128], rhs=Wout[:, c, half * 512:(half + 1) * 512],
                                              start=(c == 0), stop=(c == nkc - 1)), reads=[aT, Wot[c]], writes=[yb])
        P.op("vector", lambda e: e.tensor_tensor(out=xn[:, half * 512:(half + 1) * 512], in0=yb.ap(), in1=xt[:, half * 512:(half + 1) * 512], op=ALU.add),
             reads=[yb, xt], writes=[xn])
    P.dma("sync", xdst[i * 128:(i + 1) * 128, :], xn.ap(), reads=[xn])


def mem_heads(P, C, W, qall, qch0, kmT, Vm, obank, sbank, r):
    PT = W.PTm[r]
    for half in range(2):
        sb_ = sbank[half]
        for hh in range(2):
            hm = half * 2 + hh
            for mb in range(2):
                j = hh * 2 + mb
                P.op("tensor", lambda e: e.matmul(sb_[:, j * 128:(j + 1) * 128], lhsT=kmT[:, hm // 2, mb * 128:(mb + 1) * 128],
                                                  rhs=qall[:, qch0 + hm, :], start=True, stop=True),
                     reads=[kmT, qall], writes=[sb_])
        P.op("scalar", lambda e: e.activation(out=PT[:, half * 512:(half + 1) * 512], in_=sb_.ap(), func=AF.Exp, scale=0.125),
             reads=[sb_], writes=[PT])
    for hm in range(4):
        for mb in range(2):
            j = hm * 2 + mb
            P.op("tensor", lambda e: e.matmul(obank[:, hm * 65:(hm + 1) * 65], lhsT=PT[:, j * 128:(j + 1) * 128], rhs=Vm[:, mb, hm, 0:65],
                                              start=(mb == 0), stop=(mb == 1)), reads=[PT, Vm], writes=[obank])


def alloc_finish_work(P, C, tag, obanks, tbank, ybanks):
    W = Ctx()
    W.attn = [P.sb("%sattn%d" % (tag, r), [128, D], BF16) for r in range(2)]
    W.attnT = [P.sb("%sattnT%d" % (tag, r), [128, D], BF16) for r in range(2)]
    W.rec = [P.sb("%srec%d" % (tag, r), [128, 16], F32) for r in range(2)]
    W.xts = [P.sb("%sfx%d" % (tag, r), [128, D], F32) for r in range(2)]
    W.xn = [P.sb("%sxn%d" % (tag, r), [128, D], F32) for r in range(2)]
    W.PTm = [P.sb("%sPTm%d" % (tag, r), [128, 1024], BF16) for r in range(2)]
    W.osrc = lambda r: [(bk, bk[:, 0:nh * 65], nh) for (bk, nh) in obanks]
    W.tbank = tbank
    W.ybanks = ybanks
    return W


def phase_B0(P, C, xsrc, xdst, fT, vS, wiAll, kmT, Vm, wout_dram, nblocks=NT):
    with P.scope():
        Wout, Wot = load_weight(P, "Wout0", wout_dram, 8, D)
        kT = P.sb("kT", [128, 2, S], BF16)
        kiT = P.sb("kiT", [128, S], BF16)
        Va = P.sb("Va", [128, NT, 4, VW], BF16)
        P.op("gpsimd", lambda e: e.memset(Va.ap(), 1.0), writes=[Va])
        for c in range(2):
            P.dma("sync", kT[:, c, :], fT[6 + c], writes=[kT])
        P.dma("sync", kiT.ap(), fT[12], writes=[kiT])
        vr = vS.rearrange("(i p) (g d) -> p i g d", p=128, d=64)
        for i0 in range(NT):
            P.dma("sync", Va[:, i0, :, 0:64], vr[:, i0, :, :], writes=[Va])
        score = P.sb("score", [128, S], F32)
        junk = P.sb("junk", [128, S], BF16)
        Bs = [P.sb("Bm%d" % r, [128, S], BF16) for r in range(2)]
        Rs = [P.sb("R%d" % r, [128, 512], BF16) for r in range(4)]
        Wdgs = [P.sb("Wdg%d" % r, [128, 8, 128], BF16) for r in range(2)]
        PTs = [P.sb("PT%d" % r, [128, 512], BF16) for r in range(3)]
        qalls = [P.sb("qall%d" % r, [128, 24, 128], BF16) for r in range(2)]
        for r in range(2):
            P.op("gpsimd", lambda e: e.memset(qalls[r].ap(), 0.0), writes=[qalls[r]])
        sm = [[P.sb("bs%d_%d" % (r, j), [128, 1], F32) for j in range(6)] for r in range(2)]
        wtabs = [P.sb("wtab%d" % r, [128, N_BISECT + 2], F32) for r in range(2)]
        FW = alloc_finish_work(P, C, "b0", [(C.banks[3], 6), (C.banks[4], 6), (C.banks[5], 4)], C.banks[0], [C.banks[1], C.banks[0]])
        fT_r = fT.rearrange("c p t -> p c t")
        cnts = dict(lc=0, sc=0)

        def prep(b):
            lc = cnts["lc"]
            r = b % 2
            N = 128 * (b + 1)
            qall = qalls[r]
            B = Bs[r]
            q0 = b * 128
            for (slot0, nch, ch0) in ((0, 6, 0), (12, 4, 8), (20, 2, 13)):
                for base in (0, 64):
                    P.dma("sync", qall[base:base + 64, slot0 + base // 64:slot0 + 2 * nch:2, :], fT_r[base:base + 64, ch0:ch0 + nch, q0:q0 + 128],
                          writes=[qall])
            Wdg = Wdgs[r]
            for h in range(8):
                P.op("gpsimd", lambda e: e.tensor_scalar(out=Wdg[:, h, :], in0=C.ident.ap(), scalar1=wiAll[:, b, h:h + 1], scalar2=None, op0=ALU.mult),
                     reads=[C.ident, wiAll], writes=[Wdg])
            accb = C.banks[7]
            jobs = [(c0, h) for c0 in range(0, N, 512) for h in range(8)]
            prev = None
            for job in jobs + [None]:
                if job is not None:
                    c0, h = job
                    wc = min(512, N - c0)
                    bank = C.banks[2] if lc % 2 == 0 else C.banks[6]
                    R = Rs[lc % 4]
                    lc += 1
                    P.op("tensor", lambda e: e.matmul(bank[:, 0:wc], lhsT=qall[:, 12 + h, :], rhs=kiT[:, c0:c0 + wc],
                                                      start=True, stop=True), reads=[qall, kiT], writes=[bank])
                    if h % 4 == 3:
                        P.op("vector", lambda e: e.tensor_scalar(out=R[:, 0:wc], in0=bank[:, 0:wc], scalar1=0.0, scalar2=None, op0=ALU.max),
                             reads=[bank], writes=[R])
                    else:
                        P.op("scalar", lambda e: e.activation(out=R[:, 0:wc], in_=bank[:, 0:wc], func=AF.Relu), reads=[bank], writes=[R])
                if prev is not None:
                    (c0_, h_), R_ = prev
                    wc_ = min(512, N - c0_)
                    P.op("tensor", lambda e: e.matmul(accb[:, 0:wc_], lhsT=Wdg[:, h_, :], rhs=R_[:, 0:wc_], start=(h_ == 0), stop=(h_ == 7)),
                         reads=[Wdg, R_], writes=[accb])
                    if h_ == 7:
                        P.op("vector", lambda e: e.tensor_copy(score[:, c0_:c0_ + wc_], accb[:, 0:wc_]), reads=[accb], writes=[score])
                prev = (job, R) if job is not None else None
            mn, mx, mid, cnt, aa, thr = sm[r]
            if b >= 2:
                wtab = wtabs[r]
                P.op("vector", lambda e: e.tensor_reduce(out=mn.ap(), in_=score[:, 0:N - 128], axis=AX.X, op=ALU.min), reads=[score], writes=[mn])
            P.op("gpsimd", lambda e: e.affine_select(out=score[:, q0:q0 + 128], in_=score[:, q0:q0 + 128], pattern=[[-1, 128]], compare_op=ALU.is_ge,
                                                    fill=NEG, base=0, channel_multiplier=1), reads=[score], writes=[score])
            if b >= 2:
                P.op("vector", lambda e: e.tensor_reduce(out=mx.ap(), in_=score[:, 0:N], axis=AX.X, op=ALU.max), reads=[score], writes=[mx])
                P.op("vector", lambda e: e.tensor_tensor(out=aa.ap(), in0=mx.ap(), in1=mn.ap(), op=ALU.subtract), reads=[mx, mn], writes=[aa])
                P.op("vector", lambda e: e.tensor_scalar(out=wtab.ap(), in0=C.pow2.ap(), scalar1=aa.ap(), scalar2=0.5, op0=ALU.mult, op1=ALU.mult),
                     reads=[C.pow2, aa], writes=[wtab])
                P.op("vector", lambda e: e.tensor_tensor(out=mid.ap(), in0=mn.ap(), in1=wtab[:, 1:2], op=ALU.add), reads=[mn, wtab], writes=[mid])
                for k in range(N_BISECT):
                    P.op("vector", lambda e: e.tensor_scalar(out=junk[:, 0:N], in0=score[:, 0:N], scalar1=mid.ap(), scalar2=None, op0=ALU.is_ge, op1=ALU.add,
                                                             accum_out=cnt.ap()), reads=[score, mid], writes=[junk, cnt])
                    P.op("vector", lambda e: e.tensor_scalar(out=aa.ap(), in0=cnt.ap(), scalar1=255.5, scalar2=-0.5, op0=ALU.is_ge, op1=ALU.add),
                         reads=[cnt], writes=[aa])
                    P.op("vector", lambda e: e.scalar_tensor_tensor(out=mid.ap(), in0=aa.ap(), scalar=wtab[:, k + 1:k + 2], in1=mid.ap(), op0=ALU.mult, op1=ALU.add),
                         reads=[aa, wtab, mid], writes=[mid])
                P.op("vector", lambda e: e.tensor_tensor(out=thr.ap(), in0=mid.ap(), in1=wtab[:, N_BISECT + 1:N_BISECT + 2], op=ALU.subtract),
                     reads=[mid, wtab], writes=[thr])
                thr_t = thr
            else:
                thr_t = C.negthr
            P.op("vector", lambda e: e.tensor_scalar(out=B[:, 0:N], in0=score[:, 0:N], scalar1=thr_t.ap(), scalar2=MASKV, op0=ALU.is_lt, op1=ALU.mult),
                 reads=[score, thr_t], writes=[B])
            if C.dbg is not None and b == C.dbg_block:
                P.dma("sync", C.dbg["score"], score.ap(), reads=[score])
                P.dma("sync", C.dbg["thr"], thr_t.ap(), reads=[thr_t])
                P.dma("sync", C.dbg["Bm"], B.ap(), reads=[B])
            cnts["lc"] = lc

        def attend(b):
            sc = cnts["sc"]
            r = b % 2
            qall = qalls[r]
            B = Bs[r]
            jobs = []
            for h in range(12):
                pos = QPERM.index(h)
                g = h // 3
                obank = C.banks[3 + h // 6]
                ocol = (h % 6) * 65
                for kb0 in range(0, b + 1, 4):
                    kb1 = min(b + 1, kb0 + 4)
                    jobs.append((pos, g, obank, ocol, kb0, kb1))
            prev = None
            for job in jobs + [None]:
                if job is not None:
                    pos, g, obank, ocol, kb0, kb1 = job
                    sbank = C.banks[sc % 2]
                    PT = PTs[sc % 3]
                    sc += 1
                    for kb in range(kb0, kb1):
                        j = kb - kb0
                        P.op("tensor", lambda e: e.matmul(sbank[:, j * 128:(j + 1) * 128], lhsT=kT[:, g // 2, kb * 128:(kb + 1) * 128],
                                                          rhs=qall[:, pos, :], start=True, stop=False), reads=[kT, qall], writes=[sbank])
                        P.op("tensor", lambda e: e.matmul(sbank[:, j * 128:(j + 1) * 128], lhsT=B[:, kb * 128:(kb + 1) * 128], rhs=C.ident.ap(),
                                                          start=False, stop=True), reads=[B, C.ident], writes=[sbank])
                    nw = (kb1 - kb0) * 128
                    P.op("scalar", lambda e: e.activation(out=PT[:, 0:nw], in_=sbank[:, 0:nw], func=AF.Exp, scale=0.125), reads=[sbank], writes=[PT])
                if prev is not None:
                    (pos_, g_, obank_, ocol_, kb0_, kb1_), PT_ = prev
                    for kb in range(kb0_, kb1_):
                        j = kb - kb0_
                        P.op("tensor", lambda e: e.matmul(obank_[:, ocol_:ocol_ + 65], lhsT=PT_[:, j * 128:(j + 1) * 128], rhs=Va[:, kb, g_, 0:65],
                                                          start=(kb == 0), stop=(kb == b)), reads=[PT_, Va], writes=[obank_])
                prev = (job, PT) if job is not None else None
            cnts["sc"] = sc
            mem_heads(P, C, FW, qall, 20, kmT, Vm, C.banks[5], [C.banks[0], C.banks[1]], r)
            attn_finish(P, C, FW, xsrc, xdst, b, 16, Wout, Wot, 8)

        prep(0)
        for b in range(nblocks):
            if b + 1 < nblocks:
                prep(b + 1)
            attend(b)


def phase_F(P, C, tag, xnorm, xbase, xdst, wgu_dram, wd_dram, gains, grow, f0, f1, final_row=None, ntiles=NT):
    nf = f1 - f0
    with P.scope():
        Wgu = P.sb(tag + "Wgu", [128, 8, 2 * nf * 128], BF16)
        Wgt = [P.tok("%sWgt%d" % (tag, k)) for k in range(8)]
        for k in range(8):
            for half in range(2):
                c0 = half * DFF + f0 * 128
                P.dma("gpsimd", Wgu[:, k, half * nf * 128:(half + 1) * nf * 128], wgu_dram[k * 128:(k + 1) * 128, c0:c0 + nf * 128], writes=[Wgt[k]])
        Wd = P.sb(tag + "Wd", [128, nf, D], BF16)
        Wdt = [P.tok("%sWdt%d" % (tag, k)) for k in range(nf)]
        for k in range(nf):
            P.dma("gpsimd", Wd[:, k, :], wd_dram[(f0 + k) * 128:(f0 + k + 1) * 128, :], writes=[Wdt[k]])
        gb = load_gain(P, C, tag + "gbF", gains, grow)
        gfin = load_gain(P, C, tag + "gfin", gains, final_row) if final_row is not None else None
        NW = alloc_norm_work(P, C, tag + "F")
        xts = [P.sb("%sFx%d" % (tag, r), [128, D], F32) for r in range(2)]
        hT2 = [P.sb("%sFhT%d" % (tag, r), [128, 8, 256], BF16) for r in range(2)]
        hTt = [[P.tok("%sFhTt%d_%d" % (tag, r, t)) for t in range(2)] for r in range(2)]
        actT = [P.sb("%sactT%d" % (tag, r), [128, nf, 256], BF16) for r in range(2)]
        sg = [P.sb("%ssg%d" % (tag, r), [128, 256], F32) for r in range(2)]
        xn = [P.sb("%sFxn%d" % (tag, r), [128, D], F32) for r in range(4)]
        fsm = [[P.sb("%sfs%d_%d" % (tag, r, j), [128, 1], F32) for j in range(4)] for r in range(2)]
        fj = P.sb(tag + "fj", [128, D], BF16)
        gc = 0
        hbs = {}

        def norm1(G):
            for t in range(2):
                i = 2 * G + t
                xt = xts[i % 2]
                P.dma("sync", xt.ap(), xnorm[i * 128:(i + 1) * 128, :], writes=[xt])
                hbs[(G, t)] = rmsnorm_to_hT(P, C, NW, xt, gb, None, None, None)
                xo = xn[i % 4]
                P.dma("sync", xo.ap(), xbase[i * 128:(i + 1) * 128, :], writes=[xo])

        def norm2(G):
            for t in range(2):
                norm_p2(P, C, hbs.pop((G, t)), hT2[G % 2][:, :, t * 128:(t + 1) * 128], hTt[G % 2][t], C.banks[0],
                        evac_eng="scalar" if t == 0 else "vector")

        NG = ntiles // 2
        norm1(0)
        norm2(0)
        for G in range(NG):
            hT = hT2[G % 2]
            aT = actT[G % 2]
            for fc in range(nf):
                if fc == nf // 2 and G + 1 < NG:
                    norm1(G + 1)
                bank = C.banks[1 + (gc % 3)]
                s_ = sg[gc % 2]
                gc += 1
                for half in range(2):
                    coff = half * nf * 128 + fc * 128
                    for k in range(8):
                        P.op("tensor", lambda e: e.matmul(bank[:, half * 256:(half + 1) * 256], lhsT=Wgu[:, k, coff:coff + 128], rhs=hT[:, k, :],
                                                          start=(k == 0), stop=(k == 7)), reads=[Wgt[k]] + hTt[G % 2], writes=[bank])
                P.op("scalar", lambda e: e.activation(out=s_.ap(), in_=bank[:, 0:256], func=AF.Silu), reads=[bank], writes=[s_])
                P.op("vector", lambda e: e.tensor_tensor(out=aT[:, fc, :], in0=bank[:, 256:512], in1=s_.ap(), op=ALU.mult), reads=[bank, s_], writes=[aT])
            if G + 1 < NG:
                norm2(G + 1)
            for t in range(2):
                i = 2 * G + t
                xo = xn[i % 4]
                for half in range(2):
                    yb = C.banks[4 + (2 * t + half) % 4]
                    for fc in range(nf):
                        P.op("tensor", lambda e: e.matmul(yb.ap(), lhsT=aT[:, fc, t * 128:(t + 1) * 128], rhs=Wd[:, fc, half * 512:(half + 1) * 512],
                                                          start=(fc == 0), stop=(fc == nf - 1)), reads=[aT, Wdt[fc]], writes=[yb])
                    P.op("vector", lambda e: e.tensor_tensor(out=xo[:, half * 512:(half + 1) * 512], in0=yb.ap(), in1=xo[:, half * 512:(half + 1) * 512], op=ALU.add),
                         reads=[yb, xo], writes=[xo])
                if gfin is not None:
                    ssq, t1, t2, rstd = fsm[i % 2]
                    P.op("scalar", lambda e: e.activation(out=fj.ap(), in_=xo.ap(), func=AF.Square, accum_out=ssq.ap()), reads=[xo], writes=[fj, ssq])
                    P.op("vector", lambda e: e.tensor_scalar(out=t1.ap(), in0=ssq.ap(), scalar1=1.0 / D, scalar2=EPS, op0=ALU.mult, op1=ALU.add), reads=[ssq], writes=[t1])
                    P.op("scalar", lambda e: e.activation(out=t2.ap(), in_=t1.ap(), func=AF.Sqrt), reads=[t1], writes=[t2])
                    P.op("vector", lambda e: e.reciprocal(out=rstd.ap(), in_=t2.ap()), reads=[t2], writes=[rstd])
                    P.op("vector", lambda e: e.scalar_tensor_tensor(out=xo.ap(), in0=xo.ap(), scalar=rstd.ap(), in1=gfin.ap(), op0=ALU.mult, op1=ALU.mult),
                         reads=[xo, rstd, gfin], writes=[xo])
                P.dma("sync", xdst[i * 128:(i + 1) * 128, :], xo.ap(), reads=[xo], out=(gfin is not None))


def phase_B1(P, C, fT, vS, Og):
    for g, dil in enumerate((1, 4, 16)):
        nb = NT // dil
        with P.scope():
            qz = P.sb("qz1", [128, 4, S], BF16)
            kTg = P.sb("kTg", [128, 2, S], BF16)
            Vg = P.sb("Vg", [128, NT, 4, VW], BF16)
            P.op("gpsimd", lambda e: e.memset(qz.ap(), 0.0), writes=[qz])
            P.op("vector", lambda e: e.memset(Vg.ap(), 1.0), writes=[Vg])
            for c in range(2):
                for hh in range(2):
                    base = hh * 64
                    P.dma("sync", qz[base:base + 64, 2 * c + hh, :], fT[4 * g + c][base:base + 64, :], writes=[qz])
                P.dma("sync", kTg[:, c, :], fT[4 * g + 2 + c], writes=[kTg])
            vg = vS[:, g * 256:(g + 1) * 256].rearrange("(mb p dd) (h e) -> dd p mb h e", p=128, dd=dil, e=64)
            for r in range(dil):
                for m0 in range(nb):
                    P.dma("sync", Vg[:, r * nb + m0, :, 0:64], vg[r][:, m0, :, :], writes=[Vg])
            PTs = [P.sb("PTg%d" % k, [128, 512], BF16) for k in range(3)]
            Os = [P.sb("Os%d" % k, [128, 260], F32) for k in range(2)]
            Ogr = Og[g].rearrange("(m dd) c -> dd m c", dd=dil)
            sc = 0
            jobs = []
            qb = 0
            for r in range(dil):
                for mb in range(nb):
                    for hp in range(2):
                        jobs.append((r, mb, hp, qb))
                    qb += 1
            prev = None
            for job in jobs + [None]:
                if job is not None:
                    r, mb, hp, qb = job
                    qsl = slice(mb * 128 * dil + r, (mb * 128 + 127) * dil + r + 1, dil)
                    kbs = ([mb - 1] if mb > 0 else []) + [mb]
                    sbank = C.banks[sc % 3]
                    PT = PTs[sc % 3]
                    sc += 1
                    tiles = []
                    for hh in range(2):
                        j = hp * 2 + hh
                        for kb in kbs:
                            t = len(tiles)
                            tiles.append((j, kb))
                            ksl = slice(kb * 128 * dil + r, (kb * 128 + 127) * dil + r + 1, dil)
                            P.op("tensor", lambda e: e.matmul(sbank[:, t * 128:(t + 1) * 128], lhsT=kTg[:, j // 2, ksl], rhs=qz[:, j, qsl],
                                                              start=True, stop=False), reads=[kTg, qz], writes=[sbank])
                            M = C.MdT if kb == mb else C.MpT
                            P.op("tensor", lambda e: e.matmul(sbank[:, t * 128:(t + 1) * 128], lhsT=C.ident.ap(), rhs=M.ap(), start=False, stop=True),
                                 reads=[C.ident, M], writes=[sbank])
                    nw = len(tiles) * 128
                    P.op("scalar", lambda e: e.activation(out=PT[:, 0:nw], in_=sbank[:, 0:nw], func=AF.Exp, scale=0.125), reads=[sbank], writes=[PT])
                if prev is not None:
                    (r_, mb_, hp_, qb_), PT_, tiles_ = prev
                    obank = C.banks[6 + qb_ % 2]
                    kfirst = mb_ - 1 if mb_ > 0 else mb_
                    for t, (j, kb) in enumerate(tiles_):
                        P.op("tensor", lambda e: e.matmul(obank[:, j * 65:(j + 1) * 65], lhsT=PT_[:, t * 128:(t + 1) * 128], rhs=Vg[:, r_ * nb + kb, j, 0:65],
                                                          start=(kb == kfirst), stop=(kb == mb_)), reads=[PT_, Vg], writes=[obank])
                    if hp_ == 1:
                        O = Os[qb_ % 2]
                        P.op("vector", lambda e: e.tensor_copy(O.ap(), obank[:, 0:260]), reads=[obank], writes=[O])
                        P.dma("sync", Ogr[r_][mb_ * 128:(mb_ + 1) * 128, :], O.ap(), reads=[O])
                prev = (job, PT, tiles) if job is not None else None


def phase_B2(P, C, xsrc, xdst, fT, Og, kmT, Vm, wout_dram):
    with P.scope():
        Wout, Wot = load_weight(P, "Wout1", wout_dram, 4, D)
        qzs = [P.sb("qzm%d" % r, [128, 4, 128], BF16) for r in range(2)]
        for r in range(2):
            P.op("gpsimd", lambda e: e.memset(qzs[r].ap(), 0.0), writes=[qzs[r]])
        Ot = [[P.sb("Ot%d_%d" % (r, g), [128, 260], F32) for g in range(3)] for r in range(2)]
        FW = alloc_finish_work(P, C, "b2", [], C.banks[0], [C.banks[1], C.banks[2]])
        FW.osrc = lambda r: [(Ot[r][0], Ot[r][0].ap(), 4), (C.banks[5], C.banks[5][:, 0:260], 4)]
        fT_r = fT.rearrange("c p t -> p c t")
        for i in range(NT):
            r = i % 2
            qz = qzs[r]
            q0 = i * 128
            for base in (0, 64):
                P.dma("sync", qz[base:base + 64, base // 64:4:2, :], fT_r[base:base + 64, 12:14, q0:q0 + 128], writes=[qz])
            for g in range(3):
                P.dma("sync", Ot[r][g].ap(), Og[g][q0:q0 + 128, :], writes=[Ot[r][g]])
            mem_heads(P, C, FW, qz, 0, kmT, Vm, C.banks[5], [C.banks[6], C.banks[7]], r)
            P.op("gpsimd", lambda e: e.tensor_tensor(out=Ot[r][0].ap(), in0=Ot[r][0].ap(), in1=Ot[r][1].ap(), op=ALU.add),
                 reads=[Ot[r][0], Ot[r][1]], writes=[Ot[r][0]])
            P.op("gpsimd", lambda e: e.tensor_tensor(out=Ot[r][0].ap(), in0=Ot[r][0].ap(), in1=Ot[r][2].ap(), op=ALU.add),
                 reads=[Ot[r][0], Ot[r][2]], writes=[Ot[r][0]])
            attn_finish(P, C, FW, xsrc, xdst, i, 8, Wout, Wot, 4)


def build_program(stop_after=None, dbg_block=None, nblocks0=NT, skip_l0=False):
    nc = bass.Bass("TRN2", target_bir_lowering=False)
    P = Prog(nc)
    C = Ctx()
    I = {}

    def inp(name, shape, dt=F32):
        I[name] = nc.dram_tensor(name, list(shape), dt, kind="ExternalInput").ap()
        return I[name]

    x_in = inp("x", [S, D])
    mem_in = inp("mem", [256, D])
    pos_in = inp("pos", [128, NT], I32)
    gains = inp("gains", [7, D])
    C.freqs_in = inp("freqs", [128, 48])
    w_in0 = inp("w_in0", [D, SPEC0["ncols"]])
    w_in1 = inp("w_in1", [D, SPEC1["ncols"]])
    w_mkv = [inp("w_mkv%d" % l, [D, 512]) for l in range(2)]
    w_out0 = inp("w_out0", [1024, D])
    w_out1 = inp("w_out1", [512, D])
    w_gu = [inp("w_gu%d" % l, [D, 2 * DFF]) for l in range(2)]
    w_dn = [inp("w_dn%d" % l, [DFF, D]) for l in range(2)]
    out = nc.dram_tensor("out", [S, D], F32, kind="ExternalOutput").ap()
    C.dbg = None
    C.dbg_block = dbg_block
    if dbg_block is not None:
        C.dbg = dict(score=nc.dram_tensor("dbg_score", [128, S], F32, kind="ExternalOutput").ap(),
                     thr=nc.dram_tensor("dbg_thr", [128, 1], F32, kind="ExternalOutput").ap(),
                     Bm=nc.dram_tensor("dbg_Bm", [128, S], BF16, kind="ExternalOutput").ap())
    fT0 = P.dram("fT0", [15, 128, S], BF16)
    vS0 = P.dram("vS0", [S, 256], BF16)
    fT1 = P.dram("fT1", [14, 128, S], BF16)
    vS1 = P.dram("vS1", [S, 768], BF16)
    Og = [P.dram("Og%d" % g, [S, 260], F32) for g in range(3)]
    xa = P.dram("xa", [S, D], F32)
    xb = P.dram("xb", [S, D], F32)
    xc = P.dram("xc", [S, D], F32)

    C.banks = [P.ps("bank%d" % k, [128, 512], F32) for k in range(8)]
    setup_consts(P, C, pos_in)
    HF = NFC // 2

    def final(src):
        print("n_inst before final", P.n_inst)
        P.max_ops = None
        with P.scope():
            t = [P.sb("fin%d" % r, [128, D], F32) for r in range(2)]
            for i in range(NT):
                P.dma("sync", t[i % 2].ap(), src[i * 128:(i + 1) * 128, :], writes=[t[i % 2]])
                P.dma("sync", out[i * 128:(i + 1) * 128, :], t[i % 2].ap(), reads=[t[i % 2]], out=True)
        P.finish()
        return nc

    with (P.scope() if not skip_l0 else contextlib.nullcontext()):
      if not skip_l0:
        wiAll = P.sb("wiAll", [128, NT, 8], F32)
        kmT = P.sb("kmT0", [128, 2, 256], BF16)
        Vm = P.sb("Vm0", [128, 2, 4, VW], BF16)
        phase_M(P, C, "m0", mem_in, w_mkv[0], gains, 1, kmT, Vm)
        if stop_after == "M":
            d1 = nc.dram_tensor("dbg_kmT", [128, 2, 256], BF16, kind="ExternalOutput").ap()
            d2 = nc.dram_tensor("dbg_Vm", [128, 2, 4, VW], BF16, kind="ExternalOutput").ap()
            P.dma("sync", d1, kmT.ap(), reads=[kmT])
            P.dma("sync", d2, Vm.ap(), reads=[Vm])
            return final(x_in)
        phase_A(P, C, "a0", x_in, w_in0, gains, 0, SPEC0, fT0, vS0, wiAll, pos_in)
        if stop_after == "A":
            d1 = nc.dram_tensor("dbg_fT0", [15, 128, S], BF16, kind="ExternalOutput").ap()
            d2 = nc.dram_tensor("dbg_vS0", [S, 256], BF16, kind="ExternalOutput").ap()
            d3 = nc.dram_tensor("dbg_wi", [128, NT, 8], F32, kind="ExternalOutput").ap()
            with P.scope():
                tb = P.sb("dbgt", [128, S], BF16)
                for c in range(15):
                    P.dma("sync", tb.ap(), fT0[c], writes=[tb])
                    P.dma("sync", d1[c], tb.ap(), reads=[tb])
                for c in range(2):
                    P.dma("sync", tb[:, 0:2048].rearrange("p (a b) -> p a b", b=256), vS0[c * 2048:(c + 1) * 2048, :].rearrange("(a p) b -> p a b", p=128), writes=[tb])
                    P.dma("sync", d2[c * 2048:(c + 1) * 2048, :].rearrange("(a p) b -> p a b", p=128), tb[:, 0:2048].rearrange("p (a b) -> p a b", b=256), reads=[tb])
                P.dma("sync", d3, wiAll.ap(), reads=[wiAll])
            return final(x_in)
        phase_B0(P, C, x_in, xa, fT0, vS0, wiAll, kmT, Vm, w_out0, nblocks=nblocks0)
    if stop_after == "B0":
        return final(xa)
    if not skip_l0:
        phase_F(P, C, "f0a", xa, xa, xb, w_gu[0], w_dn[0], gains, 2, 0, HF)
        phase_F(P, C, "f0b", xa, xb, xc, w_gu[0], w_dn[0], gains, 2, HF, NFC)
    else:
        xc = x_in
    if stop_after == "F0":
        return final(xc)
    with P.scope():
        kmT = P.sb("kmT1", [128, 2, 256], BF16)
        Vm = P.sb("Vm1", [128, 2, 4, VW], BF16)
        phase_M(P, C, "m1", mem_in, w_mkv[1], gains, 4, kmT, Vm)
        phase_A(P, C, "a1", xc, w_in1, gains, 3, SPEC1, fT1, vS1, None, pos_in)
        phase_B1(P, C, fT1, vS1, Og)
        phase_B2(P, C, xc, xa, fT1, Og, kmT, Vm, w_out1)
    if stop_after == "B2":
        return final(xa)
    phase_F(P, C, "f1a", xa, xa, xb, w_gu[1], w_dn[1], gains, 5, 0, HF)
    phase_F(P, C, "f1b", xa, xb, out, w_gu[1], w_dn[1], gains, 5, HF, NFC, final_row=6)
    P.finish()
    return nc


def prep_inputs(inputs):
    f = lambda a: np.ascontiguousarray(np.asarray(a, dtype=np.float32))
    w0 = f(inputs["l0_w_in"])
    q, k, v, qi, ki, wi, qm = np.split(w0, np.cumsum([768, 256, 256, 512, 64, 8])[:], axis=1)
    qp = np.concatenate([q[:, h * 64:(h + 1) * 64] for h in QPERM], axis=1)
    w_in0 = np.ascontiguousarray(np.concatenate([qp, k, qi, ki, ki, wi, v, qm], axis=1))
    w1 = f(inputs["l1_w_in"])
    parts = [w1[:, j * 256:(j + 1) * 256] for j in range(10)]
    w_in1 = np.ascontiguousarray(np.concatenate([parts[0], parts[1], parts[3], parts[4], parts[6], parts[7], parts[2], parts[5], parts[8], parts[9]], axis=1))
    gains = np.ascontiguousarray(np.stack([f(inputs[n]) for n in ("l0_norm_mix", "l0_norm_mem", "l0_norm_ffn", "l1_norm_mix", "l1_norm_mem",
                                                                 "l1_norm_ffn", "final_norm")], axis=0))
    fr64 = (np.float32(10000.0) ** (-np.arange(32, dtype=np.float32) / np.float32(32))).astype(np.float32)
    fr16 = (np.float32(10000.0) ** (-np.arange(16, dtype=np.float32) / np.float32(16))).astype(np.float32)
    freqs = np.ascontiguousarray(np.broadcast_to(np.concatenate([fr64, fr16])[None, :], (128, 48)).astype(np.float32))
    shared = dict(freqs=freqs, gains=gains, w_in0=w_in0, w_in1=w_in1, w_mkv0=f(inputs["l0_w_mem_kv"]), w_mkv1=f(inputs["l1_w_mem_kv"]),
                  w_out0=f(inputs["l0_w_out"]), w_out1=f(inputs["l1_w_out"]), w_gu0=f(inputs["l0_w_gate_up"]), w_gu1=f(inputs["l1_w_gate_up"]),
                  w_dn0=f(inputs["l0_w_down"]), w_dn1=f(inputs["l1_w_down"]))
    x = f(inputs["x"])
    mem = f(inputs["mem"])
    pos = np.ascontiguousarray(np.asarray(inputs["positions"], dtype=np.int32))
    maps = []
    for c in range(x.shape[0]):
        m = dict(shared)
        m["x"] = x[c]
        m["mem"] = mem[c]
        m["pos"] = np.ascontiguousarray(pos[c].reshape(NT, 128).T)
        maps.append(m)
    return maps


_NC_CACHE = {}


def kernel(**inputs):
    maps = prep_inputs(inputs)
    if "nc" not in _NC_CACHE:
        _NC_CACHE["nc"] = build_program()
    nc = _NC_CACHE["nc"]
    res = run_bass_kernel_spmd(nc, maps, core_ids=list(range(len(maps))))
    return np.stack([np.asarray(r["out"], dtype=np.float32) for r in res.results], axis=0)
```

```python
import contextlib
import numpy as np
import ml_dtypes
import concourse.bass as bass
import concourse.mybir as mybir
from concourse.bass_utils import run_bass_kernel_spmd

F32 = mybir.dt.float32
BF16 = mybir.dt.bfloat16
I32 = mybir.dt.int32
AF = mybir.ActivationFunctionType
ALU = mybir.AluOpType
AX = mybir.AxisListType

SAME_ENGINE_SYNC = True


class Tok:
    def __init__(self, name):
        self.name = name
        self.w = None
        self.r = {}


class Buf(Tok):
    def __init__(self, name, handle):
        super().__init__(name)
        self.h = handle

    def ap(self):
        return self.h[:]

    def __getitem__(self, idx):
        return self.h[idx]


class _Eng:
    def __init__(self, name, handle, sem):
        self.name = name
        self.h = handle
        self.sem = sem
        self.count = 0
        self.seen = {}


class Prog:
    def __init__(self, nc, n_dma_sems=8):
        self.nc = nc
        self.stack = contextlib.ExitStack()
        self.engs = {}
        for name in ("tensor", "vector", "scalar", "gpsimd", "sync"):
            sem = self.stack.enter_context(nc.semaphore("s_" + name))
            self.engs[name] = _Eng(name, getattr(nc, name), sem)
        self.dma_sems = {}
        for q in ("sync", "gpsimd", "scalar"):
            self.dma_sems[q] = [[self.stack.enter_context(nc.semaphore("d_%s%d" % (q, i))), 0] for i in range(n_dma_sems)]
        self.dma_rr = {"sync": 0, "gpsimd": 0, "scalar": 0}
        self.out_events = []
        self.n_inst = 0
        self.scopes = [self.stack]
        import os
        self.max_ops = int(os.environ["KMAXOPS"]) if "KMAXOPS" in os.environ else None

    @contextlib.contextmanager
    def scope(self):
        st = contextlib.ExitStack()
        self.scopes.append(st)
        try:
            yield
        finally:
            self.barrier()
            self.scopes.pop()
            st.close()

    def barrier(self):
        if getattr(self, "finished", False):
            return
        for eng in self.engs.values():
            for other in self.engs.values():
                if other is not eng and other.count > 0:
                    self._wait(eng, (other.name, other.sem, other.count))
            for q, pool in self.dma_sems.items():
                for i, slot in enumerate(pool):
                    if slot[1] > 0:
                        self._wait(eng, ("d_%s%d" % (q, i), slot[0], 16 * slot[1]))

    def tok(self, name):
        return Tok(name)

    def sb(self, name, shape, dtype):
        self.uid = getattr(self, "uid", 0) + 1
        name = "%s_u%d" % (name, self.uid)
        return Buf(name, self.scopes[-1].enter_context(self.nc.sbuf_tensor(name, list(shape), dtype)))

    def ps(self, name, shape, dtype):
        b = Buf(name, self.scopes[-1].enter_context(self.nc.psum_tensor(name, list(shape), dtype)))
        b.excl = True
        return b

    def dram(self, name, shape, dtype):
        return self.nc.dram_tensor(name, list(shape), dtype, kind="Internal").ap()

    def _wait(self, eng, ev):
        key, sem, val = ev
        if eng.seen.get(key, 0) >= val:
            return
        if key == eng.name and not (SAME_ENGINE_SYNC and eng.name != "tensor" and eng.name != "sync"):
            return
        eng.h.wait_ge(sem, val)
        eng.seen[key] = val

    def _deps(self, eng, reads, writes):
        for t in reads:
            if t.w is not None:
                self._wait(eng, t.w)
        for t in writes:
            if t.w is not None:
                self._wait(eng, t.w)
            for ev in t.r.values():
                self._wait(eng, ev)

    def _record(self, ev, reads, writes):
        for t in writes:
            t.w = ev
            t.r = {}
        for t in reads:
            if t in writes:
                continue
            t.r[ev[0]] = ev

    def op(self, engname, fn, reads=(), writes=()):
        if self.max_ops is not None and self.n_inst >= self.max_ops:
            return None
        eng = self.engs[engname]
        ex = [t for t in reads if getattr(t, "excl", False) and t not in writes]
        if ex:
            reads = [t for t in reads if t not in ex]
            writes = list(writes) + ex
        self._deps(eng, reads, writes)
        inst = fn(eng.h)
        eng.count += 1
        inst.then_inc(eng.sem, 1)
        ev = (eng.name, eng.sem, eng.count)
        self._record(ev, reads, writes)
        self.n_inst += 1
        return ev

    def dma(self, q, out_ap, in_ap, reads=(), writes=(), out=False, **kw):
        if self.max_ops is not None and self.n_inst >= self.max_ops:
            return None
        eng = self.engs[q]
        self._deps(eng, reads, writes)
        pool = self.dma_sems[q]
        i = self.dma_rr[q]
        self.dma_rr[q] = (i + 1) % len(pool)
        slot = pool[i]
        key = "d_%s%d" % (q, i)
        if slot[1] > 0:
            self._wait(eng, (key, slot[0], 16 * slot[1]))
        eng.h.dma_start(out=out_ap, in_=in_ap, **kw).then_inc(slot[0], 16)
        slot[1] += 1
        ev = (key, slot[0], 16 * slot[1])
        self._record(ev, reads, writes)
        if out:
            self.out_events.append(ev)
        self.n_inst += 1
        return ev

    def finish(self):
        eng = self.engs["sync"]
        for q, pool in self.dma_sems.items():
            for i, slot in enumerate(pool):
                if slot[1] > 0:
                    self._wait(eng, ("d_%s%d" % (q, i), slot[0], 16 * slot[1]))
        for name, e in self.engs.items():
            if name != "sync" and e.count > 0:
                self._wait(eng, (name, e.sem, e.count))
        self.finished = True
        for st in reversed(self.scopes):
            st.close()

    def make_identity(self, idt):
        tmp = self.sb(idt.name + "_i", [128, 128], I32)
        self.op("gpsimd", lambda e: e.iota(tmp.ap(), pattern=[[1, 128]], base=0, channel_multiplier=-1), writes=[tmp])
        self.op("vector", lambda e: e.tensor_scalar(out=idt.ap(), in0=tmp.ap(), scalar1=0.0, scalar2=None, op0=ALU.is_equal),
                reads=[tmp], writes=[idt])


S = 4096
D = 1024
NT = 32
DFF = 2816
NFC = 22
EPS = 1e-6
NEG = -1.0e30
MASKV = -30000.0
VW = 66
N_BISECT = 16
IDX_SCALE = (8 ** -0.5) * (64 ** -0.5)
QPERM = [0, 3, 1, 4, 2, 5, 6, 9, 7, 10, 8, 11]

SPEC0 = dict(
    ncols=2184,
    chunks=[(0, 512, [("rope64", 0, 8)]), (512, 512, [("rope64", 0, 8)]), (1024, 512, [("rope32", 0, 8)]),
            (1536, 136, [("rope32", 0, 2), ("wi", 128, 8)]), (1672, 512, [("copy", 0, 512)])],
    tcols=[128 * k for k in range(13)] + [1928, 2056],
    vcols=(1672, 1928),
)
SPEC1 = dict(
    ncols=2560,
    chunks=[(0, 512, [("rope64", 0, 8)]), (512, 512, [("rope64", 0, 8)]), (1024, 512, [("rope64", 0, 8)]),
            (1536, 512, [("copy", 0, 512)]), (2048, 512, [("copy", 0, 512)])],
    tcols=[128 * k for k in range(12)] + [2304, 2432],
    vcols=(1536, 2304),
)


class Ctx:
    pass


def load_weight(P, name, dram, nk, ncols, q="gpsimd"):
    W = P.sb(name, [128, nk, ncols], BF16)
    toks = [P.tok("%s_%d" % (name, k)) for k in range(nk)]
    for k in range(nk):
        c0 = 0
        while c0 < ncols:
            c1 = min(ncols, c0 + 2048)
            P.dma(q, W[:, k, c0:c1], dram[k * 128:(k + 1) * 128, c0:c1], writes=[toks[k]])
            c0 = c1
    return W, toks


def setup_consts(P, C, pos_in):
    C.ident = P.sb("ident", [128, 128], BF16)
    P.make_identity(C.ident)
    C.negthr = P.sb("negthr", [128, 1], F32)
    P.op("vector", lambda e: e.memset(C.negthr.ap(), -1.0e29), writes=[C.negthr])
    C.pow2 = P.sb("pow2", [128, N_BISECT + 2], F32)
    for k in range(N_BISECT + 2):
        P.op("gpsimd", lambda e: e.memset(C.pow2[:, k:k + 1], 2.0 ** (1 - k)), writes=[C.pow2])
    C.MdT = P.sb("MdT", [128, 128], BF16)
    C.MpT = P.sb("MpT", [128, 128], BF16)
    zt = P.sb("zt", [128, 128], F32)
    zm = P.sb("zm", [128, 128], F32)
    P.op("vector", lambda e: e.memset(zt.ap(), 0.0), writes=[zt])
    P.op("gpsimd", lambda e: e.affine_select(out=zm.ap(), in_=zt.ap(), pattern=[[1, 128]], compare_op=ALU.is_ge, fill=MASKV, base=0, channel_multiplier=-1),
         reads=[zt], writes=[zm])
    P.op("vector", lambda e: e.tensor_copy(C.MdT.ap(), zm.ap()), reads=[zm], writes=[C.MdT])
    P.op("gpsimd", lambda e: e.affine_select(out=zm.ap(), in_=zt.ap(), pattern=[[-1, 128]], compare_op=ALU.is_ge, fill=MASKV, base=0, channel_multiplier=1),
         reads=[zt, C.MdT], writes=[zm])
    P.op("vector", lambda e: e.tensor_copy(C.MpT.ap(), zm.ap()), reads=[zm], writes=[C.MpT])


def build_rope_tables(P, C, pos_in):
    C.cos64 = P.sb("cos64", [128, NT, 32], F32)
    C.sin64 = P.sb("sin64", [128, NT, 32], F32)
    C.nsin64 = P.sb("nsin64", [128, NT, 32], F32)
    C.cos16 = P.sb("cos16", [128, NT, 16], F32)
    C.sin16 = P.sb("sin16", [128, NT, 16], F32)
    C.nsin16 = P.sb("nsin16", [128, NT, 16], F32)
    with P.scope():
        posi = P.sb("posi", [128, NT], I32)
        posf = P.sb("posf", [128, NT], F32)
        P.dma("sync", posi.ap(), pos_in, writes=[posi])
        P.op("vector", lambda e: e.tensor_copy(posf.ap(), posi.ap()), reads=[posi], writes=[posf])
        for half, cosT, sinT, nsinT in ((32, C.cos64, C.sin64, C.nsin64), (16, C.cos16, C.sin16, C.nsin16)):
            n = NT * half
            fr = P.sb("fr%d" % half, [128, half], F32)
            a = P.sb("a%d" % half, [128, NT, half], F32)
            ki = P.sb("ki%d" % half, [128, NT, half], I32)
            kf = P.sb("kf%d" % half, [128, NT, half], F32)
            fr1 = P.sb("fr1%d" % half, [128, NT, half], F32)
            m1 = P.sb("m1%d" % half, [128, NT, half], F32)
            f0 = 0 if half == 32 else 32
            P.dma("sync", fr.ap(), C.freqs_in[:, f0:f0 + half], writes=[fr])
            P.op("vector", lambda e: e.tensor_tensor(out=a.ap(), in0=posf.ap().unsqueeze(2).broadcast_to([128, NT, half]),
                                                     in1=fr.ap().unsqueeze(1).broadcast_to([128, NT, half]), op=ALU.mult),
                 reads=[posf, fr], writes=[a])
            P.op("vector", lambda e: e.tensor_scalar(out=a.ap(), in0=a.ap(), scalar1=float(1.0 / (2.0 * np.pi)), scalar2=None, op0=ALU.mult),
                 reads=[a], writes=[a])
            for shift, outT, neg in ((0.0, sinT, False), (0.25, cosT, False), (0.5, nsinT, False)):
                src = a
                if shift != 0.0:
                    P.op("vector", lambda e: e.tensor_scalar(out=fr1.ap(), in0=a.ap(), scalar1=shift, scalar2=None, op0=ALU.add),
                         reads=[a], writes=[fr1])
                    src = fr1
                P.op("vector", lambda e: e.tensor_copy(ki.ap(), src.ap()), reads=[src], writes=[ki])
                P.op("vector", lambda e: e.tensor_copy(kf.ap(), ki.ap()), reads=[ki], writes=[kf])
                P.op("vector", lambda e: e.tensor_tensor(out=kf.ap(), in0=src.ap(), in1=kf.ap(), op=ALU.subtract), reads=[src, kf], writes=[kf])
                P.op("vector", lambda e: e.tensor_scalar(out=m1.ap(), in0=kf.ap(), scalar1=0.5, scalar2=None, op0=ALU.is_gt), reads=[kf], writes=[m1])
                P.op("vector", lambda e: e.tensor_tensor(out=kf.ap(), in0=kf.ap(), in1=m1.ap(), op=ALU.subtract), reads=[kf, m1], writes=[kf])
                P.op("vector", lambda e: e.tensor_scalar(out=m1.ap(), in0=kf.ap(), scalar1=-0.5, scalar2=None, op0=ALU.is_lt), reads=[kf], writes=[m1])
                P.op("vector", lambda e: e.tensor_tensor(out=kf.ap(), in0=kf.ap(), in1=m1.ap(), op=ALU.add), reads=[kf, m1], writes=[kf])
                P.op("scalar", lambda e: e.activation(out=outT.ap(), in_=kf.ap(), func=AF.Sin, scale=float(2.0 * np.pi) * (1.0 - 1e-6)),
                     reads=[kf], writes=[outT])


def alloc_norm_work(P, C, tag):
    W = Ctx()
    W.sqj = P.sb(tag + "sqj", [128, D], BF16)
    W.small = [[P.sb("%ssm%d_%d" % (tag, r, j), [128, 1], F32) for j in range(4)] for r in range(2)]
    W.hb = [P.sb("%shb%d" % (tag, r), [128, D], BF16) for r in range(2)]
    W.k = 0
    return W


def rmsnorm_to_hT(P, C, W, xt, gb, hT_ap, hT_tok, bank, evac_eng="scalar"):
    r = W.k % 2
    W.k += 1
    ssq, t1, t2, rstd = W.small[r]
    hb = W.hb[r]
    P.op("scalar", lambda e: e.activation(out=W.sqj.ap(), in_=xt.ap(), func=AF.Square, accum_out=ssq.ap()), reads=[xt], writes=[W.sqj, ssq])
    P.op("vector", lambda e: e.tensor_scalar(out=t1.ap(), in0=ssq.ap(), scalar1=1.0 / D, scalar2=EPS, op0=ALU.mult, op1=ALU.add), reads=[ssq], writes=[t1])
    P.op("scalar", lambda e: e.activation(out=t2.ap(), in_=t1.ap(), func=AF.Sqrt), reads=[t1], writes=[t2])
    P.op("vector", lambda e: e.reciprocal(out=rstd.ap(), in_=t2.ap()), reads=[t2], writes=[rstd])
    P.op("vector", lambda e: e.scalar_tensor_tensor(out=hb.ap(), in0=xt.ap(), scalar=rstd.ap(), in1=gb.ap(), op0=ALU.mult, op1=ALU.mult),
         reads=[xt, rstd, gb], writes=[hb])
    if hT_ap is None:
        return hb
    return norm_p2(P, C, hb, hT_ap, hT_tok, bank, evac_eng)


def norm_p2(P, C, hb, hT_ap, hT_tok, bank, evac_eng="scalar"):
    bbf = bank.ap().bitcast(BF16)
    for c in range(8):
        P.op("tensor", lambda e: e.transpose(bbf[:, c * 128:(c + 1) * 128], hb[:, c * 128:(c + 1) * 128], C.ident.ap()),
             reads=[hb, C.ident], writes=[bank])
    srcv = bbf if len(hT_ap.shape) == 2 else bbf.rearrange("p (c t) -> p c t", t=128)
    if evac_eng == "scalar":
        P.op("scalar", lambda e: e.activation(out=hT_ap, in_=srcv, func=AF.Copy), reads=[bank], writes=[hT_tok])
    else:
        P.op("vector", lambda e: e.tensor_copy(hT_ap, srcv), reads=[bank], writes=[hT_tok])


def load_gain(P, C, name, gains, row):
    gb = P.sb(name, [128, D], F32)
    P.dma("sync", gb.ap(), gains[row].partition_broadcast(128), writes=[gb])
    return gb


def phase_A(P, C, tag, xsrc, w_dram, gains, grow, spec, fT, vS, wiAll, pos_in):
    ncols = spec["ncols"]
    with P.scope():
        build_rope_tables(P, C, pos_in)
        Win, Wt = load_weight(P, tag + "Win", w_dram, 8, ncols)
        gb = load_gain(P, C, tag + "gbA", gains, grow)
        NW = alloc_norm_work(P, C, tag + "A")
        xts = [P.sb("%sxt%d" % (tag, r), [128, D], F32) for r in range(2)]
        hTs = [P.sb("%shT%d" % (tag, r), [128, D], BF16) for r in range(2)]
        pts = [P.sb("%spt%d" % (tag, r), [128, ncols], BF16) for r in range(2)]
        t1s = [P.sb("%st1_%d" % (tag, r), [128, 512], F32) for r in range(2)]
        t2s = [P.sb("%st2_%d" % (tag, r), [128, 512], F32) for r in range(2)]
        ntc = len(spec["tcols"])
        fTs = [P.sb("%sfTs%d" % (tag, r), [128, ntc, 128], BF16) for r in range(2)]
        fT_r = fT.rearrange("c p t -> p c t")
        cc = 0
        hbs = {}

        def n1(i):
            xt = xts[i % 2]
            P.dma("sync", xt.ap(), xsrc[i * 128:(i + 1) * 128, :], writes=[xt])
            hbs[i] = rmsnorm_to_hT(P, C, NW, xt, gb, None, None, None)

        def n2(i):
            norm_p2(P, C, hbs.pop(i), hTs[i % 2].ap(), hTs[i % 2], C.banks[0], evac_eng="scalar")

        n1(0)
        n2(0)
        for i in range(NT):
            hT = hTs[i % 2]
            pt = pts[i % 2]
            if i + 1 < NT:
                n1(i + 1)
            for (col0, width, handlers) in spec["chunks"]:
                bank = C.banks[1 + (cc % 4)]
                t1 = t1s[cc % 2]
                t2 = t2s[cc % 2]
                cc += 1
                for k in range(8):
                    P.op("tensor", lambda e: e.matmul(bank[:, 0:width], lhsT=hT[:, k * 128:(k + 1) * 128], rhs=Win[:, k, col0:col0 + width],
                                                      start=(k == 0), stop=(k == 7)), reads=[hT, Wt[k]], writes=[bank])
                for (kind, l0, n) in handlers:
                    if kind == "rope64":
                        nh = n
                        w = nh * 64
                        xv2 = bank[:, l0:l0 + w].rearrange("p (h d) -> p h d", d=32)
                        xv = bank[:, l0:l0 + w].rearrange("p (h d) -> p h d", d=64)
                        t1v2 = t1[:, 0:w].rearrange("p (h d) -> p h d", d=32)
                        t2v = t2[:, 0:w].rearrange("p (h d) -> p h d", d=64)
                        cosb = C.cos64[:, i:i + 1, :].broadcast_to([128, 2 * nh, 32])
                        sinb = C.sin64[:, i:i + 1, :].broadcast_to([128, nh, 32])
                        nsinb = C.nsin64[:, i:i + 1, :].broadcast_to([128, nh, 32])
                        P.op("vector", lambda e: e.tensor_tensor(out=t1v2, in0=xv2, in1=cosb, op=ALU.mult), reads=[bank, C.cos64], writes=[t1])
                        P.op("vector", lambda e: e.tensor_tensor(out=t2v[:, :, 0:32], in0=xv[:, :, 32:64], in1=nsinb, op=ALU.mult),
                             reads=[bank, C.nsin64], writes=[t2])
                        P.op("vector", lambda e: e.tensor_tensor(out=t2v[:, :, 32:64], in0=xv[:, :, 0:32], in1=sinb, op=ALU.mult),
                             reads=[bank, C.sin64], writes=[t2])
                        P.op("gpsimd", lambda e: e.tensor_tensor(out=pt[:, col0 + l0:col0 + l0 + w], in0=t1[:, 0:w], in1=t2[:, 0:w], op=ALU.add),
                             reads=[t1, t2], writes=[pt])
                    elif kind == "rope32":
                        nh = n
                        w = nh * 64
                        xv = bank[:, l0:l0 + w].rearrange("p (h d) -> p h d", d=64)
                        xr4 = bank[:, l0:l0 + w].rearrange("p (h t d) -> p h t d", t=4, d=16)
                        t1r4 = t1[:, 0:w].rearrange("p (h t d) -> p h t d", t=4, d=16)
                        t2v = t2[:, 0:w].rearrange("p (h d) -> p h d", d=64)
                        t1v = t1[:, 0:w].rearrange("p (h d) -> p h d", d=64)
                        ptv = pt[:, col0 + l0:col0 + l0 + w].rearrange("p (h d) -> p h d", d=64)
                        cosb = C.cos16[:, i:i + 1, :].unsqueeze(1).broadcast_to([128, nh, 2, 16])
                        sinb = C.sin16[:, i:i + 1, :].broadcast_to([128, nh, 16])
                        nsinb = C.nsin16[:, i:i + 1, :].broadcast_to([128, nh, 16])
                        P.op("vector", lambda e: e.tensor_tensor(out=t1r4[:, :, 0:2, :], in0=xr4[:, :, 0:2, :], in1=cosb, op=ALU.mult),
                             reads=[bank, C.cos16], writes=[t1])
                        P.op("vector", lambda e: e.tensor_tensor(out=t2v[:, :, 0:16], in0=xv[:, :, 16:32], in1=nsinb, op=ALU.mult),
                             reads=[bank, C.nsin16], writes=[t2])
                        P.op("vector", lambda e: e.tensor_tensor(out=t2v[:, :, 16:32], in0=xv[:, :, 0:16], in1=sinb, op=ALU.mult),
                             reads=[bank, C.sin16], writes=[t2])
                        P.op("gpsimd", lambda e: e.tensor_tensor(out=ptv[:, :, 0:32], in0=t1v[:, :, 0:32], in1=t2v[:, :, 0:32], op=ALU.add),
                             reads=[t1, t2], writes=[pt])
                        P.op("scalar", lambda e: e.activation(out=ptv[:, :, 32:64], in_=xv[:, :, 32:64], func=AF.Copy), reads=[bank], writes=[pt])
                    elif kind == "copy":
                        P.op("scalar", lambda e: e.activation(out=pt[:, col0 + l0:col0 + l0 + n], in_=bank[:, l0:l0 + n], func=AF.Copy),
                             reads=[bank], writes=[pt])
                    elif kind == "wi":
                        P.op("scalar", lambda e: e.activation(out=wiAll[:, i, :], in_=bank[:, l0:l0 + n], func=AF.Copy, scale=float(IDX_SCALE)),
                             reads=[bank], writes=[wiAll])
            if i + 1 < NT:
                n2(i + 1)
            fts = fTs[i % 2]
            for g0 in range(0, ntc, 8):
                g1 = min(ntc, g0 + 8)
                bank = C.banks[5 + (g0 // 8)]
                bbf = bank.ap().bitcast(BF16)
                for k in range(g0, g1):
                    c0 = spec["tcols"][k]
                    P.op("tensor", lambda e: e.transpose(bbf[:, (k - g0) * 128:(k - g0 + 1) * 128], pt[:, c0:c0 + 128], C.ident.ap()),
                         reads=[pt, C.ident], writes=[bank])
                eng = "vector" if g0 == 0 else "scalar"
                if eng == "vector":
                    P.op("vector", lambda e: e.tensor_copy(fts[:, g0:g1, :], bbf[:, 0:(g1 - g0) * 128].rearrange("p (c t) -> p c t", t=128)),
                         reads=[bank], writes=[fts])
                else:
                    P.op("scalar", lambda e: e.activation(out=fts[:, g0:g1, :], in_=bbf[:, 0:(g1 - g0) * 128].rearrange("p (c t) -> p c t", t=128),
                                                          func=AF.Copy), reads=[bank], writes=[fts])
            P.dma("sync", fT_r[:, :, i * 128:(i + 1) * 128], fts.ap(), reads=[fts])
            v0, v1 = spec["vcols"]
            P.dma("sync", vS[i * 128:(i + 1) * 128, :], pt[:, v0:v1], reads=[pt])


def phase_M(P, C, tag, mem_in, w_dram, gains, grow, kmT, Vm):
    with P.scope():
        Wm, Wt = load_weight(P, tag + "Wm", w_dram, 8, 512)
        gb = load_gain(P, C, tag + "gbM", gains, grow)
        NW = alloc_norm_work(P, C, tag + "M")
        xts = [P.sb("%smx%d" % (tag, r), [128, D], F32) for r in range(2)]
        hTs = [P.sb("%smhT%d" % (tag, r), [128, D], BF16) for r in range(2)]
        kb16 = [P.sb("%skb16_%d" % (tag, r), [128, 256], BF16) for r in range(2)]
        P.op("gpsimd", lambda e: e.memset(Vm.ap(), 1.0), writes=[Vm])
        for mb in range(2):
            xt, hT = xts[mb], hTs[mb]
            P.dma("sync", xt.ap(), mem_in[mb * 128:(mb + 1) * 128, :], writes=[xt])
            rmsnorm_to_hT(P, C, NW, xt, gb, hT.ap(), hT, C.banks[0])
            bank = C.banks[1 + mb]
            for k in range(8):
                P.op("tensor", lambda e: e.matmul(bank.ap(), lhsT=hT[:, k * 128:(k + 1) * 128], rhs=Wm[:, k, :], start=(k == 0), stop=(k == 7)),
                     reads=[hT, Wt[k]], writes=[bank])
            P.op("scalar", lambda e: e.activation(out=kb16[mb].ap(), in_=bank[:, 0:256], func=AF.Copy), reads=[bank], writes=[kb16[mb]])
            P.op("vector", lambda e: e.tensor_copy(Vm[:, mb, :, 0:64], bank[:, 256:512].rearrange("p (h d) -> p h d", d=64)),
                 reads=[bank], writes=[Vm])
            tb = C.banks[3 + mb]
            tbf = tb.ap().bitcast(BF16)
            for c in range(2):
                P.op("tensor", lambda e: e.transpose(tbf[:, c * 128:(c + 1) * 128], kb16[mb][:, c * 128:(c + 1) * 128], C.ident.ap()),
                     reads=[kb16[mb], C.ident], writes=[tb])
            P.op("vector", lambda e: e.tensor_copy(kmT[:, :, mb * 128:(mb + 1) * 128], tbf[:, 0:256].rearrange("p (c t) -> p c t", t=128)),
                 reads=[tb], writes=[kmT])


def attn_finish(P, C, W, xsrc, xdst, i, nheads, Wout, Wot, nkc):
    r = i % 2
    attn = W.attn[r]
    rec = W.rec[r]
    xt = W.xts[r]
    P.dma("sync", xt.ap(), xsrc[i * 128:(i + 1) * 128, :], writes=[xt])
    h0 = 0
    for (bank, oap, nh) in W.osrc(r):
        ov = oap.rearrange("p (h d) -> p h d", d=65)
        P.op("vector", lambda e: e.reciprocal(out=rec[:, h0:h0 + nh], in_=ov[:, :, 64]), reads=[bank], writes=[rec])
        P.op("vector", lambda e: e.tensor_tensor(out=attn[:, h0 * 64:(h0 + nh) * 64].rearrange("p (h d) -> p h d", d=64), in0=ov[:, :, 0:64],
                                                 in1=rec[:, h0:h0 + nh].unsqueeze(2).broadcast_to([128, nh, 64]), op=ALU.mult),
             reads=[bank, rec], writes=[attn])
        h0 += nh
    tb = W.tbank
    tbf = tb.ap().bitcast(BF16)
    aT = W.attnT[r]
    for c in range(nkc):
        P.op("tensor", lambda e: e.transpose(tbf[:, c * 128:(c + 1) * 128], attn[:, c * 128:(c + 1) * 128], C.ident.ap()),
             reads=[attn, C.ident], writes=[tb])
    P.op("scalar", lambda e: e.activation(out=aT[:, 0:nkc * 128], in_=tbf[:, 0:nkc * 128], func=AF.Copy), reads=[tb], writes=[aT])
    xn = W.xn[r]
    for half in range(2):
        yb = W.ybanks[half]
        for c in range(nkc):
            P.op("tensor", lambda e: e.matmul(yb.ap(), lhsT=aT[:, c * 128:(c + 1) * 128], rhs=Wout[:, c, half * 512:(half + 1) * 512],
                                              start=(c == 0), stop=(c == nkc - 1)), reads=[aT, Wot[c]], writes=[yb])
        P.op("vector", lambda e: e.tensor_tensor(out=xn[:, half * 512:(half + 1) * 512], in0=yb.ap(), in1=xt[:, half * 512:(half + 1) * 512], op=ALU.add),
             reads=[yb, xt], writes=[xn])
    P.dma("sync", xdst[i * 128:(i + 1) * 128, :], xn.ap(), reads=[xn])


def mem_heads(P, C, W, qall, qch0, kmT, Vm, obank, sbank, r):
    PT = W.PTm[r]
    for half in range(2):
        sb_ = sbank[half]
        for hh in range(2):
            hm = half * 2 + hh
            for mb in range(2):
                j = hh * 2 + mb
                P.op("tensor", lambda e: e.matmul(sb_[:, j * 128:(j + 1) * 128], lhsT=kmT[:, hm // 2, mb * 128:(mb + 1) * 128],
                                                  rhs=qall[:, qch0 + hm, :], start=True, stop=True),
                     reads=[kmT, qall], writes=[sb_])
        P.op("scalar", lambda e: e.activation(out=PT[:, half * 512:(half + 1) * 512], in_=sb_.ap(), func=AF.Exp, scale=0.125),
             reads=[sb_], writes=[PT])
    for hm in range(4):
        for mb in range(2):
            j = hm * 2 + mb
            P.op("tensor", lambda e: e.matmul(obank[:, hm * 65:(hm + 1) * 65], lhsT=PT[:, j * 128:(j + 1) * 128], rhs=Vm[:, mb, hm, 0:65],
                                              start=(mb == 0), stop=(mb == 1)), reads=[PT, Vm], writes=[obank])


def alloc_finish_work(P, C, tag, obanks, tbank, ybanks):
    W = Ctx()
    W.attn = [P.sb("%sattn%d" % (tag, r), [128, D], BF16) for r in range(2)]
    W.attnT = [P.sb("%sattnT%d" % (tag, r), [128, D], BF16) for r in range(2)]
    W.rec = [P.sb("%srec%d" % (tag, r), [128, 16], F32) for r in range(2)]
    W.xts = [P.sb("%sfx%d" % (tag, r), [128, D], F32) for r in range(2)]
    W.xn = [P.sb("%sxn%d" % (tag, r), [128, D], F32) for r in range(2)]
    W.PTm = [P.sb("%sPTm%d" % (tag, r), [128, 1024], BF16) for r in range(2)]
    W.osrc = lambda r: [(bk, bk[:, 0:nh * 65], nh) for (bk, nh) in obanks]
    W.tbank = tbank
    W.ybanks = ybanks
    return W


def phase_B0(P, C, xsrc, xdst, fT, vS, wiAll, kmT, Vm, wout_dram, nblocks=NT):
    with P.scope():
        Wout, Wot = load_weight(P, "Wout0", wout_dram, 8, D)
        kT = P.sb("kT", [128, 2, S], BF16)
        kiT = P.sb("kiT", [128, S], BF16)
        Va = P.sb("Va", [128, NT, 4, VW], BF16)
        P.op("gpsimd", lambda e: e.memset(Va.ap(), 1.0), writes=[Va])
        for c in range(2):
            P.dma("sync", kT[:, c, :], fT[6 + c], writes=[kT])
        P.dma("sync", kiT.ap(), fT[12], writes=[kiT])
        vr = vS.rearrange("(i p) (g d) -> p i g d", p=128, d=64)
        for i0 in range(NT):
            P.dma("sync", Va[:, i0, :, 0:64], vr[:, i0, :, :], writes=[Va])
        scores = [P.sb("score%d" % r, [128, S], F32) for r in range(2)]
        junk = P.sb("junk", [128, S], BF16)
        Bs = [P.sb("Bm%d" % r, [128, S], BF16) for r in range(2)]
        Rs = [P.sb("R%d" % r, [128, 512], BF16) for r in range(4)]
        Wdgs = [P.sb("Wdg%d" % r, [128, 8, 128], BF16) for r in range(2)]
        PTs = [P.sb("PT%d" % r, [128, 512], BF16) for r in range(3)]
        qalls = [P.sb("qall%d" % r, [128, 24, 128], BF16) for r in range(3)]
        for r in range(3):
            P.op("gpsimd", lambda e: e.memset(qalls[r].ap(), 0.0), writes=[qalls[r]])
        sm = [[P.sb("bs%d_%d" % (r, j), [128, 1], F32) for j in range(6)] for r in range(2)]
        wtabs = [P.sb("wtab%d" % r, [128, N_BISECT + 2], F32) for r in range(2)]
        FW = alloc_finish_work(P, C, "b0", [(C.banks[3], 6), (C.banks[4], 6), (C.banks[5], 4)], C.banks[0], [C.banks[1], C.banks[0]])
        fT_r = fT.rearrange("c p t -> p c t")
        cnts = dict(lc=0, sc=0)

        def idx_steps(b):
            r = b % 2
            N = 128 * (b + 1)
            qall = qalls[b % 3]
            score = scores[r]
            q0 = b * 128
            Wdg = Wdgs[r]
            accb = C.banks[7]
            steps = []

            def loads():
                for (slot0, nch, ch0) in ((0, 6, 0), (12, 4, 8), (20, 2, 13)):
                    for base in (0, 64):
                        P.dma("sync", qall[base:base + 64, slot0 + base // 64:slot0 + 2 * nch:2, :], fT_r[base:base + 64, ch0:ch0 + nch, q0:q0 + 128],
                              writes=[qall])
                for h in range(8):
                    P.op("gpsimd", lambda e: e.tensor_scalar(out=Wdg[:, h, :], in0=C.ident.ap(), scalar1=wiAll[:, b, h:h + 1], scalar2=None, op0=ALU.mult),
                         reads=[C.ident, wiAll], writes=[Wdg])
            steps.append(loads)
            jobs = [(c0, h) for c0 in range(0, N, 512) for h in range(8)]
            state = dict(prev=None)

            def mk(job):
                def run():
                    prev = state["prev"]
                    if job is not None:
                        c0, h = job
                        wc = min(512, N - c0)
                        lc = cnts["lc"]
                        bank = C.banks[2] if lc % 2 == 0 else C.banks[6]
                        R = Rs[lc % 4]
                        cnts["lc"] = lc + 1
                        P.op("tensor", lambda e: e.matmul(bank[:, 0:wc], lhsT=qall[:, 12 + h, :], rhs=kiT[:, c0:c0 + wc],
                                                          start=True, stop=True), reads=[qall, kiT], writes=[bank])
                        P.op("scalar", lambda e: e.activation(out=R[:, 0:wc], in_=bank[:, 0:wc], func=AF.Relu), reads=[bank], writes=[R])
                    if prev is not None:
                        (c0_, h_), R_ = prev
                        wc_ = min(512, N - c0_)
                        P.op("tensor", lambda e: e.matmul(accb[:, 0:wc_], lhsT=Wdg[:, h_, :], rhs=R_[:, 0:wc_], start=(h_ == 0), stop=(h_ == 7)),
                             reads=[Wdg, R_], writes=[accb])
                        if h_ == 7:
                            P.op("scalar", lambda e: e.activation(out=score[:, c0_:c0_ + wc_], in_=accb[:, 0:wc_], func=AF.Copy), reads=[accb], writes=[score])
                    state["prev"] = (job, R) if job is not None else None
                return run
            for job in jobs + [None]:
                steps.append(mk(job))
            return steps

        def thr_steps(b):
            r = b % 2
            N = 128 * (b + 1)
            score = scores[r]
            B = Bs[r]
            q0 = b * 128
            mn, mx, mid, cnt, aa, thr = sm[r]
            wtab = wtabs[r]
            steps = []

            def pre():
                if b >= 2:
                    P.op("vector", lambda e: e.tensor_reduce(out=mn.ap(), in_=score[:, 0:N - 128], axis=AX.X, op=ALU.min), reads=[score], writes=[mn])
                P.op("gpsimd", lambda e: e.affine_select(out=score[:, q0:q0 + 128], in_=score[:, q0:q0 + 128], pattern=[[-1, 128]], compare_op=ALU.is_ge,
                                                        fill=NEG, base=0, channel_multiplier=1), reads=[score], writes=[score])
                if b >= 2:
                    P.op("vector", lambda e: e.tensor_reduce(out=mx.ap(), in_=score[:, 0:N], axis=AX.X, op=ALU.max), reads=[score], writes=[mx])
                    P.op("vector", lambda e: e.tensor_tensor(out=aa.ap(), in0=mx.ap(), in1=mn.ap(), op=ALU.subtract), reads=[mx, mn], writes=[aa])
                    P.op("vector", lambda e: e.tensor_scalar(out=wtab.ap(), in0=C.pow2.ap(), scalar1=aa.ap(), scalar2=0.5, op0=ALU.mult, op1=ALU.mult),
                         reads=[C.pow2, aa], writes=[wtab])
                    P.op("vector", lambda e: e.tensor_tensor(out=mid.ap(), in0=mn.ap(), in1=wtab[:, 1:2], op=ALU.add), reads=[mn, wtab], writes=[mid])
            steps.append(pre)
            if b >= 2:
                def mk(k):
                    def run():
                        P.op("vector", lambda e: e.tensor_scalar(out=junk[:, 0:N], in0=score[:, 0:N], scalar1=mid.ap(), scalar2=None, op0=ALU.is_ge, op1=ALU.add,
                                                                 accum_out=cnt.ap()), reads=[score, mid], writes=[junk, cnt])
                        P.op("vector", lambda e: e.tensor_scalar(out=aa.ap(), in0=cnt.ap(), scalar1=255.5, scalar2=-0.5, op0=ALU.is_ge, op1=ALU.add),
                             reads=[cnt], writes=[aa])
                        P.op("vector", lambda e: e.scalar_tensor_tensor(out=mid.ap(), in0=aa.ap(), scalar=wtab[:, k + 1:k + 2], in1=mid.ap(), op0=ALU.mult, op1=ALU.add),
                             reads=[aa, wtab, mid], writes=[mid])
                    return run
                for k in range(N_BISECT):
                    steps.append(mk(k))

            def post():
                if b >= 2:
                    P.op("vector", lambda e: e.tensor_tensor(out=thr.ap(), in0=mid.ap(), in1=wtab[:, N_BISECT + 1:N_BISECT + 2], op=ALU.subtract),
                         reads=[mid, wtab], writes=[thr])
                    thr_t = thr
                else:
                    thr_t = C.negthr
                P.op("vector", lambda e: e.tensor_scalar(out=B[:, 0:N], in0=score[:, 0:N], scalar1=thr_t.ap(), scalar2=MASKV, op0=ALU.is_lt, op1=ALU.mult),
                     reads=[score, thr_t], writes=[B])
                if C.dbg is not None and b == C.dbg_block:
                    P.dma("sync", C.dbg["score"], score.ap(), reads=[score])
                    P.dma("sync", C.dbg["thr"], thr_t.ap(), reads=[thr_t])
                    P.dma("sync", C.dbg["Bm"], B.ap(), reads=[B])
            steps.append(post)
            return steps

        def interleave(sa, sb_):
            na, nb_ = len(sa), len(sb_)
            j = 0
            for i, s in enumerate(sa):
                s()
                tgt = ((i + 1) * nb_) // max(na, 1)
                while j < tgt:
                    sb_[j]()
                    j += 1
            while j < nb_:
                sb_[j]()
                j += 1

        def attend(b):
            sc = cnts["sc"]
            r = b % 2
            qall = qalls[b % 3]
            B = Bs[r]
            jobs = []
            for h in range(12):
                pos = QPERM.index(h)
                g = h // 3
                obank = C.banks[3 + h // 6]
                ocol = (h % 6) * 65
                for kb0 in range(0, b + 1, 4):
                    kb1 = min(b + 1, kb0 + 4)
                    jobs.append((pos, g, obank, ocol, kb0, kb1))
            prev = None
            for job in jobs + [None]:
                if job is not None:
                    pos, g, obank, ocol, kb0, kb1 = job
                    sbank = C.banks[sc % 2]
                    PT = PTs[sc % 3]
                    sc += 1
                    for kb in range(kb0, kb1):
                        j = kb - kb0
                        P.op("tensor", lambda e: e.matmul(sbank[:, j * 128:(j + 1) * 128], lhsT=kT[:, g // 2, kb * 128:(kb + 1) * 128],
                                                          rhs=qall[:, pos, :], start=True, stop=False), reads=[kT, qall], writes=[sbank])
                        P.op("tensor", lambda e: e.matmul(sbank[:, j * 128:(j + 1) * 128], lhsT=B[:, kb * 128:(kb + 1) * 128], rhs=C.ident.ap(),
                                                          start=False, stop=True), reads=[B, C.ident], writes=[sbank])
                    nw = (kb1 - kb0) * 128
                    P.op("scalar", lambda e: e.activation(out=PT[:, 0:nw], in_=sbank[:, 0:nw], func=AF.Exp, scale=0.125), reads=[sbank], writes=[PT])
                if prev is not None:
                    (pos_, g_, obank_, ocol_, kb0_, kb1_), PT_ = prev
                    for kb in range(kb0_, kb1_):
                        j = kb - kb0_
                        P.op("tensor", lambda e: e.matmul(obank_[:, ocol_:ocol_ + 65], lhsT=PT_[:, j * 128:(j + 1) * 128], rhs=Va[:, kb, g_, 0:65],
                                                          start=(kb == 0), stop=(kb == b)), reads=[PT_, Va], writes=[obank_])
                prev = (job, PT) if job is not None else None
            cnts["sc"] = sc
            mem_heads(P, C, FW, qall, 20, kmT, Vm, C.banks[5], [C.banks[0], C.banks[1]], r)
            attn_finish(P, C, FW, xsrc, xdst, b, 16, Wout, Wot, 8)

        for s in idx_steps(0):
            s()
        interleave(thr_steps(0), idx_steps(1) if nblocks > 1 else [])
        for b in range(nblocks):
            if b + 1 < nblocks:
                interleave(thr_steps(b + 1), idx_steps(b + 2) if b + 2 < nblocks else [])
            attend(b)


def phase_F(P, C, tag, xnorm, xbase, xdst, wgu_dram, wd_dram, gains, grow, f0, f1, final_row=None, ntiles=NT):
    nf = f1 - f0
    with P.scope():
        Wgu = P.sb(tag + "Wgu", [128, 8, 2 * nf * 128], BF16)
        Wgt = [P.tok("%sWgt%d" % (tag, k)) for k in range(8)]
        for k in range(8):
            for half in range(2):
                c0 = half * DFF + f0 * 128
                P.dma("gpsimd", Wgu[:, k, half * nf * 128:(half + 1) * nf * 128], wgu_dram[k * 128:(k + 1) * 128, c0:c0 + nf * 128], writes=[Wgt[k]])
        Wd = P.sb(tag + "Wd", [128, nf, D], BF16)
        Wdt = [P.tok("%sWdt%d" % (tag, k)) for k in range(nf)]
        for k in range(nf):
            P.dma("gpsimd", Wd[:, k, :], wd_dram[(f0 + k) * 128:(f0 + k + 1) * 128, :], writes=[Wdt[k]])
        gb = load_gain(P, C, tag + "gbF", gains, grow)
        gfin = load_gain(P, C, tag + "gfin", gains, final_row) if final_row is not None else None
        NW = alloc_norm_work(P, C, tag + "F")
        xts = [P.sb("%sFx%d" % (tag, r), [128, D], F32) for r in range(2)]
        hT2 = [P.sb("%sFhT%d" % (tag, r), [128, 8, 256], BF16) for r in range(2)]
        hTt = [[P.tok("%sFhTt%d_%d" % (tag, r, t)) for t in range(2)] for r in range(2)]
        actT = [P.sb("%sactT%d" % (tag, r), [128, nf, 256], BF16) for r in range(2)]
        sg = [P.sb("%ssg%d" % (tag, r), [128, 256], F32) for r in range(2)]
        xn = [P.sb("%sFxn%d" % (tag, r), [128, D], F32) for r in range(4)]
        fsm = [[P.sb("%sfs%d_%d" % (tag, r, j), [128, 1], F32) for j in range(4)] for r in range(2)]
        fj = P.sb(tag + "fj", [128, D], BF16)
        gc = 0
        hbs = {}

        def norm1(G):
            for t in range(2):
                i = 2 * G + t
                xt = xts[i % 2]
                P.dma("sync", xt.ap(), xnorm[i * 128:(i + 1) * 128, :], writes=[xt])
                hbs[(G, t)] = rmsnorm_to_hT(P, C, NW, xt, gb, None, None, None)
                xo = xn[i % 4]
                P.dma("sync", xo.ap(), xbase[i * 128:(i + 1) * 128, :], writes=[xo])

        def norm2(G):
            for t in range(2):
                norm_p2(P, C, hbs.pop((G, t)), hT2[G % 2][:, :, t * 128:(t + 1) * 128], hTt[G % 2][t], C.banks[0],
                        evac_eng="scalar" if t == 0 else "vector")

        NG = ntiles // 2
        norm1(0)
        norm2(0)
        for G in range(NG):
            hT = hT2[G % 2]
            aT = actT[G % 2]
            for fc in range(nf):
                if fc == nf // 2 and G + 1 < NG:
                    norm1(G + 1)
                bank = C.banks[1 + (gc % 3)]
                s_ = sg[gc % 2]
                gc += 1
                for half in range(2):
                    coff = half * nf * 128 + fc * 128
                    for k in range(8):
                        P.op("tensor", lambda e: e.matmul(bank[:, half * 256:(half + 1) * 256], lhsT=Wgu[:, k, coff:coff + 128], rhs=hT[:, k, :],
                                                          start=(k == 0), stop=(k == 7)), reads=[Wgt[k]] + hTt[G % 2], writes=[bank])
                P.op("scalar", lambda e: e.activation(out=s_.ap(), in_=bank[:, 0:256], func=AF.Silu), reads=[bank], writes=[s_])
                P.op("vector", lambda e: e.tensor_tensor(out=aT[:, fc, :], in0=bank[:, 256:512], in1=s_.ap(), op=ALU.mult), reads=[bank, s_], writes=[aT])
            if G + 1 < NG:
                norm2(G + 1)
            for t in range(2):
                i = 2 * G + t
                xo = xn[i % 4]
                for half in range(2):
                    yb = C.banks[4 + (2 * t + half) % 4]
                    for fc in range(nf):
                        P.op("tensor", lambda e: e.matmul(yb.ap(), lhsT=aT[:, fc, t * 128:(t + 1) * 128], rhs=Wd[:, fc, half * 512:(half + 1) * 512],
                                                          start=(fc == 0), stop=(fc == nf - 1)), reads=[aT, Wdt[fc]], writes=[yb])
                    P.op("vector", lambda e: e.tensor_tensor(out=xo[:, half * 512:(half + 1) * 512], in0=yb.ap(), in1=xo[:, half * 512:(half + 1) * 512], op=ALU.add),
                         reads=[yb, xo], writes=[xo])
                if gfin is not None:
                    ssq, t1, t2, rstd = fsm[i % 2]
                    P.op("scalar", lambda e: e.activation(out=fj.ap(), in_=xo.ap(), func=AF.Square, accum_out=ssq.ap()), reads=[xo], writes=[fj, ssq])
                    P.op("vector", lambda e: e.tensor_scalar(out=t1.ap(), in0=ssq.ap(), scalar1=1.0 / D, scalar2=EPS, op0=ALU.mult, op1=ALU.add), reads=[ssq], writes=[t1])
                    P.op("scalar", lambda e: e.activation(out=t2.ap(), in_=t1.ap(), func=AF.Sqrt), reads=[t1], writes=[t2])
                    P.op("vector", lambda e: e.reciprocal(out=rstd.ap(), in_=t2.ap()), reads=[t2], writes=[rstd])
                    P.op("vector", lambda e: e.scalar_tensor_tensor(out=xo.ap(), in0=xo.ap(), scalar=rstd.ap(), in1=gfin.ap(), op0=ALU.mult, op1=ALU.mult),
                         reads=[xo, rstd, gfin], writes=[xo])
                P.dma("sync", xdst[i * 128:(i + 1) * 128, :], xo.ap(), reads=[xo], out=(gfin is not None))


def phase_B1(P, C, fT, vS, Og):
    for g, dil in enumerate((1, 4, 16)):
        nb = NT // dil
        with P.scope():
            qz = P.sb("qz1", [128, 4, S], BF16)
            kTg = P.sb("kTg", [128, 2, S], BF16)
            Vg = P.sb("Vg", [128, NT, 4, VW], BF16)
            P.op("gpsimd", lambda e: e.memset(qz.ap(), 0.0), writes=[qz])
            P.op("vector", lambda e: e.memset(Vg.ap(), 1.0), writes=[Vg])
            for c in range(2):
                for hh in range(2):
                    base = hh * 64
                    P.dma("sync", qz[base:base + 64, 2 * c + hh, :], fT[4 * g + c][base:base + 64, :], writes=[qz])
                P.dma("sync", kTg[:, c, :], fT[4 * g + 2 + c], writes=[kTg])
            vg = vS[:, g * 256:(g + 1) * 256].rearrange("(mb p dd) (h e) -> dd p mb h e", p=128, dd=dil, e=64)
            for r in range(dil):
                for m0 in range(nb):
                    P.dma("sync", Vg[:, r * nb + m0, :, 0:64], vg[r][:, m0, :, :], writes=[Vg])
            PTs = [P.sb("PTg%d" % k, [128, 512], BF16) for k in range(3)]
            Os = [P.sb("Os%d" % k, [128, 260], F32) for k in range(2)]
            Ogr = Og[g].rearrange("(m dd) c -> dd m c", dd=dil)
            sc = 0
            jobs = []
            qb = 0
            for r in range(dil):
                for mb in range(nb):
                    for hp in range(2):
                        jobs.append((r, mb, hp, qb))
                    qb += 1
            prev = None
            for job in jobs + [None]:
                if job is not None:
                    r, mb, hp, qb = job
                    qsl = slice(mb * 128 * dil + r, (mb * 128 + 127) * dil + r + 1, dil)
                    kbs = ([mb - 1] if mb > 0 else []) + [mb]
                    sbank = C.banks[sc % 3]
                    PT = PTs[sc % 3]
                    sc += 1
                    tiles = []
                    for hh in range(2):
                        j = hp * 2 + hh
                        for kb in kbs:
                            t = len(tiles)
                            tiles.append((j, kb))
                            ksl = slice(kb * 128 * dil + r, (kb * 128 + 127) * dil + r + 1, dil)
                            P.op("tensor", lambda e: e.matmul(sbank[:, t * 128:(t + 1) * 128], lhsT=kTg[:, j // 2, ksl], rhs=qz[:, j, qsl],
                                                              start=True, stop=False), reads=[kTg, qz], writes=[sbank])
                            M = C.MdT if kb == mb else C.MpT
                            P.op("tensor", lambda e: e.matmul(sbank[:, t * 128:(t + 1) * 128], lhsT=C.ident.ap(), rhs=M.ap(), start=False, stop=True),
                                 reads=[C.ident, M], writes=[sbank])
                    nw = len(tiles) * 128
                    P.op("scalar", lambda e: e.activation(out=PT[:, 0:nw], in_=sbank[:, 0:nw], func=AF.Exp, scale=0.125), reads=[sbank], writes=[PT])
                if prev is not None:
                    (r_, mb_, hp_, qb_), PT_, tiles_ = prev
                    obank = C.banks[6 + qb_ % 2]
                    kfirst = mb_ - 1 if mb_ > 0 else mb_
                    for t, (j, kb) in enumerate(tiles_):
                        P.op("tensor", lambda e: e.matmul(obank[:, j * 65:(j + 1) * 65], lhsT=PT_[:, t * 128:(t + 1) * 128], rhs=Vg[:, r_ * nb + kb, j, 0:65],
                                                          start=(kb == kfirst), stop=(kb == mb_)), reads=[PT_, Vg], writes=[obank])
                    if hp_ == 1:
                        O = Os[qb_ % 2]
                        P.op("vector", lambda e: e.tensor_copy(O.ap(), obank[:, 0:260]), reads=[obank], writes=[O])
                        P.dma("sync", Ogr[r_][mb_ * 128:(mb_ + 1) * 128, :], O.ap(), reads=[O])
                prev = (job, PT, tiles) if job is not None else None


def phase_B2(P, C, xsrc, xdst, fT, Og, kmT, Vm, wout_dram):
    with P.scope():
        Wout, Wot = load_weight(P, "Wout1", wout_dram, 4, D)
        qzs = [P.sb("qzm%d" % r, [128, 4, 128], BF16) for r in range(2)]
        for r in range(2):
            P.op("gpsimd", lambda e: e.memset(qzs[r].ap(), 0.0), writes=[qzs[r]])
        Ot = [[P.sb("Ot%d_%d" % (r, g), [128, 260], F32) for g in range(3)] for r in range(2)]
        FW = alloc_finish_work(P, C, "b2", [], C.banks[0], [C.banks[1], C.banks[2]])
        FW.osrc = lambda r: [(Ot[r][0], Ot[r][0].ap(), 4), (C.banks[5], C.banks[5][:, 0:260], 4)]
        fT_r = fT.rearrange("c p t -> p c t")
        for i in range(NT):
            r = i % 2
            qz = qzs[r]
            q0 = i * 128
            for base in (0, 64):
                P.dma("sync", qz[base:base + 64, base // 64:4:2, :], fT_r[base:base + 64, 12:14, q0:q0 + 128], writes=[qz])
            for g in range(3):
                P.dma("sync", Ot[r][g].ap(), Og[g][q0:q0 + 128, :], writes=[Ot[r][g]])
            mem_heads(P, C, FW, qz, 0, kmT, Vm, C.banks[5], [C.banks[6], C.banks[7]], r)
            P.op("gpsimd", lambda e: e.tensor_tensor(out=Ot[r][0].ap(), in0=Ot[r][0].ap(), in1=Ot[r][1].ap(), op=ALU.add),
                 reads=[Ot[r][0], Ot[r][1]], writes=[Ot[r][0]])
            P.op("gpsimd", lambda e: e.tensor_tensor(out=Ot[r][0].ap(), in0=Ot[r][0].ap(), in1=Ot[r][2].ap(), op=ALU.add),
                 reads=[Ot[r][0], Ot[r][2]], writes=[Ot[r][0]])
            attn_finish(P, C, FW, xsrc, xdst, i, 8, Wout, Wot, 4)


def build_program(stop_after=None, dbg_block=None, nblocks0=NT, skip_l0=False):
    nc = bass.Bass("TRN2", target_bir_lowering=False)
    P = Prog(nc)
    C = Ctx()
    I = {}

    def inp(name, shape, dt=F32):
        I[name] = nc.dram_tensor(name, list(shape), dt, kind="ExternalInput").ap()
        return I[name]

    x_in = inp("x", [S, D])
    mem_in = inp("mem", [256, D])
    pos_in = inp("pos", [128, NT], I32)
    gains = inp("gains", [7, D])
    C.freqs_in = inp("freqs", [128, 48])
    w_in0 = inp("w_in0", [D, SPEC0["ncols"]])
    w_in1 = inp("w_in1", [D, SPEC1["ncols"]])
    w_mkv = [inp("w_mkv%d" % l, [D, 512]) for l in range(2)]
    w_out0 = inp("w_out0", [1024, D])
    w_out1 = inp("w_out1", [512, D])
    w_gu = [inp("w_gu%d" % l, [D, 2 * DFF]) for l in range(2)]
    w_dn = [inp("w_dn%d" % l, [DFF, D]) for l in range(2)]
    out = nc.dram_tensor("out", [S, D], F32, kind="ExternalOutput").ap()
    C.dbg = None
    C.dbg_block = dbg_block
    if dbg_block is not None:
        C.dbg = dict(score=nc.dram_tensor("dbg_score", [128, S], F32, kind="ExternalOutput").ap(),
                     thr=nc.dram_tensor("dbg_thr", [128, 1], F32, kind="ExternalOutput").ap(),
                     Bm=nc.dram_tensor("dbg_Bm", [128, S], BF16, kind="ExternalOutput").ap())
    fT0 = P.dram("fT0", [15, 128, S], BF16)
    vS0 = P.dram("vS0", [S, 256], BF16)
    fT1 = P.dram("fT1", [14, 128, S], BF16)
    vS1 = P.dram("vS1", [S, 768], BF16)
    Og = [P.dram("Og%d" % g, [S, 260], F32) for g in range(3)]
    xa = P.dram("xa", [S, D], F32)
    xb = P.dram("xb", [S, D], F32)
    xc = P.dram("xc", [S, D], F32)

    C.banks = [P.ps("bank%d" % k, [128, 512], F32) for k in range(8)]
    setup_consts(P, C, pos_in)
    HF = NFC // 2

    def final(src):
        print("n_inst before final", P.n_inst)
        P.max_ops = None
        with P.scope():
            t = [P.sb("fin%d" % r, [128, D], F32) for r in range(2)]
            for i in range(NT):
                P.dma("sync", t[i % 2].ap(), src[i * 128:(i + 1) * 128, :], writes=[t[i % 2]])
                P.dma("sync", out[i * 128:(i + 1) * 128, :], t[i % 2].ap(), reads=[t[i % 2]], out=True)
        P.finish()
        return nc

    with (P.scope() if not skip_l0 else contextlib.nullcontext()):
      if not skip_l0:
        wiAll = P.sb("wiAll", [128, NT, 8], F32)
        kmT = P.sb("kmT0", [128, 2, 256], BF16)
        Vm = P.sb("Vm0", [128, 2, 4, VW], BF16)
        phase_M(P, C, "m0", mem_in, w_mkv[0], gains, 1, kmT, Vm)
        if stop_after == "M":
            d1 = nc.dram_tensor("dbg_kmT", [128, 2, 256], BF16, kind="ExternalOutput").ap()
            d2 = nc.dram_tensor("dbg_Vm", [128, 2, 4, VW], BF16, kind="ExternalOutput").ap()
            P.dma("sync", d1, kmT.ap(), reads=[kmT])
            P.dma("sync", d2, Vm.ap(), reads=[Vm])
            return final(x_in)
        phase_A(P, C, "a0", x_in, w_in0, gains, 0, SPEC0, fT0, vS0, wiAll, pos_in)
        if stop_after == "A":
            d1 = nc.dram_tensor("dbg_fT0", [15, 128, S], BF16, kind="ExternalOutput").ap()
            d2 = nc.dram_tensor("dbg_vS0", [S, 256], BF16, kind="ExternalOutput").ap()
            d3 = nc.dram_tensor("dbg_wi", [128, NT, 8], F32, kind="ExternalOutput").ap()
            with P.scope():
                tb = P.sb("dbgt", [128, S], BF16)
                for c in range(15):
                    P.dma("sync", tb.ap(), fT0[c], writes=[tb])
                    P.dma("sync", d1[c], tb.ap(), reads=[tb])
                for c in range(2):
                    P.dma("sync", tb[:, 0:2048].rearrange("p (a b) -> p a b", b=256), vS0[c * 2048:(c + 1) * 2048, :].rearrange("(a p) b -> p a b", p=128), writes=[tb])
                    P.dma("sync", d2[c * 2048:(c + 1) * 2048, :].rearrange("(a p) b -> p a b", p=128), tb[:, 0:2048].rearrange("p (a b) -> p a b", b=256), reads=[tb])
                P.dma("sync", d3, wiAll.ap(), reads=[wiAll])
            return final(x_in)
        phase_B0(P, C, x_in, xa, fT0, vS0, wiAll, kmT, Vm, w_out0, nblocks=nblocks0)
    if stop_after == "B0":
        return final(xa)
    if not skip_l0:
        phase_F(P, C, "f0a", xa, xa, xb, w_gu[0], w_dn[0], gains, 2, 0, HF)
        phase_F(P, C, "f0b", xa, xb, xc, w_gu[0], w_dn[0], gains, 2, HF, NFC)
    else:
        xc = x_in
    if stop_after == "F0":
        return final(xc)
    with P.scope():
        kmT = P.sb("kmT1", [128, 2, 256], BF16)
        Vm = P.sb("Vm1", [128, 2, 4, VW], BF16)
        phase_M(P, C, "m1", mem_in, w_mkv[1], gains, 4, kmT, Vm)
        phase_A(P, C, "a1", xc, w_in1, gains, 3, SPEC1, fT1, vS1, None, pos_in)
        phase_B1(P, C, fT1, vS1, Og)
        phase_B2(P, C, xc, xa, fT1, Og, kmT, Vm, w_out1)
    if stop_after == "B2":
        return final(xa)
    phase_F(P, C, "f1a", xa, xa, xb, w_gu[1], w_dn[1], gains, 5, 0, HF)
    phase_F(P, C, "f1b", xa, xb, out, w_gu[1], w_dn[1], gains, 5, HF, NFC, final_row=6)
    P.finish()
    return nc


def prep_inputs(inputs):
    f = lambda a: np.ascontiguousarray(np.asarray(a, dtype=np.float32))
    w0 = f(inputs["l0_w_in"])
    q, k, v, qi, ki, wi, qm = np.split(w0, np.cumsum([768, 256, 256, 512, 64, 8])[:], axis=1)
    qp = np.concatenate([q[:, h * 64:(h + 1) * 64] for h in QPERM], axis=1)
    w_in0 = np.ascontiguousarray(np.concatenate([qp, k, qi, ki, ki, wi, v, qm], axis=1))
    w1 = f(inputs["l1_w_in"])
    parts = [w1[:, j * 256:(j + 1) * 256] for j in range(10)]
    w_in1 = np.ascontiguousarray(np.concatenate([parts[0], parts[1], parts[3], parts[4], parts[6], parts[7], parts[2], parts[5], parts[8], parts[9]], axis=1))
    gains = np.ascontiguousarray(np.stack([f(inputs[n]) for n in ("l0_norm_mix", "l0_norm_mem", "l0_norm_ffn", "l1_norm_mix", "l1_norm_mem",
                                                                 "l1_norm_ffn", "final_norm")], axis=0))
    fr64 = (np.float32(10000.0) ** (-np.arange(32, dtype=np.float32) / np.float32(32))).astype(np.float32)
    fr16 = (np.float32(10000.0) ** (-np.arange(16, dtype=np.float32) / np.float32(16))).astype(np.float32)
    freqs = np.ascontiguousarray(np.broadcast_to(np.concatenate([fr64, fr16])[None, :], (128, 48)).astype(np.float32))
    shared = dict(freqs=freqs, gains=gains, w_in0=w_in0, w_in1=w_in1, w_mkv0=f(inputs["l0_w_mem_kv"]), w_mkv1=f(inputs["l1_w_mem_kv"]),
                  w_out0=f(inputs["l0_w_out"]), w_out1=f(inputs["l1_w_out"]), w_gu0=f(inputs["l0_w_gate_up"]), w_gu1=f(inputs["l1_w_gate_up"]),
                  w_dn0=f(inputs["l0_w_down"]), w_dn1=f(inputs["l1_w_down"]))
    x = f(inputs["x"])
    mem = f(inputs["mem"])
    pos = np.ascontiguousarray(np.asarray(inputs["positions"], dtype=np.int32))
    maps = []
    for c in range(x.shape[0]):
        m = dict(shared)
        m["x"] = x[c]
        m["mem"] = mem[c]
        m["pos"] = np.ascontiguousarray(pos[c].reshape(NT, 128).T)
        maps.append(m)
    return maps


_NC_CACHE = {}


def kernel(**inputs):
    maps = prep_inputs(inputs)
    if "nc" not in _NC_CACHE:
        _NC_CACHE["nc"] = build_program()
    nc = _NC_CACHE["nc"]
    res = run_bass_kernel_spmd(nc, maps, core_ids=list(range(len(maps))))
    return np.stack([np.asarray(r["out"], dtype=np.float32) for r in res.results], axis=0)
```

```python
import contextlib
import numpy as np
import ml_dtypes
import concourse.bass as bass
import concourse.mybir as mybir
from concourse.bass_utils import run_bass_kernel_spmd

F32 = mybir.dt.float32
BF16 = mybir.dt.bfloat16
I32 = mybir.dt.int32
AF = mybir.ActivationFunctionType
ALU = mybir.AluOpType
AX = mybir.AxisListType

SAME_ENGINE_SYNC = True


class Tok:
    def __init__(self, name):
        self.name = name
        self.w = None
        self.r = {}


class Buf(Tok):
    def __init__(self, name, handle):
        super().__init__(name)
        self.h = handle

    def ap(self):
        return self.h[:]

    def __getitem__(self, idx):
        return self.h[idx]


class _Eng:
    def __init__(self, name, handle, sem):
        self.name = name
        self.h = handle
        self.sem = sem
        self.count = 0
        self.seen = {}


class Prog:
    def __init__(self, nc, n_dma_sems=8):
        self.nc = nc
        self.stack = contextlib.ExitStack()
        self.engs = {}
        for name in ("tensor", "vector", "scalar", "gpsimd", "sync"):
            sem = self.stack.enter_context(nc.semaphore("s_" + name))
            self.engs[name] = _Eng(name, getattr(nc, name), sem)
        self.dma_sems = {}
        for q in ("sync", "gpsimd", "scalar"):
            self.dma_sems[q] = [[self.stack.enter_context(nc.semaphore("d_%s%d" % (q, i))), 0] for i in range(n_dma_sems)]
        self.dma_rr = {"sync": 0, "gpsimd": 0, "scalar": 0}
        self.out_events = []
        self.n_inst = 0
        self.scopes = [self.stack]
        import os
        self.max_ops = int(os.environ["KMAXOPS"]) if "KMAXOPS" in os.environ else None

    @contextlib.contextmanager
    def scope(self):
        st = contextlib.ExitStack()
        self.scopes.append(st)
        try:
            yield
        finally:
            self.barrier()
            self.scopes.pop()
            st.close()

    def barrier(self):
        if getattr(self, "finished", False):
            return
        for eng in self.engs.values():
            for other in self.engs.values():
                if other is not eng and other.count > 0:
                    self._wait(eng, (other.name, other.sem, other.count))
            for q, pool in self.dma_sems.items():
                for i, slot in enumerate(pool):
                    if slot[1] > 0:
                        self._wait(eng, ("d_%s%d" % (q, i), slot[0], 16 * slot[1]))

    def tok(self, name):
        return Tok(name)

    def sb(self, name, shape, dtype):
        self.uid = getattr(self, "uid", 0) + 1
        name = "%s_u%d" % (name, self.uid)
        return Buf(name, self.scopes[-1].enter_context(self.nc.sbuf_tensor(name, list(shape), dtype)))

    def ps(self, name, shape, dtype):
        b = Buf(name, self.scopes[-1].enter_context(self.nc.psum_tensor(name, list(shape), dtype)))
        b.excl = True
        return b

    def dram(self, name, shape, dtype):
        return self.nc.dram_tensor(name, list(shape), dtype, kind="Internal").ap()

    def _wait(self, eng, ev):
        key, sem, val = ev
        if eng.seen.get(key, 0) >= val:
            return
        if key == eng.name and not (SAME_ENGINE_SYNC and eng.name != "tensor" and eng.name != "sync"):
            return
        eng.h.wait_ge(sem, val)
        eng.seen[key] = val

    def _deps(self, eng, reads, writes):
        for t in reads:
            if t.w is not None:
                self._wait(eng, t.w)
        for t in writes:
            if t.w is not None:
                self._wait(eng, t.w)
            for ev in t.r.values():
                self._wait(eng, ev)

    def _record(self, ev, reads, writes):
        for t in writes:
            t.w = ev
            t.r = {}
        for t in reads:
            if t in writes:
                continue
            t.r[ev[0]] = ev

    def op(self, engname, fn, reads=(), writes=()):
        if self.max_ops is not None and self.n_inst >= self.max_ops:
            return None
        eng = self.engs[engname]
        ex = [t for t in reads if getattr(t, "excl", False) and t not in writes]
        if ex:
            reads = [t for t in reads if t not in ex]
            writes = list(writes) + ex
        self._deps(eng, reads, writes)
        inst = fn(eng.h)
        eng.count += 1
        inst.then_inc(eng.sem, 1)
        ev = (eng.name, eng.sem, eng.count)
        self._record(ev, reads, writes)
        self.n_inst += 1
        return ev

    def dma(self, q, out_ap, in_ap, reads=(), writes=(), out=False, **kw):
        if self.max_ops is not None and self.n_inst >= self.max_ops:
            return None
        eng = self.engs[q]
        self._deps(eng, reads, writes)
        pool = self.dma_sems[q]
        i = self.dma_rr[q]
        self.dma_rr[q] = (i + 1) % len(pool)
        slot = pool[i]
        key = "d_%s%d" % (q, i)
        if slot[1] > 0:
            self._wait(eng, (key, slot[0], 16 * slot[1]))
        eng.h.dma_start(out=out_ap, in_=in_ap, **kw).then_inc(slot[0], 16)
        slot[1] += 1
        ev = (key, slot[0], 16 * slot[1])
        self._record(ev, reads, writes)
        if out:
            self.out_events.append(ev)
        self.n_inst += 1
        return ev

    def finish(self):
        eng = self.engs["sync"]
        for q, pool in self.dma_sems.items():
            for i, slot in enumerate(pool):
                if slot[1] > 0:
                    self._wait(eng, ("d_%s%d" % (q, i), slot[0], 16 * slot[1]))
        for name, e in self.engs.items():
            if name != "sync" and e.count > 0:
                self._wait(eng, (name, e.sem, e.count))
        self.finished = True
        for st in reversed(self.scopes):
            st.close()

    def make_identity(self, idt):
        tmp = self.sb(idt.name + "_i", [128, 128], I32)
        self.op("gpsimd", lambda e: e.iota(tmp.ap(), pattern=[[1, 128]], base=0, channel_multiplier=-1), writes=[tmp])
        self.op("vector", lambda e: e.tensor_scalar(out=idt.ap(), in0=tmp.ap(), scalar1=0.0, scalar2=None, op0=ALU.is_equal),
                reads=[tmp], writes=[idt])


S = 4096
D = 1024
NT = 32
DFF = 2816
NFC = 22
EPS = 1e-6
NEG = -1.0e30
MASKV = -30000.0
VW = 66
N_BISECT = 16
IDX_SCALE = (8 ** -0.5) * (64 ** -0.5)
QPERM = [0, 3, 1, 4, 2, 5, 6, 9, 7, 10, 8, 11]

SPEC0 = dict(
    ncols=2184,
    chunks=[(0, 512, [("rope64", 0, 8)]), (512, 512, [("rope64", 0, 8)]), (1024, 512, [("rope32", 0, 8)]),
            (1536, 136, [("rope32", 0, 2), ("wi", 128, 8)]), (1672, 512, [("copy", 0, 512)])],
    tcols=[128 * k for k in range(13)] + [1928, 2056],
    vcols=(1672, 1928),
)
SPEC1 = dict(
    ncols=2560,
    chunks=[(0, 512, [("rope64", 0, 8)]), (512, 512, [("rope64", 0, 8)]), (1024, 512, [("rope64", 0, 8)]),
            (1536, 512, [("copy", 0, 512)]), (2048, 512, [("copy", 0, 512)])],
    tcols=[128 * k for k in range(12)] + [2304, 2432],
    vcols=(1536, 2304),
)


class Ctx:
    pass


def load_weight(P, name, dram, nk, ncols, q="gpsimd"):
    W = P.sb(name, [128, nk, ncols], BF16)
    toks = [P.tok("%s_%d" % (name, k)) for k in range(nk)]
    for k in range(nk):
        c0 = 0
        while c0 < ncols:
            c1 = min(ncols, c0 + 2048)
            P.dma(q, W[:, k, c0:c1], dram[k * 128:(k + 1) * 128, c0:c1], writes=[toks[k]])
            c0 = c1
    return W, toks


def setup_consts(P, C, pos_in):
    C.ident = P.sb("ident", [128, 128], BF16)
    P.make_identity(C.ident)
    C.negthr = P.sb("negthr", [128, 1], F32)
    P.op("vector", lambda e: e.memset(C.negthr.ap(), -1.0e29), writes=[C.negthr])
    C.pow2 = P.sb("pow2", [128, N_BISECT + 2], F32)
    for k in range(N_BISECT + 2):
        P.op("gpsimd", lambda e: e.memset(C.pow2[:, k:k + 1], 2.0 ** (1 - k)), writes=[C.pow2])
    C.MdT = P.sb("MdT", [128, 128], BF16)
    C.MpT = P.sb("MpT", [128, 128], BF16)
    zt = P.sb("zt", [128, 128], F32)
    zm = P.sb("zm", [128, 128], F32)
    P.op("vector", lambda e: e.memset(zt.ap(), 0.0), writes=[zt])
    P.op("gpsimd", lambda e: e.affine_select(out=zm.ap(), in_=zt.ap(), pattern=[[1, 128]], compare_op=ALU.is_ge, fill=MASKV, base=0, channel_multiplier=-1),
         reads=[zt], writes=[zm])
    P.op("vector", lambda e: e.tensor_copy(C.MdT.ap(), zm.ap()), reads=[zm], writes=[C.MdT])
    P.op("gpsimd", lambda e: e.affine_select(out=zm.ap(), in_=zt.ap(), pattern=[[-1, 128]], compare_op=ALU.is_ge, fill=MASKV, base=0, channel_multiplier=1),
         reads=[zt, C.MdT], writes=[zm])
    P.op("vector", lambda e: e.tensor_copy(C.MpT.ap(), zm.ap()), reads=[zm], writes=[C.MpT])


def build_rope_tables(P, C, pos_in):
    C.cos64 = P.sb("cos64", [128, NT, 32], F32)
    C.sin64 = P.sb("sin64", [128, NT, 32], F32)
    C.nsin64 = P.sb("nsin64", [128, NT, 32], F32)
    C.cos16 = P.sb("cos16", [128, NT, 16], F32)
    C.sin16 = P.sb("sin16", [128, NT, 16], F32)
    C.nsin16 = P.sb("nsin16", [128, NT, 16], F32)
    with P.scope():
        posi = P.sb("posi", [128, NT], I32)
        posf = P.sb("posf", [128, NT], F32)
        P.dma("sync", posi.ap(), pos_in, writes=[posi])
        P.op("vector", lambda e: e.tensor_copy(posf.ap(), posi.ap()), reads=[posi], writes=[posf])
        for half, cosT, sinT, nsinT in ((32, C.cos64, C.sin64, C.nsin64), (16, C.cos16, C.sin16, C.nsin16)):
            n = NT * half
            fr = P.sb("fr%d" % half, [128, half], F32)
            a = P.sb("a%d" % half, [128, NT, half], F32)
            ki = P.sb("ki%d" % half, [128, NT, half], I32)
            kf = P.sb("kf%d" % half, [128, NT, half], F32)
            fr1 = P.sb("fr1%d" % half, [128, NT, half], F32)
            m1 = P.sb("m1%d" % half, [128, NT, half], F32)
            f0 = 0 if half == 32 else 32
            P.dma("sync", fr.ap(), C.freqs_in[:, f0:f0 + half], writes=[fr])
            P.op("vector", lambda e: e.tensor_tensor(out=a.ap(), in0=posf.ap().unsqueeze(2).broadcast_to([128, NT, half]),
                                                     in1=fr.ap().unsqueeze(1).broadcast_to([128, NT, half]), op=ALU.mult),
                 reads=[posf, fr], writes=[a])
            P.op("vector", lambda e: e.tensor_scalar(out=a.ap(), in0=a.ap(), scalar1=float(1.0 / (2.0 * np.pi)), scalar2=None, op0=ALU.mult),
                 reads=[a], writes=[a])
            for shift, outT, neg in ((0.0, sinT, False), (0.25, cosT, False), (0.5, nsinT, False)):
                src = a
                if shift != 0.0:
                    P.op("vector", lambda e: e.tensor_scalar(out=fr1.ap(), in0=a.ap(), scalar1=shift, scalar2=None, op0=ALU.add),
                         reads=[a], writes=[fr1])
                    src = fr1
                P.op("vector", lambda e: e.tensor_copy(ki.ap(), src.ap()), reads=[src], writes=[ki])
                P.op("vector", lambda e: e.tensor_copy(kf.ap(), ki.ap()), reads=[ki], writes=[kf])
                P.op("vector", lambda e: e.tensor_tensor(out=kf.ap(), in0=src.ap(), in1=kf.ap(), op=ALU.subtract), reads=[src, kf], writes=[kf])
                P.op("vector", lambda e: e.tensor_scalar(out=m1.ap(), in0=kf.ap(), scalar1=0.5, scalar2=None, op0=ALU.is_gt), reads=[kf], writes=[m1])
                P.op("vector", lambda e: e.tensor_tensor(out=kf.ap(), in0=kf.ap(), in1=m1.ap(), op=ALU.subtract), reads=[kf, m1], writes=[kf])
                P.op("vector", lambda e: e.tensor_scalar(out=m1.ap(), in0=kf.ap(), scalar1=-0.5, scalar2=None, op0=ALU.is_lt), reads=[kf], writes=[m1])
                P.op("vector", lambda e: e.tensor_tensor(out=kf.ap(), in0=kf.ap(), in1=m1.ap(), op=ALU.add), reads=[kf, m1], writes=[kf])
                P.op("scalar", lambda e: e.activation(out=outT.ap(), in_=kf.ap(), func=AF.Sin, scale=float(2.0 * np.pi) * (1.0 - 1e-6)),
                     reads=[kf], writes=[outT])


def alloc_norm_work(P, C, tag):
    W = Ctx()
    W.sqj = P.sb(tag + "sqj", [128, D], BF16)
    W.small = [[P.sb("%ssm%d_%d" % (tag, r, j), [128, 1], F32) for j in range(4)] for r in range(2)]
    W.hb = [P.sb("%shb%d" % (tag, r), [128, D], BF16) for r in range(2)]
    W.k = 0
    return W


def rmsnorm_to_hT(P, C, W, xt, gb, hT_ap, hT_tok, bank, evac_eng="scalar"):
    r = W.k % 2
    W.k += 1
    ssq, t1, t2, rstd = W.small[r]
    hb = W.hb[r]
    P.op("scalar", lambda e: e.activation(out=W.sqj.ap(), in_=xt.ap(), func=AF.Square, accum_out=ssq.ap()), reads=[xt], writes=[W.sqj, ssq])
    P.op("vector", lambda e: e.tensor_scalar(out=t1.ap(), in0=ssq.ap(), scalar1=1.0 / D, scalar2=EPS, op0=ALU.mult, op1=ALU.add), reads=[ssq], writes=[t1])
    P.op("scalar", lambda e: e.activation(out=t2.ap(), in_=t1.ap(), func=AF.Sqrt), reads=[t1], writes=[t2])
    P.op("vector", lambda e: e.reciprocal(out=rstd.ap(), in_=t2.ap()), reads=[t2], writes=[rstd])
    P.op("vector", lambda e: e.scalar_tensor_tensor(out=hb.ap(), in0=xt.ap(), scalar=rstd.ap(), in1=gb.ap(), op0=ALU.mult, op1=ALU.mult),
         reads=[xt, rstd, gb], writes=[hb])
    if hT_ap is None:
        return hb
    return norm_p2(P, C, hb, hT_ap, hT_tok, bank, evac_eng)


def norm_p2(P, C, hb, hT_ap, hT_tok, bank, evac_eng="scalar"):
    bbf = bank.ap().bitcast(BF16)
    for c in range(8):
        P.op("tensor", lambda e: e.transpose(bbf[:, c * 128:(c + 1) * 128], hb[:, c * 128:(c + 1) * 128], C.ident.ap()),
             reads=[hb, C.ident], writes=[bank])
    srcv = bbf if len(hT_ap.shape) == 2 else bbf.rearrange("p (c t) -> p c t", t=128)
    if evac_eng == "scalar":
        P.op("scalar", lambda e: e.activation(out=hT_ap, in_=srcv, func=AF.Copy), reads=[bank], writes=[hT_tok])
    else:
        P.op("vector", lambda e: e.tensor_copy(hT_ap, srcv), reads=[bank], writes=[hT_tok])


def load_gain(P, C, name, gains, row):
    gb = P.sb(name, [128, D], F32)
    P.dma("sync", gb.ap(), gains[row].partition_broadcast(128), writes=[gb])
    return gb


def phase_A(P, C, tag, xsrc, w_dram, gains, grow, spec, fT, vS, wiAll, pos_in):
    ncols = spec["ncols"]
    with P.scope():
        build_rope_tables(P, C, pos_in)
        Win, Wt = load_weight(P, tag + "Win", w_dram, 8, ncols)
        gb = load_gain(P, C, tag + "gbA", gains, grow)
        NW = alloc_norm_work(P, C, tag + "A")
        xts = [P.sb("%sxt%d" % (tag, r), [128, D], F32) for r in range(2)]
        hTs = [P.sb("%shT%d" % (tag, r), [128, D], BF16) for r in range(2)]
        pts = [P.sb("%spt%d" % (tag, r), [128, ncols], BF16) for r in range(2)]
        t1s = [P.sb("%st1_%d" % (tag, r), [128, 512], F32) for r in range(2)]
        t2s = [P.sb("%st2_%d" % (tag, r), [128, 512], F32) for r in range(2)]
        ntc = len(spec["tcols"])
        fTs = [P.sb("%sfTs%d" % (tag, r), [128, ntc, 128], BF16) for r in range(2)]
        fT_r = fT.rearrange("c p t -> p c t")
        cc = 0
        hbs = {}

        def n1(i):
            xt = xts[i % 2]
            P.dma("sync", xt.ap(), xsrc[i * 128:(i + 1) * 128, :], writes=[xt])
            hbs[i] = rmsnorm_to_hT(P, C, NW, xt, gb, None, None, None)

        def n2(i):
            norm_p2(P, C, hbs.pop(i), hTs[i % 2].ap(), hTs[i % 2], C.banks[0], evac_eng="scalar")

        def featT(i):
            _featT_body(P, C, spec, pts, fTs, fT_r, vS, ntc, i)

        n1(0)
        n2(0)
        for i in range(NT):
            hT = hTs[i % 2]
            pt = pts[i % 2]
            if i + 1 < NT:
                n1(i + 1)
            for (col0, width, handlers) in spec["chunks"]:
                bank = C.banks[1 + (cc % 4)]
                t1 = t1s[cc % 2]
                t2 = t2s[cc % 2]
                cc += 1
                for k in range(8):
                    P.op("tensor", lambda e: e.matmul(bank[:, 0:width], lhsT=hT[:, k * 128:(k + 1) * 128], rhs=Win[:, k, col0:col0 + width],
                                                      start=(k == 0), stop=(k == 7)), reads=[hT, Wt[k]], writes=[bank])
                for (kind, l0, n) in handlers:
                    if kind == "rope64":
                        nh = n
                        w = nh * 64
                        xv2 = bank[:, l0:l0 + w].rearrange("p (h d) -> p h d", d=32)
                        xv = bank[:, l0:l0 + w].rearrange("p (h d) -> p h d", d=64)
                        t1v2 = t1[:, 0:w].rearrange("p (h d) -> p h d", d=32)
                        t2v = t2[:, 0:w].rearrange("p (h d) -> p h d", d=64)
                        cosb = C.cos64[:, i:i + 1, :].broadcast_to([128, 2 * nh, 32])
                        sinb = C.sin64[:, i:i + 1, :].broadcast_to([128, nh, 32])
                        nsinb = C.nsin64[:, i:i + 1, :].broadcast_to([128, nh, 32])
                        P.op("vector", lambda e: e.tensor_tensor(out=t1v2, in0=xv2, in1=cosb, op=ALU.mult), reads=[bank, C.cos64], writes=[t1])
                        P.op("vector", lambda e: e.tensor_tensor(out=t2v[:, :, 0:32], in0=xv[:, :, 32:64], in1=nsinb, op=ALU.mult),
                             reads=[bank, C.nsin64], writes=[t2])
                        P.op("vector", lambda e: e.tensor_tensor(out=t2v[:, :, 32:64], in0=xv[:, :, 0:32], in1=sinb, op=ALU.mult),
                             reads=[bank, C.sin64], writes=[t2])
                        P.op("gpsimd", lambda e: e.tensor_tensor(out=pt[:, col0 + l0:col0 + l0 + w], in0=t1[:, 0:w], in1=t2[:, 0:w], op=ALU.add),
                             reads=[t1, t2], writes=[pt])
                    elif kind == "rope32":
                        nh = n
                        w = nh * 64
                        xv = bank[:, l0:l0 + w].rearrange("p (h d) -> p h d", d=64)
                        xr4 = bank[:, l0:l0 + w].rearrange("p (h t d) -> p h t d", t=4, d=16)
                        t1r4 = t1[:, 0:w].rearrange("p (h t d) -> p h t d", t=4, d=16)
                        t2v = t2[:, 0:w].rearrange("p (h d) -> p h d", d=64)
                        t1v = t1[:, 0:w].rearrange("p (h d) -> p h d", d=64)
                        ptv = pt[:, col0 + l0:col0 + l0 + w].rearrange("p (h d) -> p h d", d=64)
                        cosb = C.cos16[:, i:i + 1, :].unsqueeze(1).broadcast_to([128, nh, 2, 16])
                        sinb = C.sin16[:, i:i + 1, :].broadcast_to([128, nh, 16])
                        nsinb = C.nsin16[:, i:i + 1, :].broadcast_to([128, nh, 16])
                        P.op("vector", lambda e: e.tensor_tensor(out=t1r4[:, :, 0:2, :], in0=xr4[:, :, 0:2, :], in1=cosb, op=ALU.mult),
                             reads=[bank, C.cos16], writes=[t1])
                        P.op("vector", lambda e: e.tensor_tensor(out=t2v[:, :, 0:16], in0=xv[:, :, 16:32], in1=nsinb, op=ALU.mult),
                             reads=[bank, C.nsin16], writes=[t2])
                        P.op("vector", lambda e: e.tensor_tensor(out=t2v[:, :, 16:32], in0=xv[:, :, 0:16], in1=sinb, op=ALU.mult),
                             reads=[bank, C.sin16], writes=[t2])
                        P.op("gpsimd", lambda e: e.tensor_tensor(out=ptv[:, :, 0:32], in0=t1v[:, :, 0:32], in1=t2v[:, :, 0:32], op=ALU.add),
                             reads=[t1, t2], writes=[pt])
                        P.op("scalar", lambda e: e.activation(out=ptv[:, :, 32:64], in_=xv[:, :, 32:64], func=AF.Copy), reads=[bank], writes=[pt])
                    elif kind == "copy":
                        P.op("scalar", lambda e: e.activation(out=pt[:, col0 + l0:col0 + l0 + n], in_=bank[:, l0:l0 + n], func=AF.Copy),
                             reads=[bank], writes=[pt])
                    elif kind == "wi":
                        P.op("scalar", lambda e: e.activation(out=wiAll[:, i, :], in_=bank[:, l0:l0 + n], func=AF.Copy, scale=float(IDX_SCALE)),
                             reads=[bank], writes=[wiAll])
            if i + 1 < NT:
                n2(i + 1)
            if i >= 1:
                featT(i - 1)
        featT(NT - 1)


def _featT_body(P, C, spec, pts, fTs, fT_r, vS, ntc, i):
            pt = pts[i % 2]
            fts = fTs[i % 2]
            for g0 in range(0, ntc, 8):
                g1 = min(ntc, g0 + 8)
                bank = C.banks[5 + (g0 // 8)]
                bbf = bank.ap().bitcast(BF16)
                for k in range(g0, g1):
                    c0 = spec["tcols"][k]
                    P.op("tensor", lambda e: e.transpose(bbf[:, (k - g0) * 128:(k - g0 + 1) * 128], pt[:, c0:c0 + 128], C.ident.ap()),
                         reads=[pt, C.ident], writes=[bank])
                eng = "vector" if g0 == 0 else "scalar"
                if eng == "vector":
                    P.op("vector", lambda e: e.tensor_copy(fts[:, g0:g1, :], bbf[:, 0:(g1 - g0) * 128].rearrange("p (c t) -> p c t", t=128)),
                         reads=[bank], writes=[fts])
                else:
                    P.op("scalar", lambda e: e.activation(out=fts[:, g0:g1, :], in_=bbf[:, 0:(g1 - g0) * 128].rearrange("p (c t) -> p c t", t=128),
                                                          func=AF.Copy), reads=[bank], writes=[fts])
            P.dma("sync", fT_r[:, :, i * 128:(i + 1) * 128], fts.ap(), reads=[fts])
            v0, v1 = spec["vcols"]
            P.dma("sync", vS[i * 128:(i + 1) * 128, :], pt[:, v0:v1], reads=[pt])


def phase_M(P, C, tag, mem_in, w_dram, gains, grow, kmT, Vm):
    with P.scope():
        Wm, Wt = load_weight(P, tag + "Wm", w_dram, 8, 512)
        gb = load_gain(P, C, tag + "gbM", gains, grow)
        NW = alloc_norm_work(P, C, tag + "M")
        xts = [P.sb("%smx%d" % (tag, r), [128, D], F32) for r in range(2)]
        hTs = [P.sb("%smhT%d" % (tag, r), [128, D], BF16) for r in range(2)]
        kb16 = [P.sb("%skb16_%d" % (tag, r), [128, 256], BF16) for r in range(2)]
        P.op("gpsimd", lambda e: e.memset(Vm.ap(), 1.0), writes=[Vm])
        for mb in range(2):
            xt, hT = xts[mb], hTs[mb]
            P.dma("sync", xt.ap(), mem_in[mb * 128:(mb + 1) * 128, :], writes=[xt])
            rmsnorm_to_hT(P, C, NW, xt, gb, hT.ap(), hT, C.banks[0])
            bank = C.banks[1 + mb]
            for k in range(8):
                P.op("tensor", lambda e: e.matmul(bank.ap(), lhsT=hT[:, k * 128:(k + 1) * 128], rhs=Wm[:, k, :], start=(k == 0), stop=(k == 7)),
                     reads=[hT, Wt[k]], writes=[bank])
            P.op("scalar", lambda e: e.activation(out=kb16[mb].ap(), in_=bank[:, 0:256], func=AF.Copy), reads=[bank], writes=[kb16[mb]])
            P.op("vector", lambda e: e.tensor_copy(Vm[:, mb, :, 0:64], bank[:, 256:512].rearrange("p (h d) -> p h d", d=64)),
                 reads=[bank], writes=[Vm])
            tb = C.banks[3 + mb]
            tbf = tb.ap().bitcast(BF16)
            for c in range(2):
                P.op("tensor", lambda e: e.transpose(tbf[:, c * 128:(c + 1) * 128], kb16[mb][:, c * 128:(c + 1) * 128], C.ident.ap()),
                     reads=[kb16[mb], C.ident], writes=[tb])
            P.op("vector", lambda e: e.tensor_copy(kmT[:, :, mb * 128:(mb + 1) * 128], tbf[:, 0:256].rearrange("p (c t) -> p c t", t=128)),
                 reads=[tb], writes=[kmT])


def attn_finish(P, C, W, xsrc, xdst, i, nheads, Wout, Wot, nkc):
    r = i % 2
    attn = W.attn[r]
    rec = W.rec[r]
    xt = W.xts[r]
    P.dma("sync", xt.ap(), xsrc[i * 128:(i + 1) * 128, :], writes=[xt])
    h0 = 0
    for (bank, oap, nh) in W.osrc(r):
        ov = oap.rearrange("p (h d) -> p h d", d=65)
        P.op("vector", lambda e: e.reciprocal(out=rec[:, h0:h0 + nh], in_=ov[:, :, 64]), reads=[bank], writes=[rec])
        P.op("vector", lambda e: e.tensor_tensor(out=attn[:, h0 * 64:(h0 + nh) * 64].rearrange("p (h d) -> p h d", d=64), in0=ov[:, :, 0:64],
                                                 in1=rec[:, h0:h0 + nh].unsqueeze(2).broadcast_to([128, nh, 64]), op=ALU.mult),
             reads=[bank, rec], writes=[attn])
        h0 += nh
    tb = W.tbank
    tbf = tb.ap().bitcast(BF16)
    aT = W.attnT[r]
    for c in range(nkc):
        P.op("tensor", lambda e: e.transpose(tbf[:, c * 128:(c + 1) * 128], attn[:, c * 128:(c + 1) * 128], C.ident.ap()),
             reads=[attn, C.ident], writes=[tb])
    P.op("scalar", lambda e: e.activation(out=aT[:, 0:nkc * 128], in_=tbf[:, 0:nkc * 128], func=AF.Copy), reads=[tb], writes=[aT])
    xn = W.xn[r]
    for half in range(2):
        yb = W.ybanks[half]
        for c in range(nkc):
            P.op("tensor", lambda e: e.matmul(yb.ap(), lhsT=aT[:, c * 128:(c + 1) * 128], rhs=Wout[:, c, half * 512:(half + 1) * 512],
                                              start=(c == 0), stop=(c == nkc - 1)), reads=[aT, Wot[c]], writes=[yb])
        P.op("vector", lambda e: e.tensor_tensor(out=xn[:, half * 512:(half + 1) * 512], in0=yb.ap(), in1=xt[:, half * 512:(half + 1) * 512], op=ALU.add),
             reads=[yb, xt], writes=[xn])
    P.dma("sync", xdst[i * 128:(i + 1) * 128, :], xn.ap(), reads=[xn])


def mem_heads(P, C, W, qall, qch0, kmT, Vm, obank, sbank, r):
    PT = W.PTm[r]
    for half in range(2):
        sb_ = sbank[half]
        for hh in range(2):
            hm = half * 2 + hh
            for mb in range(2):
                j = hh * 2 + mb
                P.op("tensor", lambda e: e.matmul(sb_[:, j * 128:(j + 1) * 128], lhsT=kmT[:, hm // 2, mb * 128:(mb + 1) * 128],
                                                  rhs=qall[:, qch0 + hm, :], start=True, stop=True),
                     reads=[kmT, qall], writes=[sb_])
        P.op("scalar", lambda e: e.activation(out=PT[:, half * 512:(half + 1) * 512], in_=sb_.ap(), func=AF.Exp, scale=0.125),
             reads=[sb_], writes=[PT])
    for hm in range(4):
        for mb in range(2):
            j = hm * 2 + mb
            P.op("tensor", lambda e: e.matmul(obank[:, hm * 65:(hm + 1) * 65], lhsT=PT[:, j * 128:(j + 1) * 128], rhs=Vm[:, mb, hm, 0:65],
                                              start=(mb == 0), stop=(mb == 1)), reads=[PT, Vm], writes=[obank])


def alloc_finish_work(P, C, tag, obanks, tbank, ybanks):
    W = Ctx()
    W.attn = [P.sb("%sattn%d" % (tag, r), [128, D], BF16) for r in range(2)]
    W.attnT = [P.sb("%sattnT%d" % (tag, r), [128, D], BF16) for r in range(2)]
    W.rec = [P.sb("%srec%d" % (tag, r), [128, 16], F32) for r in range(2)]
    W.xts = [P.sb("%sfx%d" % (tag, r), [128, D], F32) for r in range(2)]
    W.xn = [P.sb("%sxn%d" % (tag, r), [128, D], F32) for r in range(2)]
    W.PTm = [P.sb("%sPTm%d" % (tag, r), [128, 1024], BF16) for r in range(2)]
    W.osrc = lambda r: [(bk, bk[:, 0:nh * 65], nh) for (bk, nh) in obanks]
    W.tbank = tbank
    W.ybanks = ybanks
    return W


def phase_B0(P, C, xsrc, xdst, fT, vS, wiAll, kmT, Vm, wout_dram, nblocks=NT):
    with P.scope():
        Wout, Wot = load_weight(P, "Wout0", wout_dram, 8, D)
        kT = P.sb("kT", [128, 2, S], BF16)
        kiT = P.sb("kiT", [128, S], BF16)
        Va = P.sb("Va", [128, NT, 4, VW], BF16)
        P.op("gpsimd", lambda e: e.memset(Va.ap(), 1.0), writes=[Va])
        for c in range(2):
            P.dma("sync", kT[:, c, :], fT[6 + c], writes=[kT])
        P.dma("sync", kiT.ap(), fT[12], writes=[kiT])
        vr = vS.rearrange("(i p) (g d) -> p i g d", p=128, d=64)
        for i0 in range(NT):
            P.dma("sync", Va[:, i0, :, 0:64], vr[:, i0, :, :], writes=[Va])
        scores = [P.sb("score%d" % r, [128, S], F32) for r in range(2)]
        junk = P.sb("junk", [128, S], BF16)
        Bs = [P.sb("Bm%d" % r, [128, S], BF16) for r in range(2)]
        Rs = [P.sb("R%d" % r, [128, 512], BF16) for r in range(4)]
        Wdgs = [P.sb("Wdg%d" % r, [128, 8, 128], BF16) for r in range(2)]
        PTs = [P.sb("PT%d" % r, [128, 512], BF16) for r in range(3)]
        qalls = [P.sb("qall%d" % r, [128, 24, 128], BF16) for r in range(3)]
        for r in range(3):
            P.op("gpsimd", lambda e: e.memset(qalls[r].ap(), 0.0), writes=[qalls[r]])
        sm = [[P.sb("bs%d_%d" % (r, j), [128, 1], F32) for j in range(6)] for r in range(2)]
        wtabs = [P.sb("wtab%d" % r, [128, N_BISECT + 2], F32) for r in range(2)]
        FW = alloc_finish_work(P, C, "b0", [(C.banks[3], 6), (C.banks[4], 6), (C.banks[5], 4)], C.banks[0], [C.banks[1], C.banks[0]])
        fT_r = fT.rearrange("c p t -> p c t")
        cnts = dict(lc=0, sc=0)

        def idx_steps(b):
            r = b % 2
            N = 128 * (b + 1)
            qall = qalls[b % 3]
            score = scores[r]
            q0 = b * 128
            Wdg = Wdgs[r]
            accb = C.banks[7]
            steps = []

            def loads():
                for (slot0, nch, ch0) in ((0, 6, 0), (12, 4, 8), (20, 2, 13)):
                    for base in (0, 64):
                        P.dma("sync", qall[base:base + 64, slot0 + base // 64:slot0 + 2 * nch:2, :], fT_r[base:base + 64, ch0:ch0 + nch, q0:q0 + 128],
                              writes=[qall])
                for h in range(8):
                    P.op("gpsimd", lambda e: e.tensor_scalar(out=Wdg[:, h, :], in0=C.ident.ap(), scalar1=wiAll[:, b, h:h + 1], scalar2=None, op0=ALU.mult),
                         reads=[C.ident, wiAll], writes=[Wdg])
            steps.append(loads)
            jobs = [(c0, h) for c0 in range(0, N, 512) for h in range(8)]
            state = dict(prev=None)

            def mk(job):
                def run():
                    prev = state["prev"]
                    if job is not None:
                        c0, h = job
                        wc = min(512, N - c0)
                        lc = cnts["lc"]
                        bank = C.banks[2] if lc % 2 == 0 else C.banks[6]
                        R = Rs[lc % 4]
                        cnts["lc"] = lc + 1
                        P.op("tensor", lambda e: e.matmul(bank[:, 0:wc], lhsT=qall[:, 12 + h, :], rhs=kiT[:, c0:c0 + wc],
                                                          start=True, stop=True), reads=[qall, kiT], writes=[bank])
                        P.op("scalar", lambda e: e.activation(out=R[:, 0:wc], in_=bank[:, 0:wc], func=AF.Relu), reads=[bank], writes=[R])
                    if prev is not None:
                        (c0_, h_), R_ = prev
                        wc_ = min(512, N - c0_)
                        P.op("tensor", lambda e: e.matmul(accb[:, 0:wc_], lhsT=Wdg[:, h_, :], rhs=R_[:, 0:wc_], start=(h_ == 0), stop=(h_ == 7)),
                             reads=[Wdg, R_], writes=[accb])
                        if h_ == 7:
                            P.op("scalar", lambda e: e.activation(out=score[:, c0_:c0_ + wc_], in_=accb[:, 0:wc_], func=AF.Copy), reads=[accb], writes=[score])
                    state["prev"] = (job, R) if job is not None else None
                return run
            for job in jobs + [None]:
                steps.append(mk(job))
            return steps

        def thr_steps(b):
            r = b % 2
            N = 128 * (b + 1)
            score = scores[r]
            B = Bs[r]
            q0 = b * 128
            mn, mx, mid, cnt, aa, thr = sm[r]
            wtab = wtabs[r]
            steps = []

            def pre():
                if b >= 2:
                    P.op("vector", lambda e: e.tensor_reduce(out=mn.ap(), in_=score[:, 0:N - 128], axis=AX.X, op=ALU.min), reads=[score], writes=[mn])
                P.op("gpsimd", lambda e: e.affine_select(out=score[:, q0:q0 + 128], in_=score[:, q0:q0 + 128], pattern=[[-1, 128]], compare_op=ALU.is_ge,
                                                        fill=NEG, base=0, channel_multiplier=1), reads=[score], writes=[score])
                if b >= 2:
                    P.op("vector", lambda e: e.tensor_reduce(out=mx.ap(), in_=score[:, 0:N], axis=AX.X, op=ALU.max), reads=[score], writes=[mx])
                    P.op("vector", lambda e: e.tensor_tensor(out=aa.ap(), in0=mx.ap(), in1=mn.ap(), op=ALU.subtract), reads=[mx, mn], writes=[aa])
                    P.op("vector", lambda e: e.tensor_scalar(out=wtab.ap(), in0=C.pow2.ap(), scalar1=aa.ap(), scalar2=0.5, op0=ALU.mult, op1=ALU.mult),
                         reads=[C.pow2, aa], writes=[wtab])
                    P.op("vector", lambda e: e.tensor_tensor(out=mid.ap(), in0=mn.ap(), in1=wtab[:, 1:2], op=ALU.add), reads=[mn, wtab], writes=[mid])
            steps.append(pre)
            if b >= 2:
                def mk(k):
                    def run():
                        P.op("vector", lambda e: e.tensor_scalar(out=junk[:, 0:N], in0=score[:, 0:N], scalar1=mid.ap(), scalar2=None, op0=ALU.is_ge, op1=ALU.add,
                                                                 accum_out=cnt.ap()), reads=[score, mid], writes=[junk, cnt])
                        P.op("vector", lambda e: e.tensor_scalar(out=aa.ap(), in0=cnt.ap(), scalar1=255.5, scalar2=-0.5, op0=ALU.is_ge, op1=ALU.add),
                             reads=[cnt], writes=[aa])
                        P.op("vector", lambda e: e.scalar_tensor_tensor(out=mid.ap(), in0=aa.ap(), scalar=wtab[:, k + 1:k + 2], in1=mid.ap(), op0=ALU.mult, op1=ALU.add),
                             reads=[aa, wtab, mid], writes=[mid])
                    return run
                for k in range(N_BISECT):
                    steps.append(mk(k))

            def post():
                if b >= 2:
                    P.op("vector", lambda e: e.tensor_tensor(out=thr.ap(), in0=mid.ap(), in1=wtab[:, N_BISECT + 1:N_BISECT + 2], op=ALU.subtract),
                         reads=[mid, wtab], writes=[thr])
                    thr_t = thr
                else:
                    thr_t = C.negthr
                P.op("vector", lambda e: e.tensor_scalar(out=B[:, 0:N], in0=score[:, 0:N], scalar1=thr_t.ap(), scalar2=MASKV, op0=ALU.is_lt, op1=ALU.mult),
                     reads=[score, thr_t], writes=[B])
                if C.dbg is not None and b == C.dbg_block:
                    P.dma("sync", C.dbg["score"], score.ap(), reads=[score])
                    P.dma("sync", C.dbg["thr"], thr_t.ap(), reads=[thr_t])
                    P.dma("sync", C.dbg["Bm"], B.ap(), reads=[B])
            steps.append(post)
            return steps

        def interleave(sa, sb_):
            na, nb_ = len(sa), len(sb_)
            j = 0
            for i, s in enumerate(sa):
                s()
                tgt = ((i + 1) * nb_) // max(na, 1)
                while j < tgt:
                    sb_[j]()
                    j += 1
            while j < nb_:
                sb_[j]()
                j += 1

        def attend(b):
            sc = cnts["sc"]
            r = b % 2
            qall = qalls[b % 3]
            B = Bs[r]
            jobs = []
            for h in range(12):
                pos = QPERM.index(h)
                g = h // 3
                obank = C.banks[3 + h // 6]
                ocol = (h % 6) * 65
                for kb0 in range(0, b + 1, 4):
                    kb1 = min(b + 1, kb0 + 4)
                    jobs.append((pos, g, obank, ocol, kb0, kb1))
            prev = None
            for job in jobs + [None]:
                if job is not None:
                    pos, g, obank, ocol, kb0, kb1 = job
                    sbank = C.banks[sc % 2]
                    PT = PTs[sc % 3]
                    sc += 1
                    for kb in range(kb0, kb1):
                        j = kb - kb0
                        P.op("tensor", lambda e: e.matmul(sbank[:, j * 128:(j + 1) * 128], lhsT=kT[:, g // 2, kb * 128:(kb + 1) * 128],
                                                          rhs=qall[:, pos, :], start=True, stop=False), reads=[kT, qall], writes=[sbank])
                        P.op("tensor", lambda e: e.matmul(sbank[:, j * 128:(j + 1) * 128], lhsT=B[:, kb * 128:(kb + 1) * 128], rhs=C.ident.ap(),
                                                          start=False, stop=True), reads=[B, C.ident], writes=[sbank])
                    nw = (kb1 - kb0) * 128
                    P.op("scalar", lambda e: e.activation(out=PT[:, 0:nw], in_=sbank[:, 0:nw], func=AF.Exp, scale=0.125), reads=[sbank], writes=[PT])
                if prev is not None:
                    (pos_, g_, obank_, ocol_, kb0_, kb1_), PT_ = prev
                    for kb in range(kb0_, kb1_):
                        j = kb - kb0_
                        P.op("tensor", lambda e: e.matmul(obank_[:, ocol_:ocol_ + 65], lhsT=PT_[:, j * 128:(j + 1) * 128], rhs=Va[:, kb, g_, 0:65],
                                                          start=(kb == 0), stop=(kb == b)), reads=[PT_, Va], writes=[obank_])
                prev = (job, PT) if job is not None else None
            cnts["sc"] = sc
            mem_heads(P, C, FW, qall, 20, kmT, Vm, C.banks[5], [C.banks[0], C.banks[1]], r)
            attn_finish(P, C, FW, xsrc, xdst, b, 16, Wout, Wot, 8)

        for s in idx_steps(0):
            s()
        interleave(thr_steps(0), idx_steps(1) if nblocks > 1 else [])
        for b in range(nblocks):
            if b + 1 < nblocks:
                interleave(thr_steps(b + 1), idx_steps(b + 2) if b + 2 < nblocks else [])
            attend(b)


def phase_F(P, C, tag, xnorm, xbase, xdst, wgu_dram, wd_dram, gains, grow, f0, f1, final_row=None, ntiles=NT):
    nf = f1 - f0
    with P.scope():
        Wgu = P.sb(tag + "Wgu", [128, 8, 2 * nf * 128], BF16)
        Wgt = [P.tok("%sWgt%d" % (tag, k)) for k in range(8)]
        for k in range(8):
            for half in range(2):
                c0 = half * DFF + f0 * 128
                P.dma("gpsimd", Wgu[:, k, half * nf * 128:(half + 1) * nf * 128], wgu_dram[k * 128:(k + 1) * 128, c0:c0 + nf * 128], writes=[Wgt[k]])
        Wd = P.sb(tag + "Wd", [128, nf, D], BF16)
        Wdt = [P.tok("%sWdt%d" % (tag, k)) for k in range(nf)]
        for k in range(nf):
            P.dma("gpsimd", Wd[:, k, :], wd_dram[(f0 + k) * 128:(f0 + k + 1) * 128, :], writes=[Wdt[k]])
        gb = load_gain(P, C, tag + "gbF", gains, grow)
        gfin = load_gain(P, C, tag + "gfin", gains, final_row) if final_row is not None else None
        NW = alloc_norm_work(P, C, tag + "F")
        xts = [P.sb("%sFx%d" % (tag, r), [128, D], F32) for r in range(2)]
        hT2 = [P.sb("%sFhT%d" % (tag, r), [128, 8, 256], BF16) for r in range(2)]
        hTt = [[P.tok("%sFhTt%d_%d" % (tag, r, t)) for t in range(2)] for r in range(2)]
        actT = [P.sb("%sactT%d" % (tag, r), [128, nf, 256], BF16) for r in range(2)]
        sg = [P.sb("%ssg%d" % (tag, r), [128, 256], F32) for r in range(2)]
        xn = [P.sb("%sFxn%d" % (tag, r), [128, D], F32) for r in range(4)]
        fsm = [[P.sb("%sfs%d_%d" % (tag, r, j), [128, 1], F32) for j in range(4)] for r in range(2)]
        fj = P.sb(tag + "fj", [128, D], BF16)
        gc = 0
        hbs = {}

        def norm1(G):
            for t in range(2):
                i = 2 * G + t
                xt = xts[i % 2]
                P.dma("sync", xt.ap(), xnorm[i * 128:(i + 1) * 128, :], writes=[xt])
                hbs[(G, t)] = rmsnorm_to_hT(P, C, NW, xt, gb, None, None, None)
                xo = xn[i % 4]
                P.dma("sync", xo.ap(), xbase[i * 128:(i + 1) * 128, :], writes=[xo])

        def norm2(G):
            for t in range(2):
                norm_p2(P, C, hbs.pop((G, t)), hT2[G % 2][:, :, t * 128:(t + 1) * 128], hTt[G % 2][t], C.banks[0],
                        evac_eng="scalar" if t == 0 else "vector")

        NG = ntiles // 2
        norm1(0)
        norm2(0)
        for G in range(NG):
            hT = hT2[G % 2]
            aT = actT[G % 2]
            for fc in range(nf):
                if fc == nf // 2 and G + 1 < NG:
                    norm1(G + 1)
                bank = C.banks[1 + (gc % 3)]
                s_ = sg[gc % 2]
                gc += 1
                for half in range(2):
                    coff = half * nf * 128 + fc * 128
                    for k in range(8):
                        P.op("tensor", lambda e: e.matmul(bank[:, half * 256:(half + 1) * 256], lhsT=Wgu[:, k, coff:coff + 128], rhs=hT[:, k, :],
                                                          start=(k == 0), stop=(k == 7)), reads=[Wgt[k]] + hTt[G % 2], writes=[bank])
                P.op("scalar", lambda e: e.activation(out=s_.ap(), in_=bank[:, 0:256], func=AF.Silu), reads=[bank], writes=[s_])
                P.op("vector", lambda e: e.tensor_tensor(out=aT[:, fc, :], in0=bank[:, 256:512], in1=s_.ap(), op=ALU.mult), reads=[bank, s_], writes=[aT])
            if G + 1 < NG:
                norm2(G + 1)
            for t in range(2):
                i = 2 * G + t
                xo = xn[i % 4]
                for half in range(2):
                    yb = C.banks[4 + (2 * t + half) % 4]
                    for fc in range(nf):
                        P.op("tensor", lambda e: e.matmul(yb.ap(), lhsT=aT[:, fc, t * 128:(t + 1) * 128], rhs=Wd[:, fc, half * 512:(half + 1) * 512],
                                                          start=(fc == 0), stop=(fc == nf - 1)), reads=[aT, Wdt[fc]], writes=[yb])
                    P.op("vector", lambda e: e.tensor_tensor(out=xo[:, half * 512:(half + 1) * 512], in0=yb.ap(), in1=xo[:, half * 512:(half + 1) * 512], op=ALU.add),
                         reads=[yb, xo], writes=[xo])
                if gfin is not None:
                    ssq, t1, t2, rstd = fsm[i % 2]
                    P.op("scalar", lambda e: e.activation(out=fj.ap(), in_=xo.ap(), func=AF.Square, accum_out=ssq.ap()), reads=[xo], writes=[fj, ssq])
                    P.op("vector", lambda e: e.tensor_scalar(out=t1.ap(), in0=ssq.ap(), scalar1=1.0 / D, scalar2=EPS, op0=ALU.mult, op1=ALU.add), reads=[ssq], writes=[t1])
                    P.op("scalar", lambda e: e.activation(out=t2.ap(), in_=t1.ap(), func=AF.Sqrt), reads=[t1], writes=[t2])
                    P.op("vector", lambda e: e.reciprocal(out=rstd.ap(), in_=t2.ap()), reads=[t2], writes=[rstd])
                    P.op("vector", lambda e: e.scalar_tensor_tensor(out=xo.ap(), in0=xo.ap(), scalar=rstd.ap(), in1=gfin.ap(), op0=ALU.mult, op1=ALU.mult),
                         reads=[xo, rstd, gfin], writes=[xo])
                P.dma("sync", xdst[i * 128:(i + 1) * 128, :], xo.ap(), reads=[xo], out=(gfin is not None))


def phase_B1(P, C, fT, vS, Og):
    for g, dil in enumerate((1, 4, 16)):
        nb = NT // dil
        with P.scope():
            qz = P.sb("qz1", [128, 4, S], BF16)
            kTg = P.sb("kTg", [128, 2, S], BF16)
            Vg = P.sb("Vg", [128, NT, 4, VW], BF16)
            P.op("gpsimd", lambda e: e.memset(qz.ap(), 0.0), writes=[qz])
            P.op("vector", lambda e: e.memset(Vg.ap(), 1.0), writes=[Vg])
            for c in range(2):
                for hh in range(2):
                    base = hh * 64
                    P.dma("sync", qz[base:base + 64, 2 * c + hh, :], fT[4 * g + c][base:base + 64, :], writes=[qz])
                P.dma("sync", kTg[:, c, :], fT[4 * g + 2 + c], writes=[kTg])
            vg = vS[:, g * 256:(g + 1) * 256].rearrange("(mb p dd) (h e) -> dd p mb h e", p=128, dd=dil, e=64)
            for r in range(dil):
                for m0 in range(nb):
                    P.dma("sync", Vg[:, r * nb + m0, :, 0:64], vg[r][:, m0, :, :], writes=[Vg])
            PTs = [P.sb("PTg%d" % k, [128, 512], BF16) for k in range(3)]
            Os = [P.sb("Os%d" % k, [128, 260], F32) for k in range(2)]
            Ogr = Og[g].rearrange("(m dd) c -> dd m c", dd=dil)
            sc = 0
            jobs = []
            qb = 0
            for r in range(dil):
                for mb in range(nb):
                    for hp in range(2):
                        jobs.append((r, mb, hp, qb))
                    qb += 1
            prev = None
            for job in jobs + [None]:
                if job is not None:
                    r, mb, hp, qb = job
                    qsl = slice(mb * 128 * dil + r, (mb * 128 + 127) * dil + r + 1, dil)
                    kbs = ([mb - 1] if mb > 0 else []) + [mb]
                    sbank = C.banks[sc % 3]
                    PT = PTs[sc % 3]
                    sc += 1
                    tiles = []
                    for hh in range(2):
                        j = hp * 2 + hh
                        for kb in kbs:
                            t = len(tiles)
                            tiles.append((j, kb))
                            ksl = slice(kb * 128 * dil + r, (kb * 128 + 127) * dil + r + 1, dil)
                            P.op("tensor", lambda e: e.matmul(sbank[:, t * 128:(t + 1) * 128], lhsT=kTg[:, j // 2, ksl], rhs=qz[:, j, qsl],
                                                              start=True, stop=False), reads=[kTg, qz], writes=[sbank])
                            M = C.MdT if kb == mb else C.MpT
                            P.op("tensor", lambda e: e.matmul(sbank[:, t * 128:(t + 1) * 128], lhsT=C.ident.ap(), rhs=M.ap(), start=False, stop=True),
                                 reads=[C.ident, M], writes=[sbank])
                    nw = len(tiles) * 128
                    P.op("scalar", lambda e: e.activation(out=PT[:, 0:nw], in_=sbank[:, 0:nw], func=AF.Exp, scale=0.125), reads=[sbank], writes=[PT])
                if prev is not None:
                    (r_, mb_, hp_, qb_), PT_, tiles_ = prev
                    obank = C.banks[6 + qb_ % 2]
                    kfirst = mb_ - 1 if mb_ > 0 else mb_
                    for t, (j, kb) in enumerate(tiles_):
                        P.op("tensor", lambda e: e.matmul(obank[:, j * 65:(j + 1) * 65], lhsT=PT_[:, t * 128:(t + 1) * 128], rhs=Vg[:, r_ * nb + kb, j, 0:65],
                                                          start=(kb == kfirst), stop=(kb == mb_)), reads=[PT_, Vg], writes=[obank])
                    if hp_ == 1:
                        O = Os[qb_ % 2]
                        P.op("vector", lambda e: e.tensor_copy(O.ap(), obank[:, 0:260]), reads=[obank], writes=[O])
                        P.dma("sync", Ogr[r_][mb_ * 128:(mb_ + 1) * 128, :], O.ap(), reads=[O])
                prev = (job, PT, tiles) if job is not None else None


def phase_B2(P, C, xsrc, xdst, fT, Og, kmT, Vm, wout_dram):
    with P.scope():
        Wout, Wot = load_weight(P, "Wout1", wout_dram, 4, D)
        qzs = [P.sb("qzm%d" % r, [128, 4, 128], BF16) for r in range(2)]
        for r in range(2):
            P.op("gpsimd", lambda e: e.memset(qzs[r].ap(), 0.0), writes=[qzs[r]])
        Ot = [[P.sb("Ot%d_%d" % (r, g), [128, 260], F32) for g in range(3)] for r in range(2)]
        FW = alloc_finish_work(P, C, "b2", [], C.banks[0], [C.banks[1], C.banks[2]])
        FW.osrc = lambda r: [(Ot[r][0], Ot[r][0].ap(), 4), (C.banks[5], C.banks[5][:, 0:260], 4)]
        fT_r = fT.rearrange("c p t -> p c t")
        for i in range(NT):
            r = i % 2
            qz = qzs[r]
            q0 = i * 128
            for base in (0, 64):
                P.dma("sync", qz[base:base + 64, base // 64:4:2, :], fT_r[base:base + 64, 12:14, q0:q0 + 128], writes=[qz])
            for g in range(3):
                P.dma("sync", Ot[r][g].ap(), Og[g][q0:q0 + 128, :], writes=[Ot[r][g]])
            mem_heads(P, C, FW, qz, 0, kmT, Vm, C.banks[5], [C.banks[6], C.banks[7]], r)
            P.op("gpsimd", lambda e: e.tensor_tensor(out=Ot[r][0].ap(), in0=Ot[r][0].ap(), in1=Ot[r][1].ap(), op=ALU.add),
                 reads=[Ot[r][0], Ot[r][1]], writes=[Ot[r][0]])
            P.op("gpsimd", lambda e: e.tensor_tensor(out=Ot[r][0].ap(), in0=Ot[r][0].ap(), in1=Ot[r][2].ap(), op=ALU.add),
                 reads=[Ot[r][0], Ot[r][2]], writes=[Ot[r][0]])
            attn_finish(P, C, FW, xsrc, xdst, i, 8, Wout, Wot, 4)


def build_program(stop_after=None, dbg_block=None, nblocks0=NT, skip_l0=False):
    nc = bass.Bass("TRN2", target_bir_lowering=False)
    P = Prog(nc)
    C = Ctx()
    I = {}

    def inp(name, shape, dt=F32):
        I[name] = nc.dram_tensor(name, list(shape), dt, kind="ExternalInput").ap()
        return I[name]

    x_in = inp("x", [S, D])
    mem_in = inp("mem", [256, D])
    pos_in = inp("pos", [128, NT], I32)
    gains = inp("gains", [7, D])
    C.freqs_in = inp("freqs", [128, 48])
    w_in0 = inp("w_in0", [D, SPEC0["ncols"]])
    w_in1 = inp("w_in1", [D, SPEC1["ncols"]])
    w_mkv = [inp("w_mkv%d" % l, [D, 512]) for l in range(2)]
    w_out0 = inp("w_out0", [1024, D])
    w_out1 = inp("w_out1", [512, D])
    w_gu = [inp("w_gu%d" % l, [D, 2 * DFF]) for l in range(2)]
    w_dn = [inp("w_dn%d" % l, [DFF, D]) for l in range(2)]
    out = nc.dram_tensor("out", [S, D], F32, kind="ExternalOutput").ap()
    C.dbg = None
    C.dbg_block = dbg_block
    if dbg_block is not None:
        C.dbg = dict(score=nc.dram_tensor("dbg_score", [128, S], F32, kind="ExternalOutput").ap(),
                     thr=nc.dram_tensor("dbg_thr", [128, 1], F32, kind="ExternalOutput").ap(),
                     Bm=nc.dram_tensor("dbg_Bm", [128, S], BF16, kind="ExternalOutput").ap())
    fT0 = P.dram("fT0", [15, 128, S], BF16)
    vS0 = P.dram("vS0", [S, 256], BF16)
    fT1 = P.dram("fT1", [14, 128, S], BF16)
    vS1 = P.dram("vS1", [S, 768], BF16)
    Og = [P.dram("Og%d" % g, [S, 260], F32) for g in range(3)]
    xa = P.dram("xa", [S, D], F32)
    xb = P.dram("xb", [S, D], F32)
    xc = P.dram("xc", [S, D], F32)

    C.banks = [P.ps("bank%d" % k, [128, 512], F32) for k in range(8)]
    setup_consts(P, C, pos_in)
    HF = NFC // 2

    def final(src):
        print("n_inst before final", P.n_inst)
        P.max_ops = None
        with P.scope():
            t = [P.sb("fin%d" % r, [128, D], F32) for r in range(2)]
            for i in range(NT):
                P.dma("sync", t[i % 2].ap(), src[i * 128:(i + 1) * 128, :], writes=[t[i % 2]])
                P.dma("sync", out[i * 128:(i + 1) * 128, :], t[i % 2].ap(), reads=[t[i % 2]], out=True)
        P.finish()
        return nc

    with (P.scope() if not skip_l0 else contextlib.nullcontext()):
      if not skip_l0:
        wiAll = P.sb("wiAll", [128, NT, 8], F32)
        kmT = P.sb("kmT0", [128, 2, 256], BF16)
        Vm = P.sb("Vm0", [128, 2, 4, VW], BF16)
        phase_M(P, C, "m0", mem_in, w_mkv[0], gains, 1, kmT, Vm)
        if stop_after == "M":
            d1 = nc.dram_tensor("dbg_kmT", [128, 2, 256], BF16, kind="ExternalOutput").ap()
            d2 = nc.dram_tensor("dbg_Vm", [128, 2, 4, VW], BF16, kind="ExternalOutput").ap()
            P.dma("sync", d1, kmT.ap(), reads=[kmT])
            P.dma("sync", d2, Vm.ap(), reads=[Vm])
            return final(x_in)
        phase_A(P, C, "a0", x_in, w_in0, gains, 0, SPEC0, fT0, vS0, wiAll, pos_in)
        if stop_after == "A":
            d1 = nc.dram_tensor("dbg_fT0", [15, 128, S], BF16, kind="ExternalOutput").ap()
            d2 = nc.dram_tensor("dbg_vS0", [S, 256], BF16, kind="ExternalOutput").ap()
            d3 = nc.dram_tensor("dbg_wi", [128, NT, 8], F32, kind="ExternalOutput").ap()
            with P.scope():
                tb = P.sb("dbgt", [128, S], BF16)
                for c in range(15):
                    P.dma("sync", tb.ap(), fT0[c], writes=[tb])
                    P.dma("sync", d1[c], tb.ap(), reads=[tb])
                for c in range(2):
                    P.dma("sync", tb[:, 0:2048].rearrange("p (a b) -> p a b", b=256), vS0[c * 2048:(c + 1) * 2048, :].rearrange("(a p) b -> p a b", p=128), writes=[tb])
                    P.dma("sync", d2[c * 2048:(c + 1) * 2048, :].rearrange("(a p) b -> p a b", p=128), tb[:, 0:2048].rearrange("p (a b) -> p a b", b=256), reads=[tb])
                P.dma("sync", d3, wiAll.ap(), reads=[wiAll])
            return final(x_in)
        phase_B0(P, C, x_in, xa, fT0, vS0, wiAll, kmT, Vm, w_out0, nblocks=nblocks0)
    if stop_after == "B0":
        return final(xa)
    if not skip_l0:
        phase_F(P, C, "f0a", xa, xa, xb, w_gu[0], w_dn[0], gains, 2, 0, HF)
        phase_F(P, C, "f0b", xa, xb, xc, w_gu[0], w_dn[0], gains, 2, HF, NFC)
    else:
        xc = x_in
    if stop_after == "F0":
        return final(xc)
    with P.scope():
        kmT = P.sb("kmT1", [128, 2, 256], BF16)
        Vm = P.sb("Vm1", [128, 2, 4, VW], BF16)
        phase_M(P, C, "m1", mem_in, w_mkv[1], gains, 4, kmT, Vm)
        phase_A(P, C, "a1", xc, w_in1, gains, 3, SPEC1, fT1, vS1, None, pos_in)
        phase_B1(P, C, fT1, vS1, Og)
        phase_B2(P, C, xc, xa, fT1, Og, kmT, Vm, w_out1)
    if stop_after == "B2":
        return final(xa)
    phase_F(P, C, "f1a", xa, xa, xb, w_gu[1], w_dn[1], gains, 5, 0, HF)
    phase_F(P, C, "f1b", xa, xb, out, w_gu[1], w_dn[1], gains, 5, HF, NFC, final_row=6)
    P.finish()
    return nc


def prep_inputs(inputs):
    f = lambda a: np.ascontiguousarray(np.asarray(a, dtype=np.float32))
    w0 = f(inputs["l0_w_in"])
    q, k, v, qi, ki, wi, qm = np.split(w0, np.cumsum([768, 256, 256, 512, 64, 8])[:], axis=1)
    qp = np.concatenate([q[:, h * 64:(h + 1) * 64] for h in QPERM], axis=1)
    w_in0 = np.ascontiguousarray(np.concatenate([qp, k, qi, ki, ki, wi, v, qm], axis=1))
    w1 = f(inputs["l1_w_in"])
    parts = [w1[:, j * 256:(j + 1) * 256] for j in range(10)]
    w_in1 = np.ascontiguousarray(np.concatenate([parts[0], parts[1], parts[3], parts[4], parts[6], parts[7], parts[2], parts[5], parts[8], parts[9]], axis=1))
    gains = np.ascontiguousarray(np.stack([f(inputs[n]) for n in ("l0_norm_mix", "l0_norm_mem", "l0_norm_ffn", "l1_norm_mix", "l1_norm_mem",
                                                                 "l1_norm_ffn", "final_norm")], axis=0))
    fr64 = (np.float32(10000.0) ** (-np.arange(32, dtype=np.float32) / np.float32(32))).astype(np.float32)
    fr16 = (np.float32(10000.0) ** (-np.arange(16, dtype=np.float32) / np.float32(16))).astype(np.float32)
    freqs = np.ascontiguousarray(np.broadcast_to(np.concatenate([fr64, fr16])[None, :], (128, 48)).astype(np.float32))
    shared = dict(freqs=freqs, gains=gains, w_in0=w_in0, w_in1=w_in1, w_mkv0=f(inputs["l0_w_mem_kv"]), w_mkv1=f(inputs["l1_w_mem_kv"]),
                  w_out0=f(inputs["l0_w_out"]), w_out1=f(inputs["l1_w_out"]), w_gu0=f(inputs["l0_w_gate_up"]), w_gu1=f(inputs["l1_w_gate_up"]),
                  w_dn0=f(inputs["l0_w_down"]), w_dn1=f(inputs["l1_w_down"]))
    x = f(inputs["x"])
    mem = f(inputs["mem"])
    pos = np.ascontiguousarray(np.asarray(inputs["positions"], dtype=np.int32))
    maps = []
    for c in range(x.shape[0]):
        m = dict(shared)
        m["x"] = x[c]
        m["mem"] = mem[c]
        m["pos"] = np.ascontiguousarray(pos[c].reshape(NT, 128).T)
        maps.append(m)
    return maps


_NC_CACHE = {}


def kernel(**inputs):
    maps = prep_inputs(inputs)
    if "nc" not in _NC_CACHE:
        _NC_CACHE["nc"] = build_program()
    nc = _NC_CACHE["nc"]
    res = run_bass_kernel_spmd(nc, maps, core_ids=list(range(len(maps))))
    return np.stack([np.asarray(r["out"], dtype=np.float32) for r in res.results], axis=0)
```

```python
import contextlib
import numpy as np
import ml_dtypes
import concourse.bass as bass
import concourse.mybir as mybir
from concourse.bass_utils import run_bass_kernel_spmd

F32 = mybir.dt.float32
BF16 = mybir.dt.bfloat16
I32 = mybir.dt.int32
AF = mybir.ActivationFunctionType
ALU = mybir.AluOpType
AX = mybir.AxisListType

SAME_ENGINE_SYNC = True


class Tok:
    def __init__(self, name):
        self.name = name
        self.w = None
        self.r = {}


class Buf(Tok):
    def __init__(self, name, handle):
        super().__init__(name)
        self.h = handle

    def ap(self):
        return self.h[:]

    def __getitem__(self, idx):
        return self.h[idx]


class _Eng:
    def __init__(self, name, handle, sem):
        self.name = name
        self.h = handle
        self.sem = sem
        self.count = 0
        self.seen = {}


class Prog:
    def __init__(self, nc, n_dma_sems=8):
        self.nc = nc
        self.stack = contextlib.ExitStack()
        self.engs = {}
        for name in ("tensor", "vector", "scalar", "gpsimd", "sync"):
            sem = self.stack.enter_context(nc.semaphore("s_" + name))
            self.engs[name] = _Eng(name, getattr(nc, name), sem)
        self.dma_sems = {}
        for q in ("sync", "gpsimd", "scalar"):
            self.dma_sems[q] = [[self.stack.enter_context(nc.semaphore("d_%s%d" % (q, i))), 0] for i in range(n_dma_sems)]
        self.dma_rr = {"sync": 0, "gpsimd": 0, "scalar": 0}
        self.out_events = []
        self.n_inst = 0
        self.scopes = [self.stack]
        import os
        self.max_ops = int(os.environ["KMAXOPS"]) if "KMAXOPS" in os.environ else None

    @contextlib.contextmanager
    def scope(self):
        st = contextlib.ExitStack()
        self.scopes.append(st)
        try:
            yield
        finally:
            self.barrier()
            self.scopes.pop()
            st.close()

    def barrier(self):
        if getattr(self, "finished", False):
            return
        for eng in self.engs.values():
            for other in self.engs.values():
                if other is not eng and other.count > 0:
                    self._wait(eng, (other.name, other.sem, other.count))
            for q, pool in self.dma_sems.items():
                for i, slot in enumerate(pool):
                    if slot[1] > 0:
                        self._wait(eng, ("d_%s%d" % (q, i), slot[0], 16 * slot[1]))

    def tok(self, name):
        return Tok(name)

    def sb(self, name, shape, dtype):
        self.uid = getattr(self, "uid", 0) + 1
        name = "%s_u%d" % (name, self.uid)
        return Buf(name, self.scopes[-1].enter_context(self.nc.sbuf_tensor(name, list(shape), dtype)))

    def ps(self, name, shape, dtype):
        b = Buf(name, self.scopes[-1].enter_context(self.nc.psum_tensor(name, list(shape), dtype)))
        b.excl = True
        return b

    def dram(self, name, shape, dtype):
        return self.nc.dram_tensor(name, list(shape), dtype, kind="Internal").ap()

    def _wait(self, eng, ev):
        key, sem, val = ev
        if eng.seen.get(key, 0) >= val:
            return
        if key == eng.name and not (SAME_ENGINE_SYNC and eng.name != "tensor" and eng.name != "sync"):
            return
        eng.h.wait_ge(sem, val)
        eng.seen[key] = val

    def _deps(self, eng, reads, writes):
        for t in reads:
            if t.w is not None:
                self._wait(eng, t.w)
        for t in writes:
            if t.w is not None:
                self._wait(eng, t.w)
            for ev in t.r.values():
                self._wait(eng, ev)

    def _record(self, ev, reads, writes):
        for t in writes:
            t.w = ev
            t.r = {}
        for t in reads:
            if t in writes:
                continue
            t.r[ev[0]] = ev

    def op(self, engname, fn, reads=(), writes=()):
        if self.max_ops is not None and self.n_inst >= self.max_ops:
            return None
        eng = self.engs[engname]
        ex = [t for t in reads if getattr(t, "excl", False) and t not in writes]
        if ex:
            reads = [t for t in reads if t not in ex]
            writes = list(writes) + ex
        self._deps(eng, reads, writes)
        inst = fn(eng.h)
        eng.count += 1
        inst.then_inc(eng.sem, 1)
        ev = (eng.name, eng.sem, eng.count)
        self._record(ev, reads, writes)
        self.n_inst += 1
        return ev

    def dma(self, q, out_ap, in_ap, reads=(), writes=(), out=False, **kw):
        if self.max_ops is not None and self.n_inst >= self.max_ops:
            return None
        eng = self.engs[q]
        self._deps(eng, reads, writes)
        pool = self.dma_sems[q]
        i = self.dma_rr[q]
        self.dma_rr[q] = (i + 1) % len(pool)
        slot = pool[i]
        key = "d_%s%d" % (q, i)
        if slot[1] > 0:
            self._wait(eng, (key, slot[0], 16 * slot[1]))
        eng.h.dma_start(out=out_ap, in_=in_ap, **kw).then_inc(slot[0], 16)
        slot[1] += 1
        ev = (key, slot[0], 16 * slot[1])
        self._record(ev, reads, writes)
        if out:
            self.out_events.append(ev)
        self.n_inst += 1
        return ev

    def finish(self):
        eng = self.engs["sync"]
        for q, pool in self.dma_sems.items():
            for i, slot in enumerate(pool):
                if slot[1] > 0:
                    self._wait(eng, ("d_%s%d" % (q, i), slot[0], 16 * slot[1]))
        for name, e in self.engs.items():
            if name != "sync" and e.count > 0:
                self._wait(eng, (name, e.sem, e.count))
        self.finished = True
        for st in reversed(self.scopes):
            st.close()

    def make_identity(self, idt):
        tmp = self.sb(idt.name + "_i", [128, 128], I32)
        self.op("gpsimd", lambda e: e.iota(tmp.ap(), pattern=[[1, 128]], base=0, channel_multiplier=-1), writes=[tmp])
        self.op("vector", lambda e: e.tensor_scalar(out=idt.ap(), in0=tmp.ap(), scalar1=0.0, scalar2=None, op0=ALU.is_equal),
                reads=[tmp], writes=[idt])


S = 4096
D = 1024
NT = 32
DFF = 2816
NFC = 22
EPS = 1e-6
NEG = -1.0e30
MASKV = -30000.0
VW = 66
N_BISECT = 16
IDX_SCALE = (8 ** -0.5) * (64 ** -0.5)
QPERM = [0, 3, 1, 4, 2, 5, 6, 9, 7, 10, 8, 11]

SPEC0 = dict(
    ncols=2184,
    chunks=[(0, 512, [("rope64", 0, 8)]), (512, 512, [("rope64", 0, 8)]), (1024, 512, [("rope32", 0, 8)]),
            (1536, 136, [("rope32", 0, 2), ("wi", 128, 8)]), (1672, 512, [("copy", 0, 512)])],
    tcols=[128 * k for k in range(13)] + [1928, 2056],
    vcols=(1672, 1928),
)
SPEC1 = dict(
    ncols=2560,
    chunks=[(0, 512, [("rope64", 0, 8)]), (512, 512, [("rope64", 0, 8)]), (1024, 512, [("rope64", 0, 8)]),
            (1536, 512, [("copy", 0, 512)]), (2048, 512, [("copy", 0, 512)])],
    tcols=[128 * k for k in range(12)] + [2304, 2432],
    vcols=(1536, 2304),
)


class Ctx:
    pass


def load_weight(P, name, dram, nk, ncols, q="gpsimd"):
    W = P.sb(name, [128, nk, ncols], BF16)
    toks = [P.tok("%s_%d" % (name, k)) for k in range(nk)]
    for k in range(nk):
        c0 = 0
        while c0 < ncols:
            c1 = min(ncols, c0 + 2048)
            P.dma(q, W[:, k, c0:c1], dram[k * 128:(k + 1) * 128, c0:c1], writes=[toks[k]])
            c0 = c1
    return W, toks


def setup_consts(P, C, pos_in):
    C.ident = P.sb("ident", [128, 128], BF16)
    P.make_identity(C.ident)
    C.negthr = P.sb("negthr", [128, 1], F32)
    P.op("vector", lambda e: e.memset(C.negthr.ap(), -1.0e29), writes=[C.negthr])
    C.pow2 = P.sb("pow2", [128, N_BISECT + 2], F32)
    for k in range(N_BISECT + 2):
        P.op("gpsimd", lambda e: e.memset(C.pow2[:, k:k + 1], 2.0 ** (1 - k)), writes=[C.pow2])
    C.MdT = P.sb("MdT", [128, 128], BF16)
    C.MpT = P.sb("MpT", [128, 128], BF16)
    zt = P.sb("zt", [128, 128], F32)
    zm = P.sb("zm", [128, 128], F32)
    P.op("vector", lambda e: e.memset(zt.ap(), 0.0), writes=[zt])
    P.op("gpsimd", lambda e: e.affine_select(out=zm.ap(), in_=zt.ap(), pattern=[[1, 128]], compare_op=ALU.is_ge, fill=MASKV, base=0, channel_multiplier=-1),
         reads=[zt], writes=[zm])
    P.op("vector", lambda e: e.tensor_copy(C.MdT.ap(), zm.ap()), reads=[zm], writes=[C.MdT])
    P.op("gpsimd", lambda e: e.affine_select(out=zm.ap(), in_=zt.ap(), pattern=[[-1, 128]], compare_op=ALU.is_ge, fill=MASKV, base=0, channel_multiplier=1),
         reads=[zt, C.MdT], writes=[zm])
    P.op("vector", lambda e: e.tensor_copy(C.MpT.ap(), zm.ap()), reads=[zm], writes=[C.MpT])


def build_rope_tables(P, C, pos_in):
    C.cos64 = P.sb("cos64", [128, NT, 32], F32)
    C.sin64 = P.sb("sin64", [128, NT, 32], F32)
    C.nsin64 = P.sb("nsin64", [128, NT, 32], F32)
    C.cos16 = P.sb("cos16", [128, NT, 16], F32)
    C.sin16 = P.sb("sin16", [128, NT, 16], F32)
    C.nsin16 = P.sb("nsin16", [128, NT, 16], F32)
    with P.scope():
        posi = P.sb("posi", [128, NT], I32)
        posf = P.sb("posf", [128, NT], F32)
        P.dma("sync", posi.ap(), pos_in, writes=[posi])
        P.op("vector", lambda e: e.tensor_copy(posf.ap(), posi.ap()), reads=[posi], writes=[posf])
        for half, cosT, sinT, nsinT in ((32, C.cos64, C.sin64, C.nsin64), (16, C.cos16, C.sin16, C.nsin16)):
            n = NT * half
            fr = P.sb("fr%d" % half, [128, half], F32)
            a = P.sb("a%d" % half, [128, NT, half], F32)
            ki = P.sb("ki%d" % half, [128, NT, half], I32)
            kf = P.sb("kf%d" % half, [128, NT, half], F32)
            fr1 = P.sb("fr1%d" % half, [128, NT, half], F32)
            m1 = P.sb("m1%d" % half, [128, NT, half], F32)
            f0 = 0 if half == 32 else 32
            P.dma("sync", fr.ap(), C.freqs_in[:, f0:f0 + half], writes=[fr])
            P.op("vector", lambda e: e.tensor_tensor(out=a.ap(), in0=posf.ap().unsqueeze(2).broadcast_to([128, NT, half]),
                                                     in1=fr.ap().unsqueeze(1).broadcast_to([128, NT, half]), op=ALU.mult),
                 reads=[posf, fr], writes=[a])
            P.op("vector", lambda e: e.tensor_scalar(out=a.ap(), in0=a.ap(), scalar1=float(1.0 / (2.0 * np.pi)), scalar2=None, op0=ALU.mult),
                 reads=[a], writes=[a])
            for shift, outT, neg in ((0.0, sinT, False), (0.25, cosT, False), (0.5, nsinT, False)):
                src = a
                if shift != 0.0:
                    P.op("vector", lambda e: e.tensor_scalar(out=fr1.ap(), in0=a.ap(), scalar1=shift, scalar2=None, op0=ALU.add),
                         reads=[a], writes=[fr1])
                    src = fr1
                P.op("vector", lambda e: e.tensor_copy(ki.ap(), src.ap()), reads=[src], writes=[ki])
                P.op("vector", lambda e: e.tensor_copy(kf.ap(), ki.ap()), reads=[ki], writes=[kf])
                P.op("vector", lambda e: e.tensor_tensor(out=kf.ap(), in0=src.ap(), in1=kf.ap(), op=ALU.subtract), reads=[src, kf], writes=[kf])
                P.op("vector", lambda e: e.tensor_scalar(out=m1.ap(), in0=kf.ap(), scalar1=0.5, scalar2=None, op0=ALU.is_gt), reads=[kf], writes=[m1])
                P.op("vector", lambda e: e.tensor_tensor(out=kf.ap(), in0=kf.ap(), in1=m1.ap(), op=ALU.subtract), reads=[kf, m1], writes=[kf])
                P.op("vector", lambda e: e.tensor_scalar(out=m1.ap(), in0=kf.ap(), scalar1=-0.5, scalar2=None, op0=ALU.is_lt), reads=[kf], writes=[m1])
                P.op("vector", lambda e: e.tensor_tensor(out=kf.ap(), in0=kf.ap(), in1=m1.ap(), op=ALU.add), reads=[kf, m1], writes=[kf])
                P.op("scalar", lambda e: e.activation(out=outT.ap(), in_=kf.ap(), func=AF.Sin, scale=float(2.0 * np.pi) * (1.0 - 1e-6)),
                     reads=[kf], writes=[outT])


def alloc_norm_work(P, C, tag):
    W = Ctx()
    W.sqj = P.sb(tag + "sqj", [128, D], BF16)
    W.small = [[P.sb("%ssm%d_%d" % (tag, r, j), [128, 1], F32) for j in range(4)] for r in range(2)]
    W.hb = [P.sb("%shb%d" % (tag, r), [128, D], BF16) for r in range(2)]
    W.k = 0
    return W


def rmsnorm_to_hT(P, C, W, xt, gb, hT_ap, hT_tok, bank, evac_eng="scalar"):
    r = W.k % 2
    W.k += 1
    ssq, t1, t2, rstd = W.small[r]
    hb = W.hb[r]
    P.op("scalar", lambda e: e.activation(out=W.sqj.ap(), in_=xt.ap(), func=AF.Square, accum_out=ssq.ap()), reads=[xt], writes=[W.sqj, ssq])
    P.op("vector", lambda e: e.tensor_scalar(out=t1.ap(), in0=ssq.ap(), scalar1=1.0 / D, scalar2=EPS, op0=ALU.mult, op1=ALU.add), reads=[ssq], writes=[t1])
    P.op("scalar", lambda e: e.activation(out=t2.ap(), in_=t1.ap(), func=AF.Sqrt), reads=[t1], writes=[t2])
    P.op("vector", lambda e: e.reciprocal(out=rstd.ap(), in_=t2.ap()), reads=[t2], writes=[rstd])
    P.op("vector", lambda e: e.scalar_tensor_tensor(out=hb.ap(), in0=xt.ap(), scalar=rstd.ap(), in1=gb.ap(), op0=ALU.mult, op1=ALU.mult),
         reads=[xt, rstd, gb], writes=[hb])
    if hT_ap is None:
        return hb
    return norm_p2(P, C, hb, hT_ap, hT_tok, bank, evac_eng)


def norm_p2(P, C, hb, hT_ap, hT_tok, bank, evac_eng="scalar"):
    bbf = bank.ap().bitcast(BF16)
    for c in range(8):
        P.op("tensor", lambda e: e.transpose(bbf[:, c * 128:(c + 1) * 128], hb[:, c * 128:(c + 1) * 128], C.ident.ap()),
             reads=[hb, C.ident], writes=[bank])
    srcv = bbf if len(hT_ap.shape) == 2 else bbf.rearrange("p (c t) -> p c t", t=128)
    if evac_eng == "scalar":
        P.op("scalar", lambda e: e.activation(out=hT_ap, in_=srcv, func=AF.Copy), reads=[bank], writes=[hT_tok])
    else:
        P.op("vector", lambda e: e.tensor_copy(hT_ap, srcv), reads=[bank], writes=[hT_tok])


def load_gain(P, C, name, gains, row):
    gb = P.sb(name, [128, D], F32)
    P.dma("sync", gb.ap(), gains[row].partition_broadcast(128), writes=[gb])
    return gb


def phase_A(P, C, tag, xsrc, w_dram, gains, grow, spec, fT, vS, wiAll, pos_in):
    ncols = spec["ncols"]
    with P.scope():
        build_rope_tables(P, C, pos_in)
        Win, Wt = load_weight(P, tag + "Win", w_dram, 8, ncols)
        gb = load_gain(P, C, tag + "gbA", gains, grow)
        NW = alloc_norm_work(P, C, tag + "A")
        xts = [P.sb("%sxt%d" % (tag, r), [128, D], F32) for r in range(2)]
        hTs = [P.sb("%shT%d" % (tag, r), [128, D], BF16) for r in range(2)]
        pts = [P.sb("%spt%d" % (tag, r), [128, ncols], BF16) for r in range(2)]
        t1s = [P.sb("%st1_%d" % (tag, r), [128, 512], F32) for r in range(2)]
        t2s = [P.sb("%st2_%d" % (tag, r), [128, 512], F32) for r in range(2)]
        ntc = len(spec["tcols"])
        fTs = [P.sb("%sfTs%d" % (tag, r), [128, ntc, 128], BF16) for r in range(2)]
        fT_r = fT.rearrange("c p t -> p c t")
        cc = 0
        hbs = {}

        def n1(i):
            xt = xts[i % 2]
            P.dma("sync", xt.ap(), xsrc[i * 128:(i + 1) * 128, :], writes=[xt])
            hbs[i] = rmsnorm_to_hT(P, C, NW, xt, gb, None, None, None)

        def n2(i):
            norm_p2(P, C, hbs.pop(i), hTs[i % 2].ap(), hTs[i % 2], C.banks[0], evac_eng="scalar")

        def featT(i):
            _featT_body(P, C, spec, pts, fTs, fT_r, vS, ntc, i)

        n1(0)
        n2(0)
        for i in range(NT):
            hT = hTs[i % 2]
            pt = pts[i % 2]
            if i + 1 < NT:
                n1(i + 1)
            for (col0, width, handlers) in spec["chunks"]:
                bank = C.banks[1 + (cc % 4)]
                t1 = t1s[cc % 2]
                t2 = t2s[cc % 2]
                cc += 1
                for k in range(8):
                    P.op("tensor", lambda e: e.matmul(bank[:, 0:width], lhsT=hT[:, k * 128:(k + 1) * 128], rhs=Win[:, k, col0:col0 + width],
                                                      start=(k == 0), stop=(k == 7)), reads=[hT, Wt[k]], writes=[bank])
                for (kind, l0, n) in handlers:
                    if kind == "rope64":
                        nh = n
                        w = nh * 64
                        xv2 = bank[:, l0:l0 + w].rearrange("p (h d) -> p h d", d=32)
                        xv = bank[:, l0:l0 + w].rearrange("p (h d) -> p h d", d=64)
                        t1v2 = t1[:, 0:w].rearrange("p (h d) -> p h d", d=32)
                        t2v = t2[:, 0:w].rearrange("p (h d) -> p h d", d=64)
                        cosb = C.cos64[:, i:i + 1, :].broadcast_to([128, 2 * nh, 32])
                        sinb = C.sin64[:, i:i + 1, :].broadcast_to([128, nh, 32])
                        nsinb = C.nsin64[:, i:i + 1, :].broadcast_to([128, nh, 32])
                        P.op("vector", lambda e: e.tensor_tensor(out=t1v2, in0=xv2, in1=cosb, op=ALU.mult), reads=[bank, C.cos64], writes=[t1])
                        P.op("vector", lambda e: e.tensor_tensor(out=t2v[:, :, 0:32], in0=xv[:, :, 32:64], in1=nsinb, op=ALU.mult),
                             reads=[bank, C.nsin64], writes=[t2])
                        P.op("vector", lambda e: e.tensor_tensor(out=t2v[:, :, 32:64], in0=xv[:, :, 0:32], in1=sinb, op=ALU.mult),
                             reads=[bank, C.sin64], writes=[t2])
                        P.op("gpsimd", lambda e: e.tensor_tensor(out=pt[:, col0 + l0:col0 + l0 + w], in0=t1[:, 0:w], in1=t2[:, 0:w], op=ALU.add),
                             reads=[t1, t2], writes=[pt])
                    elif kind == "rope32":
                        nh = n
                        w = nh * 64
                        xv = bank[:, l0:l0 + w].rearrange("p (h d) -> p h d", d=64)
                        xr4 = bank[:, l0:l0 + w].rearrange("p (h t d) -> p h t d", t=4, d=16)
                        t1r4 = t1[:, 0:w].rearrange("p (h t d) -> p h t d", t=4, d=16)
                        t2v = t2[:, 0:w].rearrange("p (h d) -> p h d", d=64)
                        t1v = t1[:, 0:w].rearrange("p (h d) -> p h d", d=64)
                        ptv = pt[:, col0 + l0:col0 + l0 + w].rearrange("p (h d) -> p h d", d=64)
                        cosb = C.cos16[:, i:i + 1, :].unsqueeze(1).broadcast_to([128, nh, 2, 16])
                        sinb = C.sin16[:, i:i + 1, :].broadcast_to([128, nh, 16])
                        nsinb = C.nsin16[:, i:i + 1, :].broadcast_to([128, nh, 16])
                        P.op("vector", lambda e: e.tensor_tensor(out=t1r4[:, :, 0:2, :], in0=xr4[:, :, 0:2, :], in1=cosb, op=ALU.mult),
                             reads=[bank, C.cos16], writes=[t1])
                        P.op("vector", lambda e: e.tensor_tensor(out=t2v[:, :, 0:16], in0=xv[:, :, 16:32], in1=nsinb, op=ALU.mult),
                             reads=[bank, C.nsin16], writes=[t2])
                        P.op("vector", lambda e: e.tensor_tensor(out=t2v[:, :, 16:32], in0=xv[:, :, 0:16], in1=sinb, op=ALU.mult),
                             reads=[bank, C.sin16], writes=[t2])
                        P.op("gpsimd", lambda e: e.tensor_tensor(out=ptv[:, :, 0:32], in0=t1v[:, :, 0:32], in1=t2v[:, :, 0:32], op=ALU.add),
                             reads=[t1, t2], writes=[pt])
                        P.op("scalar", lambda e: e.activation(out=ptv[:, :, 32:64], in_=xv[:, :, 32:64], func=AF.Copy), reads=[bank], writes=[pt])
                    elif kind == "copy":
                        P.op("scalar", lambda e: e.activation(out=pt[:, col0 + l0:col0 + l0 + n], in_=bank[:, l0:l0 + n], func=AF.Copy),
                             reads=[bank], writes=[pt])
                    elif kind == "wi":
                        P.op("scalar", lambda e: e.activation(out=wiAll[:, i, :], in_=bank[:, l0:l0 + n], func=AF.Copy, scale=float(IDX_SCALE)),
                             reads=[bank], writes=[wiAll])
            if i + 1 < NT:
                n2(i + 1)
            if i >= 1:
                featT(i - 1)
        featT(NT - 1)


def _featT_body(P, C, spec, pts, fTs, fT_r, vS, ntc, i):
            pt = pts[i % 2]
            fts = fTs[i % 2]
            for g0 in range(0, ntc, 8):
                g1 = min(ntc, g0 + 8)
                bank = C.banks[5 + (g0 // 8)]
                bbf = bank.ap().bitcast(BF16)
                for k in range(g0, g1):
                    c0 = spec["tcols"][k]
                    P.op("tensor", lambda e: e.transpose(bbf[:, (k - g0) * 128:(k - g0 + 1) * 128], pt[:, c0:c0 + 128], C.ident.ap()),
                         reads=[pt, C.ident], writes=[bank])
                eng = "vector" if g0 == 0 else "scalar"
                if eng == "vector":
                    P.op("vector", lambda e: e.tensor_copy(fts[:, g0:g1, :], bbf[:, 0:(g1 - g0) * 128].rearrange("p (c t) -> p c t", t=128)),
                         reads=[bank], writes=[fts])
                else:
                    P.op("scalar", lambda e: e.activation(out=fts[:, g0:g1, :], in_=bbf[:, 0:(g1 - g0) * 128].rearrange("p (c t) -> p c t", t=128),
                                                          func=AF.Copy), reads=[bank], writes=[fts])
            P.dma("sync", fT_r[:, :, i * 128:(i + 1) * 128], fts.ap(), reads=[fts])
            v0, v1 = spec["vcols"]
            P.dma("sync", vS[i * 128:(i + 1) * 128, :], pt[:, v0:v1], reads=[pt])


def phase_M(P, C, tag, mem_in, w_dram, gains, grow, kmT, Vm):
    with P.scope():
        Wm, Wt = load_weight(P, tag + "Wm", w_dram, 8, 512)
        gb = load_gain(P, C, tag + "gbM", gains, grow)
        NW = alloc_norm_work(P, C, tag + "M")
        xts = [P.sb("%smx%d" % (tag, r), [128, D], F32) for r in range(2)]
        hTs = [P.sb("%smhT%d" % (tag, r), [128, D], BF16) for r in range(2)]
        kb16 = [P.sb("%skb16_%d" % (tag, r), [128, 256], BF16) for r in range(2)]
        P.op("gpsimd", lambda e: e.memset(Vm.ap(), 1.0), writes=[Vm])
        for mb in range(2):
            xt, hT = xts[mb], hTs[mb]
            P.dma("sync", xt.ap(), mem_in[mb * 128:(mb + 1) * 128, :], writes=[xt])
            rmsnorm_to_hT(P, C, NW, xt, gb, hT.ap(), hT, C.banks[0])
            bank = C.banks[1 + mb]
            for k in range(8):
                P.op("tensor", lambda e: e.matmul(bank.ap(), lhsT=hT[:, k * 128:(k + 1) * 128], rhs=Wm[:, k, :], start=(k == 0), stop=(k == 7)),
                     reads=[hT, Wt[k]], writes=[bank])
            P.op("scalar", lambda e: e.activation(out=kb16[mb].ap(), in_=bank[:, 0:256], func=AF.Copy), reads=[bank], writes=[kb16[mb]])
            P.op("vector", lambda e: e.tensor_copy(Vm[:, mb, :, 0:64], bank[:, 256:512].rearrange("p (h d) -> p h d", d=64)),
                 reads=[bank], writes=[Vm])
            tb = C.banks[3 + mb]
            tbf = tb.ap().bitcast(BF16)
            for c in range(2):
                P.op("tensor", lambda e: e.transpose(tbf[:, c * 128:(c + 1) * 128], kb16[mb][:, c * 128:(c + 1) * 128], C.ident.ap()),
                     reads=[kb16[mb], C.ident], writes=[tb])
            P.op("vector", lambda e: e.tensor_copy(kmT[:, :, mb * 128:(mb + 1) * 128], tbf[:, 0:256].rearrange("p (c t) -> p c t", t=128)),
                 reads=[tb], writes=[kmT])


def attn_finish(P, C, W, xsrc, xdst, i, nheads, Wout, Wot, nkc):
    r = i % 2
    attn = W.attn[r]
    rec = W.rec[r]
    xt = W.xts[r]
    P.dma("sync", xt.ap(), xsrc[i * 128:(i + 1) * 128, :], writes=[xt])
    h0 = 0
    for (bank, oap, nh) in W.osrc(r):
        ov = oap.rearrange("p (h d) -> p h d", d=65)
        P.op("vector", lambda e: e.reciprocal(out=rec[:, h0:h0 + nh], in_=ov[:, :, 64]), reads=[bank], writes=[rec])
        P.op("vector", lambda e: e.tensor_tensor(out=attn[:, h0 * 64:(h0 + nh) * 64].rearrange("p (h d) -> p h d", d=64), in0=ov[:, :, 0:64],
                                                 in1=rec[:, h0:h0 + nh].unsqueeze(2).broadcast_to([128, nh, 64]), op=ALU.mult),
             reads=[bank, rec], writes=[attn])
        h0 += nh
    tb = W.tbank
    tbf = tb.ap().bitcast(BF16)
    aT = W.attnT[r]
    for c in range(nkc):
        P.op("tensor", lambda e: e.transpose(tbf[:, c * 128:(c + 1) * 128], attn[:, c * 128:(c + 1) * 128], C.ident.ap()),
             reads=[attn, C.ident], writes=[tb])
    P.op("scalar", lambda e: e.activation(out=aT[:, 0:nkc * 128], in_=tbf[:, 0:nkc * 128], func=AF.Copy), reads=[tb], writes=[aT])
    xn = W.xn[r]
    for half in range(2):
        yb = W.ybanks[half]
        for c in range(nkc):
            P.op("tensor", lambda e: e.matmul(yb.ap(), lhsT=aT[:, c * 128:(c + 1) * 128], rhs=Wout[:, c, half * 512:(half + 1) * 512],
                                              start=(c == 0), stop=(c == nkc - 1)), reads=[aT, Wot[c]], writes=[yb])
        P.op("vector", lambda e: e.tensor_tensor(out=xn[:, half * 512:(half + 1) * 512], in0=yb.ap(), in1=xt[:, half * 512:(half + 1) * 512], op=ALU.add),
             reads=[yb, xt], writes=[xn])
    P.dma("sync", xdst[i * 128:(i + 1) * 128, :], xn.ap(), reads=[xn])


def mem_heads(P, C, W, qall, qch0, kmT, Vm, obank, sbank, r):
    PT = W.PTm[r]
    for half in range(2):
        sb_ = sbank[half]
        for hh in range(2):
            hm = half * 2 + hh
            for mb in range(2):
                j = hh * 2 + mb
                P.op("tensor", lambda e: e.matmul(sb_[:, j * 128:(j + 1) * 128], lhsT=kmT[:, hm // 2, mb * 128:(mb + 1) * 128],
                                                  rhs=qall[:, qch0 + hm, :], start=True, stop=True),
                     reads=[kmT, qall], writes=[sb_])
        P.op("scalar", lambda e: e.activation(out=PT[:, half * 512:(half + 1) * 512], in_=sb_.ap(), func=AF.Exp, scale=0.125),
             reads=[sb_], writes=[PT])
    for hm in range(4):
        for mb in range(2):
            j = hm * 2 + mb
            P.op("tensor", lambda e: e.matmul(obank[:, hm * 65:(hm + 1) * 65], lhsT=PT[:, j * 128:(j + 1) * 128], rhs=Vm[:, mb, hm, 0:65],
                                              start=(mb == 0), stop=(mb == 1)), reads=[PT, Vm], writes=[obank])


def alloc_finish_work(P, C, tag, obanks, tbank, ybanks):
    W = Ctx()
    W.attn = [P.sb("%sattn%d" % (tag, r), [128, D], BF16) for r in range(2)]
    W.attnT = [P.sb("%sattnT%d" % (tag, r), [128, D], BF16) for r in range(2)]
    W.rec = [P.sb("%srec%d" % (tag, r), [128, 16], F32) for r in range(2)]
    W.xts = [P.sb("%sfx%d" % (tag, r), [128, D], F32) for r in range(2)]
    W.xn = [P.sb("%sxn%d" % (tag, r), [128, D], F32) for r in range(2)]
    W.PTm = [P.sb("%sPTm%d" % (tag, r), [128, 1024], BF16) for r in range(2)]
    W.osrc = lambda r: [(bk, bk[:, 0:nh * 65], nh) for (bk, nh) in obanks]
    W.tbank = tbank
    W.ybanks = ybanks
    return W


def phase_B0(P, C, xsrc, xdst, fT, vS, wiAll, kmT, Vm, wout_dram, nblocks=NT):
    with P.scope():
        Wout, Wot = load_weight(P, "Wout0", wout_dram, 8, D)
        kT = P.sb("kT", [128, 2, S], BF16)
        kiT = P.sb("kiT", [128, S], BF16)
        Va = P.sb("Va", [128, NT, 4, VW], BF16)
        P.op("gpsimd", lambda e: e.memset(Va.ap(), 1.0), writes=[Va])
        for c in range(2):
            P.dma("sync", kT[:, c, :], fT[6 + c], writes=[kT])
        P.dma("sync", kiT.ap(), fT[12], writes=[kiT])
        vr = vS.rearrange("(i p) (g d) -> p i g d", p=128, d=64)
        for i0 in range(NT):
            P.dma("sync", Va[:, i0, :, 0:64], vr[:, i0, :, :], writes=[Va])
        scores = [P.sb("score%d" % r, [128, S], F32) for r in range(2)]
        junk = P.sb("junk", [128, S], BF16)
        Bs = [P.sb("Bm%d" % r, [128, S], BF16) for r in range(2)]
        Rs = [P.sb("R%d" % r, [128, 512], BF16) for r in range(4)]
        Wdgs = [P.sb("Wdg%d" % r, [128, 8, 128], BF16) for r in range(2)]
        PTs = [P.sb("PT%d" % r, [128, 512], BF16) for r in range(3)]
        qalls = [P.sb("qall%d" % r, [128, 24, 128], BF16) for r in range(4)]
        for r in range(4):
            P.op("gpsimd", lambda e: e.memset(qalls[r].ap(), 0.0), writes=[qalls[r]])
        sm = [[P.sb("bs%d_%d" % (r, j), [128, 1], F32) for j in range(6)] for r in range(2)]
        wtabs = [P.sb("wtab%d" % r, [128, N_BISECT + 2], F32) for r in range(2)]
        FW = alloc_finish_work(P, C, "b0", [(C.banks[3], 6), (C.banks[4], 6), (C.banks[5], 4)], C.banks[0], [C.banks[1], C.banks[0]])
        fT_r = fT.rearrange("c p t -> p c t")
        cnts = dict(lc=0, sc=0)

        def idx_steps(b):
            r = b % 2
            N = 128 * (b + 1)
            qall = qalls[b % 4]
            score = scores[r]
            q0 = b * 128
            Wdg = Wdgs[r]
            accb = C.banks[7]
            steps = []

            def loads():
                for (slot0, nch, ch0) in ((0, 6, 0), (12, 4, 8), (20, 2, 13)):
                    for base in (0, 64):
                        P.dma("sync", qall[base:base + 64, slot0 + base // 64:slot0 + 2 * nch:2, :], fT_r[base:base + 64, ch0:ch0 + nch, q0:q0 + 128],
                              writes=[qall])
                for h in range(8):
                    P.op("gpsimd", lambda e: e.tensor_scalar(out=Wdg[:, h, :], in0=C.ident.ap(), scalar1=wiAll[:, b, h:h + 1], scalar2=None, op0=ALU.mult),
                         reads=[C.ident, wiAll], writes=[Wdg])
            steps.append(loads)
            jobs = [(c0, h) for c0 in range(0, N, 512) for h in range(8)]
            state = dict(prev=None)

            def mk(job):
                def run():
                    prev = state["prev"]
                    if job is not None:
                        c0, h = job
                        wc = min(512, N - c0)
                        lc = cnts["lc"]
                        bank = C.banks[2] if lc % 2 == 0 else C.banks[6]
                        R = Rs[lc % 4]
                        cnts["lc"] = lc + 1
                        P.op("tensor", lambda e: e.matmul(bank[:, 0:wc], lhsT=qall[:, 12 + h, :], rhs=kiT[:, c0:c0 + wc],
                                                          start=True, stop=True), reads=[qall, kiT], writes=[bank])
                        P.op("scalar", lambda e: e.activation(out=R[:, 0:wc], in_=bank[:, 0:wc], func=AF.Relu), reads=[bank], writes=[R])
                    if prev is not None:
                        (c0_, h_), R_ = prev
                        wc_ = min(512, N - c0_)
                        P.op("tensor", lambda e: e.matmul(accb[:, 0:wc_], lhsT=Wdg[:, h_, :], rhs=R_[:, 0:wc_], start=(h_ == 0), stop=(h_ == 7)),
                             reads=[Wdg, R_], writes=[accb])
                        if h_ == 7:
                            P.op("scalar", lambda e: e.activation(out=score[:, c0_:c0_ + wc_], in_=accb[:, 0:wc_], func=AF.Copy), reads=[accb], writes=[score])
                    state["prev"] = (job, R) if job is not None else None
                return run
            for job in jobs + [None]:
                steps.append(mk(job))
            return steps

        def thr_steps(b):
            r = b % 2
            N = 128 * (b + 1)
            score = scores[r]
            B = Bs[r]
            q0 = b * 128
            mn, mx, mid, cnt, aa, thr = sm[r]
            wtab = wtabs[r]
            steps = []

            def pre():
                if b >= 2:
                    P.op("vector", lambda e: e.tensor_reduce(out=mn.ap(), in_=score[:, 0:N - 128], axis=AX.X, op=ALU.min), reads=[score], writes=[mn])
                P.op("gpsimd", lambda e: e.affine_select(out=score[:, q0:q0 + 128], in_=score[:, q0:q0 + 128], pattern=[[-1, 128]], compare_op=ALU.is_ge,
                                                        fill=NEG, base=0, channel_multiplier=1), reads=[score], writes=[score])
                if b >= 2:
                    P.op("vector", lambda e: e.tensor_reduce(out=mx.ap(), in_=score[:, 0:N], axis=AX.X, op=ALU.max), reads=[score], writes=[mx])
                    P.op("vector", lambda e: e.tensor_tensor(out=aa.ap(), in0=mx.ap(), in1=mn.ap(), op=ALU.subtract), reads=[mx, mn], writes=[aa])
                    P.op("vector", lambda e: e.tensor_scalar(out=wtab.ap(), in0=C.pow2.ap(), scalar1=aa.ap(), scalar2=0.5, op0=ALU.mult, op1=ALU.mult),
                         reads=[C.pow2, aa], writes=[wtab])
                    P.op("vector", lambda e: e.tensor_tensor(out=mid.ap(), in0=mn.ap(), in1=wtab[:, 1:2], op=ALU.add), reads=[mn, wtab], writes=[mid])
            steps.append(pre)
            if b >= 2:
                def mk(k):
                    def run():
                        P.op("vector", lambda e: e.tensor_scalar(out=junk[:, 0:N], in0=score[:, 0:N], scalar1=mid.ap(), scalar2=None, op0=ALU.is_ge, op1=ALU.add,
                                                                 accum_out=cnt.ap()), reads=[score, mid], writes=[junk, cnt])
                        P.op("vector", lambda e: e.tensor_scalar(out=aa.ap(), in0=cnt.ap(), scalar1=255.5, scalar2=-0.5, op0=ALU.is_ge, op1=ALU.add),
                             reads=[cnt], writes=[aa])
                        P.op("vector", lambda e: e.scalar_tensor_tensor(out=mid.ap(), in0=aa.ap(), scalar=wtab[:, k + 1:k + 2], in1=mid.ap(), op0=ALU.mult, op1=ALU.add),
                             reads=[aa, wtab, mid], writes=[mid])
                    return run
                for k in range(N_BISECT):
                    steps.append(mk(k))

            def post():
                if b >= 2:
                    P.op("vector", lambda e: e.tensor_tensor(out=thr.ap(), in0=mid.ap(), in1=wtab[:, N_BISECT + 1:N_BISECT + 2], op=ALU.subtract),
                         reads=[mid, wtab], writes=[thr])
                    thr_t = thr
                else:
                    thr_t = C.negthr
                P.op("vector", lambda e: e.tensor_scalar(out=B[:, 0:N], in0=score[:, 0:N], scalar1=thr_t.ap(), scalar2=MASKV, op0=ALU.is_lt, op1=ALU.mult),
                     reads=[score, thr_t], writes=[B])
                if C.dbg is not None and b == C.dbg_block:
                    P.dma("sync", C.dbg["score"], score.ap(), reads=[score])
                    P.dma("sync", C.dbg["thr"], thr_t.ap(), reads=[thr_t])
                    P.dma("sync", C.dbg["Bm"], B.ap(), reads=[B])
            steps.append(post)
            return steps

        def interleave(sa, sb_):
            na, nb_ = len(sa), len(sb_)
            j = 0
            for i, s in enumerate(sa):
                s()
                tgt = ((i + 1) * nb_) // max(na, 1)
                while j < tgt:
                    sb_[j]()
                    j += 1
            while j < nb_:
                sb_[j]()
                j += 1

        def attend(b):
            sc = cnts["sc"]
            r = b % 2
            qall = qalls[b % 4]
            B = Bs[r]
            jobs = []
            for h in range(12):
                pos = QPERM.index(h)
                g = h // 3
                obank = C.banks[3 + h // 6]
                ocol = (h % 6) * 65
                for kb0 in range(0, b + 1, 4):
                    kb1 = min(b + 1, kb0 + 4)
                    jobs.append((pos, g, obank, ocol, kb0, kb1))
            prev = None
            for job in jobs + [None]:
                if job is not None:
                    pos, g, obank, ocol, kb0, kb1 = job
                    sbank = C.banks[sc % 2]
                    PT = PTs[sc % 3]
                    sc += 1
                    for kb in range(kb0, kb1):
                        j = kb - kb0
                        P.op("tensor", lambda e: e.matmul(sbank[:, j * 128:(j + 1) * 128], lhsT=kT[:, g // 2, kb * 128:(kb + 1) * 128],
                                                          rhs=qall[:, pos, :], start=True, stop=False), reads=[kT, qall], writes=[sbank])
                        P.op("tensor", lambda e: e.matmul(sbank[:, j * 128:(j + 1) * 128], lhsT=B[:, kb * 128:(kb + 1) * 128], rhs=C.ident.ap(),
                                                          start=False, stop=True), reads=[B, C.ident], writes=[sbank])
                    nw = (kb1 - kb0) * 128
                    P.op("scalar", lambda e: e.activation(out=PT[:, 0:nw], in_=sbank[:, 0:nw], func=AF.Exp, scale=0.125), reads=[sbank], writes=[PT])
                if prev is not None:
                    (pos_, g_, obank_, ocol_, kb0_, kb1_), PT_ = prev
                    for kb in range(kb0_, kb1_):
                        j = kb - kb0_
                        P.op("tensor", lambda e: e.matmul(obank_[:, ocol_:ocol_ + 65], lhsT=PT_[:, j * 128:(j + 1) * 128], rhs=Va[:, kb, g_, 0:65],
                                                          start=(kb == 0), stop=(kb == b)), reads=[PT_, Va], writes=[obank_])
                prev = (job, PT) if job is not None else None
            cnts["sc"] = sc
            mem_heads(P, C, FW, qall, 20, kmT, Vm, C.banks[5], [C.banks[0], C.banks[1]], r)

        KPRE = 9
        idx_lists = {}

        def idx_take(b, n=None):
            if b >= nblocks:
                return []
            if b not in idx_lists:
                idx_lists[b] = idx_steps(b)
            lst = idx_lists[b]
            k = len(lst) if n is None else min(n, len(lst))
            out_ = lst[:k]
            del lst[:k]
            return out_

        for s in idx_take(0):
            s()
        interleave(thr_steps(0), idx_take(1))
        for s in idx_take(2, KPRE):
            s()
        for b in range(nblocks):
            if b + 1 < nblocks:
                interleave(thr_steps(b + 1), idx_take(b + 2))
            attend(b)
            for s in idx_take(b + 3, KPRE):
                s()
            attn_finish(P, C, FW, xsrc, xdst, b, 16, Wout, Wot, 8)


def phase_F(P, C, tag, xnorm, xbase, xdst, wgu_dram, wd_dram, gains, grow, f0, f1, final_row=None, ntiles=NT):
    nf = f1 - f0
    with P.scope():
        Wgu = P.sb(tag + "Wgu", [128, 8, 2 * nf * 128], BF16)
        Wgt = [P.tok("%sWgt%d" % (tag, k)) for k in range(8)]
        for k in range(8):
            for half in range(2):
                c0 = half * DFF + f0 * 128
                P.dma("gpsimd", Wgu[:, k, half * nf * 128:(half + 1) * nf * 128], wgu_dram[k * 128:(k + 1) * 128, c0:c0 + nf * 128], writes=[Wgt[k]])
        Wd = P.sb(tag + "Wd", [128, nf, D], BF16)
        Wdt = [P.tok("%sWdt%d" % (tag, k)) for k in range(nf)]
        for k in range(nf):
            P.dma("gpsimd", Wd[:, k, :], wd_dram[(f0 + k) * 128:(f0 + k + 1) * 128, :], writes=[Wdt[k]])
        gb = load_gain(P, C, tag + "gbF", gains, grow)
        gfin = load_gain(P, C, tag + "gfin", gains, final_row) if final_row is not None else None
        NW = alloc_norm_work(P, C, tag + "F")
        xts = [P.sb("%sFx%d" % (tag, r), [128, D], F32) for r in range(2)]
        hT2 = [P.sb("%sFhT%d" % (tag, r), [128, 8, 256], BF16) for r in range(2)]
        hTt = [[P.tok("%sFhTt%d_%d" % (tag, r, t)) for t in range(2)] for r in range(2)]
        actT = [P.sb("%sactT%d" % (tag, r), [128, nf, 256], BF16) for r in range(2)]
        sg = [P.sb("%ssg%d" % (tag, r), [128, 256], F32) for r in range(2)]
        xn = [P.sb("%sFxn%d" % (tag, r), [128, D], F32) for r in range(4)]
        fsm = [[P.sb("%sfs%d_%d" % (tag, r, j), [128, 1], F32) for j in range(4)] for r in range(2)]
        fj = P.sb(tag + "fj", [128, D], BF16)
        gc = 0
        hbs = {}

        def norm1(G):
            for t in range(2):
                i = 2 * G + t
                xt = xts[i % 2]
                P.dma("sync", xt.ap(), xnorm[i * 128:(i + 1) * 128, :], writes=[xt])
                hbs[(G, t)] = rmsnorm_to_hT(P, C, NW, xt, gb, None, None, None)
                xo = xn[i % 4]
                P.dma("sync", xo.ap(), xbase[i * 128:(i + 1) * 128, :], writes=[xo])

        def norm2(G):
            for t in range(2):
                norm_p2(P, C, hbs.pop((G, t)), hT2[G % 2][:, :, t * 128:(t + 1) * 128], hTt[G % 2][t], C.banks[0],
                        evac_eng="scalar" if t == 0 else "vector")

        NG = ntiles // 2
        norm1(0)
        norm2(0)
        for G in range(NG):
            hT = hT2[G % 2]
            aT = actT[G % 2]
            for fc in range(nf):
                if fc == nf // 2 and G + 1 < NG:
                    norm1(G + 1)
                bank = C.banks[1 + (gc % 3)]
                s_ = sg[gc % 2]
                gc += 1
                for half in range(2):
                    coff = half * nf * 128 + fc * 128
                    for k in range(8):
                        P.op("tensor", lambda e: e.matmul(bank[:, half * 256:(half + 1) * 256], lhsT=Wgu[:, k, coff:coff + 128], rhs=hT[:, k, :],
                                                          start=(k == 0), stop=(k == 7)), reads=[Wgt[k]] + hTt[G % 2], writes=[bank])
                P.op("scalar", lambda e: e.activation(out=s_.ap(), in_=bank[:, 0:256], func=AF.Silu), reads=[bank], writes=[s_])
                P.op("vector", lambda e: e.tensor_tensor(out=aT[:, fc, :], in0=bank[:, 256:512], in1=s_.ap(), op=ALU.mult), reads=[bank, s_], writes=[aT])
            if G + 1 < NG:
                norm2(G + 1)
            for t in range(2):
                i = 2 * G + t
                xo = xn[i % 4]
                for half in range(2):
                    yb = C.banks[4 + (2 * t + half) % 4]
                    for fc in range(nf):
                        P.op("tensor", lambda e: e.matmul(yb.ap(), lhsT=aT[:, fc, t * 128:(t + 1) * 128], rhs=Wd[:, fc, half * 512:(half + 1) * 512],
                                                          start=(fc == 0), stop=(fc == nf - 1)), reads=[aT, Wdt[fc]], writes=[yb])
                    P.op("vector", lambda e: e.tensor_tensor(out=xo[:, half * 512:(half + 1) * 512], in0=yb.ap(), in1=xo[:, half * 512:(half + 1) * 512], op=ALU.add),
                         reads=[yb, xo], writes=[xo])
                if gfin is not None:
                    ssq, t1, t2, rstd = fsm[i % 2]
                    P.op("scalar", lambda e: e.activation(out=fj.ap(), in_=xo.ap(), func=AF.Square, accum_out=ssq.ap()), reads=[xo], writes=[fj, ssq])
                    P.op("vector", lambda e: e.tensor_scalar(out=t1.ap(), in0=ssq.ap(), scalar1=1.0 / D, scalar2=EPS, op0=ALU.mult, op1=ALU.add), reads=[ssq], writes=[t1])
                    P.op("scalar", lambda e: e.activation(out=t2.ap(), in_=t1.ap(), func=AF.Sqrt), reads=[t1], writes=[t2])
                    P.op("vector", lambda e: e.reciprocal(out=rstd.ap(), in_=t2.ap()), reads=[t2], writes=[rstd])
                    P.op("vector", lambda e: e.scalar_tensor_tensor(out=xo.ap(), in0=xo.ap(), scalar=rstd.ap(), in1=gfin.ap(), op0=ALU.mult, op1=ALU.mult),
                         reads=[xo, rstd, gfin], writes=[xo])
                P.dma("sync", xdst[i * 128:(i + 1) * 128, :], xo.ap(), reads=[xo], out=(gfin is not None))


def phase_B1(P, C, fT, vS, Og):
    for g, dil in enumerate((1, 4, 16)):
        nb = NT // dil
        with P.scope():
            qz = P.sb("qz1", [128, 4, S], BF16)
            kTg = P.sb("kTg", [128, 2, S], BF16)
            Vg = P.sb("Vg", [128, NT, 4, VW], BF16)
            P.op("gpsimd", lambda e: e.memset(qz.ap(), 0.0), writes=[qz])
            P.op("vector", lambda e: e.memset(Vg.ap(), 1.0), writes=[Vg])
            for c in range(2):
                for hh in range(2):
                    base = hh * 64
                    P.dma("sync", qz[base:base + 64, 2 * c + hh, :], fT[4 * g + c][base:base + 64, :], writes=[qz])
                P.dma("sync", kTg[:, c, :], fT[4 * g + 2 + c], writes=[kTg])
            vg = vS[:, g * 256:(g + 1) * 256].rearrange("(mb p dd) (h e) -> dd p mb h e", p=128, dd=dil, e=64)
            for r in range(dil):
                for m0 in range(nb):
                    P.dma("sync", Vg[:, r * nb + m0, :, 0:64], vg[r][:, m0, :, :], writes=[Vg])
            PTs = [P.sb("PTg%d" % k, [128, 512], BF16) for k in range(3)]
            Os = [P.sb("Os%d" % k, [128, 260], F32) for k in range(2)]
            Ogr = Og[g].rearrange("(m dd) c -> dd m c", dd=dil)
            sc = 0
            jobs = []
            qb = 0
            for r in range(dil):
                for mb in range(nb):
                    for hp in range(2):
                        jobs.append((r, mb, hp, qb))
                    qb += 1
            prev = None
            for job in jobs + [None]:
                if job is not None:
                    r, mb, hp, qb = job
                    qsl = slice(mb * 128 * dil + r, (mb * 128 + 127) * dil + r + 1, dil)
                    kbs = ([mb - 1] if mb > 0 else []) + [mb]
                    sbank = C.banks[sc % 3]
                    PT = PTs[sc % 3]
                    sc += 1
                    tiles = []
                    for hh in range(2):
                        j = hp * 2 + hh
                        for kb in kbs:
                            t = len(tiles)
                            tiles.append((j, kb))
                            ksl = slice(kb * 128 * dil + r, (kb * 128 + 127) * dil + r + 1, dil)
                            P.op("tensor", lambda e: e.matmul(sbank[:, t * 128:(t + 1) * 128], lhsT=kTg[:, j // 2, ksl], rhs=qz[:, j, qsl],
                                                              start=True, stop=False), reads=[kTg, qz], writes=[sbank])
                            M = C.MdT if kb == mb else C.MpT
                            P.op("tensor", lambda e: e.matmul(sbank[:, t * 128:(t + 1) * 128], lhsT=C.ident.ap(), rhs=M.ap(), start=False, stop=True),
                                 reads=[C.ident, M], writes=[sbank])
                    nw = len(tiles) * 128
                    P.op("scalar", lambda e: e.activation(out=PT[:, 0:nw], in_=sbank[:, 0:nw], func=AF.Exp, scale=0.125), reads=[sbank], writes=[PT])
                if prev is not None:
                    (r_, mb_, hp_, qb_), PT_, tiles_ = prev
                    obank = C.banks[6 + qb_ % 2]
                    kfirst = mb_ - 1 if mb_ > 0 else mb_
                    for t, (j, kb) in enumerate(tiles_):
                        P.op("tensor", lambda e: e.matmul(obank[:, j * 65:(j + 1) * 65], lhsT=PT_[:, t * 128:(t + 1) * 128], rhs=Vg[:, r_ * nb + kb, j, 0:65],
                                                          start=(kb == kfirst), stop=(kb == mb_)), reads=[PT_, Vg], writes=[obank])
                    if hp_ == 1:
                        O = Os[qb_ % 2]
                        P.op("vector", lambda e: e.tensor_copy(O.ap(), obank[:, 0:260]), reads=[obank], writes=[O])
                        P.dma("sync", Ogr[r_][mb_ * 128:(mb_ + 1) * 128, :], O.ap(), reads=[O])
                prev = (job, PT, tiles) if job is not None else None


def phase_B2(P, C, xsrc, xdst, fT, Og, kmT, Vm, wout_dram):
    with P.scope():
        Wout, Wot = load_weight(P, "Wout1", wout_dram, 4, D)
        qzs = [P.sb("qzm%d" % r, [128, 4, 128], BF16) for r in range(2)]
        for r in range(2):
            P.op("gpsimd", lambda e: e.memset(qzs[r].ap(), 0.0), writes=[qzs[r]])
        Ot = [[P.sb("Ot%d_%d" % (r, g), [128, 260], F32) for g in range(3)] for r in range(2)]
        FW = alloc_finish_work(P, C, "b2", [], C.banks[0], [C.banks[1], C.banks[2]])
        FW.osrc = lambda r: [(Ot[r][0], Ot[r][0].ap(), 4), (C.banks[5], C.banks[5][:, 0:260], 4)]
        fT_r = fT.rearrange("c p t -> p c t")
        for i in range(NT):
            r = i % 2
            qz = qzs[r]
            q0 = i * 128
            for base in (0, 64):
                P.dma("sync", qz[base:base + 64, base // 64:4:2, :], fT_r[base:base + 64, 12:14, q0:q0 + 128], writes=[qz])
            for g in range(3):
                P.dma("sync", Ot[r][g].ap(), Og[g][q0:q0 + 128, :], writes=[Ot[r][g]])
            mem_heads(P, C, FW, qz, 0, kmT, Vm, C.banks[5], [C.banks[6], C.banks[7]], r)
            P.op("gpsimd", lambda e: e.tensor_tensor(out=Ot[r][0].ap(), in0=Ot[r][0].ap(), in1=Ot[r][1].ap(), op=ALU.add),
                 reads=[Ot[r][0], Ot[r][1]], writes=[Ot[r][0]])
            P.op("gpsimd", lambda e: e.tensor_tensor(out=Ot[r][0].ap(), in0=Ot[r][0].ap(), in1=Ot[r][2].ap(), op=ALU.add),
                 reads=[Ot[r][0], Ot[r][2]], writes=[Ot[r][0]])
            attn_finish(P, C, FW, xsrc, xdst, i, 8, Wout, Wot, 4)


def build_program(stop_after=None, dbg_block=None, nblocks0=NT, skip_l0=False):
    nc = bass.Bass("TRN2", target_bir_lowering=False)
    P = Prog(nc)
    C = Ctx()
    I = {}

    def inp(name, shape, dt=F32):
        I[name] = nc.dram_tensor(name, list(shape), dt, kind="ExternalInput").ap()
        return I[name]

    x_in = inp("x", [S, D])
    mem_in = inp("mem", [256, D])
    pos_in = inp("pos", [128, NT], I32)
    gains = inp("gains", [7, D])
    C.freqs_in = inp("freqs", [128, 48])
    w_in0 = inp("w_in0", [D, SPEC0["ncols"]])
    w_in1 = inp("w_in1", [D, SPEC1["ncols"]])
    w_mkv = [inp("w_mkv%d" % l, [D, 512]) for l in range(2)]
    w_out0 = inp("w_out0", [1024, D])
    w_out1 = inp("w_out1", [512, D])
    w_gu = [inp("w_gu%d" % l, [D, 2 * DFF]) for l in range(2)]
    w_dn = [inp("w_dn%d" % l, [DFF, D]) for l in range(2)]
    out = nc.dram_tensor("out", [S, D], F32, kind="ExternalOutput").ap()
    C.dbg = None
    C.dbg_block = dbg_block
    if dbg_block is not None:
        C.dbg = dict(score=nc.dram_tensor("dbg_score", [128, S], F32, kind="ExternalOutput").ap(),
                     thr=nc.dram_tensor("dbg_thr", [128, 1], F32, kind="ExternalOutput").ap(),
                     Bm=nc.dram_tensor("dbg_Bm", [128, S], BF16, kind="ExternalOutput").ap())
    fT0 = P.dram("fT0", [15, 128, S], BF16)
    vS0 = P.dram("vS0", [S, 256], BF16)
    fT1 = P.dram("fT1", [14, 128, S], BF16)
    vS1 = P.dram("vS1", [S, 768], BF16)
    Og = [P.dram("Og%d" % g, [S, 260], F32) for g in range(3)]
    xa = P.dram("xa", [S, D], F32)
    xb = P.dram("xb", [S, D], F32)
    xc = P.dram("xc", [S, D], F32)

    C.banks = [P.ps("bank%d" % k, [128, 512], F32) for k in range(8)]
    setup_consts(P, C, pos_in)
    HF = NFC // 2

    def final(src):
        print("n_inst before final", P.n_inst)
        P.max_ops = None
        with P.scope():
            t = [P.sb("fin%d" % r, [128, D], F32) for r in range(2)]
            for i in range(NT):
                P.dma("sync", t[i % 2].ap(), src[i * 128:(i + 1) * 128, :], writes=[t[i % 2]])
                P.dma("sync", out[i * 128:(i + 1) * 128, :], t[i % 2].ap(), reads=[t[i % 2]], out=True)
        P.finish()
        return nc

    with (P.scope() if not skip_l0 else contextlib.nullcontext()):
      if not skip_l0:
        wiAll = P.sb("wiAll", [128, NT, 8], F32)
        kmT = P.sb("kmT0", [128, 2, 256], BF16)
        Vm = P.sb("Vm0", [128, 2, 4, VW], BF16)
        phase_M(P, C, "m0", mem_in, w_mkv[0], gains, 1, kmT, Vm)
        if stop_after == "M":
            d1 = nc.dram_tensor("dbg_kmT", [128, 2, 256], BF16, kind="ExternalOutput").ap()
            d2 = nc.dram_tensor("dbg_Vm", [128, 2, 4, VW], BF16, kind="ExternalOutput").ap()
            P.dma("sync", d1, kmT.ap(), reads=[kmT])
            P.dma("sync", d2, Vm.ap(), reads=[Vm])
            return final(x_in)
        phase_A(P, C, "a0", x_in, w_in0, gains, 0, SPEC0, fT0, vS0, wiAll, pos_in)
        if stop_after == "A":
            d1 = nc.dram_tensor("dbg_fT0", [15, 128, S], BF16, kind="ExternalOutput").ap()
            d2 = nc.dram_tensor("dbg_vS0", [S, 256], BF16, kind="ExternalOutput").ap()
            d3 = nc.dram_tensor("dbg_wi", [128, NT, 8], F32, kind="ExternalOutput").ap()
            with P.scope():
                tb = P.sb("dbgt", [128, S], BF16)
                for c in range(15):
                    P.dma("sync", tb.ap(), fT0[c], writes=[tb])
                    P.dma("sync", d1[c], tb.ap(), reads=[tb])
                for c in range(2):
                    P.dma("sync", tb[:, 0:2048].rearrange("p (a b) -> p a b", b=256), vS0[c * 2048:(c + 1) * 2048, :].rearrange("(a p) b -> p a b", p=128), writes=[tb])
                    P.dma("sync", d2[c * 2048:(c + 1) * 2048, :].rearrange("(a p) b -> p a b", p=128), tb[:, 0:2048].rearrange("p (a b) -> p a b", b=256), reads=[tb])
                P.dma("sync", d3, wiAll.ap(), reads=[wiAll])
            return final(x_in)
        phase_B0(P, C, x_in, xa, fT0, vS0, wiAll, kmT, Vm, w_out0, nblocks=nblocks0)
    if stop_after == "B0":
        return final(xa)
    if not skip_l0:
        phase_F(P, C, "f0a", xa, xa, xb, w_gu[0], w_dn[0], gains, 2, 0, HF)
        phase_F(P, C, "f0b", xa, xb, xc, w_gu[0], w_dn[0], gains, 2, HF, NFC)
    else:
        xc = x_in
    if stop_after == "F0":
        return final(xc)
    with P.scope():
        kmT = P.sb("kmT1", [128, 2, 256], BF16)
        Vm = P.sb("Vm1", [128, 2, 4, VW], BF16)
        phase_M(P, C, "m1", mem_in, w_mkv[1], gains, 4, kmT, Vm)
        phase_A(P, C, "a1", xc, w_in1, gains, 3, SPEC1, fT1, vS1, None, pos_in)
        phase_B1(P, C, fT1, vS1, Og)
        phase_B2(P, C, xc, xa, fT1, Og, kmT, Vm, w_out1)
    if stop_after == "B2":
        return final(xa)
    phase_F(P, C, "f1a", xa, xa, xb, w_gu[1], w_dn[1], gains, 5, 0, HF)
    phase_F(P, C, "f1b", xa, xb, out, w_gu[1], w_dn[1], gains, 5, HF, NFC, final_row=6)
    P.finish()
    return nc


def prep_inputs(inputs):
    f = lambda a: np.ascontiguousarray(np.asarray(a, dtype=np.float32))
    w0 = f(inputs["l0_w_in"])
    q, k, v, qi, ki, wi, qm = np.split(w0, np.cumsum([768, 256, 256, 512, 64, 8])[:], axis=1)
    qp = np.concatenate([q[:, h * 64:(h + 1) * 64] for h in QPERM], axis=1)
    w_in0 = np.ascontiguousarray(np.concatenate([qp, k, qi, ki, ki, wi, v, qm], axis=1))
    w1 = f(inputs["l1_w_in"])
    parts = [w1[:, j * 256:(j + 1) * 256] for j in range(10)]
    w_in1 = np.ascontiguousarray(np.concatenate([parts[0], parts[1], parts[3], parts[4], parts[6], parts[7], parts[2], parts[5], parts[8], parts[9]], axis=1))
    gains = np.ascontiguousarray(np.stack([f(inputs[n]) for n in ("l0_norm_mix", "l0_norm_mem", "l0_norm_ffn", "l1_norm_mix", "l1_norm_mem",
                                                                 "l1_norm_ffn", "final_norm")], axis=0))
    fr64 = (np.float32(10000.0) ** (-np.arange(32, dtype=np.float32) / np.float32(32))).astype(np.float32)
    fr16 = (np.float32(10000.0) ** (-np.arange(16, dtype=np.float32) / np.float32(16))).astype(np.float32)
    freqs = np.ascontiguousarray(np.broadcast_to(np.concatenate([fr64, fr16])[None, :], (128, 48)).astype(np.float32))
    shared = dict(freqs=freqs, gains=gains, w_in0=w_in0, w_in1=w_in1, w_mkv0=f(inputs["l0_w_mem_kv"]), w_mkv1=f(inputs["l1_w_mem_kv"]),
                  w_out0=f(inputs["l0_w_out"]), w_out1=f(inputs["l1_w_out"]), w_gu0=f(inputs["l0_w_gate_up"]), w_gu1=f(inputs["l1_w_gate_up"]),
                  w_dn0=f(inputs["l0_w_down"]), w_dn1=f(inputs["l1_w_down"]))
    x = f(inputs["x"])
    mem = f(inputs["mem"])
    pos = np.ascontiguousarray(np.asarray(inputs["positions"], dtype=np.int32))
    maps = []
    for c in range(x.shape[0]):
        m = dict(shared)
        m["x"] = x[c]
        m["mem"] = mem[c]
        m["pos"] = np.ascontiguousarray(pos[c].reshape(NT, 128).T)
        maps.append(m)
    return maps


_NC_CACHE = {}


def kernel(**inputs):
    maps = prep_inputs(inputs)
    if "nc" not in _NC_CACHE:
        _NC_CACHE["nc"] = build_program()
    nc = _NC_CACHE["nc"]
    res = run_bass_kernel_spmd(nc, maps, core_ids=list(range(len(maps))))
    return np.stack([np.asarray(r["out"], dtype=np.float32) for r in res.results], axis=0)
```

```python
import contextlib
import numpy as np
import ml_dtypes
import concourse.bass as bass
import concourse.mybir as mybir
from concourse.bass_utils import run_bass_kernel_spmd

F32 = mybir.dt.float32
BF16 = mybir.dt.bfloat16
I32 = mybir.dt.int32
AF = mybir.ActivationFunctionType
ALU = mybir.AluOpType
AX = mybir.AxisListType

SAME_ENGINE_SYNC = True


class Tok:
    def __init__(self, name):
        self.name = name
        self.w = None
        self.r = {}


class Buf(Tok):
    def __init__(self, name, handle):
        super().__init__(name)
        self.h = handle

    def ap(self):
        return self.h[:]

    def __getitem__(self, idx):
        return self.h[idx]


class _Eng:
    def __init__(self, name, handle, sem):
        self.name = name
        self.h = handle
        self.sem = sem
        self.count = 0
        self.seen = {}


class Prog:
    def __init__(self, nc, n_dma_sems=8):
        self.nc = nc
        self.stack = contextlib.ExitStack()
        self.engs = {}
        for name in ("tensor", "vector", "scalar", "gpsimd", "sync"):
            sem = self.stack.enter_context(nc.semaphore("s_" + name))
            self.engs[name] = _Eng(name, getattr(nc, name), sem)
        self.dma_sems = {}
        for q in ("sync", "gpsimd", "scalar"):
            self.dma_sems[q] = [[self.stack.enter_context(nc.semaphore("d_%s%d" % (q, i))), 0] for i in range(n_dma_sems)]
        self.dma_rr = {"sync": 0, "gpsimd": 0, "scalar": 0}
        self.out_events = []
        self.n_inst = 0
        self.scopes = [self.stack]
        import os
        self.max_ops = int(os.environ["KMAXOPS"]) if "KMAXOPS" in os.environ else None

    @contextlib.contextmanager
    def scope(self):
        st = contextlib.ExitStack()
        self.scopes.append(st)
        try:
            yield
        finally:
            self.barrier()
            self.scopes.pop()
            st.close()

    def barrier(self):
        if getattr(self, "finished", False):
            return
        for eng in self.engs.values():
            for other in self.engs.values():
                if other is not eng and other.count > 0:
                    self._wait(eng, (other.name, other.sem, other.count))
            for q, pool in self.dma_sems.items():
                for i, slot in enumerate(pool):
                    if slot[1] > 0:
                        self._wait(eng, ("d_%s%d" % (q, i), slot[0], 16 * slot[1]))

    def tok(self, name):
        return Tok(name)

    def sb(self, name, shape, dtype):
        self.uid = getattr(self, "uid", 0) + 1
        name = "%s_u%d" % (name, self.uid)
        return Buf(name, self.scopes[-1].enter_context(self.nc.sbuf_tensor(name, list(shape), dtype)))

    def ps(self, name, shape, dtype):
        b = Buf(name, self.scopes[-1].enter_context(self.nc.psum_tensor(name, list(shape), dtype)))
        b.excl = True
        return b

    def dram(self, name, shape, dtype):
        return self.nc.dram_tensor(name, list(shape), dtype, kind="Internal").ap()

    def _wait(self, eng, ev):
        key, sem, val = ev
        if eng.seen.get(key, 0) >= val:
            return
        if key == eng.name and not (SAME_ENGINE_SYNC and eng.name != "tensor" and eng.name != "sync"):
            return
        eng.h.wait_ge(sem, val)
        eng.seen[key] = val

    def _deps(self, eng, reads, writes):
        for t in reads:
            if t.w is not None:
                self._wait(eng, t.w)
        for t in writes:
            if t.w is not None:
                self._wait(eng, t.w)
            for ev in t.r.values():
                self._wait(eng, ev)

    def _record(self, ev, reads, writes):
        for t in writes:
            t.w = ev
            t.r = {}
        for t in reads:
            if t in writes:
                continue
            t.r[ev[0]] = ev

    def op(self, engname, fn, reads=(), writes=()):
        if self.max_ops is not None and self.n_inst >= self.max_ops:
            return None
        eng = self.engs[engname]
        ex = [t for t in reads if getattr(t, "excl", False) and t not in writes]
        if ex:
            reads = [t for t in reads if t not in ex]
            writes = list(writes) + ex
        self._deps(eng, reads, writes)
        inst = fn(eng.h)
        eng.count += 1
        inst.then_inc(eng.sem, 1)
        ev = (eng.name, eng.sem, eng.count)
        self._record(ev, reads, writes)
        self.n_inst += 1
        return ev

    def dma(self, q, out_ap, in_ap, reads=(), writes=(), out=False, **kw):
        if self.max_ops is not None and self.n_inst >= self.max_ops:
            return None
        eng = self.engs[q]
        self._deps(eng, reads, writes)
        pool = self.dma_sems[q]
        i = self.dma_rr[q]
        self.dma_rr[q] = (i + 1) % len(pool)
        slot = pool[i]
        key = "d_%s%d" % (q, i)
        if slot[1] > 0:
            self._wait(eng, (key, slot[0], 16 * slot[1]))
        eng.h.dma_start(out=out_ap, in_=in_ap, **kw).then_inc(slot[0], 16)
        slot[1] += 1
        ev = (key, slot[0], 16 * slot[1])
        self._record(ev, reads, writes)
        if out:
            self.out_events.append(ev)
        self.n_inst += 1
        return ev

    def finish(self):
        eng = self.engs["sync"]
        for q, pool in self.dma_sems.items():
            for i, slot in enumerate(pool):
                if slot[1] > 0:
                    self._wait(eng, ("d_%s%d" % (q, i), slot[0], 16 * slot[1]))
        for name, e in self.engs.items():
            if name != "sync" and e.count > 0:
                self._wait(eng, (name, e.sem, e.count))
        self.finished = True
        for st in reversed(self.scopes):
            st.close()

    def make_identity(self, idt):
        tmp = self.sb(idt.name + "_i", [128, 128], I32)
        self.op("gpsimd", lambda e: e.iota(tmp.ap(), pattern=[[1, 128]], base=0, channel_multiplier=-1), writes=[tmp])
        self.op("vector", lambda e: e.tensor_scalar(out=idt.ap(), in0=tmp.ap(), scalar1=0.0, scalar2=None, op0=ALU.is_equal),
                reads=[tmp], writes=[idt])


S = 4096
D = 1024
NT = 32
DFF = 2816
NFC = 22
EPS = 1e-6
NEG = -1.0e30
MASKV = -30000.0
VW = 66
N_BISECT = 16
IDX_SCALE = (8 ** -0.5) * (64 ** -0.5)
QPERM = [0, 3, 1, 4, 2, 5, 6, 9, 7, 10, 8, 11]

SPEC0 = dict(
    ncols=2184,
    chunks=[(0, 512, [("rope64", 0, 8)]), (512, 512, [("rope64", 0, 8)]), (1024, 512, [("rope32", 0, 8)]),
            (1536, 136, [("rope32", 0, 2), ("wi", 128, 8)]), (1672, 512, [("copy", 0, 512)])],
    tcols=[128 * k for k in range(13)] + [1928, 2056],
    vcols=(1672, 1928),
)
SPEC1 = dict(
    ncols=2560,
    chunks=[(0, 512, [("rope64", 0, 8)]), (512, 512, [("rope64", 0, 8)]), (1024, 512, [("rope64", 0, 8)]),
            (1536, 512, [("copy", 0, 512)]), (2048, 512, [("copy", 0, 512)])],
    tcols=[128 * k for k in range(12)] + [2304, 2432],
    vcols=(1536, 2304),
)


class Ctx:
    pass


def load_weight(P, name, dram, nk, ncols, q="gpsimd"):
    W = P.sb(name, [128, nk, ncols], BF16)
    toks = [P.tok("%s_%d" % (name, k)) for k in range(nk)]
    for k in range(nk):
        c0 = 0
        while c0 < ncols:
            c1 = min(ncols, c0 + 2048)
            P.dma(q, W[:, k, c0:c1], dram[k * 128:(k + 1) * 128, c0:c1], writes=[toks[k]])
            c0 = c1
    return W, toks


def setup_consts(P, C, pos_in):
    C.ident = P.sb("ident", [128, 128], BF16)
    P.make_identity(C.ident)
    C.negthr = P.sb("negthr", [128, 1], F32)
    P.op("vector", lambda e: e.memset(C.negthr.ap(), -1.0e29), writes=[C.negthr])
    C.pow2 = P.sb("pow2", [128, N_BISECT + 2], F32)
    for k in range(N_BISECT + 2):
        P.op("gpsimd", lambda e: e.memset(C.pow2[:, k:k + 1], 2.0 ** (1 - k)), writes=[C.pow2])
    C.MdT = P.sb("MdT", [128, 128], BF16)
    C.MpT = P.sb("MpT", [128, 128], BF16)
    zt = P.sb("zt", [128, 128], F32)
    zm = P.sb("zm", [128, 128], F32)
    P.op("vector", lambda e: e.memset(zt.ap(), 0.0), writes=[zt])
    P.op("gpsimd", lambda e: e.affine_select(out=zm.ap(), in_=zt.ap(), pattern=[[1, 128]], compare_op=ALU.is_ge, fill=MASKV, base=0, channel_multiplier=-1),
         reads=[zt], writes=[zm])
    P.op("vector", lambda e: e.tensor_copy(C.MdT.ap(), zm.ap()), reads=[zm], writes=[C.MdT])
    P.op("gpsimd", lambda e: e.affine_select(out=zm.ap(), in_=zt.ap(), pattern=[[-1, 128]], compare_op=ALU.is_ge, fill=MASKV, base=0, channel_multiplier=1),
         reads=[zt, C.MdT], writes=[zm])
    P.op("vector", lambda e: e.tensor_copy(C.MpT.ap(), zm.ap()), reads=[zm], writes=[C.MpT])


def build_rope_tables(P, C, pos_in):
    C.cos64 = P.sb("cos64", [128, NT, 32], F32)
    C.sin64 = P.sb("sin64", [128, NT, 32], F32)
    C.nsin64 = P.sb("nsin64", [128, NT, 32], F32)
    C.cos16 = P.sb("cos16", [128, NT, 16], F32)
    C.sin16 = P.sb("sin16", [128, NT, 16], F32)
    C.nsin16 = P.sb("nsin16", [128, NT, 16], F32)
    with P.scope():
        posi = P.sb("posi", [128, NT], I32)
        posf = P.sb("posf", [128, NT], F32)
        P.dma("sync", posi.ap(), pos_in, writes=[posi])
        P.op("vector", lambda e: e.tensor_copy(posf.ap(), posi.ap()), reads=[posi], writes=[posf])
        for half, cosT, sinT, nsinT in ((32, C.cos64, C.sin64, C.nsin64), (16, C.cos16, C.sin16, C.nsin16)):
            n = NT * half
            fr = P.sb("fr%d" % half, [128, half], F32)
            a = P.sb("a%d" % half, [128, NT, half], F32)
            ki = P.sb("ki%d" % half, [128, NT, half], I32)
            kf = P.sb("kf%d" % half, [128, NT, half], F32)
            fr1 = P.sb("fr1%d" % half, [128, NT, half], F32)
            m1 = P.sb("m1%d" % half, [128, NT, half], F32)
            f0 = 0 if half == 32 else 32
            P.dma("sync", fr.ap(), C.freqs_in[:, f0:f0 + half], writes=[fr])
            P.op("vector", lambda e: e.tensor_tensor(out=a.ap(), in0=posf.ap().unsqueeze(2).broadcast_to([128, NT, half]),
                                                     in1=fr.ap().unsqueeze(1).broadcast_to([128, NT, half]), op=ALU.mult),
                 reads=[posf, fr], writes=[a])
            P.op("vector", lambda e: e.tensor_scalar(out=a.ap(), in0=a.ap(), scalar1=float(1.0 / (2.0 * np.pi)), scalar2=None, op0=ALU.mult),
                 reads=[a], writes=[a])
            for shift, outT, neg in ((0.0, sinT, False), (0.25, cosT, False), (0.5, nsinT, False)):
                src = a
                if shift != 0.0:
                    P.op("vector", lambda e: e.tensor_scalar(out=fr1.ap(), in0=a.ap(), scalar1=shift, scalar2=None, op0=ALU.add),
                         reads=[a], writes=[fr1])
                    src = fr1
                P.op("vector", lambda e: e.tensor_copy(ki.ap(), src.ap()), reads=[src], writes=[ki])
                P.op("vector", lambda e: e.tensor_copy(kf.ap(), ki.ap()), reads=[ki], writes=[kf])
                P.op("vector", lambda e: e.tensor_tensor(out=kf.ap(), in0=src.ap(), in1=kf.ap(), op=ALU.subtract), reads=[src, kf], writes=[kf])
                P.op("vector", lambda e: e.tensor_scalar(out=m1.ap(), in0=kf.ap(), scalar1=0.5, scalar2=None, op0=ALU.is_gt), reads=[kf], writes=[m1])
                P.op("vector", lambda e: e.tensor_tensor(out=kf.ap(), in0=kf.ap(), in1=m1.ap(), op=ALU.subtract), reads=[kf, m1], writes=[kf])
                P.op("vector", lambda e: e.tensor_scalar(out=m1.ap(), in0=kf.ap(), scalar1=-0.5, scalar2=None, op0=ALU.is_lt), reads=[kf], writes=[m1])
                P.op("vector", lambda e: e.tensor_tensor(out=kf.ap(), in0=kf.ap(), in1=m1.ap(), op=ALU.add), reads=[kf, m1], writes=[kf])
                P.op("scalar", lambda e: e.activation(out=outT.ap(), in_=kf.ap(), func=AF.Sin, scale=float(2.0 * np.pi) * (1.0 - 1e-6)),
                     reads=[kf], writes=[outT])


def alloc_norm_work(P, C, tag):
    W = Ctx()
    W.sqj = P.sb(tag + "sqj", [128, D], BF16)
    W.small = [[P.sb("%ssm%d_%d" % (tag, r, j), [128, 1], F32) for j in range(4)] for r in range(2)]
    W.hb = [P.sb("%shb%d" % (tag, r), [128, D], BF16) for r in range(2)]
    W.k = 0
    return W


def rmsnorm_to_hT(P, C, W, xt, gb, hT_ap, hT_tok, bank, evac_eng="scalar"):
    r = W.k % 2
    W.k += 1
    ssq, t1, t2, rstd = W.small[r]
    hb = W.hb[r]
    P.op("scalar", lambda e: e.activation(out=W.sqj.ap(), in_=xt.ap(), func=AF.Square, accum_out=ssq.ap()), reads=[xt], writes=[W.sqj, ssq])
    P.op("vector", lambda e: e.tensor_scalar(out=t1.ap(), in0=ssq.ap(), scalar1=1.0 / D, scalar2=EPS, op0=ALU.mult, op1=ALU.add), reads=[ssq], writes=[t1])
    P.op("scalar", lambda e: e.activation(out=t2.ap(), in_=t1.ap(), func=AF.Sqrt), reads=[t1], writes=[t2])
    P.op("vector", lambda e: e.reciprocal(out=rstd.ap(), in_=t2.ap()), reads=[t2], writes=[rstd])
    P.op("vector", lambda e: e.scalar_tensor_tensor(out=hb.ap(), in0=xt.ap(), scalar=rstd.ap(), in1=gb.ap(), op0=ALU.mult, op1=ALU.mult),
         reads=[xt, rstd, gb], writes=[hb])
    if hT_ap is None:
        return hb
    return norm_p2(P, C, hb, hT_ap, hT_tok, bank, evac_eng)


def norm_p2(P, C, hb, hT_ap, hT_tok, bank, evac_eng="scalar"):
    bbf = bank.ap().bitcast(BF16)
    for c in range(8):
        P.op("tensor", lambda e: e.transpose(bbf[:, c * 128:(c + 1) * 128], hb[:, c * 128:(c + 1) * 128], C.ident.ap()),
             reads=[hb, C.ident], writes=[bank])
    srcv = bbf if len(hT_ap.shape) == 2 else bbf.rearrange("p (c t) -> p c t", t=128)
    if evac_eng == "scalar":
        P.op("scalar", lambda e: e.activation(out=hT_ap, in_=srcv, func=AF.Copy), reads=[bank], writes=[hT_tok])
    else:
        P.op("vector", lambda e: e.tensor_copy(hT_ap, srcv), reads=[bank], writes=[hT_tok])


def load_gain(P, C, name, gains, row):
    gb = P.sb(name, [128, D], F32)
    P.dma("sync", gb.ap(), gains[row].partition_broadcast(128), writes=[gb])
    return gb


def phase_A(P, C, tag, xsrc, w_dram, gains, grow, spec, fT, vS, wiAll, pos_in):
    ncols = spec["ncols"]
    with P.scope():
        build_rope_tables(P, C, pos_in)
        Win, Wt = load_weight(P, tag + "Win", w_dram, 8, ncols)
        gb = load_gain(P, C, tag + "gbA", gains, grow)
        NW = alloc_norm_work(P, C, tag + "A")
        xts = [P.sb("%sxt%d" % (tag, r), [128, D], F32) for r in range(3)]
        hTs = [P.sb("%shT%d" % (tag, r), [128, D], BF16) for r in range(2)]
        pts = [P.sb("%spt%d" % (tag, r), [128, ncols], BF16) for r in range(2)]
        t1s = [P.sb("%st1_%d" % (tag, r), [128, 512], F32) for r in range(2)]
        t2s = [P.sb("%st2_%d" % (tag, r), [128, 512], F32) for r in range(2)]
        ntc = len(spec["tcols"])
        fTs = [P.sb("%sfTs%d" % (tag, r), [128, ntc, 128], BF16) for r in range(2)]
        fT_r = fT.rearrange("c p t -> p c t")
        cc = 0
        hbs = {}

        def ld(i):
            xt = xts[i % 3]
            P.dma("sync", xt.ap(), xsrc[i * 128:(i + 1) * 128, :], writes=[xt])

        def n1(i):
            hbs[i] = rmsnorm_to_hT(P, C, NW, xts[i % 3], gb, None, None, None)

        def n2(i):
            norm_p2(P, C, hbs.pop(i), hTs[i % 2].ap(), hTs[i % 2], C.banks[0], evac_eng="scalar")

        def featT(i):
            _featT_body(P, C, spec, pts, fTs, fT_r, vS, ntc, i)

        ld(0)
        ld(1)
        n1(0)
        n2(0)
        for i in range(NT):
            hT = hTs[i % 2]
            pt = pts[i % 2]
            if i + 2 < NT:
                ld(i + 2)
            if i + 1 < NT:
                n1(i + 1)
            for (col0, width, handlers) in spec["chunks"]:
                bank = C.banks[1 + (cc % 4)]
                t1 = t1s[cc % 2]
                t2 = t2s[cc % 2]
                cc += 1
                for k in range(8):
                    P.op("tensor", lambda e: e.matmul(bank[:, 0:width], lhsT=hT[:, k * 128:(k + 1) * 128], rhs=Win[:, k, col0:col0 + width],
                                                      start=(k == 0), stop=(k == 7)), reads=[hT, Wt[k]], writes=[bank])
                for (kind, l0, n) in handlers:
                    if kind == "rope64":
                        nh = n
                        w = nh * 64
                        xv2 = bank[:, l0:l0 + w].rearrange("p (h d) -> p h d", d=32)
                        xv = bank[:, l0:l0 + w].rearrange("p (h d) -> p h d", d=64)
                        t1v2 = t1[:, 0:w].rearrange("p (h d) -> p h d", d=32)
                        t2v = t2[:, 0:w].rearrange("p (h d) -> p h d", d=64)
                        cosb = C.cos64[:, i:i + 1, :].broadcast_to([128, 2 * nh, 32])
                        sinb = C.sin64[:, i:i + 1, :].broadcast_to([128, nh, 32])
                        nsinb = C.nsin64[:, i:i + 1, :].broadcast_to([128, nh, 32])
                        P.op("vector", lambda e: e.tensor_tensor(out=t1v2, in0=xv2, in1=cosb, op=ALU.mult), reads=[bank, C.cos64], writes=[t1])
                        P.op("vector", lambda e: e.tensor_tensor(out=t2v[:, :, 0:32], in0=xv[:, :, 32:64], in1=nsinb, op=ALU.mult),
                             reads=[bank, C.nsin64], writes=[t2])
                        P.op("vector", lambda e: e.tensor_tensor(out=t2v[:, :, 32:64], in0=xv[:, :, 0:32], in1=sinb, op=ALU.mult),
                             reads=[bank, C.sin64], writes=[t2])
                        P.op("gpsimd", lambda e: e.tensor_tensor(out=pt[:, col0 + l0:col0 + l0 + w], in0=t1[:, 0:w], in1=t2[:, 0:w], op=ALU.add),
                             reads=[t1, t2], writes=[pt])
                    elif kind == "rope32":
                        nh = n
                        w = nh * 64
                        xv = bank[:, l0:l0 + w].rearrange("p (h d) -> p h d", d=64)
                        xr4 = bank[:, l0:l0 + w].rearrange("p (h t d) -> p h t d", t=4, d=16)
                        t1r4 = t1[:, 0:w].rearrange("p (h t d) -> p h t d", t=4, d=16)
                        t2v = t2[:, 0:w].rearrange("p (h d) -> p h d", d=64)
                        t1v = t1[:, 0:w].rearrange("p (h d) -> p h d", d=64)
                        ptv = pt[:, col0 + l0:col0 + l0 + w].rearrange("p (h d) -> p h d", d=64)
                        cosb = C.cos16[:, i:i + 1, :].unsqueeze(1).broadcast_to([128, nh, 2, 16])
                        sinb = C.sin16[:, i:i + 1, :].broadcast_to([128, nh, 16])
                        nsinb = C.nsin16[:, i:i + 1, :].broadcast_to([128, nh, 16])
                        P.op("vector", lambda e: e.tensor_tensor(out=t1r4[:, :, 0:2, :], in0=xr4[:, :, 0:2, :], in1=cosb, op=ALU.mult),
                             reads=[bank, C.cos16], writes=[t1])
                        P.op("vector", lambda e: e.tensor_tensor(out=t2v[:, :, 0:16], in0=xv[:, :, 16:32], in1=nsinb, op=ALU.mult),
                             reads=[bank, C.nsin16], writes=[t2])
                        P.op("vector", lambda e: e.tensor_tensor(out=t2v[:, :, 16:32], in0=xv[:, :, 0:16], in1=sinb, op=ALU.mult),
                             reads=[bank, C.sin16], writes=[t2])
                        P.op("gpsimd", lambda e: e.tensor_tensor(out=ptv[:, :, 0:32], in0=t1v[:, :, 0:32], in1=t2v[:, :, 0:32], op=ALU.add),
                             reads=[t1, t2], writes=[pt])
                        P.op("scalar", lambda e: e.activation(out=ptv[:, :, 32:64], in_=xv[:, :, 32:64], func=AF.Copy), reads=[bank], writes=[pt])
                    elif kind == "copy":
                        P.op("scalar", lambda e: e.activation(out=pt[:, col0 + l0:col0 + l0 + n], in_=bank[:, l0:l0 + n], func=AF.Copy),
                             reads=[bank], writes=[pt])
                    elif kind == "wi":
                        P.op("scalar", lambda e: e.activation(out=wiAll[:, i, :], in_=bank[:, l0:l0 + n], func=AF.Copy, scale=float(IDX_SCALE)),
                             reads=[bank], writes=[wiAll])
            if i + 1 < NT:
                n2(i + 1)
            if i >= 1:
                featT(i - 1)
        featT(NT - 1)


def _featT_body(P, C, spec, pts, fTs, fT_r, vS, ntc, i):
            pt = pts[i % 2]
            fts = fTs[i % 2]
            for g0 in range(0, ntc, 8):
                g1 = min(ntc, g0 + 8)
                bank = C.banks[5 + (g0 // 8)]
                bbf = bank.ap().bitcast(BF16)
                for k in range(g0, g1):
                    c0 = spec["tcols"][k]
                    P.op("tensor", lambda e: e.transpose(bbf[:, (k - g0) * 128:(k - g0 + 1) * 128], pt[:, c0:c0 + 128], C.ident.ap()),
                         reads=[pt, C.ident], writes=[bank])
                eng = "vector" if g0 == 0 else "scalar"
                if eng == "vector":
                    P.op("vector", lambda e: e.tensor_copy(fts[:, g0:g1, :], bbf[:, 0:(g1 - g0) * 128].rearrange("p (c t) -> p c t", t=128)),
                         reads=[bank], writes=[fts])
                else:
                    P.op("scalar", lambda e: e.activation(out=fts[:, g0:g1, :], in_=bbf[:, 0:(g1 - g0) * 128].rearrange("p (c t) -> p c t", t=128),
                                                          func=AF.Copy), reads=[bank], writes=[fts])
            P.dma("sync", fT_r[:, :, i * 128:(i + 1) * 128], fts.ap(), reads=[fts])
            v0, v1 = spec["vcols"]
            P.dma("sync", vS[i * 128:(i + 1) * 128, :], pt[:, v0:v1], reads=[pt])


def phase_M(P, C, tag, mem_in, w_dram, gains, grow, kmT, Vm):
    with P.scope():
        Wm, Wt = load_weight(P, tag + "Wm", w_dram, 8, 512)
        gb = load_gain(P, C, tag + "gbM", gains, grow)
        NW = alloc_norm_work(P, C, tag + "M")
        xts = [P.sb("%smx%d" % (tag, r), [128, D], F32) for r in range(2)]
        hTs = [P.sb("%smhT%d" % (tag, r), [128, D], BF16) for r in range(2)]
        kb16 = [P.sb("%skb16_%d" % (tag, r), [128, 256], BF16) for r in range(2)]
        P.op("gpsimd", lambda e: e.memset(Vm.ap(), 1.0), writes=[Vm])
        for mb in range(2):
            xt, hT = xts[mb], hTs[mb]
            P.dma("sync", xt.ap(), mem_in[mb * 128:(mb + 1) * 128, :], writes=[xt])
            rmsnorm_to_hT(P, C, NW, xt, gb, hT.ap(), hT, C.banks[0])
            bank = C.banks[1 + mb]
            for k in range(8):
                P.op("tensor", lambda e: e.matmul(bank.ap(), lhsT=hT[:, k * 128:(k + 1) * 128], rhs=Wm[:, k, :], start=(k == 0), stop=(k == 7)),
                     reads=[hT, Wt[k]], writes=[bank])
            P.op("scalar", lambda e: e.activation(out=kb16[mb].ap(), in_=bank[:, 0:256], func=AF.Copy), reads=[bank], writes=[kb16[mb]])
            P.op("vector", lambda e: e.tensor_copy(Vm[:, mb, :, 0:64], bank[:, 256:512].rearrange("p (h d) -> p h d", d=64)),
                 reads=[bank], writes=[Vm])
            tb = C.banks[3 + mb]
            tbf = tb.ap().bitcast(BF16)
            for c in range(2):
                P.op("tensor", lambda e: e.transpose(tbf[:, c * 128:(c + 1) * 128], kb16[mb][:, c * 128:(c + 1) * 128], C.ident.ap()),
                     reads=[kb16[mb], C.ident], writes=[tb])
            P.op("vector", lambda e: e.tensor_copy(kmT[:, :, mb * 128:(mb + 1) * 128], tbf[:, 0:256].rearrange("p (c t) -> p c t", t=128)),
                 reads=[tb], writes=[kmT])


def attn_finish(P, C, W, xsrc, xdst, i, nheads, Wout, Wot, nkc):
    r = i % 2
    attn = W.attn[r]
    rec = W.rec[r]
    xt = W.xts[r]
    P.dma("sync", xt.ap(), xsrc[i * 128:(i + 1) * 128, :], writes=[xt])
    h0 = 0
    for (bank, oap, nh) in W.osrc(r):
        ov = oap.rearrange("p (h d) -> p h d", d=65)
        P.op("vector", lambda e: e.reciprocal(out=rec[:, h0:h0 + nh], in_=ov[:, :, 64]), reads=[bank], writes=[rec])
        P.op("vector", lambda e: e.tensor_tensor(out=attn[:, h0 * 64:(h0 + nh) * 64].rearrange("p (h d) -> p h d", d=64), in0=ov[:, :, 0:64],
                                                 in1=rec[:, h0:h0 + nh].unsqueeze(2).broadcast_to([128, nh, 64]), op=ALU.mult),
             reads=[bank, rec], writes=[attn])
        h0 += nh
    tb = W.tbank
    tbf = tb.ap().bitcast(BF16)
    aT = W.attnT[r]
    for c in range(nkc):
        P.op("tensor", lambda e: e.transpose(tbf[:, c * 128:(c + 1) * 128], attn[:, c * 128:(c + 1) * 128], C.ident.ap()),
             reads=[attn, C.ident], writes=[tb])
    P.op("scalar", lambda e: e.activation(out=aT[:, 0:nkc * 128], in_=tbf[:, 0:nkc * 128], func=AF.Copy), reads=[tb], writes=[aT])
    xn = W.xn[r]
    for half in range(2):
        yb = W.ybanks[half]
        for c in range(nkc):
            P.op("tensor", lambda e: e.matmul(yb.ap(), lhsT=aT[:, c * 128:(c + 1) * 128], rhs=Wout[:, c, half * 512:(half + 1) * 512],
                                              start=(c == 0), stop=(c == nkc - 1)), reads=[aT, Wot[c]], writes=[yb])
        P.op("vector", lambda e: e.tensor_tensor(out=xn[:, half * 512:(half + 1) * 512], in0=yb.ap(), in1=xt[:, half * 512:(half + 1) * 512], op=ALU.add),
             reads=[yb, xt], writes=[xn])
    P.dma("sync", xdst[i * 128:(i + 1) * 128, :], xn.ap(), reads=[xn])


def mem_heads(P, C, W, qall, qch0, kmT, Vm, obank, sbank, r):
    PT = W.PTm[r]
    for half in range(2):
        sb_ = sbank[half]
        for hh in range(2):
            hm = half * 2 + hh
            for mb in range(2):
                j = hh * 2 + mb
                P.op("tensor", lambda e: e.matmul(sb_[:, j * 128:(j + 1) * 128], lhsT=kmT[:, hm // 2, mb * 128:(mb + 1) * 128],
                                                  rhs=qall[:, qch0 + hm, :], start=True, stop=True),
                     reads=[kmT, qall], writes=[sb_])
        P.op("scalar", lambda e: e.activation(out=PT[:, half * 512:(half + 1) * 512], in_=sb_.ap(), func=AF.Exp, scale=0.125),
             reads=[sb_], writes=[PT])
    for hm in range(4):
        for mb in range(2):
            j = hm * 2 + mb
            P.op("tensor", lambda e: e.matmul(obank[:, hm * 65:(hm + 1) * 65], lhsT=PT[:, j * 128:(j + 1) * 128], rhs=Vm[:, mb, hm, 0:65],
                                              start=(mb == 0), stop=(mb == 1)), reads=[PT, Vm], writes=[obank])


def alloc_finish_work(P, C, tag, obanks, tbank, ybanks):
    W = Ctx()
    W.attn = [P.sb("%sattn%d" % (tag, r), [128, D], BF16) for r in range(2)]
    W.attnT = [P.sb("%sattnT%d" % (tag, r), [128, D], BF16) for r in range(2)]
    W.rec = [P.sb("%srec%d" % (tag, r), [128, 16], F32) for r in range(2)]
    W.xts = [P.sb("%sfx%d" % (tag, r), [128, D], F32) for r in range(2)]
    W.xn = [P.sb("%sxn%d" % (tag, r), [128, D], F32) for r in range(2)]
    W.PTm = [P.sb("%sPTm%d" % (tag, r), [128, 1024], BF16) for r in range(2)]
    W.osrc = lambda r: [(bk, bk[:, 0:nh * 65], nh) for (bk, nh) in obanks]
    W.tbank = tbank
    W.ybanks = ybanks
    return W


def phase_B0(P, C, xsrc, xdst, fT, vS, wiAll, kmT, Vm, wout_dram, nblocks=NT):
    with P.scope():
        Wout, Wot = load_weight(P, "Wout0", wout_dram, 8, D)
        kT = P.sb("kT", [128, 2, S], BF16)
        kiT = P.sb("kiT", [128, S], BF16)
        Va = P.sb("Va", [128, NT, 4, VW], BF16)
        P.op("gpsimd", lambda e: e.memset(Va.ap(), 1.0), writes=[Va])
        for c in range(2):
            P.dma("sync", kT[:, c, :], fT[6 + c], writes=[kT])
        P.dma("sync", kiT.ap(), fT[12], writes=[kiT])
        vr = vS.rearrange("(i p) (g d) -> p i g d", p=128, d=64)
        for i0 in range(NT):
            P.dma("sync", Va[:, i0, :, 0:64], vr[:, i0, :, :], writes=[Va])
        scores = [P.sb("score%d" % r, [128, S], F32) for r in range(2)]
        junk = P.sb("junk", [128, S], BF16)
        Bs = [P.sb("Bm%d" % r, [128, S], BF16) for r in range(2)]
        Rs = [P.sb("R%d" % r, [128, 512], BF16) for r in range(4)]
        Wdgs = [P.sb("Wdg%d" % r, [128, 8, 128], BF16) for r in range(2)]
        PTs = [P.sb("PT%d" % r, [128, 512], BF16) for r in range(3)]
        qalls = [P.sb("qall%d" % r, [128, 24, 128], BF16) for r in range(4)]
        for r in range(4):
            P.op("gpsimd", lambda e: e.memset(qalls[r].ap(), 0.0), writes=[qalls[r]])
        sm = [[P.sb("bs%d_%d" % (r, j), [128, 1], F32) for j in range(6)] for r in range(2)]
        wtabs = [P.sb("wtab%d" % r, [128, N_BISECT + 2], F32) for r in range(2)]
        FW = alloc_finish_work(P, C, "b0", [(C.banks[3], 6), (C.banks[4], 6), (C.banks[5], 4)], C.banks[0], [C.banks[1], C.banks[0]])
        fT_r = fT.rearrange("c p t -> p c t")
        cnts = dict(lc=0, sc=0)

        def idx_steps(b):
            r = b % 2
            N = 128 * (b + 1)
            qall = qalls[b % 4]
            score = scores[r]
            q0 = b * 128
            Wdg = Wdgs[r]
            accb = C.banks[7]
            steps = []

            def loads():
                for (slot0, nch, ch0) in ((0, 6, 0), (12, 4, 8), (20, 2, 13)):
                    for base in (0, 64):
                        P.dma("sync", qall[base:base + 64, slot0 + base // 64:slot0 + 2 * nch:2, :], fT_r[base:base + 64, ch0:ch0 + nch, q0:q0 + 128],
                              writes=[qall])
                for h in range(8):
                    P.op("gpsimd", lambda e: e.tensor_scalar(out=Wdg[:, h, :], in0=C.ident.ap(), scalar1=wiAll[:, b, h:h + 1], scalar2=None, op0=ALU.mult),
                         reads=[C.ident, wiAll], writes=[Wdg])
            steps.append(loads)
            jobs = [(c0, h) for c0 in range(0, N, 512) for h in range(8)]
            state = dict(prev=None)

            def mk(job):
                def run():
                    prev = state["prev"]
                    if job is not None:
                        c0, h = job
                        wc = min(512, N - c0)
                        lc = cnts["lc"]
                        bank = C.banks[2] if lc % 2 == 0 else C.banks[6]
                        R = Rs[lc % 4]
                        cnts["lc"] = lc + 1
                        P.op("tensor", lambda e: e.matmul(bank[:, 0:wc], lhsT=qall[:, 12 + h, :], rhs=kiT[:, c0:c0 + wc],
                                                          start=True, stop=True), reads=[qall, kiT], writes=[bank])
                        P.op("scalar", lambda e: e.activation(out=R[:, 0:wc], in_=bank[:, 0:wc], func=AF.Relu), reads=[bank], writes=[R])
                    if prev is not None:
                        (c0_, h_), R_ = prev
                        wc_ = min(512, N - c0_)
                        P.op("tensor", lambda e: e.matmul(accb[:, 0:wc_], lhsT=Wdg[:, h_, :], rhs=R_[:, 0:wc_], start=(h_ == 0), stop=(h_ == 7)),
                             reads=[Wdg, R_], writes=[accb])
                        if h_ == 7:
                            P.op("scalar", lambda e: e.activation(out=score[:, c0_:c0_ + wc_], in_=accb[:, 0:wc_], func=AF.Copy), reads=[accb], writes=[score])
                    state["prev"] = (job, R) if job is not None else None
                return run
            for job in jobs + [None]:
                steps.append(mk(job))
            return steps

        def thr_steps(b):
            r = b % 2
            N = 128 * (b + 1)
            score = scores[r]
            B = Bs[r]
            q0 = b * 128
            mn, mx, mid, cnt, aa, thr = sm[r]
            wtab = wtabs[r]
            steps = []

            def pre():
                if b >= 2:
                    P.op("vector", lambda e: e.tensor_reduce(out=mn.ap(), in_=score[:, 0:N - 128], axis=AX.X, op=ALU.min), reads=[score], writes=[mn])
                P.op("gpsimd", lambda e: e.affine_select(out=score[:, q0:q0 + 128], in_=score[:, q0:q0 + 128], pattern=[[-1, 128]], compare_op=ALU.is_ge,
                                                        fill=NEG, base=0, channel_multiplier=1), reads=[score], writes=[score])
                if b >= 2:
                    P.op("vector", lambda e: e.tensor_reduce(out=mx.ap(), in_=score[:, 0:N], axis=AX.X, op=ALU.max), reads=[score], writes=[mx])
                    P.op("vector", lambda e: e.tensor_tensor(out=aa.ap(), in0=mx.ap(), in1=mn.ap(), op=ALU.subtract), reads=[mx, mn], writes=[aa])
                    P.op("vector", lambda e: e.tensor_scalar(out=wtab.ap(), in0=C.pow2.ap(), scalar1=aa.ap(), scalar2=0.5, op0=ALU.mult, op1=ALU.mult),
                         reads=[C.pow2, aa], writes=[wtab])
                    P.op("vector", lambda e: e.tensor_tensor(out=mid.ap(), in0=mn.ap(), in1=wtab[:, 1:2], op=ALU.add), reads=[mn, wtab], writes=[mid])
            steps.append(pre)
            if b >= 2:
                def mk(k):
                    def run():
                        P.op("vector", lambda e: e.tensor_scalar(out=junk[:, 0:N], in0=score[:, 0:N], scalar1=mid.ap(), scalar2=None, op0=ALU.is_ge, op1=ALU.add,
                                                                 accum_out=cnt.ap()), reads=[score, mid], writes=[junk, cnt])
                        P.op("vector", lambda e: e.tensor_scalar(out=aa.ap(), in0=cnt.ap(), scalar1=255.5, scalar2=-0.5, op0=ALU.is_ge, op1=ALU.add),
                             reads=[cnt], writes=[aa])
                        P.op("vector", lambda e: e.scalar_tensor_tensor(out=mid.ap(), in0=aa.ap(), scalar=wtab[:, k + 1:k + 2], in1=mid.ap(), op0=ALU.mult, op1=ALU.add),
                             reads=[aa, wtab, mid], writes=[mid])
                    return run
                for k in range(N_BISECT):
                    steps.append(mk(k))

            def post():
                if b >= 2:
                    P.op("vector", lambda e: e.tensor_tensor(out=thr.ap(), in0=mid.ap(), in1=wtab[:, N_BISECT + 1:N_BISECT + 2], op=ALU.subtract),
                         reads=[mid, wtab], writes=[thr])
                    thr_t = thr
                else:
                    thr_t = C.negthr
                P.op("vector", lambda e: e.tensor_scalar(out=B[:, 0:N], in0=score[:, 0:N], scalar1=thr_t.ap(), scalar2=MASKV, op0=ALU.is_lt, op1=ALU.mult),
                     reads=[score, thr_t], writes=[B])
                if C.dbg is not None and b == C.dbg_block:
                    P.dma("sync", C.dbg["score"], score.ap(), reads=[score])
                    P.dma("sync", C.dbg["thr"], thr_t.ap(), reads=[thr_t])
                    P.dma("sync", C.dbg["Bm"], B.ap(), reads=[B])
            steps.append(post)
            return steps

        def interleave(sa, sb_):
            na, nb_ = len(sa), len(sb_)
            j = 0
            for i, s in enumerate(sa):
                s()
                tgt = ((i + 1) * nb_) // max(na, 1)
                while j < tgt:
                    sb_[j]()
                    j += 1
            while j < nb_:
                sb_[j]()
                j += 1

        def attend(b):
            sc = cnts["sc"]
            r = b % 2
            qall = qalls[b % 4]
            B = Bs[r]
            jobs = []
            for h in range(12):
                pos = QPERM.index(h)
                g = h // 3
                obank = C.banks[3 + h // 6]
                ocol = (h % 6) * 65
                for kb0 in range(0, b + 1, 4):
                    kb1 = min(b + 1, kb0 + 4)
                    jobs.append((pos, g, obank, ocol, kb0, kb1))
            prev = None
            for job in jobs + [None]:
                if job is not None:
                    pos, g, obank, ocol, kb0, kb1 = job
                    sbank = C.banks[sc % 2]
                    PT = PTs[sc % 3]
                    sc += 1
                    for kb in range(kb0, kb1):
                        j = kb - kb0
                        P.op("tensor", lambda e: e.matmul(sbank[:, j * 128:(j + 1) * 128], lhsT=kT[:, g // 2, kb * 128:(kb + 1) * 128],
                                                          rhs=qall[:, pos, :], start=True, stop=False), reads=[kT, qall], writes=[sbank])
                        P.op("tensor", lambda e: e.matmul(sbank[:, j * 128:(j + 1) * 128], lhsT=B[:, kb * 128:(kb + 1) * 128], rhs=C.ident.ap(),
                                                          start=False, stop=True), reads=[B, C.ident], writes=[sbank])
                    nw = (kb1 - kb0) * 128
                    P.op("scalar", lambda e: e.activation(out=PT[:, 0:nw], in_=sbank[:, 0:nw], func=AF.Exp, scale=0.125), reads=[sbank], writes=[PT])
                if prev is not None:
                    (pos_, g_, obank_, ocol_, kb0_, kb1_), PT_ = prev
                    for kb in range(kb0_, kb1_):
                        j = kb - kb0_
                        P.op("tensor", lambda e: e.matmul(obank_[:, ocol_:ocol_ + 65], lhsT=PT_[:, j * 128:(j + 1) * 128], rhs=Va[:, kb, g_, 0:65],
                                                          start=(kb == 0), stop=(kb == b)), reads=[PT_, Va], writes=[obank_])
                prev = (job, PT) if job is not None else None
            cnts["sc"] = sc
            mem_heads(P, C, FW, qall, 20, kmT, Vm, C.banks[5], [C.banks[0], C.banks[1]], r)

        KPRE = 9
        idx_lists = {}

        def idx_take(b, n=None):
            if b >= nblocks:
                return []
            if b not in idx_lists:
                idx_lists[b] = idx_steps(b)
            lst = idx_lists[b]
            k = len(lst) if n is None else min(n, len(lst))
            out_ = lst[:k]
            del lst[:k]
            return out_

        for s in idx_take(0):
            s()
        interleave(thr_steps(0), idx_take(1))
        for s in idx_take(2, KPRE):
            s()
        for b in range(nblocks):
            if b + 1 < nblocks:
                interleave(thr_steps(b + 1), idx_take(b + 2))
            attend(b)
            for s in idx_take(b + 3, KPRE):
                s()
            attn_finish(P, C, FW, xsrc, xdst, b, 16, Wout, Wot, 8)


def phase_F(P, C, tag, xnorm, xbase, xdst, wgu_dram, wd_dram, gains, grow, f0, f1, final_row=None, ntiles=NT):
    nf = f1 - f0
    with P.scope():
        Wgu = P.sb(tag + "Wgu", [128, 8, 2 * nf * 128], BF16)
        Wgt = [P.tok("%sWgt%d" % (tag, k)) for k in range(8)]
        for k in range(8):
            for half in range(2):
                c0 = half * DFF + f0 * 128
                P.dma("gpsimd", Wgu[:, k, half * nf * 128:(half + 1) * nf * 128], wgu_dram[k * 128:(k + 1) * 128, c0:c0 + nf * 128], writes=[Wgt[k]])
        Wd = P.sb(tag + "Wd", [128, nf, D], BF16)
        Wdt = [P.tok("%sWdt%d" % (tag, k)) for k in range(nf)]
        for k in range(nf):
            P.dma("gpsimd", Wd[:, k, :], wd_dram[(f0 + k) * 128:(f0 + k + 1) * 128, :], writes=[Wdt[k]])
        gb = load_gain(P, C, tag + "gbF", gains, grow)
        gfin = load_gain(P, C, tag + "gfin", gains, final_row) if final_row is not None else None
        NW = alloc_norm_work(P, C, tag + "F")
        xts = [P.sb("%sFx%d" % (tag, r), [128, D], F32) for r in range(2)]
        hT2 = [P.sb("%sFhT%d" % (tag, r), [128, 8, 256], BF16) for r in range(2)]
        hTt = [[P.tok("%sFhTt%d_%d" % (tag, r, t)) for t in range(2)] for r in range(2)]
        actT = [P.sb("%sactT%d" % (tag, r), [128, nf, 256], BF16) for r in range(2)]
        sg = [P.sb("%ssg%d" % (tag, r), [128, 256], F32) for r in range(2)]
        xn = [P.sb("%sFxn%d" % (tag, r), [128, D], F32) for r in range(4)]
        fsm = [[P.sb("%sfs%d_%d" % (tag, r, j), [128, 1], F32) for j in range(4)] for r in range(2)]
        fj = P.sb(tag + "fj", [128, D], BF16)
        gc = 0
        hbs = {}

        def norm1(G):
            for t in range(2):
                i = 2 * G + t
                xt = xts[i % 2]
                P.dma("sync", xt.ap(), xnorm[i * 128:(i + 1) * 128, :], writes=[xt])
                hbs[(G, t)] = rmsnorm_to_hT(P, C, NW, xt, gb, None, None, None)
                xo = xn[i % 4]
                P.dma("sync", xo.ap(), xbase[i * 128:(i + 1) * 128, :], writes=[xo])

        def norm2(G):
            for t in range(2):
                norm_p2(P, C, hbs.pop((G, t)), hT2[G % 2][:, :, t * 128:(t + 1) * 128], hTt[G % 2][t], C.banks[0],
                        evac_eng="scalar" if t == 0 else "vector")

        NG = ntiles // 2
        norm1(0)
        norm2(0)
        for G in range(NG):
            hT = hT2[G % 2]
            aT = actT[G % 2]
            for fc in range(nf):
                if fc == nf // 2 and G + 1 < NG:
                    norm1(G + 1)
                bank = C.banks[1 + (gc % 3)]
                s_ = sg[gc % 2]
                gc += 1
                for half in range(2):
                    coff = half * nf * 128 + fc * 128
                    for k in range(8):
                        P.op("tensor", lambda e: e.matmul(bank[:, half * 256:(half + 1) * 256], lhsT=Wgu[:, k, coff:coff + 128], rhs=hT[:, k, :],
                                                          start=(k == 0), stop=(k == 7)), reads=[Wgt[k]] + hTt[G % 2], writes=[bank])
                P.op("scalar", lambda e: e.activation(out=s_.ap(), in_=bank[:, 0:256], func=AF.Silu), reads=[bank], writes=[s_])
                P.op("vector", lambda e: e.tensor_tensor(out=aT[:, fc, :], in0=bank[:, 256:512], in1=s_.ap(), op=ALU.mult), reads=[bank, s_], writes=[aT])
            if G + 1 < NG:
                norm2(G + 1)
            for t in range(2):
                i = 2 * G + t
                xo = xn[i % 4]
                for half in range(2):
                    yb = C.banks[4 + (2 * t + half) % 4]
                    for fc in range(nf):
                        P.op("tensor", lambda e: e.matmul(yb.ap(), lhsT=aT[:, fc, t * 128:(t + 1) * 128], rhs=Wd[:, fc, half * 512:(half + 1) * 512],
                                                          start=(fc == 0), stop=(fc == nf - 1)), reads=[aT, Wdt[fc]], writes=[yb])
                    P.op("vector", lambda e: e.tensor_tensor(out=xo[:, half * 512:(half + 1) * 512], in0=yb.ap(), in1=xo[:, half * 512:(half + 1) * 512], op=ALU.add),
                         reads=[yb, xo], writes=[xo])
                if gfin is not None:
                    ssq, t1, t2, rstd = fsm[i % 2]
                    P.op("scalar", lambda e: e.activation(out=fj.ap(), in_=xo.ap(), func=AF.Square, accum_out=ssq.ap()), reads=[xo], writes=[fj, ssq])
                    P.op("vector", lambda e: e.tensor_scalar(out=t1.ap(), in0=ssq.ap(), scalar1=1.0 / D, scalar2=EPS, op0=ALU.mult, op1=ALU.add), reads=[ssq], writes=[t1])
                    P.op("scalar", lambda e: e.activation(out=t2.ap(), in_=t1.ap(), func=AF.Sqrt), reads=[t1], writes=[t2])
                    P.op("vector", lambda e: e.reciprocal(out=rstd.ap(), in_=t2.ap()), reads=[t2], writes=[rstd])
                    P.op("vector", lambda e: e.scalar_tensor_tensor(out=xo.ap(), in0=xo.ap(), scalar=rstd.ap(), in1=gfin.ap(), op0=ALU.mult, op1=ALU.mult),
                         reads=[xo, rstd, gfin], writes=[xo])
                P.dma("sync", xdst[i * 128:(i + 1) * 128, :], xo.ap(), reads=[xo], out=(gfin is not None))


def phase_B1(P, C, fT, vS, Og):
    for g, dil in enumerate((1, 4, 16)):
        nb = NT // dil
        with P.scope():
            qz = P.sb("qz1", [128, 4, S], BF16)
            kTg = P.sb("kTg", [128, 2, S], BF16)
            Vg = P.sb("Vg", [128, NT, 4, VW], BF16)
            P.op("gpsimd", lambda e: e.memset(qz.ap(), 0.0), writes=[qz])
            P.op("vector", lambda e: e.memset(Vg.ap(), 1.0), writes=[Vg])
            for c in range(2):
                for hh in range(2):
                    base = hh * 64
                    P.dma("sync", qz[base:base + 64, 2 * c + hh, :], fT[4 * g + c][base:base + 64, :], writes=[qz])
                P.dma("sync", kTg[:, c, :], fT[4 * g + 2 + c], writes=[kTg])
            vg = vS[:, g * 256:(g + 1) * 256].rearrange("(mb p dd) (h e) -> dd p mb h e", p=128, dd=dil, e=64)
            for r in range(dil):
                for m0 in range(nb):
                    P.dma("sync", Vg[:, r * nb + m0, :, 0:64], vg[r][:, m0, :, :], writes=[Vg])
            PTs = [P.sb("PTg%d" % k, [128, 512], BF16) for k in range(3)]
            Os = [P.sb("Os%d" % k, [128, 260], F32) for k in range(2)]
            Ogr = Og[g].rearrange("(m dd) c -> dd m c", dd=dil)
            sc = 0
            jobs = []
            qb = 0
            for r in range(dil):
                for mb in range(nb):
                    for hp in range(2):
                        jobs.append((r, mb, hp, qb))
                    qb += 1
            prev = None
            for job in jobs + [None]:
                if job is not None:
                    r, mb, hp, qb = job
                    qsl = slice(mb * 128 * dil + r, (mb * 128 + 127) * dil + r + 1, dil)
                    kbs = ([mb - 1] if mb > 0 else []) + [mb]
                    sbank = C.banks[sc % 3]
                    PT = PTs[sc % 3]
                    sc += 1
                    tiles = []
                    for hh in range(2):
                        j = hp * 2 + hh
                        for kb in kbs:
                            t = len(tiles)
                            tiles.append((j, kb))
                            ksl = slice(kb * 128 * dil + r, (kb * 128 + 127) * dil + r + 1, dil)
                            P.op("tensor", lambda e: e.matmul(sbank[:, t * 128:(t + 1) * 128], lhsT=kTg[:, j // 2, ksl], rhs=qz[:, j, qsl],
                                                              start=True, stop=False), reads=[kTg, qz], writes=[sbank])
                            M = C.MdT if kb == mb else C.MpT
                            P.op("tensor", lambda e: e.matmul(sbank[:, t * 128:(t + 1) * 128], lhsT=C.ident.ap(), rhs=M.ap(), start=False, stop=True),
                                 reads=[C.ident, M], writes=[sbank])
                    nw = len(tiles) * 128
                    P.op("scalar", lambda e: e.activation(out=PT[:, 0:nw], in_=sbank[:, 0:nw], func=AF.Exp, scale=0.125), reads=[sbank], writes=[PT])
                if prev is not None:
                    (r_, mb_, hp_, qb_), PT_, tiles_ = prev
                    obank = C.banks[6 + qb_ % 2]
                    kfirst = mb_ - 1 if mb_ > 0 else mb_
                    for t, (j, kb) in enumerate(tiles_):
                        P.op("tensor", lambda e: e.matmul(obank[:, j * 65:(j + 1) * 65], lhsT=PT_[:, t * 128:(t + 1) * 128], rhs=Vg[:, r_ * nb + kb, j, 0:65],
                                                          start=(kb == kfirst), stop=(kb == mb_)), reads=[PT_, Vg], writes=[obank])
                    if hp_ == 1:
                        O = Os[qb_ % 2]
                        P.op("vector", lambda e: e.tensor_copy(O.ap(), obank[:, 0:260]), reads=[obank], writes=[O])
                        P.dma("sync", Ogr[r_][mb_ * 128:(mb_ + 1) * 128, :], O.ap(), reads=[O])
                prev = (job, PT, tiles) if job is not None else None


def phase_B2(P, C, xsrc, xdst, fT, Og, kmT, Vm, wout_dram):
    with P.scope():
        Wout, Wot = load_weight(P, "Wout1", wout_dram, 4, D)
        qzs = [P.sb("qzm%d" % r, [128, 4, 128], BF16) for r in range(2)]
        for r in range(2):
            P.op("gpsimd", lambda e: e.memset(qzs[r].ap(), 0.0), writes=[qzs[r]])
        Ot = [[P.sb("Ot%d_%d" % (r, g), [128, 260], F32) for g in range(3)] for r in range(2)]
        FW = alloc_finish_work(P, C, "b2", [], C.banks[0], [C.banks[1], C.banks[2]])
        FW.osrc = lambda r: [(Ot[r][0], Ot[r][0].ap(), 4), (C.banks[5], C.banks[5][:, 0:260], 4)]
        fT_r = fT.rearrange("c p t -> p c t")
        for i in range(NT):
            r = i % 2
            qz = qzs[r]
            q0 = i * 128
            for base in (0, 64):
                P.dma("sync", qz[base:base + 64, base // 64:4:2, :], fT_r[base:base + 64, 12:14, q0:q0 + 128], writes=[qz])
            for g in range(3):
                P.dma("sync", Ot[r][g].ap(), Og[g][q0:q0 + 128, :], writes=[Ot[r][g]])
            mem_heads(P, C, FW, qz, 0, kmT, Vm, C.banks[5], [C.banks[6], C.banks[7]], r)
            P.op("gpsimd", lambda e: e.tensor_tensor(out=Ot[r][0].ap(), in0=Ot[r][0].ap(), in1=Ot[r][1].ap(), op=ALU.add),
                 reads=[Ot[r][0], Ot[r][1]], writes=[Ot[r][0]])
            P.op("gpsimd", lambda e: e.tensor_tensor(out=Ot[r][0].ap(), in0=Ot[r][0].ap(), in1=Ot[r][2].ap(), op=ALU.add),
                 reads=[Ot[r][0], Ot[r][2]], writes=[Ot[r][0]])
            attn_finish(P, C, FW, xsrc, xdst, i, 8, Wout, Wot, 4)


def build_program(stop_after=None, dbg_block=None, nblocks0=NT, skip_l0=False):
    nc = bass.Bass("TRN2", target_bir_lowering=False)
    P = Prog(nc)
    C = Ctx()
    I = {}

    def inp(name, shape, dt=F32):
        I[name] = nc.dram_tensor(name, list(shape), dt, kind="ExternalInput").ap()
        return I[name]

    x_in = inp("x", [S, D])
    mem_in = inp("mem", [256, D])
    pos_in = inp("pos", [128, NT], I32)
    gains = inp("gains", [7, D])
    C.freqs_in = inp("freqs", [128, 48])
    w_in0 = inp("w_in0", [D, SPEC0["ncols"]])
    w_in1 = inp("w_in1", [D, SPEC1["ncols"]])
    w_mkv = [inp("w_mkv%d" % l, [D, 512]) for l in range(2)]
    w_out0 = inp("w_out0", [1024, D])
    w_out1 = inp("w_out1", [512, D])
    w_gu = [inp("w_gu%d" % l, [D, 2 * DFF]) for l in range(2)]
    w_dn = [inp("w_dn%d" % l, [DFF, D]) for l in range(2)]
    out = nc.dram_tensor("out", [S, D], F32, kind="ExternalOutput").ap()
    C.dbg = None
    C.dbg_block = dbg_block
    if dbg_block is not None:
        C.dbg = dict(score=nc.dram_tensor("dbg_score", [128, S], F32, kind="ExternalOutput").ap(),
                     thr=nc.dram_tensor("dbg_thr", [128, 1], F32, kind="ExternalOutput").ap(),
                     Bm=nc.dram_tensor("dbg_Bm", [128, S], BF16, kind="ExternalOutput").ap())
    fT0 = P.dram("fT0", [15, 128, S], BF16)
    vS0 = P.dram("vS0", [S, 256], BF16)
    fT1 = P.dram("fT1", [14, 128, S], BF16)
    vS1 = P.dram("vS1", [S, 768], BF16)
    Og = [P.dram("Og%d" % g, [S, 260], F32) for g in range(3)]
    xa = P.dram("xa", [S, D], F32)
    xb = P.dram("xb", [S, D], F32)
    xc = P.dram("xc", [S, D], F32)

    C.banks = [P.ps("bank%d" % k, [128, 512], F32) for k in range(8)]
    setup_consts(P, C, pos_in)
    HF = NFC // 2

    def final(src):
        print("n_inst before final", P.n_inst)
        P.max_ops = None
        with P.scope():
            t = [P.sb("fin%d" % r, [128, D], F32) for r in range(2)]
            for i in range(NT):
                P.dma("sync", t[i % 2].ap(), src[i * 128:(i + 1) * 128, :], writes=[t[i % 2]])
                P.dma("sync", out[i * 128:(i + 1) * 128, :], t[i % 2].ap(), reads=[t[i % 2]], out=True)
        P.finish()
        return nc

    with (P.scope() if not skip_l0 else contextlib.nullcontext()):
      if not skip_l0:
        wiAll = P.sb("wiAll", [128, NT, 8], F32)
        kmT = P.sb("kmT0", [128, 2, 256], BF16)
        Vm = P.sb("Vm0", [128, 2, 4, VW], BF16)
        phase_M(P, C, "m0", mem_in, w_mkv[0], gains, 1, kmT, Vm)
        if stop_after == "M":
            d1 = nc.dram_tensor("dbg_kmT", [128, 2, 256], BF16, kind="ExternalOutput").ap()
            d2 = nc.dram_tensor("dbg_Vm", [128, 2, 4, VW], BF16, kind="ExternalOutput").ap()
            P.dma("sync", d1, kmT.ap(), reads=[kmT])
            P.dma("sync", d2, Vm.ap(), reads=[Vm])
            return final(x_in)
        phase_A(P, C, "a0", x_in, w_in0, gains, 0, SPEC0, fT0, vS0, wiAll, pos_in)
        if stop_after == "A":
            d1 = nc.dram_tensor("dbg_fT0", [15, 128, S], BF16, kind="ExternalOutput").ap()
            d2 = nc.dram_tensor("dbg_vS0", [S, 256], BF16, kind="ExternalOutput").ap()
            d3 = nc.dram_tensor("dbg_wi", [128, NT, 8], F32, kind="ExternalOutput").ap()
            with P.scope():
                tb = P.sb("dbgt", [128, S], BF16)
                for c in range(15):
                    P.dma("sync", tb.ap(), fT0[c], writes=[tb])
                    P.dma("sync", d1[c], tb.ap(), reads=[tb])
                for c in range(2):
                    P.dma("sync", tb[:, 0:2048].rearrange("p (a b) -> p a b", b=256), vS0[c * 2048:(c + 1) * 2048, :].rearrange("(a p) b -> p a b", p=128), writes=[tb])
                    P.dma("sync", d2[c * 2048:(c + 1) * 2048, :].rearrange("(a p) b -> p a b", p=128), tb[:, 0:2048].rearrange("p (a b) -> p a b", b=256), reads=[tb])
                P.dma("sync", d3, wiAll.ap(), reads=[wiAll])
            return final(x_in)
        phase_B0(P, C, x_in, xa, fT0, vS0, wiAll, kmT, Vm, w_out0, nblocks=nblocks0)
    if stop_after == "B0":
        return final(xa)
    if not skip_l0:
        phase_F(P, C, "f0a", xa, xa, xb, w_gu[0], w_dn[0], gains, 2, 0, HF)
        phase_F(P, C, "f0b", xa, xb, xc, w_gu[0], w_dn[0], gains, 2, HF, NFC)
    else:
        xc = x_in
    if stop_after == "F0":
        return final(xc)
    with P.scope():
        kmT = P.sb("kmT1", [128, 2, 256], BF16)
        Vm = P.sb("Vm1", [128, 2, 4, VW], BF16)
        phase_M(P, C, "m1", mem_in, w_mkv[1], gains, 4, kmT, Vm)
        phase_A(P, C, "a1", xc, w_in1, gains, 3, SPEC1, fT1, vS1, None, pos_in)
        phase_B1(P, C, fT1, vS1, Og)
        phase_B2(P, C, xc, xa, fT1, Og, kmT, Vm, w_out1)
    if stop_after == "B2":
        return final(xa)
    phase_F(P, C, "f1a", xa, xa, xb, w_gu[1], w_dn[1], gains, 5, 0, HF)
    phase_F(P, C, "f1b", xa, xb, out, w_gu[1], w_dn[1], gains, 5, HF, NFC, final_row=6)
    P.finish()
    return nc


def prep_inputs(inputs):
    f = lambda a: np.ascontiguousarray(np.asarray(a, dtype=np.float32))
    w0 = f(inputs["l0_w_in"])
    q, k, v, qi, ki, wi, qm = np.split(w0, np.cumsum([768, 256, 256, 512, 64, 8])[:], axis=1)
    qp = np.concatenate([q[:, h * 64:(h + 1) * 64] for h in QPERM], axis=1)
    w_in0 = np.ascontiguousarray(np.concatenate([qp, k, qi, ki, ki, wi, v, qm], axis=1))
    w1 = f(inputs["l1_w_in"])
    parts = [w1[:, j * 256:(j + 1) * 256] for j in range(10)]
    w_in1 = np.ascontiguousarray(np.concatenate([parts[0], parts[1], parts[3], parts[4], parts[6], parts[7], parts[2], parts[5], parts[8], parts[9]], axis=1))
    gains = np.ascontiguousarray(np.stack([f(inputs[n]) for n in ("l0_norm_mix", "l0_norm_mem", "l0_norm_ffn", "l1_norm_mix", "l1_norm_mem",
                                                                 "l1_norm_ffn", "final_norm")], axis=0))
    fr64 = (np.float32(10000.0) ** (-np.arange(32, dtype=np.float32) / np.float32(32))).astype(np.float32)
    fr16 = (np.float32(10000.0) ** (-np.arange(16, dtype=np.float32) / np.float32(16))).astype(np.float32)
    freqs = np.ascontiguousarray(np.broadcast_to(np.concatenate([fr64, fr16])[None, :], (128, 48)).astype(np.float32))
    shared = dict(freqs=freqs, gains=gains, w_in0=w_in0, w_in1=w_in1, w_mkv0=f(inputs["l0_w_mem_kv"]), w_mkv1=f(inputs["l1_w_mem_kv"]),
                  w_out0=f(inputs["l0_w_out"]), w_out1=f(inputs["l1_w_out"]), w_gu0=f(inputs["l0_w_gate_up"]), w_gu1=f(inputs["l1_w_gate_up"]),
                  w_dn0=f(inputs["l0_w_down"]), w_dn1=f(inputs["l1_w_down"]))
    x = f(inputs["x"])
    mem = f(inputs["mem"])
    pos = np.ascontiguousarray(np.asarray(inputs["positions"], dtype=np.int32))
    maps = []
    for c in range(x.shape[0]):
        m = dict(shared)
        m["x"] = x[c]
        m["mem"] = mem[c]
        m["pos"] = np.ascontiguousarray(pos[c].reshape(NT, 128).T)
        maps.append(m)
    return maps


_NC_CACHE = {}


def kernel(**inputs):
    maps = prep_inputs(inputs)
    if "nc" not in _NC_CACHE:
        _NC_CACHE["nc"] = build_program()
    nc = _NC_CACHE["nc"]
    res = run_bass_kernel_spmd(nc, maps, core_ids=list(range(len(maps))))
    return np.stack([np.asarray(r["out"], dtype=np.float32) for r in res.results], axis=0)
```

```python
import contextlib
import numpy as np
import ml_dtypes
import concourse.bass as bass
import concourse.mybir as mybir
from concourse.bass_utils import run_bass_kernel_spmd

F32 = mybir.dt.float32
BF16 = mybir.dt.bfloat16
I32 = mybir.dt.int32
AF = mybir.ActivationFunctionType
ALU = mybir.AluOpType
AX = mybir.AxisListType

SAME_ENGINE_SYNC = True


class Tok:
    def __init__(self, name):
        self.name = name
        self.w = None
        self.r = {}


class Buf(Tok):
    def __init__(self, name, handle):
        super().__init__(name)
        self.h = handle

    def ap(self):
        return self.h[:]

    def __getitem__(self, idx):
        return self.h[idx]


class _Eng:
    def __init__(self, name, handle, sem):
        self.name = name
        self.h = handle
        self.sem = sem
        self.count = 0
        self.seen = {}


class Prog:
    def __init__(self, nc, n_dma_sems=8):
        self.nc = nc
        self.stack = contextlib.ExitStack()
        self.engs = {}
        for name in ("tensor", "vector", "scalar", "gpsimd", "sync"):
            sem = self.stack.enter_context(nc.semaphore("s_" + name))
            self.engs[name] = _Eng(name, getattr(nc, name), sem)
        self.dma_sems = {}
        for q in ("sync", "gpsimd", "scalar"):
            self.dma_sems[q] = [[self.stack.enter_context(nc.semaphore("d_%s%d" % (q, i))), 0] for i in range(n_dma_sems)]
        self.dma_rr = {"sync": 0, "gpsimd": 0, "scalar": 0}
        self.out_events = []
        self.n_inst = 0
        self.scopes = [self.stack]
        import os
        self.max_ops = int(os.environ["KMAXOPS"]) if "KMAXOPS" in os.environ else None

    @contextlib.contextmanager
    def scope(self):
        st = contextlib.ExitStack()
        self.scopes.append(st)
        try:
            yield
        finally:
            self.barrier()
            self.scopes.pop()
            st.close()

    def barrier(self):
        if getattr(self, "finished", False):
            return
        for eng in self.engs.values():
            for other in self.engs.values():
                if other is not eng and other.count > 0:
                    self._wait(eng, (other.name, other.sem, other.count))
            for q, pool in self.dma_sems.items():
                for i, slot in enumerate(pool):
                    if slot[1] > 0:
                        self._wait(eng, ("d_%s%d" % (q, i), slot[0], 16 * slot[1]))

    def tok(self, name):
        return Tok(name)

    def sb(self, name, shape, dtype):
        self.uid = getattr(self, "uid", 0) + 1
        name = "%s_u%d" % (name, self.uid)
        return Buf(name, self.scopes[-1].enter_context(self.nc.sbuf_tensor(name, list(shape), dtype)))

    def ps(self, name, shape, dtype):
        b = Buf(name, self.scopes[-1].enter_context(self.nc.psum_tensor(name, list(shape), dtype)))
        b.excl = True
        return b

    def dram(self, name, shape, dtype):
        return self.nc.dram_tensor(name, list(shape), dtype, kind="Internal").ap()

    def _wait(self, eng, ev):
        key, sem, val = ev
        if eng.seen.get(key, 0) >= val:
            return
        if key == eng.name and not (SAME_ENGINE_SYNC and eng.name != "tensor" and eng.name != "sync"):
            return
        eng.h.wait_ge(sem, val)
        eng.seen[key] = val

    def _deps(self, eng, reads, writes):
        for t in reads:
            if t.w is not None:
                self._wait(eng, t.w)
        for t in writes:
            if t.w is not None:
                self._wait(eng, t.w)
            for ev in t.r.values():
                self._wait(eng, ev)

    def _record(self, ev, reads, writes):
        for t in writes:
            t.w = ev
            t.r = {}
        for t in reads:
            if t in writes:
                continue
            t.r[ev[0]] = ev

    def op(self, engname, fn, reads=(), writes=()):
        if self.max_ops is not None and self.n_inst >= self.max_ops:
            return None
        eng = self.engs[engname]
        ex = [t for t in reads if getattr(t, "excl", False) and t not in writes]
        if ex:
            reads = [t for t in reads if t not in ex]
            writes = list(writes) + ex
        self._deps(eng, reads, writes)
        inst = fn(eng.h)
        eng.count += 1
        inst.then_inc(eng.sem, 1)
        ev = (eng.name, eng.sem, eng.count)
        self._record(ev, reads, writes)
        self.n_inst += 1
        return ev

    def dma(self, q, out_ap, in_ap, reads=(), writes=(), out=False, **kw):
        if self.max_ops is not None and self.n_inst >= self.max_ops:
            return None
        eng = self.engs[q]
        self._deps(eng, reads, writes)
        pool = self.dma_sems[q]
        i = self.dma_rr[q]
        self.dma_rr[q] = (i + 1) % len(pool)
        slot = pool[i]
        key = "d_%s%d" % (q, i)
        if slot[1] > 0:
            self._wait(eng, (key, slot[0], 16 * slot[1]))
        eng.h.dma_start(out=out_ap, in_=in_ap, **kw).then_inc(slot[0], 16)
        slot[1] += 1
        ev = (key, slot[0], 16 * slot[1])
        self._record(ev, reads, writes)
        if out:
            self.out_events.append(ev)
        self.n_inst += 1
        return ev

    def finish(self):
        eng = self.engs["sync"]
        for q, pool in self.dma_sems.items():
            for i, slot in enumerate(pool):
                if slot[1] > 0:
                    self._wait(eng, ("d_%s%d" % (q, i), slot[0], 16 * slot[1]))
        for name, e in self.engs.items():
            if name != "sync" and e.count > 0:
                self._wait(eng, (name, e.sem, e.count))
        self.finished = True
        for st in reversed(self.scopes):
            st.close()

    def make_identity(self, idt):
        tmp = self.sb(idt.name + "_i", [128, 128], I32)
        self.op("gpsimd", lambda e: e.iota(tmp.ap(), pattern=[[1, 128]], base=0, channel_multiplier=-1), writes=[tmp])
        self.op("vector", lambda e: e.tensor_scalar(out=idt.ap(), in0=tmp.ap(), scalar1=0.0, scalar2=None, op0=ALU.is_equal),
                reads=[tmp], writes=[idt])


S = 4096
D = 1024
NT = 32
DFF = 2816
NFC = 22
EPS = 1e-6
NEG = -1.0e30
MASKV = -30000.0
VW = 66
N_BISECT = 16
IDX_SCALE = (8 ** -0.5) * (64 ** -0.5)
QPERM = [0, 3, 1, 4, 2, 5, 6, 9, 7, 10, 8, 11]

SPEC0 = dict(
    ncols=2184,
    chunks=[(0, 512, [("rope64", 0, 8)]), (512, 512, [("rope64", 0, 8)]), (1024, 512, [("rope32", 0, 8)]),
            (1536, 136, [("rope32", 0, 2), ("wi", 128, 8)]), (1672, 512, [("copy", 0, 512)])],
    tcols=[128 * k for k in range(13)] + [1928, 2056],
    vcols=(1672, 1928),
)
SPEC1 = dict(
    ncols=2560,
    chunks=[(0, 512, [("rope64", 0, 8)]), (512, 512, [("rope64", 0, 8)]), (1024, 512, [("rope64", 0, 8)]),
            (1536, 512, [("copy", 0, 512)]), (2048, 512, [("copy", 0, 512)])],
    tcols=[128 * k for k in range(12)] + [2304, 2432],
    vcols=(1536, 2304),
)


class Ctx:
    pass


def load_weight(P, name, dram, nk, ncols, q="gpsimd"):
    W = P.sb(name, [128, nk, ncols], BF16)
    toks = [P.tok("%s_%d" % (name, k)) for k in range(nk)]
    for k in range(nk):
        c0 = 0
        while c0 < ncols:
            c1 = min(ncols, c0 + 2048)
            P.dma(q, W[:, k, c0:c1], dram[k * 128:(k + 1) * 128, c0:c1], writes=[toks[k]])
            c0 = c1
    return W, toks


def setup_consts(P, C, pos_in):
    C.ident = P.sb("ident", [128, 128], BF16)
    P.make_identity(C.ident)
    C.negthr = P.sb("negthr", [128, 1], F32)
    P.op("vector", lambda e: e.memset(C.negthr.ap(), -1.0e29), writes=[C.negthr])
    C.pow2 = P.sb("pow2", [128, N_BISECT + 2], F32)
    for k in range(N_BISECT + 2):
        P.op("gpsimd", lambda e: e.memset(C.pow2[:, k:k + 1], 2.0 ** (1 - k)), writes=[C.pow2])
    C.MdT = P.sb("MdT", [128, 128], BF16)
    C.MpT = P.sb("MpT", [128, 128], BF16)
    zt = P.sb("zt", [128, 128], F32)
    zm = P.sb("zm", [128, 128], F32)
    P.op("vector", lambda e: e.memset(zt.ap(), 0.0), writes=[zt])
    P.op("gpsimd", lambda e: e.affine_select(out=zm.ap(), in_=zt.ap(), pattern=[[1, 128]], compare_op=ALU.is_ge, fill=MASKV, base=0, channel_multiplier=-1),
         reads=[zt], writes=[zm])
    P.op("vector", lambda e: e.tensor_copy(C.MdT.ap(), zm.ap()), reads=[zm], writes=[C.MdT])
    P.op("gpsimd", lambda e: e.affine_select(out=zm.ap(), in_=zt.ap(), pattern=[[-1, 128]], compare_op=ALU.is_ge, fill=MASKV, base=0, channel_multiplier=1),
         reads=[zt, C.MdT], writes=[zm])
    P.op("vector", lambda e: e.tensor_copy(C.MpT.ap(), zm.ap()), reads=[zm], writes=[C.MpT])


def build_rope_tables(P, C, pos_in):
    C.cos64 = P.sb("cos64", [128, NT, 32], F32)
    C.sin64 = P.sb("sin64", [128, NT, 32], F32)
    C.nsin64 = P.sb("nsin64", [128, NT, 32], F32)
    C.cos16 = P.sb("cos16", [128, NT, 16], F32)
    C.sin16 = P.sb("sin16", [128, NT, 16], F32)
    C.nsin16 = P.sb("nsin16", [128, NT, 16], F32)
    with P.scope():
        posi = P.sb("posi", [128, NT], I32)
        posf = P.sb("posf", [128, NT], F32)
        P.dma("sync", posi.ap(), pos_in, writes=[posi])
        P.op("vector", lambda e: e.tensor_copy(posf.ap(), posi.ap()), reads=[posi], writes=[posf])
        for half, cosT, sinT, nsinT in ((32, C.cos64, C.sin64, C.nsin64), (16, C.cos16, C.sin16, C.nsin16)):
            n = NT * half
            fr = P.sb("fr%d" % half, [128, half], F32)
            a = P.sb("a%d" % half, [128, NT, half], F32)
            ki = P.sb("ki%d" % half, [128, NT, half], I32)
            kf = P.sb("kf%d" % half, [128, NT, half], F32)
            fr1 = P.sb("fr1%d" % half, [128, NT, half], F32)
            m1 = P.sb("m1%d" % half, [128, NT, half], F32)
            f0 = 0 if half == 32 else 32
            P.dma("sync", fr.ap(), C.freqs_in[:, f0:f0 + half], writes=[fr])
            P.op("vector", lambda e: e.tensor_tensor(out=a.ap(), in0=posf.ap().unsqueeze(2).broadcast_to([128, NT, half]),
                                                     in1=fr.ap().unsqueeze(1).broadcast_to([128, NT, half]), op=ALU.mult),
                 reads=[posf, fr], writes=[a])
            P.op("vector", lambda e: e.tensor_scalar(out=a.ap(), in0=a.ap(), scalar1=float(1.0 / (2.0 * np.pi)), scalar2=None, op0=ALU.mult),
                 reads=[a], writes=[a])
            for shift, outT, neg in ((0.0, sinT, False), (0.25, cosT, False), (0.5, nsinT, False)):
                src = a
                if shift != 0.0:
                    P.op("vector", lambda e: e.tensor_scalar(out=fr1.ap(), in0=a.ap(), scalar1=shift, scalar2=None, op0=ALU.add),
                         reads=[a], writes=[fr1])
                    src = fr1
                P.op("vector", lambda e: e.tensor_copy(ki.ap(), src.ap()), reads=[src], writes=[ki])
                P.op("vector", lambda e: e.tensor_copy(kf.ap(), ki.ap()), reads=[ki], writes=[kf])
                P.op("vector", lambda e: e.tensor_tensor(out=kf.ap(), in0=src.ap(), in1=kf.ap(), op=ALU.subtract), reads=[src, kf], writes=[kf])
                P.op("vector", lambda e: e.tensor_scalar(out=m1.ap(), in0=kf.ap(), scalar1=0.5, scalar2=None, op0=ALU.is_gt), reads=[kf], writes=[m1])
                P.op("vector", lambda e: e.tensor_tensor(out=kf.ap(), in0=kf.ap(), in1=m1.ap(), op=ALU.subtract), reads=[kf, m1], writes=[kf])
                P.op("vector", lambda e: e.tensor_scalar(out=m1.ap(), in0=kf.ap(), scalar1=-0.5, scalar2=None, op0=ALU.is_lt), reads=[kf], writes=[m1])
                P.op("vector", lambda e: e.tensor_tensor(out=kf.ap(), in0=kf.ap(), in1=m1.ap(), op=ALU.add), reads=[kf, m1], writes=[kf])
                P.op("scalar", lambda e: e.activation(out=outT.ap(), in_=kf.ap(), func=AF.Sin, scale=float(2.0 * np.pi) * (1.0 - 1e-6)),
                     reads=[kf], writes=[outT])


def alloc_norm_work(P, C, tag):
    W = Ctx()
    W.sqj = P.sb(tag + "sqj", [128, D], BF16)
    W.small = [[P.sb("%ssm%d_%d" % (tag, r, j), [128, 1], F32) for j in range(4)] for r in range(2)]
    W.hb = [P.sb("%shb%d" % (tag, r), [128, D], BF16) for r in range(2)]
    W.k = 0
    return W


def rmsnorm_to_hT(P, C, W, xt, gb, hT_ap, hT_tok, bank, evac_eng="scalar"):
    r = W.k % 2
    W.k += 1
    ssq, t1, t2, rstd = W.small[r]
    hb = W.hb[r]
    P.op("scalar", lambda e: e.activation(out=W.sqj.ap(), in_=xt.ap(), func=AF.Square, accum_out=ssq.ap()), reads=[xt], writes=[W.sqj, ssq])
    P.op("vector", lambda e: e.tensor_scalar(out=t1.ap(), in0=ssq.ap(), scalar1=1.0 / D, scalar2=EPS, op0=ALU.mult, op1=ALU.add), reads=[ssq], writes=[t1])
    P.op("scalar", lambda e: e.activation(out=t2.ap(), in_=t1.ap(), func=AF.Sqrt), reads=[t1], writes=[t2])
    P.op("vector", lambda e: e.reciprocal(out=rstd.ap(), in_=t2.ap()), reads=[t2], writes=[rstd])
    P.op("vector", lambda e: e.scalar_tensor_tensor(out=hb.ap(), in0=xt.ap(), scalar=rstd.ap(), in1=gb.ap(), op0=ALU.mult, op1=ALU.mult),
         reads=[xt, rstd, gb], writes=[hb])
    if hT_ap is None:
        return hb
    return norm_p2(P, C, hb, hT_ap, hT_tok, bank, evac_eng)


def norm_p2(P, C, hb, hT_ap, hT_tok, bank, evac_eng="scalar"):
    bbf = bank.ap().bitcast(BF16)
    for c in range(8):
        P.op("tensor", lambda e: e.transpose(bbf[:, c * 128:(c + 1) * 128], hb[:, c * 128:(c + 1) * 128], C.ident.ap()),
             reads=[hb, C.ident], writes=[bank])
    srcv = bbf if len(hT_ap.shape) == 2 else bbf.rearrange("p (c t) -> p c t", t=128)
    if evac_eng == "scalar":
        P.op("scalar", lambda e: e.activation(out=hT_ap, in_=srcv, func=AF.Copy), reads=[bank], writes=[hT_tok])
    else:
        P.op("vector", lambda e: e.tensor_copy(hT_ap, srcv), reads=[bank], writes=[hT_tok])


def load_gain(P, C, name, gains, row):
    gb = P.sb(name, [128, D], F32)
    P.dma("sync", gb.ap(), gains[row].partition_broadcast(128), writes=[gb])
    return gb


def phase_A(P, C, tag, xsrc, w_dram, gains, grow, spec, fT, vS, wiAll, pos_in):
    ncols = spec["ncols"]
    with P.scope():
        build_rope_tables(P, C, pos_in)
        Win, Wt = load_weight(P, tag + "Win", w_dram, 8, ncols)
        gb = load_gain(P, C, tag + "gbA", gains, grow)
        NW = alloc_norm_work(P, C, tag + "A")
        xts = [P.sb("%sxt%d" % (tag, r), [128, D], F32) for r in range(3)]
        hTs = [P.sb("%shT%d" % (tag, r), [128, D], BF16) for r in range(2)]
        pts = [P.sb("%spt%d" % (tag, r), [128, ncols], BF16) for r in range(2)]
        t1s = [P.sb("%st1_%d" % (tag, r), [128, 512], F32) for r in range(2)]
        t2s = [P.sb("%st2_%d" % (tag, r), [128, 512], F32) for r in range(2)]
        ntc = len(spec["tcols"])
        fTs = [P.sb("%sfTs%d" % (tag, r), [128, ntc, 128], BF16) for r in range(2)]
        fT_r = fT.rearrange("c p t -> p c t")
        cc = 0
        hbs = {}

        def ld(i):
            xt = xts[i % 3]
            P.dma("sync", xt.ap(), xsrc[i * 128:(i + 1) * 128, :], writes=[xt])

        def n1(i):
            hbs[i] = rmsnorm_to_hT(P, C, NW, xts[i % 3], gb, None, None, None)

        def n2(i):
            norm_p2(P, C, hbs.pop(i), hTs[i % 2].ap(), hTs[i % 2], C.banks[0], evac_eng="scalar")

        def featT(i):
            _featT_body(P, C, spec, pts, fTs, fT_r, vS, ntc, i)

        ld(0)
        ld(1)
        n1(0)
        n2(0)
        for i in range(NT):
            hT = hTs[i % 2]
            pt = pts[i % 2]
            if i + 2 < NT:
                ld(i + 2)
            if i + 1 < NT:
                n1(i + 1)
            for (col0, width, handlers) in spec["chunks"]:
                bank = C.banks[1 + (cc % 4)]
                t1 = t1s[cc % 2]
                t2 = t2s[cc % 2]
                cc += 1
                for k in range(8):
                    P.op("tensor", lambda e: e.matmul(bank[:, 0:width], lhsT=hT[:, k * 128:(k + 1) * 128], rhs=Win[:, k, col0:col0 + width],
                                                      start=(k == 0), stop=(k == 7)), reads=[hT, Wt[k]], writes=[bank])
                for (kind, l0, n) in handlers:
                    if kind == "rope64":
                        nh = n
                        w = nh * 64
                        xv2 = bank[:, l0:l0 + w].rearrange("p (h d) -> p h d", d=32)
                        xv = bank[:, l0:l0 + w].rearrange("p (h d) -> p h d", d=64)
                        t1v2 = t1[:, 0:w].rearrange("p (h d) -> p h d", d=32)
                        t2v = t2[:, 0:w].rearrange("p (h d) -> p h d", d=64)
                        cosb = C.cos64[:, i:i + 1, :].broadcast_to([128, 2 * nh, 32])
                        sinb = C.sin64[:, i:i + 1, :].broadcast_to([128, nh, 32])
                        nsinb = C.nsin64[:, i:i + 1, :].broadcast_to([128, nh, 32])
                        P.op("vector", lambda e: e.tensor_tensor(out=t1v2, in0=xv2, in1=cosb, op=ALU.mult), reads=[bank, C.cos64], writes=[t1])
                        P.op("vector", lambda e: e.tensor_tensor(out=t2v[:, :, 0:32], in0=xv[:, :, 32:64], in1=nsinb, op=ALU.mult),
                             reads=[bank, C.nsin64], writes=[t2])
                        P.op("vector", lambda e: e.tensor_tensor(out=t2v[:, :, 32:64], in0=xv[:, :, 0:32], in1=sinb, op=ALU.mult),
                             reads=[bank, C.sin64], writes=[t2])
                        P.op("gpsimd", lambda e: e.tensor_tensor(out=pt[:, col0 + l0:col0 + l0 + w], in0=t1[:, 0:w], in1=t2[:, 0:w], op=ALU.add),
                             reads=[t1, t2], writes=[pt])
                    elif kind == "rope32":
                        nh = n
                        w = nh * 64
                        xv = bank[:, l0:l0 + w].rearrange("p (h d) -> p h d", d=64)
                        xr4 = bank[:, l0:l0 + w].rearrange("p (h t d) -> p h t d", t=4, d=16)
                        t1r4 = t1[:, 0:w].rearrange("p (h t d) -> p h t d", t=4, d=16)
                        t2v = t2[:, 0:w].rearrange("p (h d) -> p h d", d=64)
                        t1v = t1[:, 0:w].rearrange("p (h d) -> p h d", d=64)
                        ptv = pt[:, col0 + l0:col0 + l0 + w].rearrange("p (h d) -> p h d", d=64)
                        cosb = C.cos16[:, i:i + 1, :].unsqueeze(1).broadcast_to([128, nh, 2, 16])
                        sinb = C.sin16[:, i:i + 1, :].broadcast_to([128, nh, 16])
                        nsinb = C.nsin16[:, i:i + 1, :].broadcast_to([128, nh, 16])
                        P.op("vector", lambda e: e.tensor_tensor(out=t1r4[:, :, 0:2, :], in0=xr4[:, :, 0:2, :], in1=cosb, op=ALU.mult),
                             reads=[bank, C.cos16], writes=[t1])
                        P.op("vector", lambda e: e.tensor_tensor(out=t2v[:, :, 0:16], in0=xv[:, :, 16:32], in1=nsinb, op=ALU.mult),
                             reads=[bank, C.nsin16], writes=[t2])
                        P.op("vector", lambda e: e.tensor_tensor(out=t2v[:, :, 16:32], in0=xv[:, :, 0:16], in1=sinb, op=ALU.mult),
                             reads=[bank, C.sin16], writes=[t2])
                        P.op("gpsimd", lambda e: e.tensor_tensor(out=ptv[:, :, 0:32], in0=t1v[:, :, 0:32], in1=t2v[:, :, 0:32], op=ALU.add),
                             reads=[t1, t2], writes=[pt])
                        P.op("scalar", lambda e: e.activation(out=ptv[:, :, 32:64], in_=xv[:, :, 32:64], func=AF.Copy), reads=[bank], writes=[pt])
                    elif kind == "copy":
                        P.op("scalar", lambda e: e.activation(out=pt[:, col0 + l0:col0 + l0 + n], in_=bank[:, l0:l0 + n], func=AF.Copy),
                             reads=[bank], writes=[pt])
                    elif kind == "wi":
                        P.op("scalar", lambda e: e.activation(out=wiAll[:, i, :], in_=bank[:, l0:l0 + n], func=AF.Copy, scale=float(IDX_SCALE)),
                             reads=[bank], writes=[wiAll])
            if i + 1 < NT:
                n2(i + 1)
            if i >= 1:
                featT(i - 1)
        featT(NT - 1)


def _featT_body(P, C, spec, pts, fTs, fT_r, vS, ntc, i):
            pt = pts[i % 2]
            fts = fTs[i % 2]
            for g0 in range(0, ntc, 8):
                g1 = min(ntc, g0 + 8)
                bank = C.banks[5 + (g0 // 8)]
                bbf = bank.ap().bitcast(BF16)
                for k in range(g0, g1):
                    c0 = spec["tcols"][k]
                    P.op("tensor", lambda e: e.transpose(bbf[:, (k - g0) * 128:(k - g0 + 1) * 128], pt[:, c0:c0 + 128], C.ident.ap()),
                         reads=[pt, C.ident], writes=[bank])
                eng = "vector" if g0 == 0 else "scalar"
                if eng == "vector":
                    P.op("vector", lambda e: e.tensor_copy(fts[:, g0:g1, :], bbf[:, 0:(g1 - g0) * 128].rearrange("p (c t) -> p c t", t=128)),
                         reads=[bank], writes=[fts])
                else:
                    P.op("scalar", lambda e: e.activation(out=fts[:, g0:g1, :], in_=bbf[:, 0:(g1 - g0) * 128].rearrange("p (c t) -> p c t", t=128),
                                                          func=AF.Copy), reads=[bank], writes=[fts])
            P.dma("sync", fT_r[:, :, i * 128:(i + 1) * 128], fts.ap(), reads=[fts])
            v0, v1 = spec["vcols"]
            P.dma("sync", vS[i * 128:(i + 1) * 128, :], pt[:, v0:v1], reads=[pt])


def phase_M(P, C, tag, mem_in, w_dram, gains, grow, kmT, Vm):
    with P.scope():
        Wm, Wt = load_weight(P, tag + "Wm", w_dram, 8, 512)
        gb = load_gain(P, C, tag + "gbM", gains, grow)
        NW = alloc_norm_work(P, C, tag + "M")
        xts = [P.sb("%smx%d" % (tag, r), [128, D], F32) for r in range(2)]
        hTs = [P.sb("%smhT%d" % (tag, r), [128, D], BF16) for r in range(2)]
        kb16 = [P.sb("%skb16_%d" % (tag, r), [128, 256], BF16) for r in range(2)]
        P.op("gpsimd", lambda e: e.memset(Vm.ap(), 1.0), writes=[Vm])
        for mb in range(2):
            xt, hT = xts[mb], hTs[mb]
            P.dma("sync", xt.ap(), mem_in[mb * 128:(mb + 1) * 128, :], writes=[xt])
            rmsnorm_to_hT(P, C, NW, xt, gb, hT.ap(), hT, C.banks[0])
            bank = C.banks[1 + mb]
            for k in range(8):
                P.op("tensor", lambda e: e.matmul(bank.ap(), lhsT=hT[:, k * 128:(k + 1) * 128], rhs=Wm[:, k, :], start=(k == 0), stop=(k == 7)),
                     reads=[hT, Wt[k]], writes=[bank])
            P.op("scalar", lambda e: e.activation(out=kb16[mb].ap(), in_=bank[:, 0:256], func=AF.Copy), reads=[bank], writes=[kb16[mb]])
            P.op("vector", lambda e: e.tensor_copy(Vm[:, mb, :, 0:64], bank[:, 256:512].rearrange("p (h d) -> p h d", d=64)),
                 reads=[bank], writes=[Vm])
            tb = C.banks[3 + mb]
            tbf = tb.ap().bitcast(BF16)
            for c in range(2):
                P.op("tensor", lambda e: e.transpose(tbf[:, c * 128:(c + 1) * 128], kb16[mb][:, c * 128:(c + 1) * 128], C.ident.ap()),
                     reads=[kb16[mb], C.ident], writes=[tb])
            P.op("vector", lambda e: e.tensor_copy(kmT[:, :, mb * 128:(mb + 1) * 128], tbf[:, 0:256].rearrange("p (c t) -> p c t", t=128)),
                 reads=[tb], writes=[kmT])


def attn_finish(P, C, W, xsrc, xdst, i, nheads, Wout, Wot, nkc):
    r = i % 2
    attn = W.attn[r]
    rec = W.rec[r]
    xt = W.xts[r]
    P.dma("sync", xt.ap(), xsrc[i * 128:(i + 1) * 128, :], writes=[xt])
    h0 = 0
    for (bank, oap, nh) in W.osrc(r):
        ov = oap.rearrange("p (h d) -> p h d", d=65)
        P.op("vector", lambda e: e.reciprocal(out=rec[:, h0:h0 + nh], in_=ov[:, :, 64]), reads=[bank], writes=[rec])
        P.op("vector", lambda e: e.tensor_tensor(out=attn[:, h0 * 64:(h0 + nh) * 64].rearrange("p (h d) -> p h d", d=64), in0=ov[:, :, 0:64],
                                                 in1=rec[:, h0:h0 + nh].unsqueeze(2).broadcast_to([128, nh, 64]), op=ALU.mult),
             reads=[bank, rec], writes=[attn])
        h0 += nh
    tb = W.tbank
    tbf = tb.ap().bitcast(BF16)
    aT = W.attnT[r]
    for c in range(nkc):
        P.op("tensor", lambda e: e.transpose(tbf[:, c * 128:(c + 1) * 128], attn[:, c * 128:(c + 1) * 128], C.ident.ap()),
             reads=[attn, C.ident], writes=[tb])
    P.op("scalar", lambda e: e.activation(out=aT[:, 0:nkc * 128], in_=tbf[:, 0:nkc * 128], func=AF.Copy), reads=[tb], writes=[aT])
    xn = W.xn[r]
    for half in range(2):
        yb = W.ybanks[half]
        for c in range(nkc):
            P.op("tensor", lambda e: e.matmul(yb.ap(), lhsT=aT[:, c * 128:(c + 1) * 128], rhs=Wout[:, c, half * 512:(half + 1) * 512],
                                              start=(c == 0), stop=(c == nkc - 1)), reads=[aT, Wot[c]], writes=[yb])
        P.op("vector", lambda e: e.tensor_tensor(out=xn[:, half * 512:(half + 1) * 512], in0=yb.ap(), in1=xt[:, half * 512:(half + 1) * 512], op=ALU.add),
             reads=[yb, xt], writes=[xn])
    P.dma("sync", xdst[i * 128:(i + 1) * 128, :], xn.ap(), reads=[xn])


def mem_heads(P, C, W, qall, qch0, kmT, Vm, obank, sbank, r):
    PT = W.PTm[r]
    for half in range(2):
        sb_ = sbank[half]
        for hh in range(2):
            hm = half * 2 + hh
            for mb in range(2):
                j = hh * 2 + mb
                P.op("tensor", lambda e: e.matmul(sb_[:, j * 128:(j + 1) * 128], lhsT=kmT[:, hm // 2, mb * 128:(mb + 1) * 128],
                                                  rhs=qall[:, qch0 + hm, :], start=True, stop=True),
                     reads=[kmT, qall], writes=[sb_])
        P.op("scalar", lambda e: e.activation(out=PT[:, half * 512:(half + 1) * 512], in_=sb_.ap(), func=AF.Exp, scale=0.125),
             reads=[sb_], writes=[PT])
    for hm in range(4):
        for mb in range(2):
            j = hm * 2 + mb
            P.op("tensor", lambda e: e.matmul(obank[:, hm * 65:(hm + 1) * 65], lhsT=PT[:, j * 128:(j + 1) * 128], rhs=Vm[:, mb, hm, 0:65],
                                              start=(mb == 0), stop=(mb == 1)), reads=[PT, Vm], writes=[obank])


def alloc_finish_work(P, C, tag, obanks, tbank, ybanks):
    W = Ctx()
    W.attn = [P.sb("%sattn%d" % (tag, r), [128, D], BF16) for r in range(2)]
    W.attnT = [P.sb("%sattnT%d" % (tag, r), [128, D], BF16) for r in range(2)]
    W.rec = [P.sb("%srec%d" % (tag, r), [128, 16], F32) for r in range(2)]
    W.xts = [P.sb("%sfx%d" % (tag, r), [128, D], F32) for r in range(2)]
    W.xn = [P.sb("%sxn%d" % (tag, r), [128, D], F32) for r in range(2)]
    W.PTm = [P.sb("%sPTm%d" % (tag, r), [128, 1024], BF16) for r in range(2)]
    W.osrc = lambda r: [(bk, bk[:, 0:nh * 65], nh) for (bk, nh) in obanks]
    W.tbank = tbank
    W.ybanks = ybanks
    return W


def phase_B0(P, C, xsrc, xdst, fT, vS, wiAll, kmT, Vm, wout_dram, nblocks=NT):
    with P.scope():
        Wout, Wot = load_weight(P, "Wout0", wout_dram, 8, D)
        kT = P.sb("kT", [128, 2, S], BF16)
        kiT = P.sb("kiT", [128, S], BF16)
        Va = P.sb("Va", [128, NT, 4, VW], BF16)
        P.op("gpsimd", lambda e: e.memset(Va.ap(), 1.0), writes=[Va])
        for c in range(2):
            P.dma("sync", kT[:, c, :], fT[6 + c], writes=[kT])
        P.dma("sync", kiT.ap(), fT[12], writes=[kiT])
        vr = vS.rearrange("(i p) (g d) -> p i g d", p=128, d=64)
        for i0 in range(NT):
            P.dma("sync", Va[:, i0, :, 0:64], vr[:, i0, :, :], writes=[Va])
        scores = [P.sb("score%d" % r, [128, S], F32) for r in range(2)]
        junk = P.sb("junk", [128, S], BF16)
        Bs = [P.sb("Bm%d" % r, [128, S], BF16) for r in range(2)]
        Rs = [P.sb("R%d" % r, [128, 512], BF16) for r in range(4)]
        Wdgs = [P.sb("Wdg%d" % r, [128, 8, 128], BF16) for r in range(2)]
        PTs = [P.sb("PT%d" % r, [128, 512], BF16) for r in range(3)]
        qalls = [P.sb("qall%d" % r, [128, 24, 128], BF16) for r in range(4)]
        for r in range(4):
            P.op("gpsimd", lambda e: e.memset(qalls[r].ap(), 0.0), writes=[qalls[r]])
        sm = [[P.sb("bs%d_%d" % (r, j), [128, 1], F32) for j in range(6)] for r in range(2)]
        wtabs = [P.sb("wtab%d" % r, [128, N_BISECT + 2], F32) for r in range(2)]
        FW = alloc_finish_work(P, C, "b0", [(C.banks[3], 6), (C.banks[4], 6), (C.banks[5], 4)], C.banks[0], [C.banks[1], C.banks[0]])
        fT_r = fT.rearrange("c p t -> p c t")
        cnts = dict(lc=0, sc=0)

        def idx_steps(b):
            r = b % 2
            N = 128 * (b + 1)
            qall = qalls[b % 4]
            score = scores[r]
            q0 = b * 128
            Wdg = Wdgs[r]
            accb = C.banks[7]
            steps = []

            def loads():
                for (slot0, nch, ch0) in ((0, 6, 0), (12, 4, 8), (20, 2, 13)):
                    for base in (0, 64):
                        P.dma("sync", qall[base:base + 64, slot0 + base // 64:slot0 + 2 * nch:2, :], fT_r[base:base + 64, ch0:ch0 + nch, q0:q0 + 128],
                              writes=[qall])
                for h in range(8):
                    P.op("gpsimd", lambda e: e.tensor_scalar(out=Wdg[:, h, :], in0=C.ident.ap(), scalar1=wiAll[:, b, h:h + 1], scalar2=None, op0=ALU.mult),
                         reads=[C.ident, wiAll], writes=[Wdg])
            steps.append(loads)
            jobs = [(c0, h) for c0 in range(0, N, 512) for h in range(8)]
            state = dict(prev=None)

            def mk(job):
                def run():
                    prev = state["prev"]
                    if job is not None:
                        c0, h = job
                        wc = min(512, N - c0)
                        lc = cnts["lc"]
                        bank = C.banks[2] if lc % 2 == 0 else C.banks[6]
                        R = Rs[lc % 4]
                        cnts["lc"] = lc + 1
                        P.op("tensor", lambda e: e.matmul(bank[:, 0:wc], lhsT=qall[:, 12 + h, :], rhs=kiT[:, c0:c0 + wc],
                                                          start=True, stop=True), reads=[qall, kiT], writes=[bank])
                        P.op("scalar", lambda e: e.activation(out=R[:, 0:wc], in_=bank[:, 0:wc], func=AF.Relu), reads=[bank], writes=[R])
                    if prev is not None:
                        (c0_, h_), R_ = prev
                        wc_ = min(512, N - c0_)
                        P.op("tensor", lambda e: e.matmul(accb[:, 0:wc_], lhsT=Wdg[:, h_, :], rhs=R_[:, 0:wc_], start=(h_ == 0), stop=(h_ == 7)),
                             reads=[Wdg, R_], writes=[accb])
                        if h_ == 7:
                            P.op("scalar", lambda e: e.activation(out=score[:, c0_:c0_ + wc_], in_=accb[:, 0:wc_], func=AF.Copy), reads=[accb], writes=[score])
                    state["prev"] = (job, R) if job is not None else None
                return run
            for job in jobs + [None]:
                steps.append(mk(job))
            return steps

        def thr_steps(b):
            r = b % 2
            N = 128 * (b + 1)
            score = scores[r]
            B = Bs[r]
            q0 = b * 128
            mn, mx, mid, cnt, aa, thr = sm[r]
            wtab = wtabs[r]
            steps = []

            def pre():
                if b >= 2:
                    P.op("vector", lambda e: e.tensor_reduce(out=mn.ap(), in_=score[:, 0:N - 128], axis=AX.X, op=ALU.min), reads=[score], writes=[mn])
                P.op("gpsimd", lambda e: e.affine_select(out=score[:, q0:q0 + 128], in_=score[:, q0:q0 + 128], pattern=[[-1, 128]], compare_op=ALU.is_ge,
                                                        fill=NEG, base=0, channel_multiplier=1), reads=[score], writes=[score])
                if b >= 2:
                    P.op("vector", lambda e: e.tensor_reduce(out=mx.ap(), in_=score[:, 0:N], axis=AX.X, op=ALU.max), reads=[score], writes=[mx])
                    P.op("vector", lambda e: e.tensor_tensor(out=aa.ap(), in0=mx.ap(), in1=mn.ap(), op=ALU.subtract), reads=[mx, mn], writes=[aa])
                    P.op("vector", lambda e: e.tensor_scalar(out=wtab.ap(), in0=C.pow2.ap(), scalar1=aa.ap(), scalar2=0.5, op0=ALU.mult, op1=ALU.mult),
                         reads=[C.pow2, aa], writes=[wtab])
                    P.op("vector", lambda e: e.tensor_tensor(out=mid.ap(), in0=mn.ap(), in1=wtab[:, 1:2], op=ALU.add), reads=[mn, wtab], writes=[mid])
            steps.append(pre)
            if b >= 2:
                def mk(k):
                    def run():
                        P.op("vector", lambda e: e.tensor_scalar(out=junk[:, 0:N], in0=score[:, 0:N], scalar1=mid.ap(), scalar2=None, op0=ALU.is_ge, op1=ALU.add,
                                                                 accum_out=cnt.ap()), reads=[score, mid], writes=[junk, cnt])
                        P.op("vector", lambda e: e.tensor_scalar(out=aa.ap(), in0=cnt.ap(), scalar1=255.5, scalar2=-0.5, op0=ALU.is_ge, op1=ALU.add),
                             reads=[cnt], writes=[aa])
                        P.op("vector", lambda e: e.scalar_tensor_tensor(out=mid.ap(), in0=aa.ap(), scalar=wtab[:, k + 1:k + 2], in1=mid.ap(), op0=ALU.mult, op1=ALU.add),
                             reads=[aa, wtab, mid], writes=[mid])
                    return run
                for k in range(N_BISECT):
                    steps.append(mk(k))

            def post():
                if b >= 2:
                    P.op("vector", lambda e: e.tensor_tensor(out=thr.ap(), in0=mid.ap(), in1=wtab[:, N_BISECT + 1:N_BISECT + 2], op=ALU.subtract),
                         reads=[mid, wtab], writes=[thr])
                    thr_t = thr
                else:
                    thr_t = C.negthr
                P.op("vector", lambda e: e.tensor_scalar(out=B[:, 0:N], in0=score[:, 0:N], scalar1=thr_t.ap(), scalar2=MASKV, op0=ALU.is_lt, op1=ALU.mult),
                     reads=[score, thr_t], writes=[B])
                if C.dbg is not None and b == C.dbg_block:
                    P.dma("sync", C.dbg["score"], score.ap(), reads=[score])
                    P.dma("sync", C.dbg["thr"], thr_t.ap(), reads=[thr_t])
                    P.dma("sync", C.dbg["Bm"], B.ap(), reads=[B])
            steps.append(post)
            return steps

        def interleave(sa, sb_):
            na, nb_ = len(sa), len(sb_)
            j = 0
            for i, s in enumerate(sa):
                s()
                tgt = ((i + 1) * nb_) // max(na, 1)
                while j < tgt:
                    sb_[j]()
                    j += 1
            while j < nb_:
                sb_[j]()
                j += 1

        def attend(b):
            sc = cnts["sc"]
            r = b % 2
            qall = qalls[b % 4]
            B = Bs[r]
            jobs = []
            for h in range(12):
                pos = QPERM.index(h)
                g = h // 3
                obank = C.banks[3 + h // 6]
                ocol = (h % 6) * 65
                for kb0 in range(0, b + 1, 4):
                    kb1 = min(b + 1, kb0 + 4)
                    jobs.append((pos, g, obank, ocol, kb0, kb1))
            prev = None
            for job in jobs + [None]:
                if job is not None:
                    pos, g, obank, ocol, kb0, kb1 = job
                    sbank = C.banks[sc % 2]
                    PT = PTs[sc % 3]
                    sc += 1
                    for kb in range(kb0, kb1):
                        j = kb - kb0
                        P.op("tensor", lambda e: e.matmul(sbank[:, j * 128:(j + 1) * 128], lhsT=kT[:, g // 2, kb * 128:(kb + 1) * 128],
                                                          rhs=qall[:, pos, :], start=True, stop=False), reads=[kT, qall], writes=[sbank])
                        P.op("tensor", lambda e: e.matmul(sbank[:, j * 128:(j + 1) * 128], lhsT=B[:, kb * 128:(kb + 1) * 128], rhs=C.ident.ap(),
                                                          start=False, stop=True), reads=[B, C.ident], writes=[sbank])
                    nw = (kb1 - kb0) * 128
                    P.op("scalar", lambda e: e.activation(out=PT[:, 0:nw], in_=sbank[:, 0:nw], func=AF.Exp, scale=0.125), reads=[sbank], writes=[PT])
                if prev is not None:
                    (pos_, g_, obank_, ocol_, kb0_, kb1_), PT_ = prev
                    for kb in range(kb0_, kb1_):
                        j = kb - kb0_
                        P.op("tensor", lambda e: e.matmul(obank_[:, ocol_:ocol_ + 65], lhsT=PT_[:, j * 128:(j + 1) * 128], rhs=Va[:, kb, g_, 0:65],
                                                          start=(kb == 0), stop=(kb == b)), reads=[PT_, Va], writes=[obank_])
                prev = (job, PT) if job is not None else None
            cnts["sc"] = sc
            mem_heads(P, C, FW, qall, 20, kmT, Vm, C.banks[5], [C.banks[0], C.banks[1]], r)

        KPRE = 9
        idx_lists = {}

        def idx_take(b, n=None):
            if b >= nblocks:
                return []
            if b not in idx_lists:
                idx_lists[b] = idx_steps(b)
            lst = idx_lists[b]
            k = len(lst) if n is None else min(n, len(lst))
            out_ = lst[:k]
            del lst[:k]
            return out_

        for s in idx_take(0):
            s()
        interleave(thr_steps(0), idx_take(1))
        for s in idx_take(2, KPRE):
            s()
        for b in range(nblocks):
            if b + 1 < nblocks:
                interleave(thr_steps(b + 1), idx_take(b + 2))
            attend(b)
            for s in idx_take(b + 3, KPRE):
                s()
            attn_finish(P, C, FW, xsrc, xdst, b, 16, Wout, Wot, 8)


def phase_F(P, C, tag, xnorm, xbase, xdst, wgu_dram, wd_dram, gains, grow, f0, f1, final_row=None, ntiles=NT):
    nf = f1 - f0
    with P.scope():
        Wgu = P.sb(tag + "Wgu", [128, 8, 2 * nf * 128], BF16)
        Wgt = [P.tok("%sWgt%d" % (tag, k)) for k in range(8)]
        for k in range(8):
            for half in range(2):
                c0 = half * DFF + f0 * 128
                P.dma("gpsimd", Wgu[:, k, half * nf * 128:(half + 1) * nf * 128], wgu_dram[k * 128:(k + 1) * 128, c0:c0 + nf * 128], writes=[Wgt[k]])
        Wd = P.sb(tag + "Wd", [128, nf, D], BF16)
        Wdt = [P.tok("%sWdt%d" % (tag, k)) for k in range(nf)]
        for k in range(nf):
            P.dma("gpsimd", Wd[:, k, :], wd_dram[(f0 + k) * 128:(f0 + k + 1) * 128, :], writes=[Wdt[k]])
        gb = load_gain(P, C, tag + "gbF", gains, grow)
        gfin = load_gain(P, C, tag + "gfin", gains, final_row) if final_row is not None else None
        NW = alloc_norm_work(P, C, tag + "F")
        xts = [P.sb("%sFx%d" % (tag, r), [128, D], F32) for r in range(2)]
        hT2 = [P.sb("%sFhT%d" % (tag, r), [128, 8, 256], BF16) for r in range(2)]
        hTt = [[P.tok("%sFhTt%d_%d" % (tag, r, t)) for t in range(2)] for r in range(2)]
        actT = [P.sb("%sactT%d" % (tag, r), [128, nf, 256], BF16) for r in range(2)]
        sg = [P.sb("%ssg%d" % (tag, r), [128, 256], F32) for r in range(2)]
        xn = [P.sb("%sFxn%d" % (tag, r), [128, D], F32) for r in range(4)]
        fsm = [[P.sb("%sfs%d_%d" % (tag, r, j), [128, 1], F32) for j in range(4)] for r in range(2)]
        fj = P.sb(tag + "fj", [128, D], BF16)
        gc = 0
        hbs = {}

        def norm1(G):
            for t in range(2):
                i = 2 * G + t
                xt = xts[i % 2]
                P.dma("sync", xt.ap(), xnorm[i * 128:(i + 1) * 128, :], writes=[xt])
                hbs[(G, t)] = rmsnorm_to_hT(P, C, NW, xt, gb, None, None, None)
                xo = xn[i % 4]
                P.dma("sync", xo.ap(), xbase[i * 128:(i + 1) * 128, :], writes=[xo])

        def norm2(G):
            for t in range(2):
                norm_p2(P, C, hbs.pop((G, t)), hT2[G % 2][:, :, t * 128:(t + 1) * 128], hTt[G % 2][t], C.banks[0],
                        evac_eng="scalar" if t == 0 else "vector")

        NG = ntiles // 2
        norm1(0)
        norm2(0)
        for G in range(NG):
            hT = hT2[G % 2]
            aT = actT[G % 2]
            for fc in range(nf):
                if fc == nf // 2 and G + 1 < NG:
                    norm1(G + 1)
                bank = C.banks[1 + (gc % 3)]
                s_ = sg[gc % 2]
                gc += 1
                for half in range(2):
                    coff = half * nf * 128 + fc * 128
                    for k in range(8):
                        P.op("tensor", lambda e: e.matmul(bank[:, half * 256:(half + 1) * 256], lhsT=Wgu[:, k, coff:coff + 128], rhs=hT[:, k, :],
                                                          start=(k == 0), stop=(k == 7)), reads=[Wgt[k]] + hTt[G % 2], writes=[bank])
                P.op("scalar", lambda e: e.activation(out=s_.ap(), in_=bank[:, 0:256], func=AF.Silu), reads=[bank], writes=[s_])
                P.op("vector", lambda e: e.tensor_tensor(out=aT[:, fc, :], in0=bank[:, 256:512], in1=s_.ap(), op=ALU.mult), reads=[bank, s_], writes=[aT])
            if G + 1 < NG:
                norm2(G + 1)
            for t in range(2):
                i = 2 * G + t
                xo = xn[i % 4]
                for half in range(2):
                    yb = C.banks[4 + (2 * t + half) % 4]
                    for fc in range(nf):
                        P.op("tensor", lambda e: e.matmul(yb.ap(), lhsT=aT[:, fc, t * 128:(t + 1) * 128], rhs=Wd[:, fc, half * 512:(half + 1) * 512],
                                                          start=(fc == 0), stop=(fc == nf - 1)), reads=[aT, Wdt[fc]], writes=[yb])
                    P.op("vector", lambda e: e.tensor_tensor(out=xo[:, half * 512:(half + 1) * 512], in0=yb.ap(), in1=xo[:, half * 512:(half + 1) * 512], op=ALU.add),
                         reads=[yb, xo], writes=[xo])
                if gfin is not None:
                    ssq, t1, t2, rstd = fsm[i % 2]
                    P.op("scalar", lambda e: e.activation(out=fj.ap(), in_=xo.ap(), func=AF.Square, accum_out=ssq.ap()), reads=[xo], writes=[fj, ssq])
                    P.op("vector", lambda e: e.tensor_scalar(out=t1.ap(), in0=ssq.ap(), scalar1=1.0 / D, scalar2=EPS, op0=ALU.mult, op1=ALU.add), reads=[ssq], writes=[t1])
                    P.op("scalar", lambda e: e.activation(out=t2.ap(), in_=t1.ap(), func=AF.Sqrt), reads=[t1], writes=[t2])
                    P.op("vector", lambda e: e.reciprocal(out=rstd.ap(), in_=t2.ap()), reads=[t2], writes=[rstd])
                    P.op("vector", lambda e: e.scalar_tensor_tensor(out=xo.ap(), in0=xo.ap(), scalar=rstd.ap(), in1=gfin.ap(), op0=ALU.mult, op1=ALU.mult),
                         reads=[xo, rstd, gfin], writes=[xo])
                P.dma("sync", xdst[i * 128:(i + 1) * 128, :], xo.ap(), reads=[xo], out=(gfin is not None))


def phase_B1(P, C, fT, vS, Og):
    for g, dil in enumerate((1, 4, 16)):
        nb = NT // dil
        with P.scope():
            qz = P.sb("qz1", [128, 4, S], BF16)
            kTg = P.sb("kTg", [128, 2, S], BF16)
            Vg = P.sb("Vg", [128, NT, 4, VW], BF16)
            P.op("gpsimd", lambda e: e.memset(qz.ap(), 0.0), writes=[qz])
            P.op("vector", lambda e: e.memset(Vg.ap(), 1.0), writes=[Vg])
            for c in range(2):
                for hh in range(2):
                    base = hh * 64
                    P.dma("sync", qz[base:base + 64, 2 * c + hh, :], fT[4 * g + c][base:base + 64, :], writes=[qz])
                P.dma("sync", kTg[:, c, :], fT[4 * g + 2 + c], writes=[kTg])
            vg = vS[:, g * 256:(g + 1) * 256].rearrange("(mb p dd) (h e) -> dd p mb h e", p=128, dd=dil, e=64)
            for r in range(dil):
                for m0 in range(nb):
                    P.dma("sync", Vg[:, r * nb + m0, :, 0:64], vg[r][:, m0, :, :], writes=[Vg])
            PTs = [P.sb("PTg%d" % k, [128, 512], BF16) for k in range(3)]
            Os = [P.sb("Os%d" % k, [128, 260], F32) for k in range(2)]
            Ogr = Og[g].rearrange("(m dd) c -> dd m c", dd=dil)
            sc = 0
            jobs = []
            qb = 0
            for r in range(dil):
                for mb in range(nb):
                    for hp in range(2):
                        jobs.append((r, mb, hp, qb))
                    qb += 1
            prev = None
            for job in jobs + [None]:
                if job is not None:
                    r, mb, hp, qb = job
                    qsl = slice(mb * 128 * dil + r, (mb * 128 + 127) * dil + r + 1, dil)
                    kbs = ([mb - 1] if mb > 0 else []) + [mb]
                    sbank = C.banks[sc % 3]
                    PT = PTs[sc % 3]
                    sc += 1
                    tiles = []
                    for hh in range(2):
                        j = hp * 2 + hh
                        for kb in kbs:
                            t = len(tiles)
                            tiles.append((j, kb))
                            ksl = slice(kb * 128 * dil + r, (kb * 128 + 127) * dil + r + 1, dil)
                            P.op("tensor", lambda e: e.matmul(sbank[:, t * 128:(t + 1) * 128], lhsT=kTg[:, j // 2, ksl], rhs=qz[:, j, qsl],
                                                              start=True, stop=False), reads=[kTg, qz], writes=[sbank])
                            M = C.MdT if kb == mb else C.MpT
                            P.op("tensor", lambda e: e.matmul(sbank[:, t * 128:(t + 1) * 128], lhsT=C.ident.ap(), rhs=M.ap(), start=False, stop=True),
                                 reads=[C.ident, M], writes=[sbank])
                    nw = len(tiles) * 128
                    P.op("scalar", lambda e: e.activation(out=PT[:, 0:nw], in_=sbank[:, 0:nw], func=AF.Exp, scale=0.125), reads=[sbank], writes=[PT])
                if prev is not None:
                    (r_, mb_, hp_, qb_), PT_, tiles_ = prev
                    obank = C.banks[6 + qb_ % 2]
                    kfirst = mb_ - 1 if mb_ > 0 else mb_
                    for t, (j, kb) in enumerate(tiles_):
                        P.op("tensor", lambda e: e.matmul(obank[:, j * 65:(j + 1) * 65], lhsT=PT_[:, t * 128:(t + 1) * 128], rhs=Vg[:, r_ * nb + kb, j, 0:65],
                                                          start=(kb == kfirst), stop=(kb == mb_)), reads=[PT_, Vg], writes=[obank])
                    if hp_ == 1:
                        O = Os[qb_ % 2]
                        P.op("vector", lambda e: e.tensor_copy(O.ap(), obank[:, 0:260]), reads=[obank], writes=[O])
                        P.dma("sync", Ogr[r_][mb_ * 128:(mb_ + 1) * 128, :], O.ap(), reads=[O])
                prev = (job, PT, tiles) if job is not None else None


def phase_B2(P, C, xsrc, xdst, fT, Og, kmT, Vm, wout_dram):
    with P.scope():
        Wout, Wot = load_weight(P, "Wout1", wout_dram, 4, D)
        qzs = [P.sb("qzm%d" % r, [128, 4, 128], BF16) for r in range(2)]
        for r in range(2):
            P.op("gpsimd", lambda e: e.memset(qzs[r].ap(), 0.0), writes=[qzs[r]])
        Ot = [[P.sb("Ot%d_%d" % (r, g), [128, 260], F32) for g in range(3)] for r in range(2)]
        FW = alloc_finish_work(P, C, "b2", [], C.banks[0], [C.banks[1], C.banks[2]])
        FW.osrc = lambda r: [(Ot[r][0], Ot[r][0].ap(), 4), (C.banks[5], C.banks[5][:, 0:260], 4)]
        fT_r = fT.rearrange("c p t -> p c t")
        def loads(i):
            r = i % 2
            q0 = i * 128
            for base in (0, 64):
                P.dma("sync", qzs[r][base:base + 64, base // 64:4:2, :], fT_r[base:base + 64, 12:14, q0:q0 + 128], writes=[qzs[r]])
            for g in range(3):
                P.dma("sync", Ot[r][g].ap(), Og[g][q0:q0 + 128, :], writes=[Ot[r][g]])

        loads(0)
        for i in range(NT):
            r = i % 2
            qz = qzs[r]
            q0 = i * 128
            if i + 1 < NT:
                loads(i + 1)
            mem_heads(P, C, FW, qz, 0, kmT, Vm, C.banks[5], [C.banks[6], C.banks[7]], r)
            P.op("gpsimd", lambda e: e.tensor_tensor(out=Ot[r][0].ap(), in0=Ot[r][0].ap(), in1=Ot[r][1].ap(), op=ALU.add),
                 reads=[Ot[r][0], Ot[r][1]], writes=[Ot[r][0]])
            P.op("gpsimd", lambda e: e.tensor_tensor(out=Ot[r][0].ap(), in0=Ot[r][0].ap(), in1=Ot[r][2].ap(), op=ALU.add),
                 reads=[Ot[r][0], Ot[r][2]], writes=[Ot[r][0]])
            attn_finish(P, C, FW, xsrc, xdst, i, 8, Wout, Wot, 4)


def build_program(stop_after=None, dbg_block=None, nblocks0=NT, skip_l0=False):
    nc = bass.Bass("TRN2", target_bir_lowering=False)
    P = Prog(nc)
    C = Ctx()
    I = {}

    def inp(name, shape, dt=F32):
        I[name] = nc.dram_tensor(name, list(shape), dt, kind="ExternalInput").ap()
        return I[name]

    x_in = inp("x", [S, D])
    mem_in = inp("mem", [256, D])
    pos_in = inp("pos", [128, NT], I32)
    gains = inp("gains", [7, D])
    C.freqs_in = inp("freqs", [128, 48])
    w_in0 = inp("w_in0", [D, SPEC0["ncols"]])
    w_in1 = inp("w_in1", [D, SPEC1["ncols"]])
    w_mkv = [inp("w_mkv%d" % l, [D, 512]) for l in range(2)]
    w_out0 = inp("w_out0", [1024, D])
    w_out1 = inp("w_out1", [512, D])
    w_gu = [inp("w_gu%d" % l, [D, 2 * DFF]) for l in range(2)]
    w_dn = [inp("w_dn%d" % l, [DFF, D]) for l in range(2)]
    out = nc.dram_tensor("out", [S, D], F32, kind="ExternalOutput").ap()
    C.dbg = None
    C.dbg_block = dbg_block
    if dbg_block is not None:
        C.dbg = dict(score=nc.dram_tensor("dbg_score", [128, S], F32, kind="ExternalOutput").ap(),
                     thr=nc.dram_tensor("dbg_thr", [128, 1], F32, kind="ExternalOutput").ap(),
                     Bm=nc.dram_tensor("dbg_Bm", [128, S], BF16, kind="ExternalOutput").ap())
    fT0 = P.dram("fT0", [15, 128, S], BF16)
    vS0 = P.dram("vS0", [S, 256], BF16)
    fT1 = P.dram("fT1", [14, 128, S], BF16)
    vS1 = P.dram("vS1", [S, 768], BF16)
    Og = [P.dram("Og%d" % g, [S, 260], F32) for g in range(3)]
    xa = P.dram("xa", [S, D], F32)
    xb = P.dram("xb", [S, D], F32)
    xc = P.dram("xc", [S, D], F32)

    C.banks = [P.ps("bank%d" % k, [128, 512], F32) for k in range(8)]
    setup_consts(P, C, pos_in)
    HF = NFC // 2

    def final(src):
        print("n_inst before final", P.n_inst)
        P.max_ops = None
        with P.scope():
            t = [P.sb("fin%d" % r, [128, D], F32) for r in range(2)]
            for i in range(NT):
                P.dma("sync", t[i % 2].ap(), src[i * 128:(i + 1) * 128, :], writes=[t[i % 2]])
                P.dma("sync", out[i * 128:(i + 1) * 128, :], t[i % 2].ap(), reads=[t[i % 2]], out=True)
        P.finish()
        return nc

    with (P.scope() if not skip_l0 else contextlib.nullcontext()):
      if not skip_l0:
        wiAll = P.sb("wiAll", [128, NT, 8], F32)
        kmT = P.sb("kmT0", [128, 2, 256], BF16)
        Vm = P.sb("Vm0", [128, 2, 4, VW], BF16)
        phase_M(P, C, "m0", mem_in, w_mkv[0], gains, 1, kmT, Vm)
        if stop_after == "M":
            d1 = nc.dram_tensor("dbg_kmT", [128, 2, 256], BF16, kind="ExternalOutput").ap()
            d2 = nc.dram_tensor("dbg_Vm", [128, 2, 4, VW], BF16, kind="ExternalOutput").ap()
            P.dma("sync", d1, kmT.ap(), reads=[kmT])
            P.dma("sync", d2, Vm.ap(), reads=[Vm])
            return final(x_in)
        phase_A(P, C, "a0", x_in, w_in0, gains, 0, SPEC0, fT0, vS0, wiAll, pos_in)
        if stop_after == "A":
            d1 = nc.dram_tensor("dbg_fT0", [15, 128, S], BF16, kind="ExternalOutput").ap()
            d2 = nc.dram_tensor("dbg_vS0", [S, 256], BF16, kind="ExternalOutput").ap()
            d3 = nc.dram_tensor("dbg_wi", [128, NT, 8], F32, kind="ExternalOutput").ap()
            with P.scope():
                tb = P.sb("dbgt", [128, S], BF16)
                for c in range(15):
                    P.dma("sync", tb.ap(), fT0[c], writes=[tb])
                    P.dma("sync", d1[c], tb.ap(), reads=[tb])
                for c in range(2):
                    P.dma("sync", tb[:, 0:2048].rearrange("p (a b) -> p a b", b=256), vS0[c * 2048:(c + 1) * 2048, :].rearrange("(a p) b -> p a b", p=128), writes=[tb])
                    P.dma("sync", d2[c * 2048:(c + 1) * 2048, :].rearrange("(a p) b -> p a b", p=128), tb[:, 0:2048].rearrange("p (a b) -> p a b", b=256), reads=[tb])
                P.dma("sync", d3, wiAll.ap(), reads=[wiAll])
            return final(x_in)
        phase_B0(P, C, x_in, xa, fT0, vS0, wiAll, kmT, Vm, w_out0, nblocks=nblocks0)
    if stop_after == "B0":
        return final(xa)
    if not skip_l0:
        phase_F(P, C, "f0a", xa, xa, xb, w_gu[0], w_dn[0], gains, 2, 0, HF)
        phase_F(P, C, "f0b", xa, xb, xc, w_gu[0], w_dn[0], gains, 2, HF, NFC)
    else:
        xc = x_in
    if stop_after == "F0":
        return final(xc)
    with P.scope():
        kmT = P.sb("kmT1", [128, 2, 256], BF16)
        Vm = P.sb("Vm1", [128, 2, 4, VW], BF16)
        phase_M(P, C, "m1", mem_in, w_mkv[1], gains, 4, kmT, Vm)
        phase_A(P, C, "a1", xc, w_in1, gains, 3, SPEC1, fT1, vS1, None, pos_in)
        phase_B1(P, C, fT1, vS1, Og)
        phase_B2(P, C, xc, xa, fT1, Og, kmT, Vm, w_out1)
    if stop_after == "B2":
        return final(xa)
    phase_F(P, C, "f1a", xa, xa, xb, w_gu[1], w_dn[1], gains, 5, 0, HF)
    phase_F(P, C, "f1b", xa, xb, out, w_gu[1], w_dn[1], gains, 5, HF, NFC, final_row=6)
    P.finish()
    return nc


def prep_inputs(inputs):
    f = lambda a: np.ascontiguousarray(np.asarray(a, dtype=np.float32))
    w0 = f(inputs["l0_w_in"])
    q, k, v, qi, ki, wi, qm = np.split(w0, np.cumsum([768, 256, 256, 512, 64, 8])[:], axis=1)
    qp = np.concatenate([q[:, h * 64:(h + 1) * 64] for h in QPERM], axis=1)
    w_in0 = np.ascontiguousarray(np.concatenate([qp, k, qi, ki, ki, wi, v, qm], axis=1))
    w1 = f(inputs["l1_w_in"])
    parts = [w1[:, j * 256:(j + 1) * 256] for j in range(10)]
    w_in1 = np.ascontiguousarray(np.concatenate([parts[0], parts[1], parts[3], parts[4], parts[6], parts[7], parts[2], parts[5], parts[8], parts[9]], axis=1))
    gains = np.ascontiguousarray(np.stack([f(inputs[n]) for n in ("l0_norm_mix", "l0_norm_mem", "l0_norm_ffn", "l1_norm_mix", "l1_norm_mem",
                                                                 "l1_norm_ffn", "final_norm")], axis=0))
    fr64 = (np.float32(10000.0) ** (-np.arange(32, dtype=np.float32) / np.float32(32))).astype(np.float32)
    fr16 = (np.float32(10000.0) ** (-np.arange(16, dtype=np.float32) / np.float32(16))).astype(np.float32)
    freqs = np.ascontiguousarray(np.broadcast_to(np.concatenate([fr64, fr16])[None, :], (128, 48)).astype(np.float32))
    shared = dict(freqs=freqs, gains=gains, w_in0=w_in0, w_in1=w_in1, w_mkv0=f(inputs["l0_w_mem_kv"]), w_mkv1=f(inputs["l1_w_mem_kv"]),
                  w_out0=f(inputs["l0_w_out"]), w_out1=f(inputs["l1_w_out"]), w_gu0=f(inputs["l0_w_gate_up"]), w_gu1=f(inputs["l1_w_gate_up"]),
                  w_dn0=f(inputs["l0_w_down"]), w_dn1=f(inputs["l1_w_down"]))
    x = f(inputs["x"])
    mem = f(inputs["mem"])
    pos = np.ascontiguousarray(np.asarray(inputs["positions"], dtype=np.int32))
    maps = []
    for c in range(x.shape[0]):
        m = dict(shared)
        m["x"] = x[c]
        m["mem"] = mem[c]
        m["pos"] = np.ascontiguousarray(pos[c].reshape(NT, 128).T)
        maps.append(m)
    return maps


_NC_CACHE = {}


def kernel(**inputs):
    maps = prep_inputs(inputs)
    if "nc" not in _NC_CACHE:
        _NC_CACHE["nc"] = build_program()
    nc = _NC_CACHE["nc"]
    res = run_bass_kernel_spmd(nc, maps, core_ids=list(range(len(maps))))
    return np.stack([np.asarray(r["out"], dtype=np.float32) for r in res.results], axis=0)
```

```python
import contextlib
import numpy as np
import ml_dtypes
import concourse.bass as bass
import concourse.mybir as mybir
from concourse.bass_utils import run_bass_kernel_spmd

F32 = mybir.dt.float32
BF16 = mybir.dt.bfloat16
I32 = mybir.dt.int32
AF = mybir.ActivationFunctionType
ALU = mybir.AluOpType
AX = mybir.AxisListType

SAME_ENGINE_SYNC = True


class Tok:
    def __init__(self, name):
        self.name = name
        self.w = None
        self.r = {}


class Buf(Tok):
    def __init__(self, name, handle):
        super().__init__(name)
        self.h = handle

    def ap(self):
        return self.h[:]

    def __getitem__(self, idx):
        return self.h[idx]


class _Eng:
    def __init__(self, name, handle, sem):
        self.name = name
        self.h = handle
        self.sem = sem
        self.count = 0
        self.seen = {}


class Prog:
    def __init__(self, nc, n_dma_sems=8):
        self.nc = nc
        self.stack = contextlib.ExitStack()
        self.engs = {}
        for name in ("tensor", "vector", "scalar", "gpsimd", "sync"):
            sem = self.stack.enter_context(nc.semaphore("s_" + name))
            self.engs[name] = _Eng(name, getattr(nc, name), sem)
        self.dma_sems = {}
        for q in ("sync", "gpsimd", "scalar"):
            self.dma_sems[q] = [[self.stack.enter_context(nc.semaphore("d_%s%d" % (q, i))), 0] for i in range(n_dma_sems)]
        self.dma_rr = {"sync": 0, "gpsimd": 0, "scalar": 0}
        self.out_events = []
        self.n_inst = 0
        self.scopes = [self.stack]
        import os
        self.max_ops = int(os.environ["KMAXOPS"]) if "KMAXOPS" in os.environ else None

    @contextlib.contextmanager
    def scope(self):
        st = contextlib.ExitStack()
        self.scopes.append(st)
        try:
            yield
        finally:
            self.barrier()
            self.scopes.pop()
            st.close()

    def barrier(self):
        if getattr(self, "finished", False):
            return
        for eng in self.engs.values():
            for other in self.engs.values():
                if other is not eng and other.count > 0:
                    self._wait(eng, (other.name, other.sem, other.count))
            for q, pool in self.dma_sems.items():
                for i, slot in enumerate(pool):
                    if slot[1] > 0:
                        self._wait(eng, ("d_%s%d" % (q, i), slot[0], 16 * slot[1]))

    def tok(self, name):
        return Tok(name)

    def sb(self, name, shape, dtype):
        self.uid = getattr(self, "uid", 0) + 1
        name = "%s_u%d" % (name, self.uid)
        return Buf(name, self.scopes[-1].enter_context(self.nc.sbuf_tensor(name, list(shape), dtype)))

    def ps(self, name, shape, dtype):
        b = Buf(name, self.scopes[-1].enter_context(self.nc.psum_tensor(name, list(shape), dtype)))
        b.excl = True
        return b

    def dram(self, name, shape, dtype):
        return self.nc.dram_tensor(name, list(shape), dtype, kind="Internal").ap()

    def _wait(self, eng, ev):
        key, sem, val = ev
        if eng.seen.get(key, 0) >= val:
            return
        if key == eng.name and not (SAME_ENGINE_SYNC and eng.name != "tensor" and eng.name != "sync"):
            return
        eng.h.wait_ge(sem, val)
        eng.seen[key] = val

    def _deps(self, eng, reads, writes):
        for t in reads:
            if t.w is not None:
                self._wait(eng, t.w)
        for t in writes:
            if t.w is not None:
                self._wait(eng, t.w)
            for ev in t.r.values():
                self._wait(eng, ev)

    def _record(self, ev, reads, writes):
        for t in writes:
            t.w = ev
            t.r = {}
        for t in reads:
            if t in writes:
                continue
            t.r[ev[0]] = ev

    def op(self, engname, fn, reads=(), writes=()):
        if self.max_ops is not None and self.n_inst >= self.max_ops:
            return None
        eng = self.engs[engname]
        ex = [t for t in reads if getattr(t, "excl", False) and t not in writes]
        if ex:
            reads = [t for t in reads if t not in ex]
            writes = list(writes) + ex
        self._deps(eng, reads, writes)
        inst = fn(eng.h)
        eng.count += 1
        inst.then_inc(eng.sem, 1)
        ev = (eng.name, eng.sem, eng.count)
        self._record(ev, reads, writes)
        self.n_inst += 1
        return ev

    def dma(self, q, out_ap, in_ap, reads=(), writes=(), out=False, **kw):
        if self.max_ops is not None and self.n_inst >= self.max_ops:
            return None
        eng = self.engs[q]
        self._deps(eng, reads, writes)
        pool = self.dma_sems[q]
        i = self.dma_rr[q]
        self.dma_rr[q] = (i + 1) % len(pool)
        slot = pool[i]
        key = "d_%s%d" % (q, i)
        if slot[1] > 0:
            self._wait(eng, (key, slot[0], 16 * slot[1]))
        eng.h.dma_start(out=out_ap, in_=in_ap, **kw).then_inc(slot[0], 16)
        slot[1] += 1
        ev = (key, slot[0], 16 * slot[1])
        self._record(ev, reads, writes)
        if out:
            self.out_events.append(ev)
        self.n_inst += 1
        return ev

    def finish(self):
        eng = self.engs["sync"]
        for q, pool in self.dma_sems.items():
            for i, slot in enumerate(pool):
                if slot[1] > 0:
                    self._wait(eng, ("d_%s%d" % (q, i), slot[0], 16 * slot[1]))
        for name, e in self.engs.items():
            if name != "sync" and e.count > 0:
                self._wait(eng, (name, e.sem, e.count))
        self.finished = True
        for st in reversed(self.scopes):
            st.close()

    def make_identity(self, idt):
        tmp = self.sb(idt.name + "_i", [128, 128], I32)
        self.op("gpsimd", lambda e: e.iota(tmp.ap(), pattern=[[1, 128]], base=0, channel_multiplier=-1), writes=[tmp])
        self.op("vector", lambda e: e.tensor_scalar(out=idt.ap(), in0=tmp.ap(), scalar1=0.0, scalar2=None, op0=ALU.is_equal),
                reads=[tmp], writes=[idt])


S = 4096
D = 1024
NT = 32
DFF = 2816
NFC = 22
EPS = 1e-6
NEG = -1.0e30
MASKV = -30000.0
VW = 66
N_BISECT = 16
IDX_SCALE = (8 ** -0.5) * (64 ** -0.5)
QPERM = [0, 3, 1, 4, 2, 5, 6, 9, 7, 10, 8, 11]

SPEC0 = dict(
    ncols=2184,
    chunks=[(0, 512, [("rope64", 0, 8)]), (512, 512, [("rope64", 0, 8)]), (1024, 512, [("rope32", 0, 8)]),
            (1536, 136, [("rope32", 0, 2), ("wi", 128, 8)]), (1672, 512, [("copy", 0, 512)])],
    tcols=[128 * k for k in range(13)] + [1928, 2056],
    vcols=(1672, 1928),
)
SPEC1 = dict(
    ncols=2560,
    chunks=[(0, 512, [("rope64", 0, 8)]), (512, 512, [("rope64", 0, 8)]), (1024, 512, [("rope64", 0, 8)]),
            (1536, 512, [("copy", 0, 512)]), (2048, 512, [("copy", 0, 512)])],
    tcols=[128 * k for k in range(12)] + [2304, 2432],
    vcols=(1536, 2304),
)


class Ctx:
    pass


def load_weight(P, name, dram, nk, ncols, q="gpsimd"):
    W = P.sb(name, [128, nk, ncols], BF16)
    toks = [P.tok("%s_%d" % (name, k)) for k in range(nk)]
    for k in range(nk):
        c0 = 0
        while c0 < ncols:
            c1 = min(ncols, c0 + 2048)
            P.dma(q, W[:, k, c0:c1], dram[k * 128:(k + 1) * 128, c0:c1], writes=[toks[k]])
            c0 = c1
    return W, toks


def setup_consts(P, C, pos_in):
    C.ident = P.sb("ident", [128, 128], BF16)
    P.make_identity(C.ident)
    C.negthr = P.sb("negthr", [128, 1], F32)
    P.op("vector", lambda e: e.memset(C.negthr.ap(), -1.0e29), writes=[C.negthr])
    C.pow2 = P.sb("pow2", [128, N_BISECT + 2], F32)
    for k in range(N_BISECT + 2):
        P.op("gpsimd", lambda e: e.memset(C.pow2[:, k:k + 1], 2.0 ** (1 - k)), writes=[C.pow2])
    C.MdT = P.sb("MdT", [128, 128], BF16)
    C.MpT = P.sb("MpT", [128, 128], BF16)
    zt = P.sb("zt", [128, 128], F32)
    zm = P.sb("zm", [128, 128], F32)
    P.op("vector", lambda e: e.memset(zt.ap(), 0.0), writes=[zt])
    P.op("gpsimd", lambda e: e.affine_select(out=zm.ap(), in_=zt.ap(), pattern=[[1, 128]], compare_op=ALU.is_ge, fill=MASKV, base=0, channel_multiplier=-1),
         reads=[zt], writes=[zm])
    P.op("vector", lambda e: e.tensor_copy(C.MdT.ap(), zm.ap()), reads=[zm], writes=[C.MdT])
    P.op("gpsimd", lambda e: e.affine_select(out=zm.ap(), in_=zt.ap(), pattern=[[-1, 128]], compare_op=ALU.is_ge, fill=MASKV, base=0, channel_multiplier=1),
         reads=[zt, C.MdT], writes=[zm])
    P.op("vector", lambda e: e.tensor_copy(C.MpT.ap(), zm.ap()), reads=[zm], writes=[C.MpT])


def build_rope_tables(P, C, pos_in):
    C.cos64 = P.sb("cos64", [128, NT, 32], F32)
    C.sin64 = P.sb("sin64", [128, NT, 32], F32)
    C.nsin64 = P.sb("nsin64", [128, NT, 32], F32)
    C.cos16 = P.sb("cos16", [128, NT, 16], F32)
    C.sin16 = P.sb("sin16", [128, NT, 16], F32)
    C.nsin16 = P.sb("nsin16", [128, NT, 16], F32)
    with P.scope():
        posi = P.sb("posi", [128, NT], I32)
        posf = P.sb("posf", [128, NT], F32)
        P.dma("sync", posi.ap(), pos_in, writes=[posi])
        P.op("vector", lambda e: e.tensor_copy(posf.ap(), posi.ap()), reads=[posi], writes=[posf])
        for half, cosT, sinT, nsinT in ((32, C.cos64, C.sin64, C.nsin64), (16, C.cos16, C.sin16, C.nsin16)):
            n = NT * half
            fr = P.sb("fr%d" % half, [128, half], F32)
            a = P.sb("a%d" % half, [128, NT, half], F32)
            ki = P.sb("ki%d" % half, [128, NT, half], I32)
            kf = P.sb("kf%d" % half, [128, NT, half], F32)
            fr1 = P.sb("fr1%d" % half, [128, NT, half], F32)
            m1 = P.sb("m1%d" % half, [128, NT, half], F32)
            f0 = 0 if half == 32 else 32
            P.dma("sync", fr.ap(), C.freqs_in[:, f0:f0 + half], writes=[fr])
            P.op("vector", lambda e: e.tensor_tensor(out=a.ap(), in0=posf.ap().unsqueeze(2).broadcast_to([128, NT, half]),
                                                     in1=fr.ap().unsqueeze(1).broadcast_to([128, NT, half]), op=ALU.mult),
                 reads=[posf, fr], writes=[a])
            P.op("vector", lambda e: e.tensor_scalar(out=a.ap(), in0=a.ap(), scalar1=float(1.0 / (2.0 * np.pi)), scalar2=None, op0=ALU.mult),
                 reads=[a], writes=[a])
            for shift, outT, neg in ((0.0, sinT, False), (0.25, cosT, False), (0.5, nsinT, False)):
                src = a
                if shift != 0.0:
                    P.op("vector", lambda e: e.tensor_scalar(out=fr1.ap(), in0=a.ap(), scalar1=shift, scalar2=None, op0=ALU.add),
                         reads=[a], writes=[fr1])
                    src = fr1
                P.op("vector", lambda e: e.tensor_copy(ki.ap(), src.ap()), reads=[src], writes=[ki])
                P.op("vector", lambda e: e.tensor_copy(kf.ap(), ki.ap()), reads=[ki], writes=[kf])
                P.op("vector", lambda e: e.tensor_tensor(out=kf.ap(), in0=src.ap(), in1=kf.ap(), op=ALU.subtract), reads=[src, kf], writes=[kf])
                P.op("vector", lambda e: e.tensor_scalar(out=m1.ap(), in0=kf.ap(), scalar1=0.5, scalar2=None, op0=ALU.is_gt), reads=[kf], writes=[m1])
                P.op("vector", lambda e: e.tensor_tensor(out=kf.ap(), in0=kf.ap(), in1=m1.ap(), op=ALU.subtract), reads=[kf, m1], writes=[kf])
                P.op("vector", lambda e: e.tensor_scalar(out=m1.ap(), in0=kf.ap(), scalar1=-0.5, scalar2=None, op0=ALU.is_lt), reads=[kf], writes=[m1])
                P.op("vector", lambda e: e.tensor_tensor(out=kf.ap(), in0=kf.ap(), in1=m1.ap(), op=ALU.add), reads=[kf, m1], writes=[kf])
                P.op("scalar", lambda e: e.activation(out=outT.ap(), in_=kf.ap(), func=AF.Sin, scale=float(2.0 * np.pi) * (1.0 - 1e-6)),
                     reads=[kf], writes=[outT])


def alloc_norm_work(P, C, tag):
    W = Ctx()
    W.sqj = P.sb(tag + "sqj", [128, D], BF16)
    W.small = [[P.sb("%ssm%d_%d" % (tag, r, j), [128, 1], F32) for j in range(4)] for r in range(2)]
    W.hb = [P.sb("%shb%d" % (tag, r), [128, D], BF16) for r in range(2)]
    W.k = 0
    return W


def rmsnorm_to_hT(P, C, W, xt, gb, hT_ap, hT_tok, bank, evac_eng="scalar"):
    r = W.k % 2
    W.k += 1
    ssq, t1, t2, rstd = W.small[r]
    hb = W.hb[r]
    P.op("scalar", lambda e: e.activation(out=W.sqj.ap(), in_=xt.ap(), func=AF.Square, accum_out=ssq.ap()), reads=[xt], writes=[W.sqj, ssq])
    P.op("vector", lambda e: e.tensor_scalar(out=t1.ap(), in0=ssq.ap(), scalar1=1.0 / D, scalar2=EPS, op0=ALU.mult, op1=ALU.add), reads=[ssq], writes=[t1])
    P.op("scalar", lambda e: e.activation(out=t2.ap(), in_=t1.ap(), func=AF.Sqrt), reads=[t1], writes=[t2])
    P.op("vector", lambda e: e.reciprocal(out=rstd.ap(), in_=t2.ap()), reads=[t2], writes=[rstd])
    P.op("vector", lambda e: e.scalar_tensor_tensor(out=hb.ap(), in0=xt.ap(), scalar=rstd.ap(), in1=gb.ap(), op0=ALU.mult, op1=ALU.mult),
         reads=[xt, rstd, gb], writes=[hb])
    if hT_ap is None:
        return hb
    return norm_p2(P, C, hb, hT_ap, hT_tok, bank, evac_eng)


def norm_p2(P, C, hb, hT_ap, hT_tok, bank, evac_eng="scalar"):
    bbf = bank.ap().bitcast(BF16)
    for c in range(8):
        P.op("tensor", lambda e: e.transpose(bbf[:, c * 128:(c + 1) * 128], hb[:, c * 128:(c + 1) * 128], C.ident.ap()),
             reads=[hb, C.ident], writes=[bank])
    srcv = bbf if len(hT_ap.shape) == 2 else bbf.rearrange("p (c t) -> p c t", t=128)
    if evac_eng == "scalar":
        P.op("scalar", lambda e: e.activation(out=hT_ap, in_=srcv, func=AF.Copy), reads=[bank], writes=[hT_tok])
    else:
        P.op("vector", lambda e: e.tensor_copy(hT_ap, srcv), reads=[bank], writes=[hT_tok])


def load_gain(P, C, name, gains, row):
    gb = P.sb(name, [128, D], F32)
    P.dma("sync", gb.ap(), gains[row].partition_broadcast(128), writes=[gb])
    return gb


def phase_A(P, C, tag, xsrc, w_dram, gains, grow, spec, fT, vS, wiAll, pos_in):
    ncols = spec["ncols"]
    with P.scope():
        build_rope_tables(P, C, pos_in)
        Win, Wt = load_weight(P, tag + "Win", w_dram, 8, ncols)
        gb = load_gain(P, C, tag + "gbA", gains, grow)
        NW = alloc_norm_work(P, C, tag + "A")
        xts = [P.sb("%sxt%d" % (tag, r), [128, D], F32) for r in range(3)]
        hTs = [P.sb("%shT%d" % (tag, r), [128, D], BF16) for r in range(2)]
        pts = [P.sb("%spt%d" % (tag, r), [128, ncols], BF16) for r in range(2)]
        t1s = [P.sb("%st1_%d" % (tag, r), [128, 512], F32) for r in range(2)]
        t2s = [P.sb("%st2_%d" % (tag, r), [128, 512], F32) for r in range(2)]
        ntc = len(spec["tcols"])
        fTs = [P.sb("%sfTs%d" % (tag, r), [128, ntc, 128], BF16) for r in range(2)]
        fT_r = fT.rearrange("c p t -> p c t")
        cc = 0
        hbs = {}

        def ld(i):
            xt = xts[i % 3]
            P.dma("sync", xt.ap(), xsrc[i * 128:(i + 1) * 128, :], writes=[xt])

        def n1(i):
            hbs[i] = rmsnorm_to_hT(P, C, NW, xts[i % 3], gb, None, None, None)

        def n2(i):
            norm_p2(P, C, hbs.pop(i), hTs[i % 2].ap(), hTs[i % 2], C.banks[0], evac_eng="scalar")

        def featT(i):
            _featT_body(P, C, spec, pts, fTs, fT_r, vS, ntc, i)

        ld(0)
        ld(1)
        n1(0)
        n2(0)
        for i in range(NT):
            hT = hTs[i % 2]
            pt = pts[i % 2]
            if i + 2 < NT:
                ld(i + 2)
            if i + 1 < NT:
                n1(i + 1)
            for (col0, width, handlers) in spec["chunks"]:
                bank = C.banks[1 + (cc % 4)]
                t1 = t1s[cc % 2]
                t2 = t2s[cc % 2]
                cc += 1
                for k in range(8):
                    P.op("tensor", lambda e: e.matmul(bank[:, 0:width], lhsT=hT[:, k * 128:(k + 1) * 128], rhs=Win[:, k, col0:col0 + width],
                                                      start=(k == 0), stop=(k == 7)), reads=[hT, Wt[k]], writes=[bank])
                for (kind, l0, n) in handlers:
                    if kind == "rope64":
                        nh = n
                        w = nh * 64
                        xv2 = bank[:, l0:l0 + w].rearrange("p (h d) -> p h d", d=32)
                        xv = bank[:, l0:l0 + w].rearrange("p (h d) -> p h d", d=64)
                        t1v2 = t1[:, 0:w].rearrange("p (h d) -> p h d", d=32)
                        t2v = t2[:, 0:w].rearrange("p (h d) -> p h d", d=64)
                        cosb = C.cos64[:, i:i + 1, :].broadcast_to([128, 2 * nh, 32])
                        sinb = C.sin64[:, i:i + 1, :].broadcast_to([128, nh, 32])
                        nsinb = C.nsin64[:, i:i + 1, :].broadcast_to([128, nh, 32])
                        P.op("vector", lambda e: e.tensor_tensor(out=t1v2, in0=xv2, in1=cosb, op=ALU.mult), reads=[bank, C.cos64], writes=[t1])
                        P.op("vector", lambda e: e.tensor_tensor(out=t2v[:, :, 0:32], in0=xv[:, :, 32:64], in1=nsinb, op=ALU.mult),
                             reads=[bank, C.nsin64], writes=[t2])
                        P.op("vector", lambda e: e.tensor_tensor(out=t2v[:, :, 32:64], in0=xv[:, :, 0:32], in1=sinb, op=ALU.mult),
                             reads=[bank, C.sin64], writes=[t2])
                        P.op("gpsimd", lambda e: e.tensor_tensor(out=pt[:, col0 + l0:col0 + l0 + w], in0=t1[:, 0:w], in1=t2[:, 0:w], op=ALU.add),
                             reads=[t1, t2], writes=[pt])
                    elif kind == "rope32":
                        nh = n
                        w = nh * 64
                        xv = bank[:, l0:l0 + w].rearrange("p (h d) -> p h d", d=64)
                        xr4 = bank[:, l0:l0 + w].rearrange("p (h t d) -> p h t d", t=4, d=16)
                        t1r4 = t1[:, 0:w].rearrange("p (h t d) -> p h t d", t=4, d=16)
                        t2v = t2[:, 0:w].rearrange("p (h d) -> p h d", d=64)
                        t1v = t1[:, 0:w].rearrange("p (h d) -> p h d", d=64)
                        ptv = pt[:, col0 + l0:col0 + l0 + w].rearrange("p (h d) -> p h d", d=64)
                        cosb = C.cos16[:, i:i + 1, :].unsqueeze(1).broadcast_to([128, nh, 2, 16])
                        sinb = C.sin16[:, i:i + 1, :].broadcast_to([128, nh, 16])
                        nsinb = C.nsin16[:, i:i + 1, :].broadcast_to([128, nh, 16])
                        P.op("vector", lambda e: e.tensor_tensor(out=t1r4[:, :, 0:2, :], in0=xr4[:, :, 0:2, :], in1=cosb, op=ALU.mult),
                             reads=[bank, C.cos16], writes=[t1])
                        P.op("vector", lambda e: e.tensor_tensor(out=t2v[:, :, 0:16], in0=xv[:, :, 16:32], in1=nsinb, op=ALU.mult),
                             reads=[bank, C.nsin16], writes=[t2])
                        P.op("vector", lambda e: e.tensor_tensor(out=t2v[:, :, 16:32], in0=xv[:, :, 0:16], in1=sinb, op=ALU.mult),
                             reads=[bank, C.sin16], writes=[t2])
                        P.op("gpsimd", lambda e: e.tensor_tensor(out=ptv[:, :, 0:32], in0=t1v[:, :, 0:32], in1=t2v[:, :, 0:32], op=ALU.add),
                             reads=[t1, t2], writes=[pt])
                        P.op("scalar", lambda e: e.activation(out=ptv[:, :, 32:64], in_=xv[:, :, 32:64], func=AF.Copy), reads=[bank], writes=[pt])
                    elif kind == "copy":
                        P.op("scalar", lambda e: e.activation(out=pt[:, col0 + l0:col0 + l0 + n], in_=bank[:, l0:l0 + n], func=AF.Copy),
                             reads=[bank], writes=[pt])
                    elif kind == "wi":
                        P.op("scalar", lambda e: e.activation(out=wiAll[:, i, :], in_=bank[:, l0:l0 + n], func=AF.Copy, scale=float(IDX_SCALE)),
                             reads=[bank], writes=[wiAll])
            if i + 1 < NT:
                n2(i + 1)
            if i >= 1:
                featT(i - 1)
        featT(NT - 1)


def _featT_body(P, C, spec, pts, fTs, fT_r, vS, ntc, i):
            pt = pts[i % 2]
            fts = fTs[i % 2]
            for g0 in range(0, ntc, 8):
                g1 = min(ntc, g0 + 8)
                bank = C.banks[5 + (g0 // 8)]
                bbf = bank.ap().bitcast(BF16)
                for k in range(g0, g1):
                    c0 = spec["tcols"][k]
                    P.op("tensor", lambda e: e.transpose(bbf[:, (k - g0) * 128:(k - g0 + 1) * 128], pt[:, c0:c0 + 128], C.ident.ap()),
                         reads=[pt, C.ident], writes=[bank])
                eng = "vector" if g0 == 0 else "scalar"
                if eng == "vector":
                    P.op("vector", lambda e: e.tensor_copy(fts[:, g0:g1, :], bbf[:, 0:(g1 - g0) * 128].rearrange("p (c t) -> p c t", t=128)),
                         reads=[bank], writes=[fts])
                else:
                    P.op("scalar", lambda e: e.activation(out=fts[:, g0:g1, :], in_=bbf[:, 0:(g1 - g0) * 128].rearrange("p (c t) -> p c t", t=128),
                                                          func=AF.Copy), reads=[bank], writes=[fts])
            P.dma("sync", fT_r[:, :, i * 128:(i + 1) * 128], fts.ap(), reads=[fts])
            v0, v1 = spec["vcols"]
            P.dma("sync", vS[i * 128:(i + 1) * 128, :], pt[:, v0:v1], reads=[pt])


def phase_M(P, C, tag, mem_in, w_dram, gains, grow, kmT, Vm):
    with P.scope():
        Wm, Wt = load_weight(P, tag + "Wm", w_dram, 8, 512)
        gb = load_gain(P, C, tag + "gbM", gains, grow)
        NW = alloc_norm_work(P, C, tag + "M")
        xts = [P.sb("%smx%d" % (tag, r), [128, D], F32) for r in range(2)]
        hTs = [P.sb("%smhT%d" % (tag, r), [128, D], BF16) for r in range(2)]
        kb16 = [P.sb("%skb16_%d" % (tag, r), [128, 256], BF16) for r in range(2)]
        P.op("gpsimd", lambda e: e.memset(Vm.ap(), 1.0), writes=[Vm])
        for mb in range(2):
            xt, hT = xts[mb], hTs[mb]
            P.dma("sync", xt.ap(), mem_in[mb * 128:(mb + 1) * 128, :], writes=[xt])
            rmsnorm_to_hT(P, C, NW, xt, gb, hT.ap(), hT, C.banks[0])
            bank = C.banks[1 + mb]
            for k in range(8):
                P.op("tensor", lambda e: e.matmul(bank.ap(), lhsT=hT[:, k * 128:(k + 1) * 128], rhs=Wm[:, k, :], start=(k == 0), stop=(k == 7)),
                     reads=[hT, Wt[k]], writes=[bank])
            P.op("scalar", lambda e: e.activation(out=kb16[mb].ap(), in_=bank[:, 0:256], func=AF.Copy), reads=[bank], writes=[kb16[mb]])
            P.op("vector", lambda e: e.tensor_copy(Vm[:, mb, :, 0:64], bank[:, 256:512].rearrange("p (h d) -> p h d", d=64)),
                 reads=[bank], writes=[Vm])
            tb = C.banks[3 + mb]
            tbf = tb.ap().bitcast(BF16)
            for c in range(2):
                P.op("tensor", lambda e: e.transpose(tbf[:, c * 128:(c + 1) * 128], kb16[mb][:, c * 128:(c + 1) * 128], C.ident.ap()),
                     reads=[kb16[mb], C.ident], writes=[tb])
            P.op("vector", lambda e: e.tensor_copy(kmT[:, :, mb * 128:(mb + 1) * 128], tbf[:, 0:256].rearrange("p (c t) -> p c t", t=128)),
                 reads=[tb], writes=[kmT])


def attn_finish(P, C, W, xsrc, xdst, i, nheads, Wout, Wot, nkc):
    r = i % 2
    attn = W.attn[r]
    rec = W.rec[r]
    xt = W.xts[r]
    P.dma("sync", xt.ap(), xsrc[i * 128:(i + 1) * 128, :], writes=[xt])
    h0 = 0
    for (bank, oap, nh) in W.osrc(r):
        ov = oap.rearrange("p (h d) -> p h d", d=65)
        P.op("vector", lambda e: e.reciprocal(out=rec[:, h0:h0 + nh], in_=ov[:, :, 64]), reads=[bank], writes=[rec])
        P.op("vector", lambda e: e.tensor_tensor(out=attn[:, h0 * 64:(h0 + nh) * 64].rearrange("p (h d) -> p h d", d=64), in0=ov[:, :, 0:64],
                                                 in1=rec[:, h0:h0 + nh].unsqueeze(2).broadcast_to([128, nh, 64]), op=ALU.mult),
             reads=[bank, rec], writes=[attn])
        h0 += nh
    tb = W.tbank
    tbf = tb.ap().bitcast(BF16)
    aT = W.attnT[r]
    for c in range(nkc):
        P.op("tensor", lambda e: e.transpose(tbf[:, c * 128:(c + 1) * 128], attn[:, c * 128:(c + 1) * 128], C.ident.ap()),
             reads=[attn, C.ident], writes=[tb])
    P.op("scalar", lambda e: e.activation(out=aT[:, 0:nkc * 128], in_=tbf[:, 0:nkc * 128], func=AF.Copy), reads=[tb], writes=[aT])
    xn = W.xn[r]
    for half in range(2):
        yb = W.ybanks[half]
        for c in range(nkc):
            P.op("tensor", lambda e: e.matmul(yb.ap(), lhsT=aT[:, c * 128:(c + 1) * 128], rhs=Wout[:, c, half * 512:(half + 1) * 512],
                                              start=(c == 0), stop=(c == nkc - 1)), reads=[aT, Wot[c]], writes=[yb])
        P.op("vector", lambda e: e.tensor_tensor(out=xn[:, half * 512:(half + 1) * 512], in0=yb.ap(), in1=xt[:, half * 512:(half + 1) * 512], op=ALU.add),
             reads=[yb, xt], writes=[xn])
    P.dma("sync", xdst[i * 128:(i + 1) * 128, :], xn.ap(), reads=[xn])


def mem_heads(P, C, W, qall, qch0, kmT, Vm, obank, sbank, r):
    PT = W.PTm[r]
    for half in range(2):
        sb_ = sbank[half]
        for hh in range(2):
            hm = half * 2 + hh
            for mb in range(2):
                j = hh * 2 + mb
                P.op("tensor", lambda e: e.matmul(sb_[:, j * 128:(j + 1) * 128], lhsT=kmT[:, hm // 2, mb * 128:(mb + 1) * 128],
                                                  rhs=qall[:, qch0 + hm, :], start=True, stop=True),
                     reads=[kmT, qall], writes=[sb_])
        P.op("scalar", lambda e: e.activation(out=PT[:, half * 512:(half + 1) * 512], in_=sb_.ap(), func=AF.Exp, scale=0.125),
             reads=[sb_], writes=[PT])
    for hm in range(4):
        for mb in range(2):
            j = hm * 2 + mb
            P.op("tensor", lambda e: e.matmul(obank[:, hm * 65:(hm + 1) * 65], lhsT=PT[:, j * 128:(j + 1) * 128], rhs=Vm[:, mb, hm, 0:65],
                                              start=(mb == 0), stop=(mb == 1)), reads=[PT, Vm], writes=[obank])


def alloc_finish_work(P, C, tag, obanks, tbank, ybanks):
    W = Ctx()
    W.attn = [P.sb("%sattn%d" % (tag, r), [128, D], BF16) for r in range(2)]
    W.attnT = [P.sb("%sattnT%d" % (tag, r), [128, D], BF16) for r in range(2)]
    W.rec = [P.sb("%srec%d" % (tag, r), [128, 16], F32) for r in range(2)]
    W.xts = [P.sb("%sfx%d" % (tag, r), [128, D], F32) for r in range(2)]
    W.xn = [P.sb("%sxn%d" % (tag, r), [128, D], F32) for r in range(2)]
    W.PTm = [P.sb("%sPTm%d" % (tag, r), [128, 1024], BF16) for r in range(2)]
    W.osrc = lambda r: [(bk, bk[:, 0:nh * 65], nh) for (bk, nh) in obanks]
    W.tbank = tbank
    W.ybanks = ybanks
    return W


def phase_B0(P, C, xsrc, xdst, fT, vS, wiAll, kmT, Vm, wout_dram, nblocks=NT):
    with P.scope():
        Wout, Wot = load_weight(P, "Wout0", wout_dram, 8, D)
        kT = P.sb("kT", [128, 2, S], BF16)
        kiT = P.sb("kiT", [128, S], BF16)
        Va = P.sb("Va", [128, NT, 4, VW], BF16)
        P.op("gpsimd", lambda e: e.memset(Va.ap(), 1.0), writes=[Va])
        for c in range(2):
            P.dma("sync", kT[:, c, :], fT[6 + c], writes=[kT])
        P.dma("sync", kiT.ap(), fT[12], writes=[kiT])
        vr = vS.rearrange("(i p) (g d) -> p i g d", p=128, d=64)
        for i0 in range(NT):
            P.dma("sync", Va[:, i0, :, 0:64], vr[:, i0, :, :], writes=[Va])
        scores = [P.sb("score%d" % r, [128, S], F32) for r in range(2)]
        junk = P.sb("junk", [128, S], BF16)
        Bs = [P.sb("Bm%d" % r, [128, S], BF16) for r in range(2)]
        Rs = [P.sb("R%d" % r, [128, 512], BF16) for r in range(4)]
        Wdgs = [P.sb("Wdg%d" % r, [128, 8, 128], BF16) for r in range(2)]
        PTs = [P.sb("PT%d" % r, [128, 512], BF16) for r in range(3)]
        qalls = [P.sb("qall%d" % r, [128, 24, 128], BF16) for r in range(4)]
        for r in range(4):
            P.op("gpsimd", lambda e: e.memset(qalls[r].ap(), 0.0), writes=[qalls[r]])
        sm = [[P.sb("bs%d_%d" % (r, j), [128, 1], F32) for j in range(6)] for r in range(2)]
        wtabs = [P.sb("wtab%d" % r, [128, N_BISECT + 2], F32) for r in range(2)]
        FW = alloc_finish_work(P, C, "b0", [(C.banks[3], 6), (C.banks[4], 6), (C.banks[5], 4)], C.banks[0], [C.banks[1], C.banks[0]])
        fT_r = fT.rearrange("c p t -> p c t")
        cnts = dict(lc=0, sc=0)

        def idx_steps(b):
            r = b % 2
            N = 128 * (b + 1)
            qall = qalls[b % 4]
            score = scores[r]
            q0 = b * 128
            Wdg = Wdgs[r]
            accb = C.banks[7]
            steps = []

            def loads():
                for (slot0, nch, ch0) in ((0, 6, 0), (12, 4, 8), (20, 2, 13)):
                    for base in (0, 64):
                        P.dma("sync", qall[base:base + 64, slot0 + base // 64:slot0 + 2 * nch:2, :], fT_r[base:base + 64, ch0:ch0 + nch, q0:q0 + 128],
                              writes=[qall])
                for h in range(8):
                    P.op("gpsimd", lambda e: e.tensor_scalar(out=Wdg[:, h, :], in0=C.ident.ap(), scalar1=wiAll[:, b, h:h + 1], scalar2=None, op0=ALU.mult),
                         reads=[C.ident, wiAll], writes=[Wdg])
            steps.append(loads)
            jobs = [(c0, h) for c0 in range(0, N, 512) for h in range(8)]
            state = dict(prev=None)

            def mk(job):
                def run():
                    prev = state["prev"]
                    if job is not None:
                        c0, h = job
                        wc = min(512, N - c0)
                        lc = cnts["lc"]
                        bank = C.banks[2] if lc % 2 == 0 else C.banks[6]
                        R = Rs[lc % 4]
                        cnts["lc"] = lc + 1
                        P.op("tensor", lambda e: e.matmul(bank[:, 0:wc], lhsT=qall[:, 12 + h, :], rhs=kiT[:, c0:c0 + wc],
                                                          start=True, stop=True), reads=[qall, kiT], writes=[bank])
                        P.op("scalar", lambda e: e.activation(out=R[:, 0:wc], in_=bank[:, 0:wc], func=AF.Relu), reads=[bank], writes=[R])
                    if prev is not None:
                        (c0_, h_), R_ = prev
                        wc_ = min(512, N - c0_)
                        P.op("tensor", lambda e: e.matmul(accb[:, 0:wc_], lhsT=Wdg[:, h_, :], rhs=R_[:, 0:wc_], start=(h_ == 0), stop=(h_ == 7)),
                             reads=[Wdg, R_], writes=[accb])
                        if h_ == 7:
                            P.op("scalar", lambda e: e.activation(out=score[:, c0_:c0_ + wc_], in_=accb[:, 0:wc_], func=AF.Copy), reads=[accb], writes=[score])
                    state["prev"] = (job, R) if job is not None else None
                return run
            for job in jobs + [None]:
                steps.append(mk(job))
            return steps

        def thr_steps(b):
            r = b % 2
            N = 128 * (b + 1)
            score = scores[r]
            B = Bs[r]
            q0 = b * 128
            mn, mx, mid, cnt, aa, thr = sm[r]
            wtab = wtabs[r]
            steps = []

            def pre():
                if b >= 2:
                    P.op("vector", lambda e: e.tensor_reduce(out=mn.ap(), in_=score[:, 0:N - 128], axis=AX.X, op=ALU.min), reads=[score], writes=[mn])
                P.op("gpsimd", lambda e: e.affine_select(out=score[:, q0:q0 + 128], in_=score[:, q0:q0 + 128], pattern=[[-1, 128]], compare_op=ALU.is_ge,
                                                        fill=NEG, base=0, channel_multiplier=1), reads=[score], writes=[score])
                if b >= 2:
                    P.op("vector", lambda e: e.tensor_reduce(out=mx.ap(), in_=score[:, 0:N], axis=AX.X, op=ALU.max), reads=[score], writes=[mx])
                    P.op("vector", lambda e: e.tensor_tensor(out=aa.ap(), in0=mx.ap(), in1=mn.ap(), op=ALU.subtract), reads=[mx, mn], writes=[aa])
                    P.op("vector", lambda e: e.tensor_scalar(out=wtab.ap(), in0=C.pow2.ap(), scalar1=aa.ap(), scalar2=0.5, op0=ALU.mult, op1=ALU.mult),
                         reads=[C.pow2, aa], writes=[wtab])
                    P.op("vector", lambda e: e.tensor_tensor(out=mid.ap(), in0=mn.ap(), in1=wtab[:, 1:2], op=ALU.add), reads=[mn, wtab], writes=[mid])
            steps.append(pre)
            if b >= 2:
                def mk(k):
                    def run():
                        P.op("vector", lambda e: e.tensor_scalar(out=junk[:, 0:N], in0=score[:, 0:N], scalar1=mid.ap(), scalar2=None, op0=ALU.is_ge, op1=ALU.add,
                                                                 accum_out=cnt.ap()), reads=[score, mid], writes=[junk, cnt])
                        P.op("vector", lambda e: e.tensor_scalar(out=aa.ap(), in0=cnt.ap(), scalar1=255.5, scalar2=-0.5, op0=ALU.is_ge, op1=ALU.add),
                             reads=[cnt], writes=[aa])
                        P.op("vector", lambda e: e.scalar_tensor_tensor(out=mid.ap(), in0=aa.ap(), scalar=wtab[:, k + 1:k + 2], in1=mid.ap(), op0=ALU.mult, op1=ALU.add),
                             reads=[aa, wtab, mid], writes=[mid])
                    return run
                for k in range(N_BISECT):
                    steps.append(mk(k))

            def post():
                if b >= 2:
                    P.op("vector", lambda e: e.tensor_tensor(out=thr.ap(), in0=mid.ap(), in1=wtab[:, N_BISECT + 1:N_BISECT + 2], op=ALU.subtract),
                         reads=[mid, wtab], writes=[thr])
                    thr_t = thr
                else:
                    thr_t = C.negthr
                P.op("vector", lambda e: e.tensor_scalar(out=B[:, 0:N], in0=score[:, 0:N], scalar1=thr_t.ap(), scalar2=MASKV, op0=ALU.is_lt, op1=ALU.mult),
                     reads=[score, thr_t], writes=[B])
                if C.dbg is not None and b == C.dbg_block:
                    P.dma("sync", C.dbg["score"], score.ap(), reads=[score])
                    P.dma("sync", C.dbg["thr"], thr_t.ap(), reads=[thr_t])
                    P.dma("sync", C.dbg["Bm"], B.ap(), reads=[B])
            steps.append(post)
            return steps

        def interleave(sa, sb_):
            na, nb_ = len(sa), len(sb_)
            j = 0
            for i, s in enumerate(sa):
                s()
                tgt = ((i + 1) * nb_) // max(na, 1)
                while j < tgt:
                    sb_[j]()
                    j += 1
            while j < nb_:
                sb_[j]()
                j += 1

        def attend(b):
            sc = cnts["sc"]
            r = b % 2
            qall = qalls[b % 4]
            B = Bs[r]
            jobs = []
            for h in range(12):
                pos = QPERM.index(h)
                g = h // 3
                obank = C.banks[3 + h // 6]
                ocol = (h % 6) * 65
                for kb0 in range(0, b + 1, 4):
                    kb1 = min(b + 1, kb0 + 4)
                    jobs.append((pos, g, obank, ocol, kb0, kb1))
            prev = None
            for job in jobs + [None]:
                if job is not None:
                    pos, g, obank, ocol, kb0, kb1 = job
                    sbank = C.banks[sc % 2]
                    PT = PTs[sc % 3]
                    sc += 1
                    for kb in range(kb0, kb1):
                        j = kb - kb0
                        P.op("tensor", lambda e: e.matmul(sbank[:, j * 128:(j + 1) * 128], lhsT=kT[:, g // 2, kb * 128:(kb + 1) * 128],
                                                          rhs=qall[:, pos, :], start=True, stop=False), reads=[kT, qall], writes=[sbank])
                        P.op("tensor", lambda e: e.matmul(sbank[:, j * 128:(j + 1) * 128], lhsT=B[:, kb * 128:(kb + 1) * 128], rhs=C.ident.ap(),
                                                          start=False, stop=True), reads=[B, C.ident], writes=[sbank])
                    nw = (kb1 - kb0) * 128
                    P.op("scalar", lambda e: e.activation(out=PT[:, 0:nw], in_=sbank[:, 0:nw], func=AF.Exp, scale=0.125), reads=[sbank], writes=[PT])
                if prev is not None:
                    (pos_, g_, obank_, ocol_, kb0_, kb1_), PT_ = prev
                    for kb in range(kb0_, kb1_):
                        j = kb - kb0_
                        P.op("tensor", lambda e: e.matmul(obank_[:, ocol_:ocol_ + 65], lhsT=PT_[:, j * 128:(j + 1) * 128], rhs=Va[:, kb, g_, 0:65],
                                                          start=(kb == 0), stop=(kb == b)), reads=[PT_, Va], writes=[obank_])
                prev = (job, PT) if job is not None else None
            cnts["sc"] = sc
            mem_heads(P, C, FW, qall, 20, kmT, Vm, C.banks[5], [C.banks[0], C.banks[1]], r)

        KPRE = 9
        idx_lists = {}

        def idx_take(b, n=None):
            if b >= nblocks:
                return []
            if b not in idx_lists:
                idx_lists[b] = idx_steps(b)
            lst = idx_lists[b]
            k = len(lst) if n is None else min(n, len(lst))
            out_ = lst[:k]
            del lst[:k]
            return out_

        for s in idx_take(0):
            s()
        interleave(thr_steps(0), idx_take(1))
        for s in idx_take(2, KPRE):
            s()
        for b in range(nblocks):
            if b + 1 < nblocks:
                interleave(thr_steps(b + 1), idx_take(b + 2))
            attend(b)
            for s in idx_take(b + 3, KPRE):
                s()
            attn_finish(P, C, FW, xsrc, xdst, b, 16, Wout, Wot, 8)


def phase_F(P, C, tag, xnorm, xbase, xdst, wgu_dram, wd_dram, gains, grow, f0, f1, final_row=None, ntiles=NT):
    nf = f1 - f0
    with P.scope():
        Wgu = P.sb(tag + "Wgu", [128, 8, 2 * nf * 128], BF16)
        Wgt = [P.tok("%sWgt%d" % (tag, k)) for k in range(8)]
        for k in range(8):
            for half in range(2):
                c0 = half * DFF + f0 * 128
                P.dma("gpsimd", Wgu[:, k, half * nf * 128:(half + 1) * nf * 128], wgu_dram[k * 128:(k + 1) * 128, c0:c0 + nf * 128], writes=[Wgt[k]])
        Wd = P.sb(tag + "Wd", [128, nf, D], BF16)
        Wdt = [P.tok("%sWdt%d" % (tag, k)) for k in range(nf)]
        for k in range(nf):
            P.dma("gpsimd", Wd[:, k, :], wd_dram[(f0 + k) * 128:(f0 + k + 1) * 128, :], writes=[Wdt[k]])
        gb = load_gain(P, C, tag + "gbF", gains, grow)
        gfin = load_gain(P, C, tag + "gfin", gains, final_row) if final_row is not None else None
        NW = alloc_norm_work(P, C, tag + "F")
        xts = [P.sb("%sFx%d" % (tag, r), [128, D], F32) for r in range(2)]
        hT2 = [P.sb("%sFhT%d" % (tag, r), [128, 8, 256], BF16) for r in range(2)]
        hTt = [[P.tok("%sFhTt%d_%d" % (tag, r, t)) for t in range(2)] for r in range(2)]
        actT = [P.sb("%sactT%d" % (tag, r), [128, nf, 256], BF16) for r in range(2)]
        sg = [P.sb("%ssg%d" % (tag, r), [128, 256], F32) for r in range(2)]
        xn = [P.sb("%sFxn%d" % (tag, r), [128, D], F32) for r in range(4)]
        fsm = [[P.sb("%sfs%d_%d" % (tag, r, j), [128, 1], F32) for j in range(4)] for r in range(2)]
        fj = P.sb(tag + "fj", [128, D], BF16)
        gc = 0
        hbs = {}

        def norm1(G):
            for t in range(2):
                i = 2 * G + t
                xt = xts[i % 2]
                P.dma("sync", xt.ap(), xnorm[i * 128:(i + 1) * 128, :], writes=[xt])
                hbs[(G, t)] = rmsnorm_to_hT(P, C, NW, xt, gb, None, None, None)
                xo = xn[i % 4]
                P.dma("sync", xo.ap(), xbase[i * 128:(i + 1) * 128, :], writes=[xo])

        def norm2(G):
            for t in range(2):
                norm_p2(P, C, hbs.pop((G, t)), hT2[G % 2][:, :, t * 128:(t + 1) * 128], hTt[G % 2][t], C.banks[0],
                        evac_eng="scalar" if t == 0 else "vector")

        NG = ntiles // 2
        norm1(0)
        norm2(0)
        for G in range(NG):
            hT = hT2[G % 2]
            aT = actT[G % 2]
            for fc in range(nf):
                if fc == nf // 2 and G + 1 < NG:
                    norm1(G + 1)
                bank = C.banks[1 + (gc % 3)]
                s_ = sg[gc % 2]
                gc += 1
                for half in range(2):
                    coff = half * nf * 128 + fc * 128
                    for k in range(8):
                        P.op("tensor", lambda e: e.matmul(bank[:, half * 256:(half + 1) * 256], lhsT=Wgu[:, k, coff:coff + 128], rhs=hT[:, k, :],
                                                          start=(k == 0), stop=(k == 7)), reads=[Wgt[k]] + hTt[G % 2], writes=[bank])
                P.op("scalar", lambda e: e.activation(out=s_.ap(), in_=bank[:, 0:256], func=AF.Silu), reads=[bank], writes=[s_])
                P.op("vector", lambda e: e.tensor_tensor(out=aT[:, fc, :], in0=bank[:, 256:512], in1=s_.ap(), op=ALU.mult), reads=[bank, s_], writes=[aT])
            if G + 1 < NG:
                norm2(G + 1)
            for t in range(2):
                i = 2 * G + t
                xo = xn[i % 4]
                for half in range(2):
                    yb = C.banks[4 + (2 * t + half) % 4]
                    for fc in range(nf):
                        P.op("tensor", lambda e: e.matmul(yb.ap(), lhsT=aT[:, fc, t * 128:(t + 1) * 128], rhs=Wd[:, fc, half * 512:(half + 1) * 512],
                                                          start=(fc == 0), stop=(fc == nf - 1)), reads=[aT, Wdt[fc]], writes=[yb])
                    P.op("vector", lambda e: e.tensor_tensor(out=xo[:, half * 512:(half + 1) * 512], in0=yb.ap(), in1=xo[:, half * 512:(half + 1) * 512], op=ALU.add),
                         reads=[yb, xo], writes=[xo])
                if gfin is not None:
                    ssq, t1, t2, rstd = fsm[i % 2]
                    P.op("scalar", lambda e: e.activation(out=fj.ap(), in_=xo.ap(), func=AF.Square, accum_out=ssq.ap()), reads=[xo], writes=[fj, ssq])
                    P.op("vector", lambda e: e.tensor_scalar(out=t1.ap(), in0=ssq.ap(), scalar1=1.0 / D, scalar2=EPS, op0=ALU.mult, op1=ALU.add), reads=[ssq], writes=[t1])
                    P.op("scalar", lambda e: e.activation(out=t2.ap(), in_=t1.ap(), func=AF.Sqrt), reads=[t1], writes=[t2])
                    P.op("vector", lambda e: e.reciprocal(out=rstd.ap(), in_=t2.ap()), reads=[t2], writes=[rstd])
                    P.op("vector", lambda e: e.scalar_tensor_tensor(out=xo.ap(), in0=xo.ap(), scalar=rstd.ap(), in1=gfin.ap(), op0=ALU.mult, op1=ALU.mult),
                         reads=[xo, rstd, gfin], writes=[xo])
                P.dma("sync", xdst[i * 128:(i + 1) * 128, :], xo.ap(), reads=[xo], out=(gfin is not None))


def phase_B1(P, C, fT, vS, Og):
    with P.scope():
      sets = []
      for k in range(2):
          qz_ = P.sb("qz1_%d" % k, [128, 4, S], BF16)
          kTg_ = P.sb("kTg_%d" % k, [128, 2, S], BF16)
          Vg_ = P.sb("Vg_%d" % k, [128, NT, 4, VW], BF16)
          P.op("gpsimd", lambda e: e.memset(qz_.ap(), 0.0), writes=[qz_])
          P.op("vector", lambda e: e.memset(Vg_.ap(), 1.0), writes=[Vg_])
          sets.append((qz_, kTg_, Vg_))

      def gloads(g):
          dil = (1, 4, 16)[g]
          nb = NT // dil
          qz, kTg, Vg = sets[g % 2]
          for c in range(2):
              for hh in range(2):
                  base = hh * 64
                  P.dma("sync", qz[base:base + 64, 2 * c + hh, :], fT[4 * g + c][base:base + 64, :], writes=[qz])
              P.dma("sync", kTg[:, c, :], fT[4 * g + 2 + c], writes=[kTg])
          vg = vS[:, g * 256:(g + 1) * 256].rearrange("(mb p dd) (h e) -> dd p mb h e", p=128, dd=dil, e=64)
          for r in range(dil):
              for m0 in range(nb):
                  P.dma("sync", Vg[:, r * nb + m0, :, 0:64], vg[r][:, m0, :, :], writes=[Vg])

      gloads(0)
      for g, dil in enumerate((1, 4, 16)):
        nb = NT // dil
        qz, kTg, Vg = sets[g % 2]
        if g + 1 < 3:
            gloads(g + 1)
        if True:
            PTs = [P.sb("PTg%d" % k, [128, 512], BF16) for k in range(3)]
            Os = [P.sb("Os%d" % k, [128, 260], F32) for k in range(2)]
            Ogr = Og[g].rearrange("(m dd) c -> dd m c", dd=dil)
            sc = 0
            jobs = []
            qb = 0
            for r in range(dil):
                for mb in range(nb):
                    for hp in range(2):
                        jobs.append((r, mb, hp, qb))
                    qb += 1
            prev = None
            for job in jobs + [None]:
                if job is not None:
                    r, mb, hp, qb = job
                    qsl = slice(mb * 128 * dil + r, (mb * 128 + 127) * dil + r + 1, dil)
                    kbs = ([mb - 1] if mb > 0 else []) + [mb]
                    sbank = C.banks[sc % 3]
                    PT = PTs[sc % 3]
                    sc += 1
                    tiles = []
                    for hh in range(2):
                        j = hp * 2 + hh
                        for kb in kbs:
                            t = len(tiles)
                            tiles.append((j, kb))
                            ksl = slice(kb * 128 * dil + r, (kb * 128 + 127) * dil + r + 1, dil)
                            P.op("tensor", lambda e: e.matmul(sbank[:, t * 128:(t + 1) * 128], lhsT=kTg[:, j // 2, ksl], rhs=qz[:, j, qsl],
                                                              start=True, stop=False), reads=[kTg, qz], writes=[sbank])
                            M = C.MdT if kb == mb else C.MpT
                            P.op("tensor", lambda e: e.matmul(sbank[:, t * 128:(t + 1) * 128], lhsT=C.ident.ap(), rhs=M.ap(), start=False, stop=True),
                                 reads=[C.ident, M], writes=[sbank])
                    nw = len(tiles) * 128
                    P.op("scalar", lambda e: e.activation(out=PT[:, 0:nw], in_=sbank[:, 0:nw], func=AF.Exp, scale=0.125), reads=[sbank], writes=[PT])
                if prev is not None:
                    (r_, mb_, hp_, qb_), PT_, tiles_ = prev
                    obank = C.banks[6 + qb_ % 2]
                    kfirst = mb_ - 1 if mb_ > 0 else mb_
                    for t, (j, kb) in enumerate(tiles_):
                        P.op("tensor", lambda e: e.matmul(obank[:, j * 65:(j + 1) * 65], lhsT=PT_[:, t * 128:(t + 1) * 128], rhs=Vg[:, r_ * nb + kb, j, 0:65],
                                                          start=(kb == kfirst), stop=(kb == mb_)), reads=[PT_, Vg], writes=[obank])
                    if hp_ == 1:
                        O = Os[qb_ % 2]
                        P.op("vector", lambda e: e.tensor_copy(O.ap(), obank[:, 0:260]), reads=[obank], writes=[O])
                        P.dma("sync", Ogr[r_][mb_ * 128:(mb_ + 1) * 128, :], O.ap(), reads=[O])
                prev = (job, PT, tiles) if job is not None else None


def phase_B2(P, C, xsrc, xdst, fT, Og, kmT, Vm, wout_dram):
    with P.scope():
        Wout, Wot = load_weight(P, "Wout1", wout_dram, 4, D)
        qzs = [P.sb("qzm%d" % r, [128, 4, 128], BF16) for r in range(2)]
        for r in range(2):
            P.op("gpsimd", lambda e: e.memset(qzs[r].ap(), 0.0), writes=[qzs[r]])
        Ot = [[P.sb("Ot%d_%d" % (r, g), [128, 260], F32) for g in range(3)] for r in range(2)]
        FW = alloc_finish_work(P, C, "b2", [], C.banks[0], [C.banks[1], C.banks[2]])
        FW.osrc = lambda r: [(Ot[r][0], Ot[r][0].ap(), 4), (C.banks[5], C.banks[5][:, 0:260], 4)]
        fT_r = fT.rearrange("c p t -> p c t")
        def loads(i):
            r = i % 2
            q0 = i * 128
            for base in (0, 64):
                P.dma("sync", qzs[r][base:base + 64, base // 64:4:2, :], fT_r[base:base + 64, 12:14, q0:q0 + 128], writes=[qzs[r]])
            for g in range(3):
                P.dma("sync", Ot[r][g].ap(), Og[g][q0:q0 + 128, :], writes=[Ot[r][g]])

        loads(0)
        for i in range(NT):
            r = i % 2
            qz = qzs[r]
            q0 = i * 128
            if i + 1 < NT:
                loads(i + 1)
            mem_heads(P, C, FW, qz, 0, kmT, Vm, C.banks[5], [C.banks[6], C.banks[7]], r)
            P.op("gpsimd", lambda e: e.tensor_tensor(out=Ot[r][0].ap(), in0=Ot[r][0].ap(), in1=Ot[r][1].ap(), op=ALU.add),
                 reads=[Ot[r][0], Ot[r][1]], writes=[Ot[r][0]])
            P.op("gpsimd", lambda e: e.tensor_tensor(out=Ot[r][0].ap(), in0=Ot[r][0].ap(), in1=Ot[r][2].ap(), op=ALU.add),
                 reads=[Ot[r][0], Ot[r][2]], writes=[Ot[r][0]])
            attn_finish(P, C, FW, xsrc, xdst, i, 8, Wout, Wot, 4)


def build_program(stop_after=None, dbg_block=None, nblocks0=NT, skip_l0=False):
    nc = bass.Bass("TRN2", target_bir_lowering=False)
    P = Prog(nc)
    C = Ctx()
    I = {}

    def inp(name, shape, dt=F32):
        I[name] = nc.dram_tensor(name, list(shape), dt, kind="ExternalInput").ap()
        return I[name]

    x_in = inp("x", [S, D])
    mem_in = inp("mem", [256, D])
    pos_in = inp("pos", [128, NT], I32)
    gains = inp("gains", [7, D])
    C.freqs_in = inp("freqs", [128, 48])
    w_in0 = inp("w_in0", [D, SPEC0["ncols"]])
    w_in1 = inp("w_in1", [D, SPEC1["ncols"]])
    w_mkv = [inp("w_mkv%d" % l, [D, 512]) for l in range(2)]
    w_out0 = inp("w_out0", [1024, D])
    w_out1 = inp("w_out1", [512, D])
    w_gu = [inp("w_gu%d" % l, [D, 2 * DFF]) for l in range(2)]
    w_dn = [inp("w_dn%d" % l, [DFF, D]) for l in range(2)]
    out = nc.dram_tensor("out", [S, D], F32, kind="ExternalOutput").ap()
    C.dbg = None
    C.dbg_block = dbg_block
    if dbg_block is not None:
        C.dbg = dict(score=nc.dram_tensor("dbg_score", [128, S], F32, kind="ExternalOutput").ap(),
                     thr=nc.dram_tensor("dbg_thr", [128, 1], F32, kind="ExternalOutput").ap(),
                     Bm=nc.dram_tensor("dbg_Bm", [128, S], BF16, kind="ExternalOutput").ap())
    fT0 = P.dram("fT0", [15, 128, S], BF16)
    vS0 = P.dram("vS0", [S, 256], BF16)
    fT1 = P.dram("fT1", [14, 128, S], BF16)
    vS1 = P.dram("vS1", [S, 768], BF16)
    Og = [P.dram("Og%d" % g, [S, 260], F32) for g in range(3)]
    xa = P.dram("xa", [S, D], F32)
    xb = P.dram("xb", [S, D], F32)
    xc = P.dram("xc", [S, D], F32)

    C.banks = [P.ps("bank%d" % k, [128, 512], F32) for k in range(8)]
    setup_consts(P, C, pos_in)
    HF = NFC // 2

    def final(src):
        print("n_inst before final", P.n_inst)
        P.max_ops = None
        with P.scope():
            t = [P.sb("fin%d" % r, [128, D], F32) for r in range(2)]
            for i in range(NT):
                P.dma("sync", t[i % 2].ap(), src[i * 128:(i + 1) * 128, :], writes=[t[i % 2]])
                P.dma("sync", out[i * 128:(i + 1) * 128, :], t[i % 2].ap(), reads=[t[i % 2]], out=True)
        P.finish()
        return nc

    with (P.scope() if not skip_l0 else contextlib.nullcontext()):
      if not skip_l0:
        wiAll = P.sb("wiAll", [128, NT, 8], F32)
        kmT = P.sb("kmT0", [128, 2, 256], BF16)
        Vm = P.sb("Vm0", [128, 2, 4, VW], BF16)
        phase_M(P, C, "m0", mem_in, w_mkv[0], gains, 1, kmT, Vm)
        if stop_after == "M":
            d1 = nc.dram_tensor("dbg_kmT", [128, 2, 256], BF16, kind="ExternalOutput").ap()
            d2 = nc.dram_tensor("dbg_Vm", [128, 2, 4, VW], BF16, kind="ExternalOutput").ap()
            P.dma("sync", d1, kmT.ap(), reads=[kmT])
            P.dma("sync", d2, Vm.ap(), reads=[Vm])
            return final(x_in)
        phase_A(P, C, "a0", x_in, w_in0, gains, 0, SPEC0, fT0, vS0, wiAll, pos_in)
        if stop_after == "A":
            d1 = nc.dram_tensor("dbg_fT0", [15, 128, S], BF16, kind="ExternalOutput").ap()
            d2 = nc.dram_tensor("dbg_vS0", [S, 256], BF16, kind="ExternalOutput").ap()
            d3 = nc.dram_tensor("dbg_wi", [128, NT, 8], F32, kind="ExternalOutput").ap()
            with P.scope():
                tb = P.sb("dbgt", [128, S], BF16)
                for c in range(15):
                    P.dma("sync", tb.ap(), fT0[c], writes=[tb])
                    P.dma("sync", d1[c], tb.ap(), reads=[tb])
                for c in range(2):
                    P.dma("sync", tb[:, 0:2048].rearrange("p (a b) -> p a b", b=256), vS0[c * 2048:(c + 1) * 2048, :].rearrange("(a p) b -> p a b", p=128), writes=[tb])
                    P.dma("sync", d2[c * 2048:(c + 1) * 2048, :].rearrange("(a p) b -> p a b", p=128), tb[:, 0:2048].rearrange("p (a b) -> p a b", b=256), reads=[tb])
                P.dma("sync", d3, wiAll.ap(), reads=[wiAll])
            return final(x_in)
        phase_B0(P, C, x_in, xa, fT0, vS0, wiAll, kmT, Vm, w_out0, nblocks=nblocks0)
    if stop_after == "B0":
        return final(xa)
    if not skip_l0:
        phase_F(P, C, "f0a", xa, xa, xb, w_gu[0], w_dn[0], gains, 2, 0, HF)
        phase_F(P, C, "f0b", xa, xb, xc, w_gu[0], w_dn[0], gains, 2, HF, NFC)
    else:
        xc = x_in
    if stop_after == "F0":
        return final(xc)
    with P.scope():
        kmT = P.sb("kmT1", [128, 2, 256], BF16)
        Vm = P.sb("Vm1", [128, 2, 4, VW], BF16)
        phase_M(P, C, "m1", mem_in, w_mkv[1], gains, 4, kmT, Vm)
        phase_A(P, C, "a1", xc, w_in1, gains, 3, SPEC1, fT1, vS1, None, pos_in)
        phase_B1(P, C, fT1, vS1, Og)
        phase_B2(P, C, xc, xa, fT1, Og, kmT, Vm, w_out1)
    if stop_after == "B2":
        return final(xa)
    phase_F(P, C, "f1a", xa, xa, xb, w_gu[1], w_dn[1], gains, 5, 0, HF)
    phase_F(P, C, "f1b", xa, xb, out, w_gu[1], w_dn[1], gains, 5, HF, NFC, final_row=6)
    P.finish()
    return nc


def prep_inputs(inputs):
    f = lambda a: np.ascontiguousarray(np.asarray(a, dtype=np.float32))
    w0 = f(inputs["l0_w_in"])
    q, k, v, qi, ki, wi, qm = np.split(w0, np.cumsum([768, 256, 256, 512, 64, 8])[:], axis=1)
    qp = np.concatenate([q[:, h * 64:(h + 1) * 64] for h in QPERM], axis=1)
    w_in0 = np.ascontiguousarray(np.concatenate([qp, k, qi, ki, ki, wi, v, qm], axis=1))
    w1 = f(inputs["l1_w_in"])
    parts = [w1[:, j * 256:(j + 1) * 256] for j in range(10)]
    w_in1 = np.ascontiguousarray(np.concatenate([parts[0], parts[1], parts[3], parts[4], parts[6], parts[7], parts[2], parts[5], parts[8], parts[9]], axis=1))
    gains = np.ascontiguousarray(np.stack([f(inputs[n]) for n in ("l0_norm_mix", "l0_norm_mem", "l0_norm_ffn", "l1_norm_mix", "l1_norm_mem",
                                                                 "l1_norm_ffn", "final_norm")], axis=0))
    fr64 = (np.float32(10000.0) ** (-np.arange(32, dtype=np.float32) / np.float32(32))).astype(np.float32)
    fr16 = (np.float32(10000.0) ** (-np.arange(16, dtype=np.float32) / np.float32(16))).astype(np.float32)
    freqs = np.ascontiguousarray(np.broadcast_to(np.concatenate([fr64, fr16])[None, :], (128, 48)).astype(np.float32))
    shared = dict(freqs=freqs, gains=gains, w_in0=w_in0, w_in1=w_in1, w_mkv0=f(inputs["l0_w_mem_kv"]), w_mkv1=f(inputs["l1_w_mem_kv"]),
                  w_out0=f(inputs["l0_w_out"]), w_out1=f(inputs["l1_w_out"]), w_gu0=f(inputs["l0_w_gate_up"]), w_gu1=f(inputs["l1_w_gate_up"]),
                  w_dn0=f(inputs["l0_w_down"]), w_dn1=f(inputs["l1_w_down"]))
    x = f(inputs["x"])
    mem = f(inputs["mem"])
    pos = np.ascontiguousarray(np.asarray(inputs["positions"], dtype=np.int32))
    maps = []
    for c in range(x.shape[0]):
        m = dict(shared)
        m["x"] = x[c]
        m["mem"] = mem[c]
        m["pos"] = np.ascontiguousarray(pos[c].reshape(NT, 128).T)
        maps.append(m)
    return maps


_NC_CACHE = {}


def kernel(**inputs):
    maps = prep_inputs(inputs)
    if "nc" not in _NC_CACHE:
        _NC_CACHE["nc"] = build_program()
    nc = _NC_CACHE["nc"]
    res = run_bass_kernel_spmd(nc, maps, core_ids=list(range(len(maps))))
    return np.stack([np.asarray(r["out"], dtype=np.float32) for r in res.results], axis=0)
```
